# Optimizing a Trainium2 kernel written in Bass

```python
import math
import jax, jax.numpy as jnp
from jax import lax
import numpy as np

D_MODEL = 2048
BATCH = 4
SEQ = 2048
DEPTH = 2
DEC_BATCH = 2
DEC_SEQ = 4096
PAST_LEN = 128

N_MIXERS = 2
N_CONV_LAYERS = (DEPTH + N_MIXERS - 1) // N_MIXERS
N_SSM_LAYERS = DEPTH // N_MIXERS
CONV_WIDTH = 31
CONV_PAD = (CONV_WIDTH - 1) // 2
SSM_GROUP = 16
SSM_GROUPS = D_MODEL // SSM_GROUP
SSM_STATE = 64
N_DIRS = 2
N_EXPERTS = 16
EC_CAPACITY_FACTOR = 2
D_FF_EXPERT = D_MODEL
RMS_EPS = 1e-6
LN_EPS = 1e-5
DT_MIN = 1e-3
DT_MAX = 1e-1

kernel_name = 'hybrid_conv_s5_ecmoe_encoder'


def rms_norm(x, g):
    xf = x.astype(jnp.float32)
    y = xf * lax.rsqrt(jnp.mean(xf * xf, axis=-1, keepdims=True) + RMS_EPS)
    return (y * g.astype(jnp.float32)).astype(x.dtype)


def layer_norm(x, g, b):
    xf = x.astype(jnp.float32)
    mu = jnp.mean(xf, axis=-1, keepdims=True)
    xc = xf - mu
    y = xc * lax.rsqrt(jnp.mean(xc * xc, axis=-1, keepdims=True) + LN_EPS)
    return (y * g.astype(jnp.float32) + b.astype(jnp.float32)).astype(x.dtype)


def modulate(h, shift, scale):
    return h * (1 + scale[:, None, :]) + shift[:, None, :]


def conv_module(h, w_in, b_in, w_dw, b_dw, ln_g, ln_b, w_out, b_out):
    u = h @ w_in + b_in
    a, g = jnp.split(u, 2, axis=-1)
    u = a * jax.nn.sigmoid(g)
    d = u.shape[-1]
    u = lax.conv_general_dilated(
        u, w_dw[:, None, :].astype(u.dtype), window_strides=(1,),
        padding=[(CONV_PAD, CONV_PAD)],
        dimension_numbers=('NWC', 'WIO', 'NWC'),
        feature_group_count=d) + b_dw
    u = jax.nn.silu(layer_norm(u, ln_g, ln_b))
    return u @ w_out + b_out


def zoh_discretise(lam_re, lam_im, log_step, b_re, b_im):
    lam_re = lam_re.astype(jnp.float32)
    lam_im = lam_im.astype(jnp.float32)
    b_re = b_re.astype(jnp.float32)
    b_im = b_im.astype(jnp.float32)
    dt = jnp.exp(log_step.astype(jnp.float32))[:, None]
    mag = jnp.exp(lam_re * dt)
    a_re = mag * jnp.cos(lam_im * dt)
    a_im = mag * jnp.sin(lam_im * dt)
    nr = a_re - 1.0
    ni = a_im
    den = lam_re * lam_re + lam_im * lam_im
    k_re = (nr * lam_re + ni * lam_im) / den
    k_im = (ni * lam_re - nr * lam_im) / den
    bb_re = k_re[..., None] * b_re - k_im[..., None] * b_im
    bb_im = k_re[..., None] * b_im + k_im[..., None] * b_re
    return a_re, a_im, bb_re, bb_im


def _ssm_combine(left, right):
    a1r, a1i, b1r, b1i = left
    a2r, a2i, b2r, b2i = right
    ar = a1r * a2r - a1i * a2i
    ai = a1r * a2i + a1i * a2r
    br = a2r * b1r - a2i * b1i + b2r
    bi = a2r * b1i + a2i * b1r + b2i
    return ar, ai, br, bi


def s5_direction(u, lam_re, lam_im, log_step, b_re, b_im, c_re, c_im, reverse):
    a_re, a_im, bb_re, bb_im = zoh_discretise(lam_re, lam_im, log_step, b_re, b_im)
    bu_re = jnp.einsum('blgc,gpc->blgp', u, bb_re)
    bu_im = jnp.einsum('blgc,gpc->blgp', u, bb_im)
    shp = (1, u.shape[1]) + a_re.shape
    _, _, x_re, x_im = lax.associative_scan(
        _ssm_combine,
        (jnp.broadcast_to(a_re, shp), jnp.broadcast_to(a_im, shp), bu_re, bu_im),
        reverse=reverse, axis=1)
    c_re = c_re.astype(jnp.float32)
    c_im = c_im.astype(jnp.float32)
    return (jnp.einsum('blgp,gcp->blgc', x_re, c_re)
            - jnp.einsum('blgp,gcp->blgc', x_im, c_im))


def s5_module(h, lam_re, lam_im, log_step, b_re, b_im, c_re, c_im, d_skip, w_glu, b_glu):
    bsz, seq, dm = h.shape
    hf = h.astype(jnp.float32)
    u = hf.reshape(bsz, seq, SSM_GROUPS, SSM_GROUP)
    y = (s5_direction(u, lam_re[0], lam_im[0], log_step[0], b_re[0], b_im[0], c_re[0], c_im[0], False)
         + s5_direction(u, lam_re[1], lam_im[1], log_step[1], b_re[1], b_im[1], c_re[1], c_im[1], True))
    y = y.reshape(bsz, seq, dm) + d_skip.astype(jnp.float32) * hf
    y = jax.nn.gelu(y).astype(h.dtype)
    v = y @ w_glu + b_glu
    a, g = jnp.split(v, 2, axis=-1)
    return a * jax.nn.sigmoid(g)


def ec_moe(h, w_router, w_gate, w_up, w_down):
    bsz, seq, dm = h.shape
    n_tok = bsz * seq
    cap = EC_CAPACITY_FACTOR * n_tok // N_EXPERTS
    t = h.reshape(n_tok, dm)
    aff = jax.nn.softmax((t @ w_router).astype(jnp.float32), axis=-1)
    gate, idx = lax.top_k(aff.T, cap)
    xs = t[idx]
    hid = (jax.nn.silu(jnp.einsum('ecd,edf->ecf', xs, w_gate))
           * jnp.einsum('ecd,edf->ecf', xs, w_up))
    out = jnp.einsum('ecf,efd->ecd', hid, w_down) * gate[..., None].astype(h.dtype)
    y = jnp.zeros((n_tok, dm), h.dtype).at[idx.reshape(-1)].add(out.reshape(-1, dm))
    return y.reshape(bsz, seq, dm)


def trunk(x, c, p):
    cond = jax.nn.silu(c)
    for i in range(DEPTH):
        mod = cond @ p['ada_w'][i] + p['ada_b'][i]
        sh1, sc1, g1, sh2, sc2, g2 = jnp.split(mod, 6, axis=-1)
        h = modulate(rms_norm(x, p['norm_mix_g'][i]), sh1, sc1)
        j = i // N_MIXERS
        if i % N_MIXERS == 0:
            o = conv_module(h, p['conv_w_in'][j], p['conv_b_in'][j], p['conv_w_dw'][j], p['conv_b_dw'][j],
                            p['conv_ln_g'][j], p['conv_ln_b'][j], p['conv_w_out'][j], p['conv_b_out'][j])
        else:
            o = s5_module(h, p['ssm_lambda_re'][j], p['ssm_lambda_im'][j], p['ssm_log_step'][j],
                          p['ssm_b_re'][j], p['ssm_b_im'][j], p['ssm_c_re'][j], p['ssm_c_im'][j],
                          p['ssm_d'][j], p['ssm_w_glu'][j], p['ssm_b_glu'][j])
        x = x + g1[:, None, :] * o
        h = modulate(rms_norm(x, p['norm_ffn_g'][i]), sh2, sc2)
        x = x + g2[:, None, :] * ec_moe(h, p['moe_w_router'][i], p['moe_w_gate'][i],
                                        p['moe_w_up'][i], p['moe_w_down'][i])
    return rms_norm(x, p['final_norm_g'])


def setup_inputs(seed: int = 0) -> dict:
    key = jax.random.key(seed)
    ks = jax.random.split(key, 31)
    f32 = jnp.float32
    D = D_MODEL

    def nrm(k, shape, scale):
        return jax.random.normal(k, shape, f32) * scale

    n_idx = jnp.arange(SSM_STATE, dtype=f32)
    ssm_shape = (N_SSM_LAYERS, N_DIRS, SSM_GROUPS, SSM_STATE)
    return {
        'x_prompt': nrm(ks[0], (BATCH, SEQ, D), 1.0),
        'x_sample': nrm(ks[1], (DEC_BATCH, DEC_SEQ, D), 1.0),
        'c_prompt': nrm(ks[2], (BATCH, D), 1.0),
        'c_sample': nrm(ks[3], (DEC_BATCH, D), 1.0),
        'ada_w': nrm(ks[4], (DEPTH, D, 6 * D), 0.5 * D ** -0.5),
        'ada_b': nrm(ks[5], (DEPTH, 6 * D), 0.02),
        'norm_mix_g': 1.0 + nrm(ks[6], (DEPTH, D), 0.02),
        'norm_ffn_g': 1.0 + nrm(ks[7], (DEPTH, D), 0.02),
        'final_norm_g': 1.0 + nrm(ks[8], (D,), 0.02),
        'conv_w_in': nrm(ks[9], (N_CONV_LAYERS, D, 2 * D), D ** -0.5),
        'conv_b_in': nrm(ks[10], (N_CONV_LAYERS, 2 * D), 0.02),
        'conv_w_dw': nrm(ks[11], (N_CONV_LAYERS, CONV_WIDTH, D), CONV_WIDTH ** -0.5),
        'conv_b_dw': nrm(ks[12], (N_CONV_LAYERS, D), 0.02),
        'conv_ln_g': 1.0 + nrm(ks[13], (N_CONV_LAYERS, D), 0.02),
        'conv_ln_b': nrm(ks[14], (N_CONV_LAYERS, D), 0.02),
        'conv_w_out': nrm(ks[15], (N_CONV_LAYERS, D, D), D ** -0.5),
        'conv_b_out': nrm(ks[16], (N_CONV_LAYERS, D), 0.02),
        'ssm_lambda_re': -0.5 + nrm(ks[17], ssm_shape, 0.01),
        'ssm_lambda_im': math.pi * n_idx + nrm(ks[18], ssm_shape, 0.01),
        'ssm_log_step': jax.random.uniform(ks[19], (N_SSM_LAYERS, N_DIRS, SSM_GROUPS), f32,
                                           math.log(DT_MIN), math.log(DT_MAX)),
        'ssm_b_re': nrm(ks[20], ssm_shape + (SSM_GROUP,), (2 * SSM_GROUP) ** -0.5),
        'ssm_b_im': nrm(ks[21], ssm_shape + (SSM_GROUP,), (2 * SSM_GROUP) ** -0.5),
        'ssm_c_re': nrm(ks[22], (N_SSM_LAYERS, N_DIRS, SSM_GROUPS, SSM_GROUP, SSM_STATE), (2 * SSM_STATE) ** -0.5),
        'ssm_c_im': nrm(ks[23], (N_SSM_LAYERS, N_DIRS, SSM_GROUPS, SSM_GROUP, SSM_STATE), (2 * SSM_STATE) ** -0.5),
        'ssm_d': nrm(ks[24], (N_SSM_LAYERS, D), 1.0),
        'ssm_w_glu': nrm(ks[25], (N_SSM_LAYERS, D, 2 * D), D ** -0.5),
        'ssm_b_glu': nrm(ks[26], (N_SSM_LAYERS, 2 * D), 0.02),
        'moe_w_router': nrm(ks[27], (DEPTH, D, N_EXPERTS), D ** -0.5),
        'moe_w_gate': nrm(ks[28], (DEPTH, N_EXPERTS, D, D_FF_EXPERT), D ** -0.5),
        'moe_w_up': nrm(ks[29], (DEPTH, N_EXPERTS, D, D_FF_EXPERT), D ** -0.5),
        'moe_w_down': nrm(ks[30], (DEPTH, N_EXPERTS, D_FF_EXPERT, D), D_FF_EXPERT ** -0.5),
    }


def reference(x_prompt, x_sample, c_prompt, c_sample, ada_w, ada_b, norm_mix_g, norm_ffn_g, final_norm_g,
              conv_w_in, conv_b_in, conv_w_dw, conv_b_dw, conv_ln_g, conv_ln_b, conv_w_out, conv_b_out,
              ssm_lambda_re, ssm_lambda_im, ssm_log_step, ssm_b_re, ssm_b_im, ssm_c_re, ssm_c_im,
              ssm_d, ssm_w_glu, ssm_b_glu, moe_w_router, moe_w_gate, moe_w_up, moe_w_down):
    p = {
        'ada_w': ada_w, 'ada_b': ada_b, 'norm_mix_g': norm_mix_g, 'norm_ffn_g': norm_ffn_g,
        'final_norm_g': final_norm_g,
        'conv_w_in': conv_w_in, 'conv_b_in': conv_b_in, 'conv_w_dw': conv_w_dw, 'conv_b_dw': conv_b_dw,
        'conv_ln_g': conv_ln_g, 'conv_ln_b': conv_ln_b, 'conv_w_out': conv_w_out, 'conv_b_out': conv_b_out,
        'ssm_lambda_re': ssm_lambda_re, 'ssm_lambda_im': ssm_lambda_im, 'ssm_log_step': ssm_log_step,
        'ssm_b_re': ssm_b_re, 'ssm_b_im': ssm_b_im, 'ssm_c_re': ssm_c_re, 'ssm_c_im': ssm_c_im,
        'ssm_d': ssm_d, 'ssm_w_glu': ssm_w_glu, 'ssm_b_glu': ssm_b_glu,
        'moe_w_router': moe_w_router, 'moe_w_gate': moe_w_gate, 'moe_w_up': moe_w_up, 'moe_w_down': moe_w_down,
    }
    y_prompt = trunk(x_prompt, c_prompt, p)
    y_sample = trunk(x_sample, c_sample, p)
    return (y_prompt, y_sample)
```

```python
import contextlib
import numpy as np
import concourse.bass as bass
import concourse.mybir as mybir
from concourse.bass_utils import run_bass_kernel_spmd

F32 = mybir.dt.float32
BF16 = mybir.dt.bfloat16
I32 = mybir.dt.int32
AF = mybir.ActivationFunctionType
ALU = mybir.AluOpType
AX = mybir.AxisListType

RMS_EPS = 1e-6
LN_EPS = 1e-5
CONV_W = 31
CONV_PAD = 15


class Cfg:
    def __init__(s, D=2048, NT=8192, E=16, depth=2):
        s.D = D
        s.FC = D // 128
        s.NT = NT
        s.UNIT = NT // 4
        s.E = E
        s.CAP = 2 * NT // E
        s.depth = depth
        s.G = D // 16


class Tile:
    def __init__(s, t):
        s.t = t
        s.w = None
        s.r = []

    def __getitem__(s, k):
        return s.t[k]


class Stream:
    def __init__(s, h, sem=None):
        s.h = h
        s.sem = sem
        s.cnt = 0
        s.seen = {}


class KB:
    NDS = 12
    verbose = False

    def __init__(s, nc):
        s.nc = nc
        s.es = contextlib.ExitStack()
        s.st = {}
        for nm, h in (("pe", nc.tensor), ("act", nc.scalar), ("dve", nc.vector), ("pool", nc.gpsimd), ("sp", nc.sync)):
            sem = s.es.enter_context(nc.semaphore("sem_" + nm))
            s.st[nm] = Stream(h, sem)
        s.dq = {}
        for q, stn in (("sp", "sp"), ("gq", "pool"), ("aq", "act")):
            sems = [s.es.enter_context(nc.semaphore(f"dq_{q}_{i}")) for i in range(s.NDS)]
            s.dq[q] = dict(stream=s.st[stn], sems=sems, cnt=[0] * s.NDS, i=0)
        s.stage = None
        s.uid = 0

    def begin(s):
        if not hasattr(s, "stk"):
            s.stk = []
        s.stk.append(s.stage)
        s.stage = contextlib.ExitStack()

    def end(s):
        s.barrier()
        if KB.verbose:
            print("stage end:", {k: v.cnt for k, v in s.st.items()}, {q: max(Q["cnt"]) for q, Q in s.dq.items()}, flush=True)
        s.stage.close()
        s.stage = s.stk.pop()

    def sb(s, name, shape, dt):
        s.uid += 1
        return Tile(s.stage.enter_context(s.nc.sbuf_tensor(f"{name}_{s.uid}", list(shape), dt)))

    def ps(s, name, shape, dt=F32):
        s.uid += 1
        return Tile(s.stage.enter_context(s.nc.psum_tensor(f"{name}_{s.uid}", list(shape), dt)))

    def _wait(s, stream, sem, val):
        key = id(sem)
        if stream.seen.get(key, 0) >= val:
            return
        stream.h.wait_ge(sem, val)
        stream.seen[key] = val

    def _deps(s, stream, r, w):
        for b in r:
            if b.w is not None:
                s._wait(stream, *b.w)
        for b in w:
            if b.w is not None:
                s._wait(stream, *b.w)
            for tok in b.r:
                s._wait(stream, *tok)

    def _mark(s, tok, r, w):
        for b in r:
            b.r.append(tok)
            if len(b.r) > 24:
                d = {}
                for sem, v in b.r:
                    d[id(sem)] = (sem, max(v, d.get(id(sem), (sem, 0))[1]))
                b.r = list(d.values())
        for b in w:
            b.w = tok
            b.r = []

    def op(s, eng, fn, r=(), w=()):
        stream = s.st[eng]
        s._deps(stream, r, w)
        inst = fn(stream.h)
        stream.cnt += 1
        inst.then_inc(stream.sem, 1)
        tok = (stream.sem, stream.cnt)
        s._mark(tok, r, w)
        return tok

    def dma(s, q, out, in_, r=(), w=(), indirect=None, **kw):
        Q = s.dq[q]
        stream = Q["stream"]
        j = Q["i"] % s.NDS
        Q["i"] += 1
        sem = Q["sems"][j]
        if Q["cnt"][j] > 0:
            s._wait(stream, sem, Q["cnt"][j])
        s._deps(stream, r, w)
        if indirect is None:
            inst = stream.h.dma_start(out=out, in_=in_, **kw)
        else:
            inst = stream.h.indirect_dma_start(out=out, in_=in_, **indirect, **kw)
        inst.then_inc(sem, 16)
        Q["cnt"][j] += 16
        tok = (sem, Q["cnt"][j])
        s._mark(tok, r, w)
        return tok

    def barrier(s):
        toks = []
        for stt in s.st.values():
            if stt.cnt > 0:
                toks.append((stt.sem, stt.cnt))
        for Q in s.dq.values():
            for sem, c in zip(Q["sems"], Q["cnt"]):
                if c > 0:
                    toks.append((sem, c))
        for stt in s.st.values():
            for sem, v in toks:
                if sem is stt.sem:
                    continue
                s._wait(stt, sem, v)


def build_program(cfg, debug_out=None):
    nc = bass.Bass("TRN2", target_bir_lowering=False)
    D, FC, NT, UNIT, E = cfg.D, cfg.FC, cfg.NT, cfg.UNIT, cfg.E
    NTT = NT // 128
    NTB = NT // 512
    D6 = 6 * D

    def inp(name, shape, dt=F32):
        return nc.dram_tensor(name, list(shape), dt, kind="ExternalInput")

    def scr(name, shape, dt):
        return nc.dram_tensor(name, list(shape), dt, kind="Internal")

    x_in = inp("x", [NT, D])
    cT = inp("cT", [128, FC, 4])
    masks = inp("masks", [128, 4])
    ada_w = inp("ada_w", [cfg.depth, D, D6])
    ada_b = inp("ada_b", [cfg.depth, D6])
    norm_mix_g = inp("norm_mix_g", [cfg.depth, D])
    norm_ffn_g = inp("norm_ffn_g", [cfg.depth, D])
    final_g = inp("final_norm_g", [1, D])
    conv_w_in = inp("conv_w_in", [D, 2 * D])
    conv_b_in = inp("conv_b_in_c", [128, 2 * FC])
    conv_w_dw = inp("conv_w_dw_c", [128, FC, CONV_W])
    conv_b_dw = inp("conv_b_dw_c", [128, FC])
    conv_ln_g = inp("conv_ln_g_c", [128, FC])
    conv_ln_b = inp("conv_ln_b_c", [128, FC])
    conv_w_out = inp("conv_w_out", [D, D])
    conv_b_out = inp("conv_b_out", [1, D])
    if debug_out != "conv":
        moe_w_router = inp("moe_w_router_c", [cfg.depth, 128, FC, E])
        EG = min(E, 8)
        moe_w = {nm: [[inp(f"moe_w_{nm}_{l}_{h}", [EG, D, D]) for h in range(E // EG)] for l in range(cfg.depth)]
                 for nm in ("gate", "up", "down")}
    G = cfg.G
    NB = NT // 8
    if debug_out not in ("conv", "moe0"):
        ssm_lre = inp("ssm_lre", [128, G])
        ssm_lim = inp("ssm_lim", [128, G])
        ssm_ls = inp("ssm_ls", [128, G])
        ssm_bre = inp("ssm_bre", [128, G, 16])
        ssm_bim = inp("ssm_bim", [128, G, 16])
        ssm_cre = inp("ssm_cre", [128, G, 16])
        ssm_cim = inp("ssm_cim", [128, G, 16])
        ssm_dcol = inp("ssm_dcol", [128, G])
        ssm_w_glu = inp("ssm_w_glu", [D, 2 * D])
        ssm_b_glu = inp("ssm_b_glu", [1, 2 * D])
        s5_keep = inp("s5_keep", [128, NB])
        s5_masks = inp("s5_masks", [128, 2, 128])
        HS = scr("HS", [NT, D], BF16)
        YS = scr("YS", [NT, D], BF16)
    y_out = nc.dram_tensor("y", [NT, D], F32, kind="ExternalOutput")
    CAP = cfg.CAP
    EXT = 2 + 2 * E
    DX = D + EXT
    OBW = min(512, D)
    NOB = D // OBW
    NST = CAP // 128
    H = scr("H", [NT, DX], BF16)
    XS = [scr(f"XS{e}", [CAP, DX], BF16) for e in range(E)]
    YB = [scr(f"YB{ob}", [NT, OBW], F32) for ob in range(NOB)]
    X2 = scr("X2", [NT, D], F32)
    X3 = scr("X3", [NT, D], F32)

    MOD = scr("MOD", [cfg.depth, 4, D6], F32)
    HT = scr("HT", [FC, 128, NT], BF16)
    UT = scr("UT", [FC, 128, NT], BF16)
    CU = scr("CU", [FC, 128, NT], F32)
    X1 = scr("X1", [NT, D], F32)

    kb = KB(nc)

    kb.begin()
    cst = kb.stage
    ident_f = kb.sb("identf", [128, 128], F32)
    ident_b = kb.sb("identb", [128, 128], BF16)
    ones_f = kb.sb("onesf", [128, 128], F32)
    kb.op("pool", lambda e: e.memset(ones_f[:], 1.0), w=[ones_f])
    iot = kb.sb("iot", [128, 128], I32)
    kb.op("pool", lambda e: e.iota(iot[:], pattern=[[1, 128]], base=0, channel_multiplier=-1), w=[iot])
    iotf = kb.sb("iotf", [128, 128], F32)
    kb.op("dve", lambda e: e.tensor_copy(iotf[:], iot[:]), r=[iot], w=[iotf])
    kb.op("dve", lambda e: e.tensor_scalar(ident_f[:], iotf[:], 0.0, None, op0=ALU.is_equal), r=[iotf], w=[ident_f])
    kb.op("dve", lambda e: e.tensor_copy(ident_b[:], ident_f[:]), r=[ident_f], w=[ident_b])
    mask_t = kb.sb("maskt", [128, 4], F32)
    kb.dma("sp", mask_t[:], masks.ap(), w=[mask_t])
    const_stack = kb.stage

    def row_bcast(tile, dram_row_ap):
        n = dram_row_ap.shape[-1]
        kb.dma("sp", tile[:, 0:n], dram_row_ap.partition_broadcast(128), w=[tile])

    def stage_mod():
        kb.begin()
        ct = kb.sb("ct", [128, FC, 4], F32)
        kb.dma("sp", ct[:], cT.ap(), w=[ct])
        cs_t = kb.sb("cs", [128, FC, 4], BF16)
        kb.op("act", lambda e: e.activation(out=cs_t[:], in_=ct[:], func=AF.Silu), r=[ct], w=[cs_t])
        NTL = D6 // 512
        wts = [kb.sb(f"adaw{i}", [128, FC, 512], BF16) for i in range(2)]
        pss = [kb.ps(f"modps{i}", [4, 512]) for i in range(2)]
        brs = [kb.sb(f"adab{i}", [4, 512], F32) for i in range(2)]
        mrs = [kb.sb(f"mrow{i}", [4, 512], F32) for i in range(2)]
        it = 0
        for li in range(cfg.depth):
            for nt in range(NTL):
                wt = wts[it % 2]
                ps = pss[it % 2]
                brow = brs[it % 2]
                mrow = mrs[it % 2]
                it += 1
                cs = slice(nt * 512, (nt + 1) * 512)
                src = ada_w.ap()[li].rearrange("(kc p) n -> p kc n", p=128)[:, :, cs]
                kb.dma("gq", wt[:], src, w=[wt])
                kb.dma("sp", brow[:], ada_b.ap()[li:li + 1, cs].partition_broadcast(4), w=[brow])
                for kc in range(FC):
                    kb.op("pe", lambda e, kc=kc: e.matmul(ps[:], lhsT=cs_t[:, kc, :], rhs=wt[:, kc, :],
                                                         start=(kc == 0), stop=(kc == FC - 1)),
                          r=[cs_t, wt], w=[ps] if kc == 0 else [], )
                    ps.w = (kb.st["pe"].sem, kb.st["pe"].cnt)
                kb.op("dve", lambda e: e.tensor_tensor(out=mrow[:], in0=ps[:], in1=brow[:], op=ALU.add),
                      r=[ps, brow], w=[mrow])
                kb.dma("sp", MOD.ap()[li, :, cs], mrow[:], r=[mrow])
        kb.end()

    def load_mod_rows(li, u, which_scale, which_shift, gain_ap, A_t, S_t, tmp):
        row_bcast(tmp, MOD.ap()[li, u:u + 1, which_scale * D:(which_scale + 1) * D])
        row_bcast(A_t, gain_ap)
        kb.op("dve", lambda e: e.scalar_tensor_tensor(out=A_t[:], in0=tmp[:], scalar=1.0, in1=A_t[:],
                                                     op0=ALU.add, op1=ALU.mult), r=[tmp, A_t], w=[A_t])
        row_bcast(S_t, MOD.ap()[li, u:u + 1, which_shift * D:(which_shift + 1) * D])

    def rms_modulate(xt, A_t, S_t, h_out, junk, ssum, rstd):
        kb.op("act", lambda e: e.activation(out=junk[:], in_=xt[:], func=AF.Square, accum_out=ssum[:, 0:1]),
              r=[xt], w=[junk, ssum])
        kb.op("dve", lambda e: e.tensor_scalar(rstd[:, 0:1], ssum[:, 0:1], 1.0 / D, RMS_EPS, op0=ALU.mult, op1=ALU.add),
              r=[ssum], w=[rstd])
        kb.op("act", lambda e: e.activation(out=rstd[:, 1:2], in_=rstd[:, 0:1], func=AF.Sqrt), r=[rstd], w=[rstd])
        kb.op("dve", lambda e: e.reciprocal(rstd[:, 2:3], rstd[:, 1:2]), r=[rstd], w=[rstd])
        kb.op("dve", lambda e: e.scalar_tensor_tensor(out=junk[:], in0=xt[:], scalar=rstd[:, 2:3], in1=A_t[:],
                                                     op0=ALU.mult, op1=ALU.mult), r=[xt, rstd, A_t], w=[junk])
        kb.op("dve", lambda e: e.tensor_tensor(out=h_out[:], in0=junk[:], in1=S_t[:], op=ALU.add),
              r=[junk, S_t], w=[h_out])

    def transpose_to_HT(h_bf, tt, psT, hT, dst):
        for fb in range(0, FC, 4):
            nb = min(4, FC - fb)
            ps = psT[(fb // 4) % 2]
            for j in range(nb):
                fc = fb + j
                kb.op("pe", lambda e, fc=fc, j=j: e.transpose(out=ps[:, j * 128:(j + 1) * 128],
                                                            in_=h_bf[:, fc * 128:(fc + 1) * 128], identity=ident_b[:]),
                      r=[h_bf, ident_b], w=[ps] if j == 0 else [])
                ps.w = (kb.st["pe"].sem, kb.st["pe"].cnt)
            kb.op("act", lambda e: e.activation(out=hT[:, fb:fb + nb, :],
                                                in_=ps[:, 0:nb * 128].rearrange("p (a b) -> p a b", b=128), func=AF.Copy),
                  r=[ps], w=[hT])
        kb.dma("sp", dst.ap()[:, :, tt * 128:(tt + 1) * 128].rearrange("fc p t -> p fc t"), hT[:], r=[hT])

    def stage_prenorm_T(li, x_src, gain, wsc, wsh):
        kb.begin()
        A_t = kb.sb("A", [128, D], F32)
        S_t = kb.sb("S", [128, D], F32)
        tmp = kb.sb("tmp", [128, D], F32)
        xts = [kb.sb(f"xt{i}", [128, D], F32) for i in range(2)]
        junk = kb.sb("junk", [128, D], F32)
        hbs = [kb.sb(f"hb{i}", [128, D], BF16) for i in range(2)]
        hTs = [kb.sb(f"hT{i}", [128, FC, 128], BF16) for i in range(2)]
        ssum = kb.sb("ssum", [128, 1], F32)
        rstd = kb.sb("rstd", [128, 3], F32)
        psT = [kb.ps(f"psT{i}", [128, 512], BF16) for i in range(2)]
        for tt in range(NTT):
            u = (tt * 128) // UNIT
            if (tt * 128) % UNIT == 0:
                load_mod_rows(li, u, wsc, wsh, gain.ap()[li:li + 1, :], A_t, S_t, tmp)
            xt = xts[tt % 2]
            kb.dma("sp", xt[:], x_src.ap()[tt * 128:(tt + 1) * 128, :], w=[xt])
            hb = hbs[tt % 2]
            rms_modulate(xt, A_t, S_t, hb, junk, ssum, rstd)
            transpose_to_HT(hb, tt, psT, hTs[tt % 2], HT)
        kb.end()

    def stage_conv_in():
        kb.begin()
        FGS = min(4, FC)
        bin_t = kb.sb("bin", [128, 2 * FC], F32)
        kb.dma("sp", bin_t[:], conv_b_in.ap(), w=[bin_t])
        was = [kb.sb(f"wa{i}", [128, FC, FGS * 128], BF16) for i in range(2)]
        wgs = [kb.sb(f"wg{i}", [128, FC, FGS * 128], BF16) for i in range(2)]
        hts = [kb.sb(f"ht{i}", [128, FC, 512], BF16) for i in range(2)]
        psa = [kb.ps(f"psa{i}", [128, 512]) for i in range(2)]
        psg = [kb.ps(f"psg{i}", [128, 512]) for i in range(2)]
        sgs = [kb.sb(f"sg{i}", [128, 512], F32) for i in range(2)]
        uts = [kb.sb(f"ut{i}", [128, 512], BF16) for i in range(2)]
        wv = conv_w_in.ap().rearrange("(kc p) n -> p kc n", p=128)
        it = 0
        for fg in range(FC // FGS):
            wa, wg = was[fg % 2], wgs[fg % 2]
            kb.dma("gq", wa[:], wv[:, :, fg * FGS * 128:(fg + 1) * FGS * 128], w=[wa])
            kb.dma("gq", wg[:], wv[:, :, D + fg * FGS * 128:D + (fg + 1) * FGS * 128], w=[wg])
            for tb in range(NTB):
                ht = hts[tb % 2]
                kb.dma("sp", ht[:], HT.ap()[:, :, tb * 512:(tb + 1) * 512].rearrange("fc p t -> p fc t"), w=[ht])
                for j in range(FGS):
                    f = fg * FGS + j
                    pa, pg, sg, ut = psa[it % 2], psg[it % 2], sgs[it % 2], uts[it % 2]
                    it += 1
                    for kc in range(FC):
                        kb.op("pe", lambda e, kc=kc: e.matmul(pa[:], lhsT=wa[:, kc, j * 128:(j + 1) * 128], rhs=ht[:, kc, :],
                                                             start=(kc == 0), stop=(kc == FC - 1)),
                              r=[wa, ht], w=[pa] if kc == 0 else [])
                        pa.w = (kb.st["pe"].sem, kb.st["pe"].cnt)
                    for kc in range(FC):
                        kb.op("pe", lambda e, kc=kc: e.matmul(pg[:], lhsT=wg[:, kc, j * 128:(j + 1) * 128], rhs=ht[:, kc, :],
                                                             start=(kc == 0), stop=(kc == FC - 1)),
                              r=[wg, ht], w=[pg] if kc == 0 else [])
                        pg.w = (kb.st["pe"].sem, kb.st["pe"].cnt)
                    kb.op("act", lambda e: e.activation(out=sg[:], in_=pg[:], func=AF.Sigmoid,
                                                        bias=bin_t[:, FC + f:FC + f + 1]), r=[pg, bin_t], w=[sg])
                    kb.op("dve", lambda e: e.scalar_tensor_tensor(out=ut[:], in0=pa[:], scalar=bin_t[:, f:f + 1], in1=sg[:],
                                                                 op0=ALU.add, op1=ALU.mult), r=[pa, sg, bin_t], w=[ut])
                    kb.dma("sp", UT.ap()[f, :, tb * 512:(tb + 1) * 512], ut[:], r=[ut])
        kb.end()

    def stage_dwconv():
        kb.begin()
        wdw = kb.sb("wdw", [128, FC, CONV_W], F32)
        kb.dma("sp", wdw[:], conv_w_dw.ap(), w=[wdw])
        bdw = kb.sb("bdw", [128, FC], F32)
        kb.dma("sp", bdw[:], conv_b_dw.ap(), w=[bdw])
        dgs = [kb.sb(f"dg{i}", [128, CONV_W, 128], BF16) for i in range(2)]
        uws = [kb.sb(f"uw{i}", [128, 512 + 2 * CONV_PAD], BF16) for i in range(3)]
        pss = [kb.ps(f"cps{i}", [128, 512]) for i in range(2)]
        cus = [kb.sb(f"cu{i}", [128, 512], F32) for i in range(2)]
        it = 0
        for fc in range(FC):
            dg = dgs[fc % 2]
            for k in range(CONV_W):
                eng = "dve" if k % 2 == 0 else "pool"
                kb.op(eng, lambda e, k=k: e.tensor_scalar(dg[:, k, :], ident_f[:], wdw[:, fc, k:k + 1], None, op0=ALU.mult),
                      r=[ident_f, wdw], w=[dg])
            for tb in range(NTB):
                uw = uws[it % 3]
                ps = pss[it % 2]
                cu = cus[it % 2]
                it += 1
                t0 = tb * 512
                lo = max(t0 - CONV_PAD, 0)
                hi_ = min(t0 + 512 + CONV_PAD, NT)
                if t0 == 0:
                    kb.op("pool", lambda e: e.memset(uw[:, 0:CONV_PAD], 0.0), w=[uw])
                if t0 + 512 == NT:
                    kb.op("pool", lambda e: e.memset(uw[:, CONV_PAD + 512:], 0.0), w=[uw])
                kb.dma("sp", uw[:, lo - (t0 - CONV_PAD):hi_ - (t0 - CONV_PAD)], UT.ap()[fc, :, lo:hi_], w=[uw])
                if t0 > 0 and t0 % UNIT == 0:
                    b = t0 // UNIT
                    kb.op("dve", lambda e, b=b: e.tensor_scalar(uw[:, 0:CONV_PAD], uw[:, 0:CONV_PAD], mask_t[:, b:b + 1],
                                                             None, op0=ALU.mult), r=[uw, mask_t], w=[uw])
                if t0 + 512 < NT and (t0 + 512) % UNIT == 0:
                    b = (t0 + 512) // UNIT
                    kb.op("dve", lambda e, b=b: e.tensor_scalar(uw[:, CONV_PAD + 512:], uw[:, CONV_PAD + 512:],
                                                             mask_t[:, b:b + 1], None, op0=ALU.mult),
                          r=[uw, mask_t], w=[uw])
                for k in range(CONV_W):
                    kb.op("pe", lambda e, k=k: e.matmul(ps[:], lhsT=dg[:, k, :], rhs=uw[:, k:k + 512],
                                                       start=(k == 0), stop=(k == CONV_W - 1)),
                          r=[dg, uw], w=[ps] if k == 0 else [])
                    ps.w = (kb.st["pe"].sem, kb.st["pe"].cnt)
                kb.op("act", lambda e: e.activation(out=cu[:], in_=ps[:], func=AF.Identity, bias=bdw[:, fc:fc + 1]),
                      r=[ps, bdw], w=[cu])
                kb.dma("sp", CU.ap()[fc, :, t0:t0 + 512], cu[:], r=[cu])
        kb.end()

    def stage_conv_out(li, x_src, x_dst):
        kb.begin()
        lng = kb.sb("lng", [128, FC], F32)
        lnb = kb.sb("lnb", [128, FC], F32)
        kb.dma("sp", lng[:], conv_ln_g.ap(), w=[lng])
        kb.dma("sp", lnb[:], conv_ln_b.ap(), w=[lnb])
        wout = kb.sb("wout", [128, FC, D], BF16)
        wv = conv_w_out.ap().rearrange("(kc p) n -> p kc n", p=128)
        for kc in range(FC):
            kb.dma("gq", wout[:, kc, :], wv[:, kc, :], w=[wout])
        brow = kb.sb("brow", [128, D], F32)
        row_bcast(brow, conv_b_out.ap())
        G_t = kb.sb("G", [128, D], F32)
        cut = kb.sb("cut", [128, FC, 512], F32)
        sq = kb.sb("sq", [128, 512], F32)
        ps1 = kb.ps("ps1", [128, 512])
        ps2 = kb.ps("ps2", [128, 512])
        mean = kb.sb("mean", [128, 512], F32)
        rstd = kb.sb("rstdln", [128, 512], F32)
        tmp = kb.sb("tmpln", [128, 512], F32)
        vT = kb.sb("vT", [128, FC, 512], BF16)
        pso = [kb.ps(f"pso{i}", [128, 512]) for i in range(2)]
        xts = [kb.sb(f"xo{i}", [128, 512], F32) for i in range(2)]
        ots = [kb.sb(f"ot{i}", [128, 512], F32) for i in range(2)]
        it = 0
        NOB = D // 512 if D >= 512 else 1
        OBW = min(512, D)
        for tb in range(NTB):
            t0 = tb * 512
            if t0 % UNIT == 0:
                row_bcast(G_t, MOD.ap()[li, t0 // UNIT:t0 // UNIT + 1, 2 * D:3 * D])
            kb.dma("sp", cut[:], CU.ap()[:, :, t0:t0 + 512].rearrange("fc p t -> p fc t"), w=[cut])
            for fc in range(FC):
                kb.op("pe", lambda e, fc=fc: e.matmul(ps1[:], lhsT=ones_f[:], rhs=cut[:, fc, :], start=(fc == 0), stop=(fc == FC - 1)),
                      r=[ones_f, cut], w=[ps1] if fc == 0 else [])
                ps1.w = (kb.st["pe"].sem, kb.st["pe"].cnt)
            for fc in range(FC):
                kb.op("act", lambda e, fc=fc: e.activation(out=sq[:], in_=cut[:, fc, :], func=AF.Square), r=[cut], w=[sq])
                kb.op("pe", lambda e, fc=fc: e.matmul(ps2[:], lhsT=ones_f[:], rhs=sq[:], start=(fc == 0), stop=(fc == FC - 1)),
                      r=[ones_f, sq], w=[ps2] if fc == 0 else [])
                ps2.w = (kb.st["pe"].sem, kb.st["pe"].cnt)
            kb.op("dve", lambda e: e.tensor_scalar(mean[:], ps1[:], 1.0 / D, None, op0=ALU.mult), r=[ps1], w=[mean])
            kb.op("dve", lambda e: e.tensor_tensor(out=tmp[:], in0=mean[:], in1=mean[:], op=ALU.mult), r=[mean], w=[tmp])
            kb.op("dve", lambda e: e.scalar_tensor_tensor(out=tmp[:], in0=ps2[:], scalar=1.0 / D, in1=tmp[:],
                                                         op0=ALU.mult, op1=ALU.subtract), r=[ps2, tmp], w=[tmp])
            kb.op("dve", lambda e: e.tensor_scalar(tmp[:], tmp[:], LN_EPS, None, op0=ALU.add), r=[tmp], w=[tmp])
            kb.op("act", lambda e: e.activation(out=tmp[:], in_=tmp[:], func=AF.Sqrt), r=[tmp], w=[tmp])
            kb.op("dve", lambda e: e.reciprocal(rstd[:], tmp[:]), r=[tmp], w=[rstd])
            for fc in range(FC):
                kb.op("dve", lambda e, fc=fc: e.tensor_tensor(out=cut[:, fc, :], in0=cut[:, fc, :], in1=mean[:], op=ALU.subtract),
                      r=[cut, mean], w=[cut])
                kb.op("pool", lambda e, fc=fc: e.tensor_tensor(out=cut[:, fc, :], in0=cut[:, fc, :], in1=rstd[:], op=ALU.mult),
                      r=[cut, rstd], w=[cut])
                kb.op("act", lambda e, fc=fc: e.activation(out=vT[:, fc, :], in_=cut[:, fc, :], func=AF.Silu,
                                                           scale=lng[:, fc:fc + 1], bias=lnb[:, fc:fc + 1]),
                      r=[cut, lng, lnb], w=[vT])
            for ts in range(4):
                r0 = t0 + ts * 128
                for ob in range(NOB):
                    ps = pso[it % 2]
                    xt = xts[it % 2]
                    ot = ots[it % 2]
                    it += 1
                    cs = slice(ob * OBW, (ob + 1) * OBW)
                    kb.dma("sp", xt[:, 0:OBW], x_src.ap()[r0:r0 + 128, cs], w=[xt])
                    for kc in range(FC):
                        kb.op("pe", lambda e, kc=kc: e.matmul(ps[:, 0:OBW], lhsT=vT[:, kc, ts * 128:(ts + 1) * 128], rhs=wout[:, kc, cs],
                                                             start=(kc == 0), stop=(kc == FC - 1)),
                              r=[vT, wout], w=[ps] if kc == 0 else [])
                        ps.w = (kb.st["pe"].sem, kb.st["pe"].cnt)
                    kb.op("dve", lambda e: e.tensor_tensor(out=ot[:, 0:OBW], in0=ps[:, 0:OBW], in1=brow[:, cs], op=ALU.add),
                          r=[ps, brow], w=[ot])
                    kb.op("pool", lambda e: e.tensor_tensor(out=ot[:, 0:OBW], in0=ot[:, 0:OBW], in1=G_t[:, cs], op=ALU.mult),
                          r=[ot, G_t], w=[ot])
                    kb.op("dve", lambda e: e.tensor_tensor(out=ot[:, 0:OBW], in0=ot[:, 0:OBW], in1=xt[:, 0:OBW], op=ALU.add),
                          r=[ot, xt], w=[ot])
                    kb.dma("sp", x_dst.ap()[r0:r0 + 128, cs], ot[:, 0:OBW], r=[ot])
        kb.end()


    def acc_group(ps, n, mk, r):
        for i in range(n):
            kb.op("pe", lambda e, i=i: mk(e, i), r=r, w=[ps] if i == 0 else [])
            ps.w = (kb.st["pe"].sem, kb.st["pe"].cnt)

    def stage_moe(li, x_src):
        kb.begin()
        AFF = kb.sb("AFF", [128, NTT, E], F32)
        SLOT = kb.sb("SLOT", [128, E, NTT], I32)
        pidx = kb.sb("pidx", [128, 1], F32)
        pidi = kb.sb("pidi", [128, 1], I32)
        kb.op("pool", lambda e: e.iota(pidi[:], pattern=[[0, 1]], base=0, channel_multiplier=1), w=[pidi])
        kb.op("dve", lambda e: e.tensor_copy(pidx[:], pidi[:]), r=[pidi], w=[pidx])
        ltri = kb.sb("ltri", [128, 128], F32)
        kb.op("dve", lambda e: e.tensor_scalar(ltri[:], iotf[:], 0.0, None, op0=ALU.is_gt), r=[iotf], w=[ltri])
        kb.begin()
        zt = kb.sb("zt", [128, OBW], F32)
        kb.op("pool", lambda e: e.memset(zt[:], 0.0), w=[zt])
        for ob in range(NOB):
            for tt in range(NTT):
                kb.dma("sp", YB[ob].ap()[tt * 128:(tt + 1) * 128, :], zt[:], r=[zt])
        A_t = kb.sb("A", [128, D], F32)
        S_t = kb.sb("S", [128, D], F32)
        tmp = kb.sb("tmp", [128, D], F32)
        xts = [kb.sb(f"xt{i}", [128, D], F32) for i in range(2)]
        junk = kb.sb("junk", [128, D], F32)
        hfs = [kb.sb(f"hf{i}", [128, D], F32) for i in range(2)]
        hxs = [kb.sb(f"hx{i}", [128, DX], BF16) for i in range(2)]
        hT = kb.sb("hTf", [128, FC, 128], F32)
        ssum = kb.sb("ssum", [128, 1], F32)
        rstd = kb.sb("rstd", [128, 3], F32)
        sm = kb.sb("sm", [128, 4], F32)
        ex = kb.sb("ex", [128, E], F32)
        wr = kb.sb("wr", [128, FC, E], F32)
        kb.dma("sp", wr[:], moe_w_router.ap()[li], w=[wr])
        psT = [kb.ps(f"psTf{i}", [128, 512], F32) for i in range(2)]
        psr = kb.ps("psr", [128, E], F32)
        for tt in range(NTT):
            u = (tt * 128) // UNIT
            if (tt * 128) % UNIT == 0:
                load_mod_rows(li, u, 4, 3, norm_ffn_g.ap()[li:li + 1, :], A_t, S_t, tmp)
            xt = xts[tt % 2]
            hf = hfs[tt % 2]
            hx = hxs[tt % 2]
            kb.dma("sp", xt[:], x_src.ap()[tt * 128:(tt + 1) * 128, :], w=[xt])
            rms_modulate(xt, A_t, S_t, hf, junk, ssum, rstd)
            kb.op("act", lambda e: e.activation(out=hx[:, 0:D], in_=hf[:], func=AF.Copy), r=[hf], w=[hx])
            kb.op("pool", lambda e, tt=tt: e.memset(hx[:, D:D + 1], float(tt)), w=[hx])
            kb.op("pool", lambda e: e.tensor_copy(hx[:, D + 1:D + 2], pidx[:]), r=[pidx], w=[hx])
            for fb in range(0, FC, 4):
                nb = min(4, FC - fb)
                ps = psT[(fb // 4) % 2]
                acc_group(ps, nb, lambda e, j, fb=fb, ps=ps: e.transpose(out=ps[:, j * 128:(j + 1) * 128],
                                                                     in_=hf[:, (fb + j) * 128:(fb + j + 1) * 128],
                                                                     identity=ident_f[:]), r=[hf, ident_f])
                kb.op("dve", lambda e, fb=fb, nb=nb, ps=ps: e.tensor_copy(
                    hT[:, fb:fb + nb, :], ps[:, 0:nb * 128].rearrange("p (a b) -> p a b", b=128)), r=[ps], w=[hT])
            acc_group(psr, FC, lambda e, kc: e.matmul(psr[:], lhsT=hT[:, kc, :], rhs=wr[:, kc, :],
                                                     start=(kc == 0), stop=(kc == FC - 1)), r=[hT, wr])
            kb.op("dve", lambda e: e.tensor_reduce(out=sm[:, 0:1], in_=psr[:], axis=AX.X, op=ALU.max), r=[psr], w=[sm])
            kb.op("dve", lambda e: e.tensor_scalar(sm[:, 1:2], sm[:, 0:1], -1.0, None, op0=ALU.mult), r=[sm], w=[sm])
            kb.op("act", lambda e: e.activation(out=ex[:], in_=psr[:], func=AF.Exp, bias=sm[:, 1:2], accum_out=sm[:, 2:3]),
                  r=[psr, sm], w=[ex, sm])
            kb.op("dve", lambda e: e.reciprocal(sm[:, 3:4], sm[:, 2:3]), r=[sm], w=[sm])
            kb.op("dve", lambda e, tt=tt: e.tensor_scalar(AFF[:, tt, :], ex[:], sm[:, 3:4], None, op0=ALU.mult),
                  r=[ex, sm], w=[AFF])
            kb.op("dve", lambda e, tt=tt: e.tensor_copy(hx[:, D + 2:DX].bitcast(F32), AFF[:, tt, :]), r=[AFF], w=[hx])
            kb.dma("sp", H.ap()[tt * 128:(tt + 1) * 128, :], hx[:], r=[hx])
        kb.end()
        kb.begin()
        AFFv = AFF[:].rearrange("p t e -> p e t")
        lo = kb.sb("lo", [128, E], F32)
        hi = kb.sb("hi", [128, E], F32)
        mid = kb.sb("mid", [128, E], F32)
        ge = kb.sb("ge", [128, E], F32)
        nge = kb.sb("nge", [128, E], F32)
        ta = kb.sb("ta", [128, E], F32)
        tb_ = kb.sb("tb", [128, E], F32)
        cnt = kb.sb("cnt", [128, E], F32)
        cmp = kb.sb("cmp", [128, E, NTT], F32)
        pst = kb.ps("pst", [128, E], F32)
        kb.op("pool", lambda e: e.memset(lo[:], 0.0), w=[lo])
        kb.op("pool", lambda e: e.memset(hi[:], 1.0), w=[hi])

        def bc(t):
            return t[:, :].unsqueeze(2).broadcast_to([128, E, NTT])

        for it in range(34):
            kb.op("dve", lambda e: e.tensor_tensor(out=mid[:], in0=lo[:], in1=hi[:], op=ALU.add), r=[lo, hi], w=[mid])
            kb.op("dve", lambda e: e.tensor_scalar(mid[:], mid[:], 0.5, None, op0=ALU.mult), r=[mid], w=[mid])
            kb.op("dve", lambda e: e.tensor_tensor(out=cmp[:], in0=AFFv, in1=bc(mid), op=ALU.is_ge), r=[AFF, mid], w=[cmp])
            kb.op("dve", lambda e: e.tensor_reduce(out=cnt[:], in_=cmp[:], axis=AX.X, op=ALU.add), r=[cmp], w=[cnt])
            kb.op("pe", lambda e: e.matmul(pst[:], lhsT=ones_f[:], rhs=cnt[:], start=True, stop=True), r=[ones_f, cnt], w=[pst])
            kb.op("dve", lambda e: e.tensor_scalar(ge[:], pst[:], float(CAP), None, op0=ALU.is_ge), r=[pst], w=[ge])
            kb.op("dve", lambda e: e.tensor_scalar(nge[:], ge[:], -1.0, 1.0, op0=ALU.mult, op1=ALU.add), r=[ge], w=[nge])
            kb.op("dve", lambda e: e.tensor_tensor(out=ta[:], in0=ge[:], in1=mid[:], op=ALU.mult), r=[ge, mid], w=[ta])
            kb.op("dve", lambda e: e.tensor_tensor(out=tb_[:], in0=nge[:], in1=lo[:], op=ALU.mult), r=[nge, lo], w=[tb_])
            kb.op("dve", lambda e: e.tensor_tensor(out=lo[:], in0=ta[:], in1=tb_[:], op=ALU.add), r=[ta, tb_], w=[lo])
            kb.op("dve", lambda e: e.tensor_tensor(out=ta[:], in0=nge[:], in1=mid[:], op=ALU.mult), r=[nge, mid], w=[ta])
            kb.op("dve", lambda e: e.tensor_tensor(out=tb_[:], in0=ge[:], in1=hi[:], op=ALU.mult), r=[ge, hi], w=[tb_])
            kb.op("dve", lambda e: e.tensor_tensor(out=hi[:], in0=ta[:], in1=tb_[:], op=ALU.add), r=[ta, tb_], w=[hi])
        rst = kb.sb("rst", [128, E, NTT], F32)
        kb.op("pool", lambda e: e.memset(rst[:], 1.0), w=[rst])
        kb.op("pool", lambda e: e.memset(rst[:, :, 0:1], 0.0), w=[rst])
        pre = kb.sb("pre", [128, E, NTT], F32)
        kb.op("dve", lambda e: e.tensor_tensor(out=cmp[:], in0=AFFv, in1=bc(lo), op=ALU.is_ge), r=[AFF, lo], w=[cmp])
        kb.op("dve", lambda e: e.tensor_tensor_scan(out=pre[:].rearrange("p a b -> p (a b)"),
                                                   data0=rst[:].rearrange("p a b -> p (a b)"),
                                                   data1=cmp[:].rearrange("p a b -> p (a b)"),
                                                   initial=0.0, op0=ALU.mult, op1=ALU.add), r=[rst, cmp], w=[pre])
        kb.op("dve", lambda e: e.tensor_copy(cnt[:], pre[:, :, NTT - 1]), r=[pre], w=[cnt])
        kb.op("pe", lambda e: e.matmul(pst[:], lhsT=ltri[:], rhs=cnt[:], start=True, stop=True), r=[ltri, cnt], w=[pst])
        kb.op("dve", lambda e: e.tensor_scalar(ta[:], pst[:], -1.0, None, op0=ALU.add), r=[pst], w=[ta])
        BIG = 1000000.0
        kb.op("dve", lambda e: e.tensor_tensor(out=pre[:], in0=pre[:], in1=bc(ta), op=ALU.add), r=[pre, ta], w=[pre])
        kb.op("dve", lambda e: e.tensor_scalar(pre[:], pre[:], -BIG, None, op0=ALU.add), r=[pre], w=[pre])
        kb.op("dve", lambda e: e.tensor_tensor(out=pre[:], in0=pre[:], in1=cmp[:], op=ALU.mult), r=[pre, cmp], w=[pre])
        kb.op("dve", lambda e: e.tensor_scalar(pre[:], pre[:], BIG, None, op0=ALU.add), r=[pre], w=[pre])
        kb.op("dve", lambda e: e.tensor_copy(SLOT[:], pre[:]), r=[pre], w=[SLOT])
        hxs = [kb.sb(f"hxd{i}", [128, DX], BF16) for i in range(3)]
        bc_reg = nc.gpsimd.to_reg(CAP - 1)
        for tt in range(NTT):
            hx = hxs[tt % 3]
            kb.dma("sp", hx[:], H.ap()[tt * 128:(tt + 1) * 128, :], w=[hx])
            for ei in range(E):
                kb.dma("gq", XS[ei].ap()[:, :], hx[:, :], r=[hx, SLOT],
                       indirect=dict(out_offset=bass.IndirectOffsetOnAxis(ap=SLOT[:, ei, tt:tt + 1], axis=0), in_offset=None,
                                     bounds_check=bc_reg, oob_is_err=False))
        kb.end()
        kb.begin()
        FBW = min(256, D)
        xsT = kb.sb("xsT", [128, FC, CAP], BF16)
        hidT = kb.sb("hidT", [128, FC, CAP], BF16)
        IDX = kb.sb("IDX", [128, NST], I32)
        GT = kb.sb("GT", [128, NST], F32)
        idf = kb.sb("idf", [128, 4], F32)
        xss = [kb.sb(f"xs{i}", [128, DX], BF16) for i in range(2)]
        wgs = [kb.sb(f"wg{i}", [128, FC, FBW], BF16) for i in range(2)]
        wus = [kb.sb(f"wu{i}", [128, FC, FBW], BF16) for i in range(2)]
        wds = [kb.sb(f"wd{i}", [128, FC, OBW], BF16) for i in range(2)]
        sgs = [kb.sb(f"sgm{i}", [128, 512], F32) for i in range(2)]
        ots = [kb.sb(f"otm{i}", [128, OBW], F32) for i in range(3)]
        psT2 = [kb.ps(f"psTb{i}", [128, 512], BF16) for i in range(2)]
        psg = [kb.ps(f"psgm{i}", [128, 512]) for i in range(2)]
        psu = [kb.ps(f"psum{i}", [128, 512]) for i in range(2)]
        pso = [kb.ps(f"psom{i}", [128, OBW]) for i in range(2)]
        ytok = kb.sb("ytok", [1, 1], F32)
        nw = 0
        nd = 0
        ni = 0
        for ei in range(E):
            for st in range(NST):
                xs = xss[st % 2]
                kb.dma("sp", xs[:], XS[ei].ap()[st * 128:(st + 1) * 128, :], w=[xs])
                for fb in range(0, FC, 4):
                    nb = min(4, FC - fb)
                    ps = psT2[(fb // 4) % 2]
                    acc_group(ps, nb, lambda e, j, fb=fb, ps=ps, xs=xs: e.transpose(
                        out=ps[:, j * 128:(j + 1) * 128], in_=xs[:, (fb + j) * 128:(fb + j + 1) * 128], identity=ident_b[:]),
                        r=[xs, ident_b])
                    kb.op("act" if (fb // 4) % 2 == 0 else "dve",
                          (lambda e, fb=fb, nb=nb, ps=ps, st=st: e.activation(
                              out=xsT[:, fb:fb + nb, st * 128:(st + 1) * 128],
                              in_=ps[:, 0:nb * 128].rearrange("p (a b) -> p a b", b=128), func=AF.Copy))
                          if (fb // 4) % 2 == 0 else
                          (lambda e, fb=fb, nb=nb, ps=ps, st=st: e.tensor_copy(
                              xsT[:, fb:fb + nb, st * 128:(st + 1) * 128],
                              ps[:, 0:nb * 128].rearrange("p (a b) -> p a b", b=128))),
                          r=[ps], w=[xsT])
                kb.op("dve", lambda e, xs=xs: e.tensor_copy(idf[:, 0:2], xs[:, D:D + 2]), r=[xs], w=[idf])
                kb.op("dve", lambda e: e.scalar_tensor_tensor(out=idf[:, 2:3], in0=idf[:, 0:1], scalar=128.0, in1=idf[:, 1:2],
                                                             op0=ALU.mult, op1=ALU.add), r=[idf], w=[idf])
                kb.op("dve", lambda e, st=st: e.tensor_copy(IDX[:, st:st + 1], idf[:, 2:3]), r=[idf], w=[IDX])
                kb.op("dve", lambda e, st=st, xs=xs, ei=ei: e.tensor_copy(
                    GT[:, st:st + 1], xs[:, D + 2 + 2 * ei:D + 4 + 2 * ei].bitcast(F32)), r=[xs], w=[GT])
            gv = moe_w["gate"][li][ei // EG].ap()[ei % EG].rearrange("(kc p) f -> p kc f", p=128)
            uv = moe_w["up"][li][ei // EG].ap()[ei % EG].rearrange("(kc p) f -> p kc f", p=128)
            dv = moe_w["down"][li][ei // EG].ap()[ei % EG].rearrange("(kc p) f -> p kc f", p=128)
            for fb in range(D // FBW):
                wg, wu = wgs[nw % 2], wus[nw % 2]
                nw += 1
                kb.dma("gq", wg[:], gv[:, :, fb * FBW:(fb + 1) * FBW], w=[wg])
                kb.dma("gq", wu[:], uv[:, :, fb * FBW:(fb + 1) * FBW], w=[wu])
                for j in range(FBW // 128):
                    f = fb * (FBW // 128) + j
                    for sb_ in range(CAP // 512):
                        pg, pu, sg = psg[ni % 2], psu[ni % 2], sgs[ni % 2]
                        ni += 1
                        cs = slice(sb_ * 512, (sb_ + 1) * 512)
                        acc_group(pg, FC, lambda e, kc, pg=pg, wg=wg, j=j, cs=cs: e.matmul(
                            pg[:], lhsT=wg[:, kc, j * 128:(j + 1) * 128], rhs=xsT[:, kc, cs],
                            start=(kc == 0), stop=(kc == FC - 1)), r=[wg, xsT])
                        acc_group(pu, FC, lambda e, kc, pu=pu, wu=wu, j=j, cs=cs: e.matmul(
                            pu[:], lhsT=wu[:, kc, j * 128:(j + 1) * 128], rhs=xsT[:, kc, cs],
                            start=(kc == 0), stop=(kc == FC - 1)), r=[wu, xsT])
                        kb.op("act", lambda e, pg=pg, sg=sg: e.activation(out=sg[:], in_=pg[:], func=AF.Silu), r=[pg], w=[sg])
                        kb.op("dve", lambda e, pu=pu, sg=sg, f=f, cs=cs: e.tensor_tensor(
                            out=hidT[:, f, cs], in0=pu[:], in1=sg[:], op=ALU.mult), r=[pu, sg], w=[hidT])
            for ob in range(NOB):
                wd = wds[nd % 2]
                nd += 1
                kb.dma("gq", wd[:], dv[:, :, ob * OBW:(ob + 1) * OBW], w=[wd])
                for st in range(NST):
                    po = pso[ni % 2]
                    ot = ots[ni % 3]
                    ni += 1
                    acc_group(po, FC, lambda e, kc, po=po, wd=wd, st=st: e.matmul(
                        po[:], lhsT=hidT[:, kc, st * 128:(st + 1) * 128], rhs=wd[:, kc, :],
                        start=(kc == 0), stop=(kc == FC - 1)), r=[hidT, wd])
                    kb.op("dve" if ni % 2 else "act",
                          (lambda e, po=po, ot=ot, st=st: e.tensor_scalar(ot[:], po[:], GT[:, st:st + 1], None, op0=ALU.mult))
                          if ni % 2 else
                          (lambda e, po=po, ot=ot, st=st: e.activation(out=ot[:], in_=po[:], func=AF.Identity, scale=GT[:, st:st + 1])),
                          r=[po, GT], w=[ot])
                    kb.dma("gq", YB[ob].ap()[:, :], ot[:, :], r=[ot, IDX], w=[ytok],
                           indirect=dict(out_offset=bass.IndirectOffsetOnAxis(ap=IDX[:, st:st + 1], axis=0), in_offset=None,
                                         compute_op=ALU.add))
        kb.end()
        kb.end()

    def stage_combine(li, x_src, x_dst, final):
        kb.begin()
        G_t = kb.sb("G2", [128, D], F32)
        F_t = kb.sb("Fg", [128, D], F32)
        if final:
            row_bcast(F_t, final_g.ap())
        xts = [kb.sb(f"xc{i}", [128, D], F32) for i in range(2)]
        yts = [kb.sb(f"yc{i}", [128, D], F32) for i in range(2)]
        junk = kb.sb("junkc", [128, D], F32)
        ssum = kb.sb("ssumc", [128, 1], F32)
        rstd = kb.sb("rstdc", [128, 3], F32)
        for tt in range(NTT):
            if (tt * 128) % UNIT == 0:
                u = (tt * 128) // UNIT
                row_bcast(G_t, MOD.ap()[li, u:u + 1, 5 * D:6 * D])
            xt, yt = xts[tt % 2], yts[tt % 2]
            kb.dma("sp", xt[:], x_src.ap()[tt * 128:(tt + 1) * 128, :], w=[xt])
            for ob in range(NOB):
                kb.dma("sp", yt[:, ob * OBW:(ob + 1) * OBW], YB[ob].ap()[tt * 128:(tt + 1) * 128, :], w=[yt])
            kb.op("pool", lambda e, yt=yt: e.tensor_tensor(out=yt[:], in0=yt[:], in1=G_t[:], op=ALU.mult), r=[yt, G_t], w=[yt])
            kb.op("dve", lambda e, xt=xt, yt=yt: e.tensor_tensor(out=xt[:], in0=xt[:], in1=yt[:], op=ALU.add), r=[xt, yt], w=[xt])
            if final:
                kb.op("act", lambda e, xt=xt: e.activation(out=junk[:], in_=xt[:], func=AF.Square, accum_out=ssum[:, 0:1]),
                      r=[xt], w=[junk, ssum])
                kb.op("dve", lambda e: e.tensor_scalar(rstd[:, 0:1], ssum[:, 0:1], 1.0 / D, RMS_EPS, op0=ALU.mult, op1=ALU.add),
                      r=[ssum], w=[rstd])
                kb.op("act", lambda e: e.activation(out=rstd[:, 1:2], in_=rstd[:, 0:1], func=AF.Sqrt), r=[rstd], w=[rstd])
                kb.op("dve", lambda e: e.reciprocal(rstd[:, 2:3], rstd[:, 1:2]), r=[rstd], w=[rstd])
                kb.op("dve", lambda e, xt=xt, yt=yt: e.scalar_tensor_tensor(out=yt[:], in0=xt[:], scalar=rstd[:, 2:3], in1=F_t[:],
                                                                       op0=ALU.mult, op1=ALU.mult), r=[xt, rstd, F_t], w=[yt])
                kb.dma("sp", x_dst.ap()[tt * 128:(tt + 1) * 128, :], yt[:], r=[yt])
            else:
                kb.dma("sp", x_dst.ap()[tt * 128:(tt + 1) * 128, :], xt[:], r=[xt])
        kb.end()


    def stage_prenorm_rows(li, x_src, gain, wsc, wsh, dst):
        kb.begin()
        A_t = kb.sb("A", [128, D], F32)
        S_t = kb.sb("S", [128, D], F32)
        tmp = kb.sb("tmp", [128, D], F32)
        xts = [kb.sb(f"xt{i}", [128, D], F32) for i in range(2)]
        junk = kb.sb("junk", [128, D], F32)
        hbs = [kb.sb(f"hb{i}", [128, D], BF16) for i in range(2)]
        ssum = kb.sb("ssum", [128, 1], F32)
        rstd = kb.sb("rstd", [128, 3], F32)
        for tt in range(NTT):
            u = (tt * 128) // UNIT
            if (tt * 128) % UNIT == 0:
                load_mod_rows(li, u, wsc, wsh, gain.ap()[li:li + 1, :], A_t, S_t, tmp)
            xt = xts[tt % 2]
            kb.dma("sp", xt[:], x_src.ap()[tt * 128:(tt + 1) * 128, :], w=[xt])
            hb = hbs[tt % 2]
            rms_modulate(xt, A_t, S_t, hb, junk, ssum, rstd)
            kb.dma("sp", dst.ap()[tt * 128:(tt + 1) * 128, :], hb[:], r=[hb])
        kb.end()

    def stage_rows_to_T(src):
        kb.begin()
        hbs = [kb.sb(f"hbr{i}", [128, D], BF16) for i in range(2)]
        hTs = [kb.sb(f"hTr{i}", [128, FC, 128], BF16) for i in range(2)]
        psT = [kb.ps(f"psTr{i}", [128, 512], BF16) for i in range(2)]
        for tt in range(NTT):
            hb = hbs[tt % 2]
            kb.dma("sp", hb[:], src.ap()[tt * 128:(tt + 1) * 128, :], w=[hb])
            transpose_to_HT(hb, tt, psT, hTs[tt % 2], HT)
        kb.end()

    def stage_s5():
        kb.begin()
        CW = min(512, NB)
        NH = NB // CW
        NNT = NB // 128
        TWO_PI = 2.0 * np.pi
        isb = kb.sb("isb", [128, 1], F32)
        pidi = kb.sb("pidi5", [128, 1], I32)
        kb.op("pool", lambda e: e.iota(pidi[:], pattern=[[0, 1]], base=0, channel_multiplier=1), w=[pidi])
        kb.op("dve", lambda e: e.tensor_copy(isb[:], pidi[:]), r=[pidi], w=[isb])
        kb.op("dve", lambda e: e.tensor_scalar(isb[:], isb[:], 63.5, None, op0=ALU.is_gt), r=[isb], w=[isb])
        sign = kb.sb("sign", [128, 1], F32)
        kb.op("dve", lambda e: e.tensor_scalar(sign[:], isb[:], 2.0, -1.0, op0=ALU.mult, op1=ALU.add), r=[isb], w=[sign])
        nsign = kb.sb("nsign", [128, 1], F32)
        kb.op("dve", lambda e: e.tensor_scalar(nsign[:], sign[:], -1.0, None, op0=ALU.mult), r=[sign], w=[nsign])
        iri = kb.sb("iri", [128, 8], I32)
        kb.op("pool", lambda e: e.iota(iri[:], pattern=[[1, 8]], base=0, channel_multiplier=0), w=[iri])
        EX = kb.sb("EX", [128, 4, 8], F32)
        kb.op("dve", lambda e: e.tensor_copy(EX[:, 0, :], iri[:]), r=[iri], w=[EX])
        kb.op("dve", lambda e: e.tensor_scalar(EX[:, 0, :], EX[:, 0, :], sign[:, 0:1], None, op0=ALU.mult), r=[EX, sign], w=[EX])
        kb.op("dve", lambda e: e.tensor_scalar(EX[:, 1, :], EX[:, 0, :], -1.0, None, op0=ALU.mult), r=[EX], w=[EX])
        off3 = kb.sb("off3", [128, 2], F32)
        kb.op("dve", lambda e: e.tensor_scalar(off3[:, 0:1], isb[:], -7.0, 7.0, op0=ALU.mult, op1=ALU.add), r=[isb], w=[off3])
        kb.op("dve", lambda e: e.tensor_scalar(off3[:, 1:2], isb[:], 7.0, 1.0, op0=ALU.mult, op1=ALU.add), r=[isb], w=[off3])
        kb.op("dve", lambda e: e.tensor_scalar(EX[:, 2, :], EX[:, 0, :], off3[:, 0:1], None, op0=ALU.add), r=[EX, off3], w=[EX])
        kb.op("dve", lambda e: e.tensor_scalar(EX[:, 3, :], EX[:, 1, :], off3[:, 1:2], None, op0=ALU.add), r=[EX, off3], w=[EX])
        tni = kb.sb("tni", [128, NB], I32)
        kb.op("pool", lambda e: e.iota(tni[:], pattern=[[1, NB]], base=0, channel_multiplier=0), w=[tni])
        nrow = kb.sb("nrow", [128, NB], F32)
        kb.op("dve", lambda e: e.tensor_copy(nrow[:], tni[:]), r=[tni], w=[nrow])
        KEEP = kb.sb("KEEP", [128, NB], F32)
        kb.dma("sp", KEEP[:], s5_keep.ap(), w=[KEEP])
        MK = kb.sb("MK", [128, 2, 128], F32)
        kb.dma("sp", MK[:], s5_masks.ap(), w=[MK])
        lre = kb.sb("lre", [128, G], F32)
        lim = kb.sb("lim", [128, G], F32)
        lst = kb.sb("lst", [128, G], F32)
        kb.dma("sp", lre[:], ssm_lre.ap(), w=[lre])
        kb.dma("sp", lim[:], ssm_lim.ap(), w=[lim])
        kb.dma("sp", lst[:], ssm_ls.ap(), w=[lst])
        dcol = kb.sb("dcol", [128, G], F32)
        kb.dma("sp", dcol[:], ssm_dcol.ap(), w=[dcol])
        dt = kb.sb("dt", [128, G], F32)
        kb.op("act", lambda e: e.activation(out=dt[:], in_=lst[:], func=AF.Exp), r=[lst], w=[dt])
        lrdt = kb.sb("lrdt", [128, G], F32)
        kb.op("dve", lambda e: e.tensor_tensor(out=lrdt[:], in0=lre[:], in1=dt[:], op=ALU.mult), r=[lre, dt], w=[lrdt])
        f0 = kb.sb("f0", [128, G], F32)
        ti = kb.sb("ti", [128, G], I32)
        tf = kb.sb("tf", [128, G], F32)

        def frac_(t_f, t_i, t_tmp, eng="dve"):
            kb.op(eng, lambda e: e.tensor_copy(t_i, t_f), r=[], w=[])
            kb.op(eng, lambda e: e.tensor_copy(t_tmp, t_i), r=[], w=[])
            kb.op(eng, lambda e: e.tensor_tensor(out=t_f, in0=t_f, in1=t_tmp, op=ALU.subtract), r=[], w=[])

        kb.op("dve", lambda e: e.tensor_tensor(out=f0[:], in0=lim[:], in1=dt[:], op=ALU.mult), r=[lim, dt], w=[f0])
        kb.op("dve", lambda e: e.tensor_scalar(f0[:], f0[:], 1.0 / TWO_PI, None, op0=ALU.mult), r=[f0], w=[f0])
        def frac_tiles(F, I_, T, eng="dve"):
            kb.op(eng, lambda e: e.tensor_copy(I_[:], F[:]), r=[F], w=[I_])
            kb.op(eng, lambda e: e.tensor_copy(T[:], I_[:]), r=[I_], w=[T])
            kb.op(eng, lambda e: e.tensor_tensor(out=F[:], in0=F[:], in1=T[:], op=ALU.subtract), r=[F, T], w=[F])

        frac_tiles(f0, ti, tf)
        are = kb.sb("are", [128, G], F32)
        aim = kb.sb("aim", [128, G], F32)
        mag = kb.sb("mag", [128, G], F32)
        fc_ = kb.sb("fcq", [128, G], F32)
        kb.op("act", lambda e: e.activation(out=mag[:], in_=lrdt[:], func=AF.Exp), r=[lrdt], w=[mag])
        kb.op("act", lambda e: e.activation(out=aim[:], in_=f0[:], func=AF.Sin, scale=TWO_PI), r=[f0], w=[aim])
        kb.op("dve", lambda e: e.tensor_scalar(fc_[:], f0[:], 0.25, None, op0=ALU.add), r=[f0], w=[fc_])
        frac_tiles(fc_, ti, tf)
        kb.op("act", lambda e: e.activation(out=are[:], in_=fc_[:], func=AF.Sin, scale=TWO_PI), r=[fc_], w=[are])
        kb.op("dve", lambda e: e.tensor_tensor(out=are[:], in0=are[:], in1=mag[:], op=ALU.mult), r=[are, mag], w=[are])
        kb.op("dve", lambda e: e.tensor_tensor(out=aim[:], in0=aim[:], in1=mag[:], op=ALU.mult), r=[aim, mag], w=[aim])
        kre = kb.sb("kre", [128, G], F32)
        kim = kb.sb("kim", [128, G], F32)
        den = kb.sb("den", [128, G], F32)
        t1 = kb.sb("t1g", [128, G], F32)
        nr = kb.sb("nr", [128, G], F32)
        kb.op("dve", lambda e: e.tensor_scalar(nr[:], are[:], -1.0, None, op0=ALU.add), r=[are], w=[nr])
        kb.op("dve", lambda e: e.tensor_tensor(out=den[:], in0=lre[:], in1=lre[:], op=ALU.mult), r=[lre], w=[den])
        kb.op("dve", lambda e: e.tensor_tensor(out=t1[:], in0=lim[:], in1=lim[:], op=ALU.mult), r=[lim], w=[t1])
        kb.op("dve", lambda e: e.tensor_tensor(out=den[:], in0=den[:], in1=t1[:], op=ALU.add), r=[den, t1], w=[den])
        kb.op("dve", lambda e: e.reciprocal(den[:], den[:]), r=[den], w=[den])
        kb.op("dve", lambda e: e.tensor_tensor(out=kre[:], in0=nr[:], in1=lre[:], op=ALU.mult), r=[nr, lre], w=[kre])
        kb.op("dve", lambda e: e.tensor_tensor(out=t1[:], in0=aim[:], in1=lim[:], op=ALU.mult), r=[aim, lim], w=[t1])
        kb.op("dve", lambda e: e.tensor_tensor(out=kre[:], in0=kre[:], in1=t1[:], op=ALU.add), r=[kre, t1], w=[kre])
        kb.op("dve", lambda e: e.tensor_tensor(out=kre[:], in0=kre[:], in1=den[:], op=ALU.mult), r=[kre, den], w=[kre])
        kb.op("dve", lambda e: e.tensor_tensor(out=kim[:], in0=aim[:], in1=lre[:], op=ALU.mult), r=[aim, lre], w=[kim])
        kb.op("dve", lambda e: e.tensor_tensor(out=t1[:], in0=nr[:], in1=lim[:], op=ALU.mult), r=[nr, lim], w=[t1])
        kb.op("dve", lambda e: e.tensor_tensor(out=kim[:], in0=kim[:], in1=t1[:], op=ALU.subtract), r=[kim, t1], w=[kim])
        kb.op("dve", lambda e: e.tensor_tensor(out=kim[:], in0=kim[:], in1=den[:], op=ALU.mult), r=[kim, den], w=[kim])
        R8 = kb.sb("R8", [128, G], F32)
        kb.op("act", lambda e: e.activation(out=R8[:], in_=lrdt[:], func=AF.Exp, scale=8.0), r=[lrdt], w=[R8])
        f8 = kb.sb("f8", [128, G], F32)
        kb.op("dve", lambda e: e.tensor_scalar(f8[:], f0[:], 8.0, None, op0=ALU.mult), r=[f0], w=[f8])
        frac_tiles(f8, ti, tf)
        kb.op("dve", lambda e: e.tensor_scalar(f8[:], f8[:], nsign[:, 0:1], None, op0=ALU.mult), r=[f8, nsign], w=[f8])

        GC = 8
        bre = kb.sb("bre", [128, GC, 16], F32)
        bim = kb.sb("bim", [128, GC, 16], F32)
        cre = kb.sb("cre", [128, GC, 16], F32)
        cim = kb.sb("cim", [128, GC, 16], F32)
        bbre = kb.sb("bbre", [128, GC, 16], F32)
        bbim = kb.sb("bbim", [128, GC, 16], F32)
        tb16 = kb.sb("tb16", [128, GC, 16], F32)
        marg = kb.sb("marg", [128, GC, 32], F32)
        turn = kb.sb("turn", [128, GC, 32], F32)
        turc = kb.sb("turc", [128, GC, 32], F32)
        tui = kb.sb("tui", [128, GC, 32], I32)
        tuf = kb.sb("tuf", [128, GC, 32], F32)
        Ere = kb.sb("Ere", [128, GC, 4, 8], F32)
        Eim = kb.sb("Eim", [128, GC, 4, 8], F32)
        Pre = kb.sb("Pre", [128, GC, 8, 16], F32)
        Pim = kb.sb("Pim", [128, GC, 8, 16], F32)
        Qre = kb.sb("Qre", [128, GC, 8, 16], F32)
        Qim = kb.sb("Qim", [128, GC, 8, 16], F32)
        Lre = kb.sb("Lre", [128, GC, 8, 16], F32)
        Lim = kb.sb("Lim", [128, GC, 8, 16], F32)
        Xre = kb.sb("QXre", [128, GC, 8, 16], F32)
        Xim = kb.sb("QXim", [128, GC, 8, 16], F32)
        QXreb = kb.sb("QXreb", [128, 2, GC, 128], BF16)
        QXimb = kb.sb("QXimb", [128, 2, GC, 128], BF16)
        nisb = kb.sb("nisb", [128, 1], F32)
        kb.op("dve", lambda e: e.tensor_scalar(nisb[:], isb[:], -1.0, 1.0, op0=ALU.mult, op1=ALU.add), r=[isb], w=[nisb])
        ta4 = kb.sb("ta4", [128, GC, 8, 16], F32)
        tb4 = kb.sb("tb4", [128, GC, 8, 16], F32)
        HN = kb.sb("HN", [128, NNT, 8, 128], BF16)
        YN = kb.sb("YN", [128, NNT, 8, 128], BF16)
        HG = kb.sb("HG", [128, NNT, 8, 128], BF16)
        Ug = [kb.sb(f"Ug{i}", [128, NB], BF16) for i in range(2)]
        W0bs = [kb.sb(f"W0b{i}", [128, 128], BF16) for i in range(2)]
        tW = kb.sb("tW", [128, 128], F32)
        LTres = [kb.sb(f"LTre{i}", [128, 128], BF16) for i in range(2)]
        LTims = [kb.sb(f"LTim{i}", [128, 128], BF16) for i in range(2)]
        cns = [kb.sb(f"cn{i}", [128, NB], F32) for i in range(2)]
        sns = [kb.sb(f"sn{i}", [128, NB], F32) for i in range(2)]
        RKs = [kb.sb(f"RK{i}", [128, NB], F32) for i in range(2)]
        tnf = kb.sb("tnf", [128, NB], F32)
        frt = kb.sb("frt", [128, NB], F32)
        abt = tnf
        wr_ = kb.sb("wr5", [128, NB], F32)
        wi_ = kb.sb("wi5", [128, NB], F32)
        zr = kb.sb("zr", [128, NB], F32)
        zi = kb.sb("zi", [128, NB], F32)
        ta = kb.sb("ta5", [128, NB], F32)
        tb2 = kb.sb("tb5", [128, NB], F32)
        XPr = kb.sb("XPr", [128, NB + 2], BF16)
        XPi = kb.sb("XPi", [128, NB + 2], BF16)
        kb.op("pool", lambda e: e.memset(XPr[:], 0.0), w=[XPr])
        kb.op("pool", lambda e: e.memset(XPi[:], 0.0), w=[XPi])
        ysk = ta
        yg = kb.sb("yg", [128, NB], BF16)
        halfpi = kb.sb("halfpi", [128, 1], F32)
        kb.op("pool", lambda e: e.memset(halfpi[:], float(np.pi / 2)), w=[halfpi])
        psS = [kb.ps(f"psS{i}", [128, CW]) for i in range(2)]
        psY = [kb.ps(f"psY{i}", [128, CW]) for i in range(NH)] if NH <= 2 else None
        psU = kb.ps("psU", [128, 512])
        psM = kb.ps("psM", [128, 512])
        psB = kb.ps("psB", [128, 1024], BF16)

        def bc3(t, gsl, n):
            return t[:, gsl].unsqueeze(2).broadcast_to([128, GC, n])

        def cmul_outer(Er, Ei, Br, Bi, outr, outi, neg_im):
            def eb(E_ap):
                return E_ap.unsqueeze(3).broadcast_to([128, GC, 8, 16])

            def bb(B):
                return B[:].unsqueeze(2).broadcast_to([128, GC, 8, 16])
            kb.op("dve", lambda e: e.tensor_tensor(out=ta4[:], in0=eb(Er), in1=bb(Br), op=ALU.mult), r=[Ere, Eim, Br], w=[ta4])
            kb.op("pool", lambda e: e.tensor_tensor(out=tb4[:], in0=eb(Ei), in1=bb(Bi), op=ALU.mult), r=[Ere, Eim, Bi], w=[tb4])
            kb.op("dve", lambda e: e.tensor_tensor(out=outr[:], in0=ta4[:], in1=tb4[:], op=ALU.subtract), r=[ta4, tb4], w=[outr])
            kb.op("dve", lambda e: e.tensor_tensor(out=ta4[:], in0=eb(Er), in1=bb(Bi), op=ALU.mult), r=[Ere, Eim, Bi, outr], w=[ta4])
            kb.op("pool", lambda e: e.tensor_tensor(out=tb4[:], in0=eb(Ei), in1=bb(Br), op=ALU.mult), r=[Ere, Eim, Br, outr], w=[tb4])
            if neg_im:
                kb.op("dve", lambda e: e.tensor_tensor(out=outi[:], in0=ta4[:], in1=tb4[:], op=ALU.add), r=[ta4, tb4], w=[outi])
                kb.op("dve", lambda e: e.tensor_scalar(outi[:], outi[:], -1.0, None, op0=ALU.mult), r=[outi], w=[outi])
            else:
                kb.op("dve", lambda e: e.tensor_tensor(out=outi[:], in0=ta4[:], in1=tb4[:], op=ALU.add), r=[ta4, tb4], w=[outi])

        for fc in range(FC):
            gsl = slice(fc * GC, (fc + 1) * GC)
            kb.dma("sp", bre[:], ssm_bre.ap()[:, gsl, :], w=[bre])
            kb.dma("sp", bim[:], ssm_bim.ap()[:, gsl, :], w=[bim])
            kb.dma("sp", cre[:], ssm_cre.ap()[:, gsl, :], w=[cre])
            kb.dma("sp", cim[:], ssm_cim.ap()[:, gsl, :], w=[cim])
            kb.op("dve", lambda e: e.tensor_tensor(out=bbre[:], in0=bre[:], in1=bc3(kre, gsl, 16), op=ALU.mult), r=[bre, kre], w=[bbre])
            kb.op("dve", lambda e: e.tensor_tensor(out=tb16[:], in0=bim[:], in1=bc3(kim, gsl, 16), op=ALU.mult), r=[bim, kim], w=[tb16])
            kb.op("dve", lambda e: e.tensor_tensor(out=bbre[:], in0=bbre[:], in1=tb16[:], op=ALU.subtract), r=[bbre, tb16], w=[bbre])
            kb.op("dve", lambda e: e.tensor_tensor(out=bbim[:], in0=bim[:], in1=bc3(kre, gsl, 16), op=ALU.mult), r=[bim, kre], w=[bbim])
            kb.op("dve", lambda e: e.tensor_tensor(out=tb16[:], in0=bre[:], in1=bc3(kim, gsl, 16), op=ALU.mult), r=[bre, kim, bbre], w=[tb16])
            kb.op("dve", lambda e: e.tensor_tensor(out=bbim[:], in0=bbim[:], in1=tb16[:], op=ALU.add), r=[bbim, tb16], w=[bbim])
            exb = EX[:].rearrange("p a b -> p (a b)").unsqueeze(1).broadcast_to([128, GC, 32])
            kb.op("dve", lambda e: e.tensor_tensor(out=marg[:], in0=bc3(lrdt, gsl, 32), in1=exb, op=ALU.mult), r=[lrdt, EX], w=[marg])
            kb.op("act", lambda e: e.activation(out=marg[:], in_=marg[:], func=AF.Exp), r=[marg], w=[marg])
            kb.op("dve", lambda e: e.tensor_tensor(out=turn[:], in0=bc3(f0, gsl, 32), in1=exb, op=ALU.mult), r=[f0, EX], w=[turn])
            frac_tiles(turn, tui, tuf)
            kb.op("dve", lambda e: e.tensor_scalar(turc[:], turn[:], 0.25, None, op0=ALU.add), r=[turn], w=[turc])
            frac_tiles(turc, tui, tuf)
            Ef_re = Ere[:].rearrange("p g a b -> p g (a b)")
            Ef_im = Eim[:].rearrange("p g a b -> p g (a b)")
            kb.op("act", lambda e: e.activation(out=Ef_im, in_=turn[:], func=AF.Sin, scale=TWO_PI), r=[turn], w=[Eim])
            kb.op("act", lambda e: e.activation(out=Ef_re, in_=turc[:], func=AF.Sin, scale=TWO_PI), r=[turc], w=[Ere])
            kb.op("dve", lambda e: e.tensor_tensor(out=Ef_im, in0=Ef_im, in1=marg[:], op=ALU.mult), r=[Eim, marg], w=[Eim])
            kb.op("dve", lambda e: e.tensor_tensor(out=Ef_re, in0=Ef_re, in1=marg[:], op=ALU.mult), r=[Ere, marg], w=[Ere])
            cmul_outer(Ere[:, :, 0, :], Eim[:, :, 0, :], bbre, bbim, Pre, Pim, False)
            cmul_outer(Ere[:, :, 1, :], Eim[:, :, 1, :], cre, cim, Qre, Qim, True)
            cmul_outer(Ere[:, :, 2, :], Eim[:, :, 2, :], bbre, bbim, Lre, Lim, False)
            cmul_outer(Ere[:, :, 3, :], Eim[:, :, 3, :], cre, cim, Xre, Xim, True)
            for d_, sc_ in ((0, nisb), (1, isb)):
                kb.op("act", lambda e, d_=d_, sc_=sc_: e.activation(out=QXreb[:, d_], in_=Xre[:].rearrange("p g a b -> p g (a b)"),
                                                                  func=AF.Copy, scale=sc_[:, 0:1]), r=[Xre, sc_], w=[QXreb])
                kb.op("act", lambda e, d_=d_, sc_=sc_: e.activation(out=QXimb[:, d_], in_=Xim[:].rearrange("p g a b -> p g (a b)"),
                                                                  func=AF.Copy, scale=sc_[:, 0:1]), r=[Xim, sc_], w=[QXimb])
            for nt in range(NNT):
                src = HS.ap()[nt * 1024:(nt + 1) * 1024, fc * 128:(fc + 1) * 128].rearrange("(n i) c -> n i c", i=8)
                kb.dma("sp", HN[:, nt, :, :], src, w=[HN])
            for nt in range(NNT):
                kb.op("pool" if nt % 2 else "act",
                      (lambda e, nt=nt: e.tensor_copy(HG[:, nt, :, :].rearrange("p g (i c) -> p g i c", c=16),
                                                      HN[:, nt, :, :].rearrange("p i (g c) -> p g i c", c=16))) if nt % 2 else
                      (lambda e, nt=nt: e.activation(out=HG[:, nt, :, :].rearrange("p g (i c) -> p g i c", c=16),
                                                     in_=HN[:, nt, :, :].rearrange("p i (g c) -> p g i c", c=16), func=AF.Copy)),
                      r=[HN], w=[HG])
            Pr = Pre[:].rearrange("p g a b -> p g (a b)")
            Pi = Pim[:].rearrange("p g a b -> p g (a b)")
            Qr = Qre[:].rearrange("p g a b -> p g (a b)")
            Qi = Qim[:].rearrange("p g a b -> p g (a b)")
            Lr = Lre[:].rearrange("p g a b -> p g (a b)")
            Li = Lim[:].rearrange("p g a b -> p g (a b)")

            def prep(g):
                gg = fc * GC + g
                U = Ug[g % 2]
                W0b, LTre, LTim = W0bs[g % 2], LTres[g % 2], LTims[g % 2]
                cn, sn, RK = cns[g % 2], sns[g % 2], RKs[g % 2]
                kb.op("dve", lambda e: e.tensor_scalar(tni[:], nrow[:], f8[:, gg:gg + 1], None, op0=ALU.mult), r=[nrow, f8], w=[tni])
                kb.op("dve", lambda e: e.tensor_copy(tnf[:], tni[:]), r=[tni], w=[tnf])
                for nb in range(0, NNT, 4):
                    k4 = min(4, NNT - nb)
                    acc_group(psU, k4, lambda e, j, nb=nb: e.matmul(
                        psU[:, j * 128:(j + 1) * 128], lhsT=HG[:, nb + j, g, :], rhs=ident_b[:],
                        start=True, stop=True), r=[HG, ident_b])
                    kb.op("act", lambda e, nb=nb, k4=k4: e.activation(out=U[:, nb * 128:(nb + k4) * 128], in_=psU[:, 0:k4 * 128],
                                                                     func=AF.Copy), r=[psU], w=[U])
                for d_ in range(2):
                    rs = slice(64 * d_, 64 * d_ + 64)
                    cs_ = slice(128 * d_, 128 * d_ + 128)
                    acc_group(psM, 2, lambda e, j, rs=rs, cs_=cs_: e.matmul(
                        psM[:, cs_], lhsT=(Pr if j == 0 else Pi)[rs, g, :], rhs=(Qr if j == 0 else Qi)[rs, g, :],
                        start=(j == 0), stop=(j == 1)), r=[Pre, Pim, Qre, Qim])
                kb.op("dve", lambda e: e.tensor_tensor(out=tW[:], in0=psM[:, 0:128], in1=MK[:, 0, :], op=ALU.mult), r=[psM, MK], w=[tW])
                kb.op("dve", lambda e: e.tensor_tensor(out=W0b[:], in0=psM[:, 128:256], in1=MK[:, 1, :], op=ALU.mult), r=[psM, MK], w=[W0b])
                kb.op("dve", lambda e: e.tensor_tensor(out=W0b[:], in0=W0b[:], in1=tW[:], op=ALU.add), r=[W0b, tW], w=[W0b])
                acc_group(psM, 2, lambda e, j: e.transpose(out=psM[:, 256 + j * 128:256 + (j + 1) * 128],
                                                           in_=(Lr if j == 0 else Li)[:, g, :], identity=ident_f[:]),
                          r=[Lre, Lim, ident_f])
                kb.op("act", lambda e: e.activation(out=LTre[:], in_=psM[:, 256:384], func=AF.Copy), r=[psM], w=[LTre])
                kb.op("act", lambda e: e.activation(out=LTim[:], in_=psM[:, 384:512], func=AF.Copy), r=[psM], w=[LTim])
                kb.op("dve", lambda e: e.scalar_tensor_tensor(out=frt[:], in0=nrow[:], scalar=f8[:, gg:gg + 1], in1=tnf[:],
                                                             op0=ALU.mult, op1=ALU.subtract), r=[nrow, f8, tnf], w=[frt])
                kb.op("act", lambda e: e.activation(out=sn[:], in_=frt[:], func=AF.Sin, scale=TWO_PI), r=[frt], w=[sn])
                kb.op("act", lambda e: e.activation(out=abt[:], in_=frt[:], func=AF.Abs), r=[frt], w=[abt])
                kb.op("act", lambda e: e.activation(out=cn[:], in_=abt[:], func=AF.Sin, scale=-TWO_PI, bias=halfpi[:, 0:1]),
                      r=[abt, halfpi], w=[cn])
                kb.op("act", lambda e: e.activation(out=RK[:], in_=KEEP[:], func=AF.Copy, scale=R8[:, gg:gg + 1]), r=[KEEP, R8], w=[RK])

            def main(g):
                gg = fc * GC + g
                U = Ug[g % 2]
                W0b, LTre, LTim = W0bs[g % 2], LTres[g % 2], LTims[g % 2]
                cn, sn, RK = cns[g % 2], sns[g % 2], RKs[g % 2]
                for h in range(NH):
                    cs_ = slice(h * CW, (h + 1) * CW)
                    kb.op("pe", lambda e, cs_=cs_: e.matmul(psS[0][:], lhsT=LTre[:], rhs=U[:, cs_], start=True, stop=True),
                          r=[LTre, U], w=[psS[0]])
                    kb.op("pe", lambda e, cs_=cs_: e.matmul(psS[1][:], lhsT=LTim[:], rhs=U[:, cs_], start=True, stop=True),
                          r=[LTim, U], w=[psS[1]])
                    kb.op("dve", lambda e, cs_=cs_: e.tensor_tensor(out=wr_[:, cs_], in0=psS[0][:], in1=cn[:, cs_], op=ALU.mult), r=[psS[0], cn], w=[wr_])
                    kb.op("dve", lambda e, cs_=cs_: e.tensor_tensor(out=ta[:, cs_], in0=psS[1][:], in1=sn[:, cs_], op=ALU.mult), r=[psS[1], sn], w=[ta])
                    kb.op("dve", lambda e, cs_=cs_: e.tensor_tensor(out=wi_[:, cs_], in0=psS[1][:], in1=cn[:, cs_], op=ALU.mult), r=[psS[1], cn], w=[wi_])
                    kb.op("dve", lambda e, cs_=cs_: e.tensor_tensor(out=tb2[:, cs_], in0=psS[0][:], in1=sn[:, cs_], op=ALU.mult), r=[psS[0], sn], w=[tb2])
                kb.op("pool", lambda e: e.tensor_tensor(out=wr_[:], in0=wr_[:], in1=ta[:], op=ALU.add), r=[wr_, ta], w=[wr_])
                kb.op("dve", lambda e: e.tensor_tensor(out=wi_[:], in0=wi_[:], in1=tb2[:], op=ALU.subtract), r=[wi_, tb2], w=[wi_])
                for (z_, w_) in ((zr, wr_), (zi, wi_)):
                    kb.op("dve", lambda e, z_=z_, w_=w_: e.tensor_tensor_scan(out=z_[0:64, :], data0=RK[0:64, :], data1=w_[0:64, :],
                                                                          initial=0.0, op0=ALU.mult, op1=ALU.add), r=[RK, w_], w=[z_])
                    kb.op("dve", lambda e, z_=z_, w_=w_: e.tensor_tensor_scan(out=z_[64:128, ::-1], data0=RK[64:128, ::-1], data1=w_[64:128, ::-1],
                                                                          initial=0.0, op0=ALU.mult, op1=ALU.add), r=[RK, w_], w=[z_])
                kb.op("dve", lambda e: e.tensor_tensor(out=ta[:], in0=zr[:], in1=cn[:], op=ALU.mult), r=[zr, cn], w=[ta])
                kb.op("pool", lambda e: e.tensor_tensor(out=tb2[:], in0=zi[:], in1=sn[:], op=ALU.mult), r=[zi, sn], w=[tb2])
                kb.op("dve", lambda e: e.tensor_tensor(out=XPr[:, 1:NB + 1], in0=ta[:], in1=tb2[:], op=ALU.subtract), r=[ta, tb2], w=[XPr])
                kb.op("dve", lambda e: e.tensor_tensor(out=ta[:], in0=zr[:], in1=sn[:], op=ALU.mult), r=[zr, sn, XPr], w=[ta])
                kb.op("pool", lambda e: e.tensor_tensor(out=tb2[:], in0=zi[:], in1=cn[:], op=ALU.mult), r=[zi, cn, XPr], w=[tb2])
                kb.op("dve", lambda e: e.tensor_tensor(out=XPi[:, 1:NB + 1], in0=ta[:], in1=tb2[:], op=ALU.add), r=[ta, tb2], w=[XPi])
                UB = UNIT // 8
                for XP in (XPr, XPi):
                    for b_ in range(1, 4):
                        c_ = b_ * UB
                        kb.op("dve", lambda e, XP=XP, c_=c_, b_=b_: e.tensor_scalar(XP[0:64, c_:c_ + 1], XP[0:64, c_:c_ + 1],
                                                                                 mask_t[0:64, b_:b_ + 1], None, op0=ALU.mult),
                              r=[XP, mask_t], w=[XP])
                        kb.op("dve", lambda e, XP=XP, c_=c_, b_=b_: e.tensor_scalar(XP[64:128, c_ + 1:c_ + 2], XP[64:128, c_ + 1:c_ + 2],
                                                                                 mask_t[64:128, b_:b_ + 1], None, op0=ALU.mult),
                              r=[XP, mask_t], w=[XP])
                for h in range(NH):
                    c0 = h * CW
                    cs_ = slice(c0, c0 + CW)
                    pY = psY[h]
                    ops = [(W0b[:], U[:, cs_]),
                           (QXreb[:, 0, g, :], XPr[:, c0:c0 + CW]), (QXreb[:, 1, g, :], XPr[:, c0 + 2:c0 + CW + 2]),
                           (QXimb[:, 0, g, :], XPi[:, c0:c0 + CW]), (QXimb[:, 1, g, :], XPi[:, c0 + 2:c0 + CW + 2])]
                    acc_group(pY, 5, lambda e, j, pY=pY, ops=ops: e.matmul(pY[:], lhsT=ops[j][0], rhs=ops[j][1],
                                                                          start=(j == 0), stop=(j == 4)),
                              r=[W0b, QXreb, QXimb, U, XPr, XPi])
                    kb.op("dve", lambda e, cs_=cs_, pY=pY: e.scalar_tensor_tensor(
                        out=ysk[:, cs_], in0=U[:, cs_], scalar=dcol[:, gg:gg + 1], in1=pY[:], op0=ALU.mult, op1=ALU.add),
                        r=[U, dcol, pY], w=[ysk])
                    kb.op("act", lambda e, cs_=cs_: e.activation(out=yg[:, cs_], in_=ysk[:, cs_], func=AF.Gelu_apprx_tanh), r=[ysk], w=[yg])
                for nb in range(0, NNT, 8):
                    k8 = min(8, NNT - nb)
                    acc_group(psB, k8, lambda e, j, nb=nb: e.transpose(out=psB[:, j * 128:(j + 1) * 128],
                                                                   in_=yg[:, (nb + j) * 128:(nb + j + 1) * 128], identity=ident_b[:]),
                              r=[yg, ident_b])
                    kb.op("act", lambda e, nb=nb, k8=k8: e.activation(
                        out=YN[:, nb:nb + k8, :, g * 16:(g + 1) * 16],
                        in_=psB[:, 0:k8 * 128].rearrange("p (a i c) -> p a i c", i=8, c=16), func=AF.Copy), r=[psB], w=[YN])

            prep(0)
            for g in range(GC):
                if g + 1 < GC:
                    prep(g + 1)
                main(g)
            for nt in range(NNT):
                dst = YS.ap()[nt * 1024:(nt + 1) * 1024, fc * 128:(fc + 1) * 128].rearrange("(n i) c -> n i c", i=8)
                kb.dma("sp", dst, YN[:, nt, :, :], r=[YN])
        kb.end()

    def stage_glu_out(li, x_src, x_dst):
        kb.begin()
        CGW = min(512, D)
        was = [kb.sb(f"wga{i}", [128, FC, CGW], BF16) for i in range(2)]
        wgs = [kb.sb(f"wgg{i}", [128, FC, CGW], BF16) for i in range(2)]
        hts = [kb.sb(f"htg{i}", [128, FC, 512], BF16) for i in range(2)]
        barow = kb.sb("barow", [128, D], F32)
        bgrow = kb.sb("bgrow", [128, D], F32)
        row_bcast(barow, ssm_b_glu.ap()[:, 0:D])
        row_bcast(bgrow, ssm_b_glu.ap()[:, D:2 * D])
        G_t = kb.sb("G1s", [128, D], F32)
        psa = [kb.ps(f"psga{i}", [128, CGW]) for i in range(2)]
        psg = [kb.ps(f"psgg{i}", [128, CGW]) for i in range(2)]
        sgs = [kb.sb(f"sgg{i}", [128, CGW], F32) for i in range(2)]
        ots = [kb.sb(f"otg{i}", [128, CGW], F32) for i in range(2)]
        xts = [kb.sb(f"xtg{i}", [128, CGW], F32) for i in range(2)]
        wv = ssm_w_glu.ap().rearrange("(kc p) n -> p kc n", p=128)
        it = 0
        for cg in range(D // CGW):
            cs = slice(cg * CGW, (cg + 1) * CGW)
            wa, wg = was[cg % 2], wgs[cg % 2]
            kb.dma("gq", wa[:], wv[:, :, cg * CGW:(cg + 1) * CGW], w=[wa])
            kb.dma("gq", wg[:], wv[:, :, D + cg * CGW:D + (cg + 1) * CGW], w=[wg])
            for tb in range(NTB):
                t0 = tb * 512
                if t0 % UNIT == 0:
                    row_bcast(G_t, MOD.ap()[li, t0 // UNIT:t0 // UNIT + 1, 2 * D:3 * D])
                ht = hts[tb % 2]
                kb.dma("sp", ht[:], HT.ap()[:, :, t0:t0 + 512].rearrange("fc p t -> p fc t"), w=[ht])
                for ts in range(4):
                    r0 = t0 + ts * 128
                    pa, pg, sg, ot, xt = psa[it % 2], psg[it % 2], sgs[it % 2], ots[it % 2], xts[it % 2]
                    it += 1
                    kb.dma("sp", xt[:], x_src.ap()[r0:r0 + 128, cs], w=[xt])
                    acc_group(pa, FC, lambda e, kc, pa=pa, wa=wa, ht=ht, ts=ts: e.matmul(
                        pa[:], lhsT=ht[:, kc, ts * 128:(ts + 1) * 128], rhs=wa[:, kc, :], start=(kc == 0), stop=(kc == FC - 1)), r=[ht, wa])
                    acc_group(pg, FC, lambda e, kc, pg=pg, wg=wg, ht=ht, ts=ts: e.matmul(
                        pg[:], lhsT=ht[:, kc, ts * 128:(ts + 1) * 128], rhs=wg[:, kc, :], start=(kc == 0), stop=(kc == FC - 1)), r=[ht, wg])
                    kb.op("dve", lambda e, pg=pg, sg=sg: e.tensor_tensor(out=sg[:], in0=pg[:], in1=bgrow[:, cs], op=ALU.add), r=[pg, bgrow], w=[sg])
                    kb.op("act", lambda e, sg=sg: e.activation(out=sg[:], in_=sg[:], func=AF.Sigmoid), r=[sg], w=[sg])
                    kb.op("dve", lambda e, pa=pa, ot=ot: e.tensor_tensor(out=ot[:], in0=pa[:], in1=barow[:, cs], op=ALU.add), r=[pa, barow], w=[ot])
                    kb.op("pool", lambda e, ot=ot, sg=sg: e.tensor_tensor(out=ot[:], in0=ot[:], in1=sg[:], op=ALU.mult), r=[ot, sg], w=[ot])
                    kb.op("pool", lambda e, ot=ot: e.tensor_tensor(out=ot[:], in0=ot[:], in1=G_t[:, cs], op=ALU.mult), r=[ot, G_t], w=[ot])
                    kb.op("dve", lambda e, ot=ot, xt=xt: e.tensor_tensor(out=ot[:], in0=ot[:], in1=xt[:], op=ALU.add), r=[ot, xt], w=[ot])
                    kb.dma("sp", x_dst.ap()[r0:r0 + 128, cs], ot[:], r=[ot])
        kb.end()

    stage_mod()
    stage_prenorm_T(0, x_in, norm_mix_g, 1, 0)
    stage_conv_in()
    stage_dwconv()
    stage_conv_out(0, x_in, y_out if debug_out == "conv" else X1)
    if debug_out != "conv":
        stage_moe(0, X1)
        stage_combine(0, X1, y_out if debug_out == "moe0" else X2, final=False)
    if debug_out not in ("conv", "moe0"):
        stage_prenorm_rows(1, X2, norm_mix_g, 1, 0, HS)
        stage_s5()
        stage_rows_to_T(YS)
        stage_glu_out(1, X2, y_out if debug_out == "s5" else X3)
        if debug_out != "s5":
            stage_moe(1, X3)
            stage_combine(1, X3, y_out, final=True)

    kb.barrier()
    const_stack.close()
    kb.es.close()
    return nc


def cols(v, FC):
    return np.ascontiguousarray(np.asarray(v, np.float32).reshape(FC, 128).T)


def make_core_inputs(cfg, x, c, unit_rows, masks, p, seq_len):
    FC, D = cfg.FC, cfg.D
    cu = np.asarray(c, np.float32)[unit_rows]
    cT = np.ascontiguousarray(cu.reshape(4, FC, 128).transpose(2, 1, 0))
    m = np.ascontiguousarray(np.broadcast_to(np.asarray(masks, np.float32)[None, :], (128, 4)))
    d = {
        "x": np.ascontiguousarray(x.reshape(-1, D), dtype=np.float32),
        "cT": cT, "masks": m,
        "ada_w": p["ada_w"], "ada_b": p["ada_b"],
        "norm_mix_g": p["norm_mix_g"], "norm_ffn_g": p["norm_ffn_g"],
        "final_norm_g": p["final_norm_g"].reshape(1, D),
        "conv_w_in": p["conv_w_in"][0],
        "conv_b_in_c": cols(p["conv_b_in"][0], 2 * FC),
        "conv_w_dw_c": np.ascontiguousarray(p["conv_w_dw"][0].T.reshape(FC, 128, CONV_W).transpose(1, 0, 2)),
        "conv_b_dw_c": cols(p["conv_b_dw"][0], FC),
        "conv_ln_g_c": cols(p["conv_ln_g"][0], FC),
        "conv_ln_b_c": cols(p["conv_ln_b"][0], FC),
        "conv_w_out": p["conv_w_out"][0],
        "conv_b_out": p["conv_b_out"][0].reshape(1, D),
        "moe_w_router_c": np.ascontiguousarray(p["moe_w_router"].reshape(cfg.depth, FC, 128, cfg.E).transpose(0, 2, 1, 3)),
    }
    G = cfg.G
    def dpg(a):
        return np.ascontiguousarray(np.asarray(a, np.float32).transpose(0, 2, 1).reshape(128, G))
    d["ssm_lre"] = dpg(p["ssm_lambda_re"][0])
    d["ssm_lim"] = dpg(p["ssm_lambda_im"][0])
    d["ssm_ls"] = dpg(np.broadcast_to(np.asarray(p["ssm_log_step"][0])[:, :, None], (2, G, 64)))
    d["ssm_bre"] = np.asarray(p["ssm_b_re"][0]).transpose(0, 2, 1, 3).reshape(128, G, 16)
    d["ssm_bim"] = np.asarray(p["ssm_b_im"][0]).transpose(0, 2, 1, 3).reshape(128, G, 16)
    d["ssm_cre"] = np.asarray(p["ssm_c_re"][0]).transpose(0, 3, 1, 2).reshape(128, G, 16)
    d["ssm_cim"] = np.asarray(p["ssm_c_im"][0]).transpose(0, 3, 1, 2).reshape(128, G, 16)
    dd = np.asarray(p["ssm_d"][0], np.float32).reshape(G, 16)
    d["ssm_dcol"] = np.ascontiguousarray(np.broadcast_to(dd.T[None, :, :], (8, 16, G)).reshape(128, G))
    d["ssm_w_glu"] = p["ssm_w_glu"][0]
    d["ssm_b_glu"] = p["ssm_b_glu"][0].reshape(1, 2 * D)
    NB = cfg.NT // 8
    seqb = seq_len // 8
    keep = np.ones((128, NB), np.float32)
    n = np.arange(NB)
    keep[:64, n % seqb == 0] = 0.0
    keep[64:, n % seqb == seqb - 1] = 0.0
    d["s5_keep"] = keep
    ip = np.arange(128) // 16
    mk = np.zeros((128, 2, 128), np.float32)
    mk[:, 0, :] = (ip[None, :] >= ip[:, None])
    mk[:, 1, :] = (ip[:, None] >= ip[None, :])
    d["s5_masks"] = mk
    EG = min(cfg.E, 8)
    for nm in ("gate", "up", "down"):
        w = p["moe_w_" + nm]
        for l in range(cfg.depth):
            for h in range(cfg.E // EG):
                d[f"moe_w_{nm}_{l}_{h}"] = w[l, h * EG:(h + 1) * EG]
    return {k: np.ascontiguousarray(v, dtype=np.float32) for k, v in d.items()}


def run(cfg, x_prompt, x_sample, c_prompt, c_sample, p, debug_out=None, n_cores=8):
    nc = build_program(cfg, debug_out=debug_out)
    bp, lp = x_prompt.shape[0], x_prompt.shape[1]
    bs, ls = x_sample.shape[0], x_sample.shape[1]

    def unit_info(b, l):
        upb = 4 // b
        rows = [u // upb for u in range(4)]
        masks = [0.0] + [1.0 if (u % upb) != 0 else 0.0 for u in range(1, 4)]
        return rows, masks

    rp, mp = unit_info(bp, lp)
    rs, ms = unit_info(bs, ls)
    inp_p = make_core_inputs(cfg, x_prompt, c_prompt, rp, mp, p, lp)
    inp_s = make_core_inputs(cfg, x_sample, c_sample, rs, ms, p, ls)
    for dct in (inp_p, inp_s):
        for k in list(dct):
            if (debug_out == "conv" and k.startswith("moe_")) or (debug_out in ("conv", "moe0") and (k.startswith("ssm_") or k.startswith("s5_"))):
                del dct[k]
    in_maps = [inp_p if (i % 2 == 0) else inp_s for i in range(n_cores)]
    res = run_bass_kernel_spmd(nc, in_maps, core_ids=list(range(n_cores)))
    yp = np.asarray(res.results[0]["y"], np.float32).reshape(x_prompt.shape)
    ys = np.asarray(res.results[1]["y"], np.float32).reshape(x_sample.shape)
    return yp, ys


def kernel(**inputs):
    cfg = Cfg()
    p = {k: np.asarray(v) for k, v in inputs.items()}
    return run(cfg, p["x_prompt"], p["x_sample"], p["c_prompt"], p["c_sample"], p, n_cores=2)
```

```python
import contextlib
import numpy as np
import concourse.bass as bass
import concourse.mybir as mybir
from concourse.bass_utils import run_bass_kernel_spmd

F32 = mybir.dt.float32
BF16 = mybir.dt.bfloat16
I32 = mybir.dt.int32
AF = mybir.ActivationFunctionType
ALU = mybir.AluOpType
AX = mybir.AxisListType

RMS_EPS = 1e-6
LN_EPS = 1e-5
CONV_W = 31
CONV_PAD = 15


class Cfg:
    def __init__(s, D=2048, NT=8192, E=16, depth=2):
        s.D = D
        s.FC = D // 128
        s.NT = NT
        s.UNIT = NT // 4
        s.E = E
        s.CAP = 2 * NT // E
        s.depth = depth
        s.G = D // 16


class Tile:
    def __init__(s, t):
        s.t = t
        s.w = None
        s.r = []

    def __getitem__(s, k):
        return s.t[k]


class Stream:
    def __init__(s, h, sem=None):
        s.h = h
        s.sem = sem
        s.cnt = 0
        s.seen = {}


class KB:
    NDS = 12
    verbose = False

    def __init__(s, nc):
        s.nc = nc
        s.es = contextlib.ExitStack()
        s.st = {}
        for nm, h in (("pe", nc.tensor), ("act", nc.scalar), ("dve", nc.vector), ("pool", nc.gpsimd), ("sp", nc.sync)):
            sem = s.es.enter_context(nc.semaphore("sem_" + nm))
            s.st[nm] = Stream(h, sem)
        s.dq = {}
        for q, stn in (("sp", "sp"), ("gq", "pool"), ("aq", "act")):
            sems = [s.es.enter_context(nc.semaphore(f"dq_{q}_{i}")) for i in range(s.NDS)]
            s.dq[q] = dict(stream=s.st[stn], sems=sems, cnt=[0] * s.NDS, i=0)
        s.stage = None
        s.uid = 0

    def begin(s):
        if not hasattr(s, "stk"):
            s.stk = []
        s.stk.append(s.stage)
        s.stage = contextlib.ExitStack()

    def end(s):
        s.barrier()
        if KB.verbose:
            print("stage end:", {k: v.cnt for k, v in s.st.items()}, {q: max(Q["cnt"]) for q, Q in s.dq.items()}, flush=True)
        s.stage.close()
        s.stage = s.stk.pop()

    def sb(s, name, shape, dt):
        s.uid += 1
        return Tile(s.stage.enter_context(s.nc.sbuf_tensor(f"{name}_{s.uid}", list(shape), dt)))

    def ps(s, name, shape, dt=F32):
        s.uid += 1
        return Tile(s.stage.enter_context(s.nc.psum_tensor(f"{name}_{s.uid}", list(shape), dt)))

    def _wait(s, stream, sem, val):
        key = id(sem)
        if stream.seen.get(key, 0) >= val:
            return
        stream.h.wait_ge(sem, val)
        stream.seen[key] = val

    def _deps(s, stream, r, w):
        for b in r:
            if b.w is not None:
                s._wait(stream, *b.w)
        for b in w:
            if b.w is not None:
                s._wait(stream, *b.w)
            for tok in b.r:
                s._wait(stream, *tok)

    def _mark(s, tok, r, w):
        for b in r:
            b.r.append(tok)
            if len(b.r) > 24:
                d = {}
                for sem, v in b.r:
                    d[id(sem)] = (sem, max(v, d.get(id(sem), (sem, 0))[1]))
                b.r = list(d.values())
        for b in w:
            b.w = tok
            b.r = []

    def op(s, eng, fn, r=(), w=()):
        stream = s.st[eng]
        s._deps(stream, r, w)
        inst = fn(stream.h)
        stream.cnt += 1
        inst.then_inc(stream.sem, 1)
        tok = (stream.sem, stream.cnt)
        s._mark(tok, r, w)
        return tok

    def dma(s, q, out, in_, r=(), w=(), indirect=None, **kw):
        Q = s.dq[q]
        stream = Q["stream"]
        j = Q["i"] % s.NDS
        Q["i"] += 1
        sem = Q["sems"][j]
        if Q["cnt"][j] > 0:
            s._wait(stream, sem, Q["cnt"][j])
        s._deps(stream, r, w)
        if indirect is None:
            inst = stream.h.dma_start(out=out, in_=in_, **kw)
        else:
            inst = stream.h.indirect_dma_start(out=out, in_=in_, **indirect, **kw)
        inst.then_inc(sem, 16)
        Q["cnt"][j] += 16
        tok = (sem, Q["cnt"][j])
        s._mark(tok, r, w)
        return tok

    def barrier(s):
        toks = []
        for stt in s.st.values():
            if stt.cnt > 0:
                toks.append((stt.sem, stt.cnt))
        for Q in s.dq.values():
            for sem, c in zip(Q["sems"], Q["cnt"]):
                if c > 0:
                    toks.append((sem, c))
        for stt in s.st.values():
            for sem, v in toks:
                if sem is stt.sem:
                    continue
                s._wait(stt, sem, v)


def build_program(cfg, debug_out=None):
    nc = bass.Bass("TRN2", target_bir_lowering=False)
    D, FC, NT, UNIT, E = cfg.D, cfg.FC, cfg.NT, cfg.UNIT, cfg.E
    NTT = NT // 128
    NTB = NT // 512
    D6 = 6 * D

    def inp(name, shape, dt=F32):
        return nc.dram_tensor(name, list(shape), dt, kind="ExternalInput")

    def scr(name, shape, dt):
        return nc.dram_tensor(name, list(shape), dt, kind="Internal")

    x_in = inp("x", [NT, D])
    cT = inp("cT", [128, FC, 4])
    masks = inp("masks", [128, 4])
    ada_w = inp("ada_w", [cfg.depth, D, D6])
    ada_b = inp("ada_b", [cfg.depth, D6])
    norm_mix_g = inp("norm_mix_g", [cfg.depth, D])
    norm_ffn_g = inp("norm_ffn_g", [cfg.depth, D])
    final_g = inp("final_norm_g", [1, D])
    conv_w_in = inp("conv_w_in", [D, 2 * D])
    conv_b_in = inp("conv_b_in_c", [128, 2 * FC])
    conv_w_dw = inp("conv_w_dw_c", [128, FC, CONV_W])
    conv_b_dw = inp("conv_b_dw_c", [128, FC])
    conv_ln_g = inp("conv_ln_g_c", [128, FC])
    conv_ln_b = inp("conv_ln_b_c", [128, FC])
    conv_w_out = inp("conv_w_out", [D, D])
    conv_b_out = inp("conv_b_out", [1, D])
    if debug_out != "conv":
        moe_w_router = inp("moe_w_router_c", [cfg.depth, 128, FC, E])
        EG = min(E, 8)
        moe_w = {nm: [[inp(f"moe_w_{nm}_{l}_{h}", [EG, D, D]) for h in range(E // EG)] for l in range(cfg.depth)]
                 for nm in ("gate", "up", "down")}
    G = cfg.G
    NB = NT // 8
    if debug_out not in ("conv", "moe0"):
        ssm_lre = inp("ssm_lre", [128, G])
        ssm_lim = inp("ssm_lim", [128, G])
        ssm_ls = inp("ssm_ls", [128, G])
        ssm_bre = inp("ssm_bre", [128, G, 16])
        ssm_bim = inp("ssm_bim", [128, G, 16])
        ssm_cre = inp("ssm_cre", [128, G, 16])
        ssm_cim = inp("ssm_cim", [128, G, 16])
        ssm_dcol = inp("ssm_dcol", [128, G])
        ssm_w_glu = inp("ssm_w_glu", [D, 2 * D])
        ssm_b_glu = inp("ssm_b_glu", [1, 2 * D])
        s5_keep = inp("s5_keep", [128, NB])
        s5_masks = inp("s5_masks", [128, 2, 128])
        HS = scr("HS", [NT, D], BF16)
        YS = scr("YS", [NT, D], BF16)
    y_out = nc.dram_tensor("y", [NT, D], F32, kind="ExternalOutput")
    CAP = cfg.CAP
    EXT = 2 + 2 * E
    DX = D + EXT
    OBW = min(512, D)
    NOB = D // OBW
    NST = CAP // 128
    H = scr("H", [NT, DX], BF16)
    XS = [scr(f"XS{e}", [CAP, DX], BF16) for e in range(E)]
    YB = [scr(f"YB{ob}", [NT, OBW], F32) for ob in range(NOB)]
    X2 = scr("X2", [NT, D], F32)
    X3 = scr("X3", [NT, D], F32)

    MOD = scr("MOD", [cfg.depth, 4, D6], F32)
    HT = scr("HT", [FC, 128, NT], BF16)
    UT = scr("UT", [FC, 128, NT], BF16)
    CU = scr("CU", [FC, 128, NT], F32)
    X1 = scr("X1", [NT, D], F32)

    kb = KB(nc)

    kb.begin()
    cst = kb.stage
    ident_f = kb.sb("identf", [128, 128], F32)
    ident_b = kb.sb("identb", [128, 128], BF16)
    ones_f = kb.sb("onesf", [128, 128], F32)
    kb.op("pool", lambda e: e.memset(ones_f[:], 1.0), w=[ones_f])
    iot = kb.sb("iot", [128, 128], I32)
    kb.op("pool", lambda e: e.iota(iot[:], pattern=[[1, 128]], base=0, channel_multiplier=-1), w=[iot])
    iotf = kb.sb("iotf", [128, 128], F32)
    kb.op("dve", lambda e: e.tensor_copy(iotf[:], iot[:]), r=[iot], w=[iotf])
    kb.op("dve", lambda e: e.tensor_scalar(ident_f[:], iotf[:], 0.0, None, op0=ALU.is_equal), r=[iotf], w=[ident_f])
    kb.op("dve", lambda e: e.tensor_copy(ident_b[:], ident_f[:]), r=[ident_f], w=[ident_b])
    mask_t = kb.sb("maskt", [128, 4], F32)
    kb.dma("sp", mask_t[:], masks.ap(), w=[mask_t])
    const_stack = kb.stage

    def row_bcast(tile, dram_row_ap):
        n = dram_row_ap.shape[-1]
        kb.dma("sp", tile[:, 0:n], dram_row_ap.partition_broadcast(128), w=[tile])

    def stage_mod():
        kb.begin()
        ct = kb.sb("ct", [128, FC, 4], F32)
        kb.dma("sp", ct[:], cT.ap(), w=[ct])
        cs_t = kb.sb("cs", [128, FC, 4], BF16)
        kb.op("act", lambda e: e.activation(out=cs_t[:], in_=ct[:], func=AF.Silu), r=[ct], w=[cs_t])
        NTL = D6 // 512
        wts = [kb.sb(f"adaw{i}", [128, FC, 512], BF16) for i in range(2)]
        pss = [kb.ps(f"modps{i}", [4, 512]) for i in range(2)]
        brs = [kb.sb(f"adab{i}", [4, 512], F32) for i in range(2)]
        mrs = [kb.sb(f"mrow{i}", [4, 512], F32) for i in range(2)]
        it = 0
        for li in range(cfg.depth):
            for nt in range(NTL):
                wt = wts[it % 2]
                ps = pss[it % 2]
                brow = brs[it % 2]
                mrow = mrs[it % 2]
                it += 1
                cs = slice(nt * 512, (nt + 1) * 512)
                src = ada_w.ap()[li].rearrange("(kc p) n -> p kc n", p=128)[:, :, cs]
                kb.dma("gq", wt[:], src, w=[wt])
                kb.dma("sp", brow[:], ada_b.ap()[li:li + 1, cs].partition_broadcast(4), w=[brow])
                for kc in range(FC):
                    kb.op("pe", lambda e, kc=kc: e.matmul(ps[:], lhsT=cs_t[:, kc, :], rhs=wt[:, kc, :],
                                                         start=(kc == 0), stop=(kc == FC - 1)),
                          r=[cs_t, wt], w=[ps] if kc == 0 else [], )
                    ps.w = (kb.st["pe"].sem, kb.st["pe"].cnt)
                kb.op("dve", lambda e: e.tensor_tensor(out=mrow[:], in0=ps[:], in1=brow[:], op=ALU.add),
                      r=[ps, brow], w=[mrow])
                kb.dma("sp", MOD.ap()[li, :, cs], mrow[:], r=[mrow])
        kb.end()

    def load_mod_rows(li, u, which_scale, which_shift, gain_ap, A_t, S_t, tmp):
        row_bcast(tmp, MOD.ap()[li, u:u + 1, which_scale * D:(which_scale + 1) * D])
        row_bcast(A_t, gain_ap)
        kb.op("dve", lambda e: e.scalar_tensor_tensor(out=A_t[:], in0=tmp[:], scalar=1.0, in1=A_t[:],
                                                     op0=ALU.add, op1=ALU.mult), r=[tmp, A_t], w=[A_t])
        row_bcast(S_t, MOD.ap()[li, u:u + 1, which_shift * D:(which_shift + 1) * D])

    def make_rms_ws():
        return dict(sq=kb.sb("rms_sq", [128, D], F32), tmp=[kb.sb(f"rms_tmp{i}", [128, D], F32) for i in range(2)],
                    ssum=[kb.sb(f"rms_ss{i}", [128, 1], F32) for i in range(2)],
                    rstd=[kb.sb(f"rms_rs{i}", [128, 3], F32) for i in range(2)], i=0)

    def rms_modulate(xt, A_t, S_t, h_out, ws):
        j = ws["i"] % 2
        ws["i"] += 1
        sq, junk, ssum, rstd = ws["sq"], ws["tmp"][j], ws["ssum"][j], ws["rstd"][j]
        kb.op("act", lambda e: e.activation(out=sq[:], in_=xt[:], func=AF.Square, accum_out=ssum[:, 0:1]),
              r=[xt], w=[sq, ssum])
        kb.op("dve", lambda e: e.tensor_scalar(rstd[:, 0:1], ssum[:, 0:1], 1.0 / D, RMS_EPS, op0=ALU.mult, op1=ALU.add),
              r=[ssum], w=[rstd])
        kb.op("act", lambda e: e.activation(out=rstd[:, 1:2], in_=rstd[:, 0:1], func=AF.Sqrt), r=[rstd], w=[rstd])
        kb.op("dve", lambda e: e.reciprocal(rstd[:, 2:3], rstd[:, 1:2]), r=[rstd], w=[rstd])
        kb.op("dve", lambda e: e.scalar_tensor_tensor(out=junk[:], in0=xt[:], scalar=rstd[:, 2:3], in1=A_t[:],
                                                     op0=ALU.mult, op1=ALU.mult), r=[xt, rstd, A_t], w=[junk])
        kb.op("dve", lambda e: e.tensor_tensor(out=h_out[:], in0=junk[:], in1=S_t[:], op=ALU.add),
              r=[junk, S_t], w=[h_out])

    def transpose_to_HT(h_bf, tt, psT, hT, dst):
        for fb in range(0, FC, 4):
            nb = min(4, FC - fb)
            ps = psT[(fb // 4) % 2]
            for j in range(nb):
                fc = fb + j
                kb.op("pe", lambda e, fc=fc, j=j: e.transpose(out=ps[:, j * 128:(j + 1) * 128],
                                                            in_=h_bf[:, fc * 128:(fc + 1) * 128], identity=ident_b[:]),
                      r=[h_bf, ident_b], w=[ps] if j == 0 else [])
                ps.w = (kb.st["pe"].sem, kb.st["pe"].cnt)
            kb.op("act", lambda e: e.activation(out=hT[:, fb:fb + nb, :],
                                                in_=ps[:, 0:nb * 128].rearrange("p (a b) -> p a b", b=128), func=AF.Copy),
                  r=[ps], w=[hT])
        kb.dma("sp", dst.ap()[:, :, tt * 128:(tt + 1) * 128].rearrange("fc p t -> p fc t"), hT[:], r=[hT])

    def stage_prenorm_T(li, x_src, gain, wsc, wsh):
        kb.begin()
        A_t = kb.sb("A", [128, D], F32)
        S_t = kb.sb("S", [128, D], F32)
        tmp = kb.sb("tmp", [128, D], F32)
        xts = [kb.sb(f"xt{i}", [128, D], F32) for i in range(2)]
        ws = make_rms_ws()
        hbs = [kb.sb(f"hb{i}", [128, D], BF16) for i in range(2)]
        hTs = [kb.sb(f"hT{i}", [128, FC, 128], BF16) for i in range(2)]
        psT = [kb.ps(f"psT{i}", [128, 512], BF16) for i in range(2)]
        for tt in range(NTT):
            u = (tt * 128) // UNIT
            if (tt * 128) % UNIT == 0:
                load_mod_rows(li, u, wsc, wsh, gain.ap()[li:li + 1, :], A_t, S_t, tmp)
            xt = xts[tt % 2]
            kb.dma("sp", xt[:], x_src.ap()[tt * 128:(tt + 1) * 128, :], w=[xt])
            hb = hbs[tt % 2]
            rms_modulate(xt, A_t, S_t, hb, ws)
            transpose_to_HT(hb, tt, psT, hTs[tt % 2], HT)
        kb.end()

    def stage_conv_in():
        kb.begin()
        FGS = min(4, FC)
        bin_t = kb.sb("bin", [128, 2 * FC], F32)
        kb.dma("sp", bin_t[:], conv_b_in.ap(), w=[bin_t])
        was = [kb.sb(f"wa{i}", [128, FC, FGS * 128], BF16) for i in range(2)]
        wgs = [kb.sb(f"wg{i}", [128, FC, FGS * 128], BF16) for i in range(2)]
        hts = [kb.sb(f"ht{i}", [128, FC, 512], BF16) for i in range(2)]
        psa = [kb.ps(f"psa{i}", [128, 512]) for i in range(2)]
        psg = [kb.ps(f"psg{i}", [128, 512]) for i in range(2)]
        sgs = [kb.sb(f"sg{i}", [128, 512], F32) for i in range(2)]
        uts = [kb.sb(f"ut{i}", [128, 512], BF16) for i in range(2)]
        wv = conv_w_in.ap().rearrange("(kc p) n -> p kc n", p=128)
        it = 0
        for fg in range(FC // FGS):
            wa, wg = was[fg % 2], wgs[fg % 2]
            kb.dma("gq", wa[:], wv[:, :, fg * FGS * 128:(fg + 1) * FGS * 128], w=[wa])
            kb.dma("gq", wg[:], wv[:, :, D + fg * FGS * 128:D + (fg + 1) * FGS * 128], w=[wg])
            for tb in range(NTB):
                ht = hts[tb % 2]
                kb.dma("sp", ht[:], HT.ap()[:, :, tb * 512:(tb + 1) * 512].rearrange("fc p t -> p fc t"), w=[ht])
                for j in range(FGS):
                    f = fg * FGS + j
                    pa, pg, sg, ut = psa[it % 2], psg[it % 2], sgs[it % 2], uts[it % 2]
                    it += 1
                    for kc in range(FC):
                        kb.op("pe", lambda e, kc=kc: e.matmul(pa[:], lhsT=wa[:, kc, j * 128:(j + 1) * 128], rhs=ht[:, kc, :],
                                                             start=(kc == 0), stop=(kc == FC - 1)),
                              r=[wa, ht], w=[pa] if kc == 0 else [])
                        pa.w = (kb.st["pe"].sem, kb.st["pe"].cnt)
                    for kc in range(FC):
                        kb.op("pe", lambda e, kc=kc: e.matmul(pg[:], lhsT=wg[:, kc, j * 128:(j + 1) * 128], rhs=ht[:, kc, :],
                                                             start=(kc == 0), stop=(kc == FC - 1)),
                              r=[wg, ht], w=[pg] if kc == 0 else [])
                        pg.w = (kb.st["pe"].sem, kb.st["pe"].cnt)
                    kb.op("act", lambda e: e.activation(out=sg[:], in_=pg[:], func=AF.Sigmoid,
                                                        bias=bin_t[:, FC + f:FC + f + 1]), r=[pg, bin_t], w=[sg])
                    kb.op("dve", lambda e: e.scalar_tensor_tensor(out=ut[:], in0=pa[:], scalar=bin_t[:, f:f + 1], in1=sg[:],
                                                                 op0=ALU.add, op1=ALU.mult), r=[pa, sg, bin_t], w=[ut])
                    kb.dma("sp", UT.ap()[f, :, tb * 512:(tb + 1) * 512], ut[:], r=[ut])
        kb.end()

    def stage_dwconv():
        kb.begin()
        wdw = kb.sb("wdw", [128, FC, CONV_W], F32)
        kb.dma("sp", wdw[:], conv_w_dw.ap(), w=[wdw])
        bdw = kb.sb("bdw", [128, FC], F32)
        kb.dma("sp", bdw[:], conv_b_dw.ap(), w=[bdw])
        dgs = [kb.sb(f"dg{i}", [128, CONV_W, 128], BF16) for i in range(2)]
        uws = [kb.sb(f"uw{i}", [128, 512 + 2 * CONV_PAD], BF16) for i in range(3)]
        pss = [kb.ps(f"cps{i}", [128, 512]) for i in range(2)]
        cus = [kb.sb(f"cu{i}", [128, 512], F32) for i in range(2)]
        it = 0
        for fc in range(FC):
            dg = dgs[fc % 2]
            for k in range(CONV_W):
                eng = "dve" if k % 2 == 0 else "pool"
                kb.op(eng, lambda e, k=k: e.tensor_scalar(dg[:, k, :], ident_f[:], wdw[:, fc, k:k + 1], None, op0=ALU.mult),
                      r=[ident_f, wdw], w=[dg])
            for tb in range(NTB):
                uw = uws[it % 3]
                ps = pss[it % 2]
                cu = cus[it % 2]
                it += 1
                t0 = tb * 512
                lo = max(t0 - CONV_PAD, 0)
                hi_ = min(t0 + 512 + CONV_PAD, NT)
                if t0 == 0:
                    kb.op("pool", lambda e: e.memset(uw[:, 0:CONV_PAD], 0.0), w=[uw])
                if t0 + 512 == NT:
                    kb.op("pool", lambda e: e.memset(uw[:, CONV_PAD + 512:], 0.0), w=[uw])
                kb.dma("sp", uw[:, lo - (t0 - CONV_PAD):hi_ - (t0 - CONV_PAD)], UT.ap()[fc, :, lo:hi_], w=[uw])
                if t0 > 0 and t0 % UNIT == 0:
                    b = t0 // UNIT
                    kb.op("dve", lambda e, b=b: e.tensor_scalar(uw[:, 0:CONV_PAD], uw[:, 0:CONV_PAD], mask_t[:, b:b + 1],
                                                             None, op0=ALU.mult), r=[uw, mask_t], w=[uw])
                if t0 + 512 < NT and (t0 + 512) % UNIT == 0:
                    b = (t0 + 512) // UNIT
                    kb.op("dve", lambda e, b=b: e.tensor_scalar(uw[:, CONV_PAD + 512:], uw[:, CONV_PAD + 512:],
                                                             mask_t[:, b:b + 1], None, op0=ALU.mult),
                          r=[uw, mask_t], w=[uw])
                for k in range(CONV_W):
                    kb.op("pe", lambda e, k=k: e.matmul(ps[:], lhsT=dg[:, k, :], rhs=uw[:, k:k + 512],
                                                       start=(k == 0), stop=(k == CONV_W - 1)),
                          r=[dg, uw], w=[ps] if k == 0 else [])
                    ps.w = (kb.st["pe"].sem, kb.st["pe"].cnt)
                kb.op("act", lambda e: e.activation(out=cu[:], in_=ps[:], func=AF.Identity, bias=bdw[:, fc:fc + 1]),
                      r=[ps, bdw], w=[cu])
                kb.dma("sp", CU.ap()[fc, :, t0:t0 + 512], cu[:], r=[cu])
        kb.end()

    def stage_conv_out(li, x_src, x_dst):
        kb.begin()
        lng = kb.sb("lng", [128, FC], F32)
        lnb = kb.sb("lnb", [128, FC], F32)
        kb.dma("sp", lng[:], conv_ln_g.ap(), w=[lng])
        kb.dma("sp", lnb[:], conv_ln_b.ap(), w=[lnb])
        wout = kb.sb("wout", [128, FC, D], BF16)
        wv = conv_w_out.ap().rearrange("(kc p) n -> p kc n", p=128)
        for kc in range(FC):
            kb.dma("gq", wout[:, kc, :], wv[:, kc, :], w=[wout])
        brow = kb.sb("brow", [128, D], F32)
        row_bcast(brow, conv_b_out.ap())
        G_t = kb.sb("G", [128, D], F32)
        cuts = [kb.sb(f"cut{i}", [128, FC, 512], F32) for i in range(2)]
        sq = kb.sb("sq", [128, 512], F32)
        ps1 = kb.ps("ps1", [128, 512])
        ps2 = kb.ps("ps2", [128, 512])
        mean = kb.sb("mean", [128, 512], F32)
        rstd = kb.sb("rstdln", [128, 512], F32)
        tmp = kb.sb("tmpln", [128, 512], F32)
        vTs = [kb.sb(f"vT{i}", [128, FC, 512], BF16) for i in range(2)]
        pso = [kb.ps(f"pso{i}", [128, 512]) for i in range(2)]
        xts = [kb.sb(f"xo{i}", [128, 512], F32) for i in range(2)]
        ots = [kb.sb(f"ot{i}", [128, 512], F32) for i in range(2)]
        it = 0
        NOB = D // 512 if D >= 512 else 1
        OBW = min(512, D)
        for tb in range(NTB):
            t0 = tb * 512
            if t0 % UNIT == 0:
                row_bcast(G_t, MOD.ap()[li, t0 // UNIT:t0 // UNIT + 1, 2 * D:3 * D])
            cut = cuts[tb % 2]
            vT = vTs[tb % 2]
            kb.dma("sp", cut[:], CU.ap()[:, :, t0:t0 + 512].rearrange("fc p t -> p fc t"), w=[cut])
            for fc in range(FC):
                kb.op("pe", lambda e, fc=fc: e.matmul(ps1[:], lhsT=ones_f[:], rhs=cut[:, fc, :], start=(fc == 0), stop=(fc == FC - 1)),
                      r=[ones_f, cut], w=[ps1] if fc == 0 else [])
                ps1.w = (kb.st["pe"].sem, kb.st["pe"].cnt)
            for fc in range(FC):
                kb.op("act", lambda e, fc=fc: e.activation(out=sq[:], in_=cut[:, fc, :], func=AF.Square), r=[cut], w=[sq])
                kb.op("pe", lambda e, fc=fc: e.matmul(ps2[:], lhsT=ones_f[:], rhs=sq[:], start=(fc == 0), stop=(fc == FC - 1)),
                      r=[ones_f, sq], w=[ps2] if fc == 0 else [])
                ps2.w = (kb.st["pe"].sem, kb.st["pe"].cnt)
            kb.op("dve", lambda e: e.tensor_scalar(mean[:], ps1[:], 1.0 / D, None, op0=ALU.mult), r=[ps1], w=[mean])
            kb.op("dve", lambda e: e.tensor_tensor(out=tmp[:], in0=mean[:], in1=mean[:], op=ALU.mult), r=[mean], w=[tmp])
            kb.op("dve", lambda e: e.scalar_tensor_tensor(out=tmp[:], in0=ps2[:], scalar=1.0 / D, in1=tmp[:],
                                                         op0=ALU.mult, op1=ALU.subtract), r=[ps2, tmp], w=[tmp])
            kb.op("dve", lambda e: e.tensor_scalar(tmp[:], tmp[:], LN_EPS, None, op0=ALU.add), r=[tmp], w=[tmp])
            kb.op("act", lambda e: e.activation(out=tmp[:], in_=tmp[:], func=AF.Sqrt), r=[tmp], w=[tmp])
            kb.op("dve", lambda e: e.reciprocal(rstd[:], tmp[:]), r=[tmp], w=[rstd])
            for fc in range(FC):
                kb.op("dve", lambda e, fc=fc: e.tensor_tensor(out=cut[:, fc, :], in0=cut[:, fc, :], in1=mean[:], op=ALU.subtract),
                      r=[cut, mean], w=[cut])
                kb.op("pool", lambda e, fc=fc: e.tensor_tensor(out=cut[:, fc, :], in0=cut[:, fc, :], in1=rstd[:], op=ALU.mult),
                      r=[cut, rstd], w=[cut])
                kb.op("act", lambda e, fc=fc: e.activation(out=vT[:, fc, :], in_=cut[:, fc, :], func=AF.Silu,
                                                           scale=lng[:, fc:fc + 1], bias=lnb[:, fc:fc + 1]),
                      r=[cut, lng, lnb], w=[vT])
            for ts in range(4):
                r0 = t0 + ts * 128
                for ob in range(NOB):
                    ps = pso[it % 2]
                    xt = xts[it % 2]
                    ot = ots[it % 2]
                    it += 1
                    cs = slice(ob * OBW, (ob + 1) * OBW)
                    kb.dma("sp", xt[:, 0:OBW], x_src.ap()[r0:r0 + 128, cs], w=[xt])
                    for kc in range(FC):
                        kb.op("pe", lambda e, kc=kc: e.matmul(ps[:, 0:OBW], lhsT=vT[:, kc, ts * 128:(ts + 1) * 128], rhs=wout[:, kc, cs],
                                                             start=(kc == 0), stop=(kc == FC - 1)),
                              r=[vT, wout], w=[ps] if kc == 0 else [])
                        ps.w = (kb.st["pe"].sem, kb.st["pe"].cnt)
                    kb.op("dve", lambda e: e.tensor_tensor(out=ot[:, 0:OBW], in0=ps[:, 0:OBW], in1=brow[:, cs], op=ALU.add),
                          r=[ps, brow], w=[ot])
                    kb.op("pool", lambda e: e.tensor_tensor(out=ot[:, 0:OBW], in0=ot[:, 0:OBW], in1=G_t[:, cs], op=ALU.mult),
                          r=[ot, G_t], w=[ot])
                    kb.op("dve", lambda e: e.tensor_tensor(out=ot[:, 0:OBW], in0=ot[:, 0:OBW], in1=xt[:, 0:OBW], op=ALU.add),
                          r=[ot, xt], w=[ot])
                    kb.dma("sp", x_dst.ap()[r0:r0 + 128, cs], ot[:, 0:OBW], r=[ot])
        kb.end()


    def acc_group(ps, n, mk, r):
        for i in range(n):
            kb.op("pe", lambda e, i=i: mk(e, i), r=r, w=[ps] if i == 0 else [])
            ps.w = (kb.st["pe"].sem, kb.st["pe"].cnt)

    def stage_moe(li, x_src):
        kb.begin()
        AFF = kb.sb("AFF", [128, NTT, E], F32)
        SLOT = kb.sb("SLOT", [128, E, NTT], I32)
        pidx = kb.sb("pidx", [128, 1], F32)
        pidi = kb.sb("pidi", [128, 1], I32)
        kb.op("pool", lambda e: e.iota(pidi[:], pattern=[[0, 1]], base=0, channel_multiplier=1), w=[pidi])
        kb.op("dve", lambda e: e.tensor_copy(pidx[:], pidi[:]), r=[pidi], w=[pidx])
        ltri = kb.sb("ltri", [128, 128], F32)
        kb.op("dve", lambda e: e.tensor_scalar(ltri[:], iotf[:], 0.0, None, op0=ALU.is_gt), r=[iotf], w=[ltri])
        kb.begin()
        zt = kb.sb("zt", [128, OBW], F32)
        kb.op("pool", lambda e: e.memset(zt[:], 0.0), w=[zt])
        for ob in range(NOB):
            for tt in range(NTT):
                kb.dma("sp", YB[ob].ap()[tt * 128:(tt + 1) * 128, :], zt[:], r=[zt])
        A_t = kb.sb("A", [128, D], F32)
        S_t = kb.sb("S", [128, D], F32)
        tmp = kb.sb("tmp", [128, D], F32)
        xts = [kb.sb(f"xt{i}", [128, D], F32) for i in range(2)]
        ws = make_rms_ws()
        hfs = [kb.sb(f"hf{i}", [128, D], F32) for i in range(2)]
        hxs = [kb.sb(f"hx{i}", [128, DX], BF16) for i in range(2)]
        hTs_ = [kb.sb(f"hTf{i}", [128, FC, 128], F32) for i in range(2)]
        sms = [kb.sb(f"sm{i}", [128, 4], F32) for i in range(2)]
        exs = [kb.sb(f"ex{i}", [128, E], F32) for i in range(2)]
        wr = kb.sb("wr", [128, FC, E], F32)
        kb.dma("sp", wr[:], moe_w_router.ap()[li], w=[wr])
        psT = [kb.ps(f"psTf{i}", [128, 512], F32) for i in range(2)]
        psr = kb.ps("psr", [128, E], F32)
        for tt in range(NTT):
            u = (tt * 128) // UNIT
            if (tt * 128) % UNIT == 0:
                load_mod_rows(li, u, 4, 3, norm_ffn_g.ap()[li:li + 1, :], A_t, S_t, tmp)
            xt = xts[tt % 2]
            hf = hfs[tt % 2]
            hx = hxs[tt % 2]
            kb.dma("sp", xt[:], x_src.ap()[tt * 128:(tt + 1) * 128, :], w=[xt])
            rms_modulate(xt, A_t, S_t, hf, ws)
            hT, sm, ex = hTs_[tt % 2], sms[tt % 2], exs[tt % 2]
            kb.op("act", lambda e: e.activation(out=hx[:, 0:D], in_=hf[:], func=AF.Copy), r=[hf], w=[hx])
            kb.op("pool", lambda e, tt=tt: e.memset(hx[:, D:D + 1], float(tt)), w=[hx])
            kb.op("pool", lambda e: e.tensor_copy(hx[:, D + 1:D + 2], pidx[:]), r=[pidx], w=[hx])
            for fb in range(0, FC, 4):
                nb = min(4, FC - fb)
                ps = psT[(fb // 4) % 2]
                acc_group(ps, nb, lambda e, j, fb=fb, ps=ps: e.transpose(out=ps[:, j * 128:(j + 1) * 128],
                                                                     in_=hf[:, (fb + j) * 128:(fb + j + 1) * 128],
                                                                     identity=ident_f[:]), r=[hf, ident_f])
                kb.op("dve", lambda e, fb=fb, nb=nb, ps=ps: e.tensor_copy(
                    hT[:, fb:fb + nb, :], ps[:, 0:nb * 128].rearrange("p (a b) -> p a b", b=128)), r=[ps], w=[hT])
            acc_group(psr, FC, lambda e, kc: e.matmul(psr[:], lhsT=hT[:, kc, :], rhs=wr[:, kc, :],
                                                     start=(kc == 0), stop=(kc == FC - 1)), r=[hT, wr])
            kb.op("dve", lambda e: e.tensor_reduce(out=sm[:, 0:1], in_=psr[:], axis=AX.X, op=ALU.max), r=[psr], w=[sm])
            kb.op("dve", lambda e: e.tensor_scalar(sm[:, 1:2], sm[:, 0:1], -1.0, None, op0=ALU.mult), r=[sm], w=[sm])
            kb.op("act", lambda e: e.activation(out=ex[:], in_=psr[:], func=AF.Exp, bias=sm[:, 1:2], accum_out=sm[:, 2:3]),
                  r=[psr, sm], w=[ex, sm])
            kb.op("dve", lambda e: e.reciprocal(sm[:, 3:4], sm[:, 2:3]), r=[sm], w=[sm])
            kb.op("dve", lambda e, tt=tt: e.tensor_scalar(AFF[:, tt, :], ex[:], sm[:, 3:4], None, op0=ALU.mult),
                  r=[ex, sm], w=[AFF])
            kb.op("dve", lambda e, tt=tt: e.tensor_copy(hx[:, D + 2:DX].bitcast(F32), AFF[:, tt, :]), r=[AFF], w=[hx])
            kb.dma("sp", H.ap()[tt * 128:(tt + 1) * 128, :], hx[:], r=[hx])
        kb.end()
        kb.begin()
        AFFv = AFF[:].rearrange("p t e -> p e t")
        lo = kb.sb("lo", [128, E], F32)
        hi = kb.sb("hi", [128, E], F32)
        mid = kb.sb("mid", [128, E], F32)
        ge = kb.sb("ge", [128, E], F32)
        nge = kb.sb("nge", [128, E], F32)
        ta = kb.sb("ta", [128, E], F32)
        tb_ = kb.sb("tb", [128, E], F32)
        cnt = kb.sb("cnt", [128, E], F32)
        cmp = kb.sb("cmp", [128, E, NTT], F32)
        pst = kb.ps("pst", [128, E], F32)
        kb.op("pool", lambda e: e.memset(lo[:], 0.0), w=[lo])
        kb.op("pool", lambda e: e.memset(hi[:], 1.0), w=[hi])

        def bc(t):
            return t[:, :].unsqueeze(2).broadcast_to([128, E, NTT])

        for it in range(34):
            kb.op("dve", lambda e: e.tensor_tensor(out=mid[:], in0=lo[:], in1=hi[:], op=ALU.add), r=[lo, hi], w=[mid])
            kb.op("dve", lambda e: e.tensor_scalar(mid[:], mid[:], 0.5, None, op0=ALU.mult), r=[mid], w=[mid])
            kb.op("dve", lambda e: e.tensor_tensor(out=cmp[:], in0=AFFv, in1=bc(mid), op=ALU.is_ge), r=[AFF, mid], w=[cmp])
            kb.op("dve", lambda e: e.tensor_reduce(out=cnt[:], in_=cmp[:], axis=AX.X, op=ALU.add), r=[cmp], w=[cnt])
            kb.op("pe", lambda e: e.matmul(pst[:], lhsT=ones_f[:], rhs=cnt[:], start=True, stop=True), r=[ones_f, cnt], w=[pst])
            kb.op("dve", lambda e: e.tensor_scalar(ge[:], pst[:], float(CAP), None, op0=ALU.is_ge), r=[pst], w=[ge])
            kb.op("dve", lambda e: e.tensor_scalar(nge[:], ge[:], -1.0, 1.0, op0=ALU.mult, op1=ALU.add), r=[ge], w=[nge])
            kb.op("dve", lambda e: e.tensor_tensor(out=ta[:], in0=ge[:], in1=mid[:], op=ALU.mult), r=[ge, mid], w=[ta])
            kb.op("dve", lambda e: e.tensor_tensor(out=tb_[:], in0=nge[:], in1=lo[:], op=ALU.mult), r=[nge, lo], w=[tb_])
            kb.op("dve", lambda e: e.tensor_tensor(out=lo[:], in0=ta[:], in1=tb_[:], op=ALU.add), r=[ta, tb_], w=[lo])
            kb.op("dve", lambda e: e.tensor_tensor(out=ta[:], in0=nge[:], in1=mid[:], op=ALU.mult), r=[nge, mid], w=[ta])
            kb.op("dve", lambda e: e.tensor_tensor(out=tb_[:], in0=ge[:], in1=hi[:], op=ALU.mult), r=[ge, hi], w=[tb_])
            kb.op("dve", lambda e: e.tensor_tensor(out=hi[:], in0=ta[:], in1=tb_[:], op=ALU.add), r=[ta, tb_], w=[hi])
        rst = kb.sb("rst", [128, E, NTT], F32)
        kb.op("pool", lambda e: e.memset(rst[:], 1.0), w=[rst])
        kb.op("pool", lambda e: e.memset(rst[:, :, 0:1], 0.0), w=[rst])
        pre = kb.sb("pre", [128, E, NTT], F32)
        kb.op("dve", lambda e: e.tensor_tensor(out=cmp[:], in0=AFFv, in1=bc(lo), op=ALU.is_ge), r=[AFF, lo], w=[cmp])
        kb.op("dve", lambda e: e.tensor_tensor_scan(out=pre[:].rearrange("p a b -> p (a b)"),
                                                   data0=rst[:].rearrange("p a b -> p (a b)"),
                                                   data1=cmp[:].rearrange("p a b -> p (a b)"),
                                                   initial=0.0, op0=ALU.mult, op1=ALU.add), r=[rst, cmp], w=[pre])
        kb.op("dve", lambda e: e.tensor_copy(cnt[:], pre[:, :, NTT - 1]), r=[pre], w=[cnt])
        kb.op("pe", lambda e: e.matmul(pst[:], lhsT=ltri[:], rhs=cnt[:], start=True, stop=True), r=[ltri, cnt], w=[pst])
        kb.op("dve", lambda e: e.tensor_scalar(ta[:], pst[:], -1.0, None, op0=ALU.add), r=[pst], w=[ta])
        BIG = 1000000.0
        kb.op("dve", lambda e: e.tensor_tensor(out=pre[:], in0=pre[:], in1=bc(ta), op=ALU.add), r=[pre, ta], w=[pre])
        kb.op("dve", lambda e: e.tensor_scalar(pre[:], pre[:], -BIG, None, op0=ALU.add), r=[pre], w=[pre])
        kb.op("dve", lambda e: e.tensor_tensor(out=pre[:], in0=pre[:], in1=cmp[:], op=ALU.mult), r=[pre, cmp], w=[pre])
        kb.op("dve", lambda e: e.tensor_scalar(pre[:], pre[:], BIG, None, op0=ALU.add), r=[pre], w=[pre])
        kb.op("dve", lambda e: e.tensor_copy(SLOT[:], pre[:]), r=[pre], w=[SLOT])
        hxs = [kb.sb(f"hxd{i}", [128, DX], BF16) for i in range(3)]
        bc_reg = nc.gpsimd.to_reg(CAP - 1)
        for tt in range(NTT):
            hx = hxs[tt % 3]
            kb.dma("sp", hx[:], H.ap()[tt * 128:(tt + 1) * 128, :], w=[hx])
            for ei in range(E):
                kb.dma("gq", XS[ei].ap()[:, :], hx[:, :], r=[hx, SLOT],
                       indirect=dict(out_offset=bass.IndirectOffsetOnAxis(ap=SLOT[:, ei, tt:tt + 1], axis=0), in_offset=None,
                                     bounds_check=bc_reg, oob_is_err=False))
        kb.end()
        kb.begin()
        FBW = min(256, D)
        xsT = kb.sb("xsT", [128, FC, CAP], BF16)
        hidT = kb.sb("hidT", [128, FC, CAP], BF16)
        IDX = kb.sb("IDX", [128, NST], I32)
        GT = kb.sb("GT", [128, NST], F32)
        idf = kb.sb("idf", [128, 4], F32)
        xss = [kb.sb(f"xs{i}", [128, DX], BF16) for i in range(2)]
        wgs = [kb.sb(f"wg{i}", [128, FC, FBW], BF16) for i in range(2)]
        wus = [kb.sb(f"wu{i}", [128, FC, FBW], BF16) for i in range(2)]
        wds = [kb.sb(f"wd{i}", [128, FC, OBW], BF16) for i in range(2)]
        sgs = [kb.sb(f"sgm{i}", [128, 512], F32) for i in range(2)]
        ots = [kb.sb(f"otm{i}", [128, OBW], F32) for i in range(3)]
        psT2 = [kb.ps(f"psTb{i}", [128, 512], BF16) for i in range(2)]
        psg = [kb.ps(f"psgm{i}", [128, 512]) for i in range(2)]
        psu = [kb.ps(f"psum{i}", [128, 512]) for i in range(2)]
        pso = [kb.ps(f"psom{i}", [128, OBW]) for i in range(2)]
        ytok = kb.sb("ytok", [1, 1], F32)
        nw = 0
        nd = 0
        ni = 0
        NFB = D // FBW
        wsched = []
        for ei_ in range(E):
            wsched += [("gu", ei_, fb_) for fb_ in range(NFB)] + [("d", ei_, ob_) for ob_ in range(NOB)]
        wstate = dict(issued=0, ngu=0, nd=0, bufs={})

        def w_issue(upto):
            while wstate["issued"] <= upto and wstate["issued"] < len(wsched):
                k = wstate["issued"]
                kind, e_, b_ = wsched[k]
                if kind == "gu":
                    gv_ = moe_w["gate"][li][e_ // EG].ap()[e_ % EG].rearrange("(kc p) f -> p kc f", p=128)
                    uv_ = moe_w["up"][li][e_ // EG].ap()[e_ % EG].rearrange("(kc p) f -> p kc f", p=128)
                    wg_, wu_ = wgs[wstate["ngu"] % 2], wus[wstate["ngu"] % 2]
                    wstate["ngu"] += 1
                    kb.dma("gq", wg_[:], gv_[:, :, b_ * FBW:(b_ + 1) * FBW], w=[wg_])
                    kb.dma("gq", wu_[:], uv_[:, :, b_ * FBW:(b_ + 1) * FBW], w=[wu_])
                    wstate["bufs"][k] = (wg_, wu_)
                else:
                    dv_ = moe_w["down"][li][e_ // EG].ap()[e_ % EG].rearrange("(kc p) f -> p kc f", p=128)
                    wd_ = wds[wstate["nd"] % 2]
                    wstate["nd"] += 1
                    kb.dma("gq", wd_[:], dv_[:, :, b_ * OBW:(b_ + 1) * OBW], w=[wd_])
                    wstate["bufs"][k] = (wd_,)
                wstate["issued"] += 1

        wk = 0
        w_issue(0)
        for ei in range(E):
            for st in range(NST):
                xs = xss[st % 2]
                kb.dma("sp", xs[:], XS[ei].ap()[st * 128:(st + 1) * 128, :], w=[xs])
                for fb in range(0, FC, 4):
                    nb = min(4, FC - fb)
                    ps = psT2[(fb // 4) % 2]
                    acc_group(ps, nb, lambda e, j, fb=fb, ps=ps, xs=xs: e.transpose(
                        out=ps[:, j * 128:(j + 1) * 128], in_=xs[:, (fb + j) * 128:(fb + j + 1) * 128], identity=ident_b[:]),
                        r=[xs, ident_b])
                    kb.op("act" if (fb // 4) % 2 == 0 else "dve",
                          (lambda e, fb=fb, nb=nb, ps=ps, st=st: e.activation(
                              out=xsT[:, fb:fb + nb, st * 128:(st + 1) * 128],
                              in_=ps[:, 0:nb * 128].rearrange("p (a b) -> p a b", b=128), func=AF.Copy))
                          if (fb // 4) % 2 == 0 else
                          (lambda e, fb=fb, nb=nb, ps=ps, st=st: e.tensor_copy(
                              xsT[:, fb:fb + nb, st * 128:(st + 1) * 128],
                              ps[:, 0:nb * 128].rearrange("p (a b) -> p a b", b=128))),
                          r=[ps], w=[xsT])
                kb.op("dve", lambda e, xs=xs: e.tensor_copy(idf[:, 0:2], xs[:, D:D + 2]), r=[xs], w=[idf])
                kb.op("dve", lambda e: e.scalar_tensor_tensor(out=idf[:, 2:3], in0=idf[:, 0:1], scalar=128.0, in1=idf[:, 1:2],
                                                             op0=ALU.mult, op1=ALU.add), r=[idf], w=[idf])
                kb.op("dve", lambda e, st=st: e.tensor_copy(IDX[:, st:st + 1], idf[:, 2:3]), r=[idf], w=[IDX])
                kb.op("dve", lambda e, st=st, xs=xs, ei=ei: e.tensor_copy(
                    GT[:, st:st + 1], xs[:, D + 2 + 2 * ei:D + 4 + 2 * ei].bitcast(F32)), r=[xs], w=[GT])
            gv = moe_w["gate"][li][ei // EG].ap()[ei % EG].rearrange("(kc p) f -> p kc f", p=128)
            uv = moe_w["up"][li][ei // EG].ap()[ei % EG].rearrange("(kc p) f -> p kc f", p=128)
            dv = moe_w["down"][li][ei // EG].ap()[ei % EG].rearrange("(kc p) f -> p kc f", p=128)
            for fb in range(D // FBW):
                w_issue(wk + 1)
                wg, wu = wstate["bufs"].pop(wk)
                wk += 1
                for j in range(FBW // 128):
                    f = fb * (FBW // 128) + j
                    for sb_ in range(CAP // 512):
                        pg, pu, sg = psg[ni % 2], psu[ni % 2], sgs[ni % 2]
                        ni += 1
                        cs = slice(sb_ * 512, (sb_ + 1) * 512)
                        acc_group(pg, FC, lambda e, kc, pg=pg, wg=wg, j=j, cs=cs: e.matmul(
                            pg[:], lhsT=wg[:, kc, j * 128:(j + 1) * 128], rhs=xsT[:, kc, cs],
                            start=(kc == 0), stop=(kc == FC - 1)), r=[wg, xsT])
                        acc_group(pu, FC, lambda e, kc, pu=pu, wu=wu, j=j, cs=cs: e.matmul(
                            pu[:], lhsT=wu[:, kc, j * 128:(j + 1) * 128], rhs=xsT[:, kc, cs],
                            start=(kc == 0), stop=(kc == FC - 1)), r=[wu, xsT])
                        kb.op("act", lambda e, pg=pg, sg=sg: e.activation(out=sg[:], in_=pg[:], func=AF.Silu), r=[pg], w=[sg])
                        kb.op("dve", lambda e, pu=pu, sg=sg, f=f, cs=cs: e.tensor_tensor(
                            out=hidT[:, f, cs], in0=pu[:], in1=sg[:], op=ALU.mult), r=[pu, sg], w=[hidT])
            for ob in range(NOB):
                w_issue(wk + 1)
                (wd,) = wstate["bufs"].pop(wk)
                wk += 1
                for st in range(NST):
                    po = pso[ni % 2]
                    ot = ots[ni % 3]
                    ni += 1
                    acc_group(po, FC, lambda e, kc, po=po, wd=wd, st=st: e.matmul(
                        po[:], lhsT=hidT[:, kc, st * 128:(st + 1) * 128], rhs=wd[:, kc, :],
                        start=(kc == 0), stop=(kc == FC - 1)), r=[hidT, wd])
                    kb.op("dve" if ni % 2 else "act",
                          (lambda e, po=po, ot=ot, st=st: e.tensor_scalar(ot[:], po[:], GT[:, st:st + 1], None, op0=ALU.mult))
                          if ni % 2 else
                          (lambda e, po=po, ot=ot, st=st: e.activation(out=ot[:], in_=po[:], func=AF.Identity, scale=GT[:, st:st + 1])),
                          r=[po, GT], w=[ot])
                    kb.dma("gq", YB[ob].ap()[:, :], ot[:, :], r=[ot, IDX], w=[ytok],
                           indirect=dict(out_offset=bass.IndirectOffsetOnAxis(ap=IDX[:, st:st + 1], axis=0), in_offset=None,
                                         compute_op=ALU.add))
        kb.end()
        kb.end()

    def stage_combine(li, x_src, x_dst, final):
        kb.begin()
        G_t = kb.sb("G2", [128, D], F32)
        F_t = kb.sb("Fg", [128, D], F32)
        if final:
            row_bcast(F_t, final_g.ap())
        xts = [kb.sb(f"xc{i}", [128, D], F32) for i in range(2)]
        yts = [kb.sb(f"yc{i}", [128, D], F32) for i in range(2)]
        junk = kb.sb("junkc", [128, D], F32)
        ssum = kb.sb("ssumc", [128, 1], F32)
        rstd = kb.sb("rstdc", [128, 3], F32)
        for tt in range(NTT):
            if (tt * 128) % UNIT == 0:
                u = (tt * 128) // UNIT
                row_bcast(G_t, MOD.ap()[li, u:u + 1, 5 * D:6 * D])
            xt, yt = xts[tt % 2], yts[tt % 2]
            kb.dma("sp", xt[:], x_src.ap()[tt * 128:(tt + 1) * 128, :], w=[xt])
            for ob in range(NOB):
                kb.dma("sp", yt[:, ob * OBW:(ob + 1) * OBW], YB[ob].ap()[tt * 128:(tt + 1) * 128, :], w=[yt])
            kb.op("pool", lambda e, yt=yt: e.tensor_tensor(out=yt[:], in0=yt[:], in1=G_t[:], op=ALU.mult), r=[yt, G_t], w=[yt])
            kb.op("dve", lambda e, xt=xt, yt=yt: e.tensor_tensor(out=xt[:], in0=xt[:], in1=yt[:], op=ALU.add), r=[xt, yt], w=[xt])
            if final:
                kb.op("act", lambda e, xt=xt: e.activation(out=junk[:], in_=xt[:], func=AF.Square, accum_out=ssum[:, 0:1]),
                      r=[xt], w=[junk, ssum])
                kb.op("dve", lambda e: e.tensor_scalar(rstd[:, 0:1], ssum[:, 0:1], 1.0 / D, RMS_EPS, op0=ALU.mult, op1=ALU.add),
                      r=[ssum], w=[rstd])
                kb.op("act", lambda e: e.activation(out=rstd[:, 1:2], in_=rstd[:, 0:1], func=AF.Sqrt), r=[rstd], w=[rstd])
                kb.op("dve", lambda e: e.reciprocal(rstd[:, 2:3], rstd[:, 1:2]), r=[rstd], w=[rstd])
                kb.op("dve", lambda e, xt=xt, yt=yt: e.scalar_tensor_tensor(out=yt[:], in0=xt[:], scalar=rstd[:, 2:3], in1=F_t[:],
                                                                       op0=ALU.mult, op1=ALU.mult), r=[xt, rstd, F_t], w=[yt])
                kb.dma("sp", x_dst.ap()[tt * 128:(tt + 1) * 128, :], yt[:], r=[yt])
            else:
                kb.dma("sp", x_dst.ap()[tt * 128:(tt + 1) * 128, :], xt[:], r=[xt])
        kb.end()


    def stage_prenorm_rows(li, x_src, gain, wsc, wsh, dst):
        kb.begin()
        A_t = kb.sb("A", [128, D], F32)
        S_t = kb.sb("S", [128, D], F32)
        tmp = kb.sb("tmp", [128, D], F32)
        xts = [kb.sb(f"xt{i}", [128, D], F32) for i in range(2)]
        ws = make_rms_ws()
        hbs = [kb.sb(f"hb{i}", [128, D], BF16) for i in range(2)]
        for tt in range(NTT):
            u = (tt * 128) // UNIT
            if (tt * 128) % UNIT == 0:
                load_mod_rows(li, u, wsc, wsh, gain.ap()[li:li + 1, :], A_t, S_t, tmp)
            xt = xts[tt % 2]
            kb.dma("sp", xt[:], x_src.ap()[tt * 128:(tt + 1) * 128, :], w=[xt])
            hb = hbs[tt % 2]
            rms_modulate(xt, A_t, S_t, hb, ws)
            kb.dma("sp", dst.ap()[tt * 128:(tt + 1) * 128, :], hb[:], r=[hb])
        kb.end()

    def stage_rows_to_T(src):
        kb.begin()
        hbs = [kb.sb(f"hbr{i}", [128, D], BF16) for i in range(2)]
        hTs = [kb.sb(f"hTr{i}", [128, FC, 128], BF16) for i in range(2)]
        psT = [kb.ps(f"psTr{i}", [128, 512], BF16) for i in range(2)]
        for tt in range(NTT):
            hb = hbs[tt % 2]
            kb.dma("sp", hb[:], src.ap()[tt * 128:(tt + 1) * 128, :], w=[hb])
            transpose_to_HT(hb, tt, psT, hTs[tt % 2], HT)
        kb.end()

    def stage_s5():
        kb.begin()
        CW = min(512, NB)
        NH = NB // CW
        NNT = NB // 128
        TWO_PI = 2.0 * np.pi
        isb = kb.sb("isb", [128, 1], F32)
        pidi = kb.sb("pidi5", [128, 1], I32)
        kb.op("pool", lambda e: e.iota(pidi[:], pattern=[[0, 1]], base=0, channel_multiplier=1), w=[pidi])
        kb.op("dve", lambda e: e.tensor_copy(isb[:], pidi[:]), r=[pidi], w=[isb])
        kb.op("dve", lambda e: e.tensor_scalar(isb[:], isb[:], 63.5, None, op0=ALU.is_gt), r=[isb], w=[isb])
        sign = kb.sb("sign", [128, 1], F32)
        kb.op("dve", lambda e: e.tensor_scalar(sign[:], isb[:], 2.0, -1.0, op0=ALU.mult, op1=ALU.add), r=[isb], w=[sign])
        nsign = kb.sb("nsign", [128, 1], F32)
        kb.op("dve", lambda e: e.tensor_scalar(nsign[:], sign[:], -1.0, None, op0=ALU.mult), r=[sign], w=[nsign])
        iri = kb.sb("iri", [128, 8], I32)
        kb.op("pool", lambda e: e.iota(iri[:], pattern=[[1, 8]], base=0, channel_multiplier=0), w=[iri])
        EX = kb.sb("EX", [128, 4, 8], F32)
        kb.op("dve", lambda e: e.tensor_copy(EX[:, 0, :], iri[:]), r=[iri], w=[EX])
        kb.op("dve", lambda e: e.tensor_scalar(EX[:, 0, :], EX[:, 0, :], sign[:, 0:1], None, op0=ALU.mult), r=[EX, sign], w=[EX])
        kb.op("dve", lambda e: e.tensor_scalar(EX[:, 1, :], EX[:, 0, :], -1.0, None, op0=ALU.mult), r=[EX], w=[EX])
        off3 = kb.sb("off3", [128, 2], F32)
        kb.op("dve", lambda e: e.tensor_scalar(off3[:, 0:1], isb[:], -7.0, 7.0, op0=ALU.mult, op1=ALU.add), r=[isb], w=[off3])
        kb.op("dve", lambda e: e.tensor_scalar(off3[:, 1:2], isb[:], 7.0, 1.0, op0=ALU.mult, op1=ALU.add), r=[isb], w=[off3])
        kb.op("dve", lambda e: e.tensor_scalar(EX[:, 2, :], EX[:, 0, :], off3[:, 0:1], None, op0=ALU.add), r=[EX, off3], w=[EX])
        kb.op("dve", lambda e: e.tensor_scalar(EX[:, 3, :], EX[:, 1, :], off3[:, 1:2], None, op0=ALU.add), r=[EX, off3], w=[EX])
        tni = kb.sb("tni", [128, NB], I32)
        kb.op("pool", lambda e: e.iota(tni[:], pattern=[[1, NB]], base=0, channel_multiplier=0), w=[tni])
        nrow = kb.sb("nrow", [128, NB], F32)
        kb.op("dve", lambda e: e.tensor_copy(nrow[:], tni[:]), r=[tni], w=[nrow])
        KEEP = kb.sb("KEEP", [128, NB], F32)
        kb.dma("sp", KEEP[:], s5_keep.ap(), w=[KEEP])
        MK = kb.sb("MK", [128, 2, 128], F32)
        kb.dma("sp", MK[:], s5_masks.ap(), w=[MK])
        lre = kb.sb("lre", [128, G], F32)
        lim = kb.sb("lim", [128, G], F32)
        lst = kb.sb("lst", [128, G], F32)
        kb.dma("sp", lre[:], ssm_lre.ap(), w=[lre])
        kb.dma("sp", lim[:], ssm_lim.ap(), w=[lim])
        kb.dma("sp", lst[:], ssm_ls.ap(), w=[lst])
        dcol = kb.sb("dcol", [128, G], F32)
        kb.dma("sp", dcol[:], ssm_dcol.ap(), w=[dcol])
        dt = kb.sb("dt", [128, G], F32)
        kb.op("act", lambda e: e.activation(out=dt[:], in_=lst[:], func=AF.Exp), r=[lst], w=[dt])
        lrdt = kb.sb("lrdt", [128, G], F32)
        kb.op("dve", lambda e: e.tensor_tensor(out=lrdt[:], in0=lre[:], in1=dt[:], op=ALU.mult), r=[lre, dt], w=[lrdt])
        f0 = kb.sb("f0", [128, G], F32)
        ti = kb.sb("ti", [128, G], I32)
        tf = kb.sb("tf", [128, G], F32)

        def frac_(t_f, t_i, t_tmp, eng="dve"):
            kb.op(eng, lambda e: e.tensor_copy(t_i, t_f), r=[], w=[])
            kb.op(eng, lambda e: e.tensor_copy(t_tmp, t_i), r=[], w=[])
            kb.op(eng, lambda e: e.tensor_tensor(out=t_f, in0=t_f, in1=t_tmp, op=ALU.subtract), r=[], w=[])

        kb.op("dve", lambda e: e.tensor_tensor(out=f0[:], in0=lim[:], in1=dt[:], op=ALU.mult), r=[lim, dt], w=[f0])
        kb.op("dve", lambda e: e.tensor_scalar(f0[:], f0[:], 1.0 / TWO_PI, None, op0=ALU.mult), r=[f0], w=[f0])
        def frac_tiles(F, I_, T, eng="dve"):
            kb.op(eng, lambda e: e.tensor_copy(I_[:], F[:]), r=[F], w=[I_])
            kb.op(eng, lambda e: e.tensor_copy(T[:], I_[:]), r=[I_], w=[T])
            kb.op(eng, lambda e: e.tensor_tensor(out=F[:], in0=F[:], in1=T[:], op=ALU.subtract), r=[F, T], w=[F])

        frac_tiles(f0, ti, tf)
        are = kb.sb("are", [128, G], F32)
        aim = kb.sb("aim", [128, G], F32)
        mag = kb.sb("mag", [128, G], F32)
        fc_ = kb.sb("fcq", [128, G], F32)
        kb.op("act", lambda e: e.activation(out=mag[:], in_=lrdt[:], func=AF.Exp), r=[lrdt], w=[mag])
        kb.op("act", lambda e: e.activation(out=aim[:], in_=f0[:], func=AF.Sin, scale=TWO_PI), r=[f0], w=[aim])
        kb.op("dve", lambda e: e.tensor_scalar(fc_[:], f0[:], 0.25, None, op0=ALU.add), r=[f0], w=[fc_])
        frac_tiles(fc_, ti, tf)
        kb.op("act", lambda e: e.activation(out=are[:], in_=fc_[:], func=AF.Sin, scale=TWO_PI), r=[fc_], w=[are])
        kb.op("dve", lambda e: e.tensor_tensor(out=are[:], in0=are[:], in1=mag[:], op=ALU.mult), r=[are, mag], w=[are])
        kb.op("dve", lambda e: e.tensor_tensor(out=aim[:], in0=aim[:], in1=mag[:], op=ALU.mult), r=[aim, mag], w=[aim])
        kre = kb.sb("kre", [128, G], F32)
        kim = kb.sb("kim", [128, G], F32)
        den = kb.sb("den", [128, G], F32)
        t1 = kb.sb("t1g", [128, G], F32)
        nr = kb.sb("nr", [128, G], F32)
        kb.op("dve", lambda e: e.tensor_scalar(nr[:], are[:], -1.0, None, op0=ALU.add), r=[are], w=[nr])
        kb.op("dve", lambda e: e.tensor_tensor(out=den[:], in0=lre[:], in1=lre[:], op=ALU.mult), r=[lre], w=[den])
        kb.op("dve", lambda e: e.tensor_tensor(out=t1[:], in0=lim[:], in1=lim[:], op=ALU.mult), r=[lim], w=[t1])
        kb.op("dve", lambda e: e.tensor_tensor(out=den[:], in0=den[:], in1=t1[:], op=ALU.add), r=[den, t1], w=[den])
        kb.op("dve", lambda e: e.reciprocal(den[:], den[:]), r=[den], w=[den])
        kb.op("dve", lambda e: e.tensor_tensor(out=kre[:], in0=nr[:], in1=lre[:], op=ALU.mult), r=[nr, lre], w=[kre])
        kb.op("dve", lambda e: e.tensor_tensor(out=t1[:], in0=aim[:], in1=lim[:], op=ALU.mult), r=[aim, lim], w=[t1])
        kb.op("dve", lambda e: e.tensor_tensor(out=kre[:], in0=kre[:], in1=t1[:], op=ALU.add), r=[kre, t1], w=[kre])
        kb.op("dve", lambda e: e.tensor_tensor(out=kre[:], in0=kre[:], in1=den[:], op=ALU.mult), r=[kre, den], w=[kre])
        kb.op("dve", lambda e: e.tensor_tensor(out=kim[:], in0=aim[:], in1=lre[:], op=ALU.mult), r=[aim, lre], w=[kim])
        kb.op("dve", lambda e: e.tensor_tensor(out=t1[:], in0=nr[:], in1=lim[:], op=ALU.mult), r=[nr, lim], w=[t1])
        kb.op("dve", lambda e: e.tensor_tensor(out=kim[:], in0=kim[:], in1=t1[:], op=ALU.subtract), r=[kim, t1], w=[kim])
        kb.op("dve", lambda e: e.tensor_tensor(out=kim[:], in0=kim[:], in1=den[:], op=ALU.mult), r=[kim, den], w=[kim])
        R8 = kb.sb("R8", [128, G], F32)
        kb.op("act", lambda e: e.activation(out=R8[:], in_=lrdt[:], func=AF.Exp, scale=8.0), r=[lrdt], w=[R8])
        f8 = kb.sb("f8", [128, G], F32)
        kb.op("dve", lambda e: e.tensor_scalar(f8[:], f0[:], 8.0, None, op0=ALU.mult), r=[f0], w=[f8])
        frac_tiles(f8, ti, tf)
        kb.op("dve", lambda e: e.tensor_scalar(f8[:], f8[:], nsign[:, 0:1], None, op0=ALU.mult), r=[f8, nsign], w=[f8])

        GC = 8
        bre = kb.sb("bre", [128, GC, 16], F32)
        bim = kb.sb("bim", [128, GC, 16], F32)
        cre = kb.sb("cre", [128, GC, 16], F32)
        cim = kb.sb("cim", [128, GC, 16], F32)
        bbre = kb.sb("bbre", [128, GC, 16], F32)
        bbim = kb.sb("bbim", [128, GC, 16], F32)
        tb16 = kb.sb("tb16", [128, GC, 16], F32)
        marg = kb.sb("marg", [128, GC, 32], F32)
        turn = kb.sb("turn", [128, GC, 32], F32)
        turc = kb.sb("turc", [128, GC, 32], F32)
        tui = kb.sb("tui", [128, GC, 32], I32)
        tuf = kb.sb("tuf", [128, GC, 32], F32)
        Ere = kb.sb("Ere", [128, GC, 4, 8], F32)
        Eim = kb.sb("Eim", [128, GC, 4, 8], F32)
        Pre = kb.sb("Pre", [128, GC, 8, 16], F32)
        Pim = kb.sb("Pim", [128, GC, 8, 16], F32)
        Qre = kb.sb("Qre", [128, GC, 8, 16], F32)
        Qim = kb.sb("Qim", [128, GC, 8, 16], F32)
        Lre = kb.sb("Lre", [128, GC, 8, 16], F32)
        Lim = kb.sb("Lim", [128, GC, 8, 16], F32)
        Xre = kb.sb("QXre", [128, GC, 8, 16], F32)
        Xim = kb.sb("QXim", [128, GC, 8, 16], F32)
        QXreb = kb.sb("QXreb", [128, 2, GC, 128], BF16)
        QXimb = kb.sb("QXimb", [128, 2, GC, 128], BF16)
        nisb = kb.sb("nisb", [128, 1], F32)
        kb.op("dve", lambda e: e.tensor_scalar(nisb[:], isb[:], -1.0, 1.0, op0=ALU.mult, op1=ALU.add), r=[isb], w=[nisb])
        ta4 = kb.sb("ta4", [128, GC, 8, 16], F32)
        tb4 = kb.sb("tb4", [128, GC, 8, 16], F32)
        HN = kb.sb("HN", [128, NNT, 8, 128], BF16)
        YN = kb.sb("YN", [128, NNT, 8, 128], BF16)
        HG = kb.sb("HG", [128, NNT, 8, 128], BF16)
        Ug = [kb.sb(f"Ug{i}", [128, NB], BF16) for i in range(2)]
        W0bs = [kb.sb(f"W0b{i}", [128, 128], BF16) for i in range(2)]
        tW = kb.sb("tW", [128, 128], F32)
        LTres = [kb.sb(f"LTre{i}", [128, 128], BF16) for i in range(2)]
        LTims = [kb.sb(f"LTim{i}", [128, 128], BF16) for i in range(2)]
        cns = [kb.sb(f"cn{i}", [128, NB], F32) for i in range(2)]
        sns = [kb.sb(f"sn{i}", [128, NB], F32) for i in range(2)]
        RKs = [kb.sb(f"RK{i}", [128, NB], F32) for i in range(2)]
        tnf = kb.sb("tnf", [128, NB], F32)
        frt = kb.sb("frt", [128, NB], F32)
        abt = tnf
        wr_ = kb.sb("wr5", [128, NB], F32)
        wi_ = kb.sb("wi5", [128, NB], F32)
        zr = kb.sb("zr", [128, NB], F32)
        zi = kb.sb("zi", [128, NB], F32)
        ta = kb.sb("ta5", [128, NB], F32)
        tb2 = kb.sb("tb5", [128, NB], F32)
        XPr = kb.sb("XPr", [128, NB + 2], BF16)
        XPi = kb.sb("XPi", [128, NB + 2], BF16)
        kb.op("pool", lambda e: e.memset(XPr[:], 0.0), w=[XPr])
        kb.op("pool", lambda e: e.memset(XPi[:], 0.0), w=[XPi])
        ysk = ta
        yg = kb.sb("yg", [128, NB], BF16)
        halfpi = kb.sb("halfpi", [128, 1], F32)
        kb.op("pool", lambda e: e.memset(halfpi[:], float(np.pi / 2)), w=[halfpi])
        psS = [kb.ps(f"psS{i}", [128, CW]) for i in range(2)]
        psY = [kb.ps(f"psY{i}", [128, CW]) for i in range(NH)] if NH <= 2 else None
        psU = kb.ps("psU", [128, 512])
        psM = kb.ps("psM", [128, 256])
        psM2 = kb.ps("psM2", [128, 256])
        psB = kb.ps("psB", [128, 1024], BF16)

        def bc3(t, gsl, n):
            return t[:, gsl].unsqueeze(2).broadcast_to([128, GC, n])

        def cmul_outer(Er, Ei, Br, Bi, outr, outi, neg_im):
            def eb(E_ap):
                return E_ap.unsqueeze(3).broadcast_to([128, GC, 8, 16])

            def bb(B):
                return B[:].unsqueeze(2).broadcast_to([128, GC, 8, 16])
            kb.op("dve", lambda e: e.tensor_tensor(out=ta4[:], in0=eb(Er), in1=bb(Br), op=ALU.mult), r=[Ere, Eim, Br], w=[ta4])
            kb.op("dve", lambda e: e.tensor_tensor(out=tb4[:], in0=eb(Ei), in1=bb(Bi), op=ALU.mult), r=[Ere, Eim, Bi], w=[tb4])
            kb.op("dve", lambda e: e.tensor_tensor(out=outr[:], in0=ta4[:], in1=tb4[:], op=ALU.subtract), r=[ta4, tb4], w=[outr])
            kb.op("dve", lambda e: e.tensor_tensor(out=ta4[:], in0=eb(Er), in1=bb(Bi), op=ALU.mult), r=[Ere, Eim, Bi, outr], w=[ta4])
            kb.op("dve", lambda e: e.tensor_tensor(out=tb4[:], in0=eb(Ei), in1=bb(Br), op=ALU.mult), r=[Ere, Eim, Br, outr], w=[tb4])
            if neg_im:
                kb.op("dve", lambda e: e.tensor_tensor(out=outi[:], in0=ta4[:], in1=tb4[:], op=ALU.add), r=[ta4, tb4], w=[outi])
                kb.op("dve", lambda e: e.tensor_scalar(outi[:], outi[:], -1.0, None, op0=ALU.mult), r=[outi], w=[outi])
            else:
                kb.op("dve", lambda e: e.tensor_tensor(out=outi[:], in0=ta4[:], in1=tb4[:], op=ALU.add), r=[ta4, tb4], w=[outi])

        for fc in range(FC):
            gsl = slice(fc * GC, (fc + 1) * GC)
            kb.dma("sp", bre[:], ssm_bre.ap()[:, gsl, :], w=[bre])
            kb.dma("sp", bim[:], ssm_bim.ap()[:, gsl, :], w=[bim])
            kb.dma("sp", cre[:], ssm_cre.ap()[:, gsl, :], w=[cre])
            kb.dma("sp", cim[:], ssm_cim.ap()[:, gsl, :], w=[cim])
            kb.op("dve", lambda e: e.tensor_tensor(out=bbre[:], in0=bre[:], in1=bc3(kre, gsl, 16), op=ALU.mult), r=[bre, kre], w=[bbre])
            kb.op("dve", lambda e: e.tensor_tensor(out=tb16[:], in0=bim[:], in1=bc3(kim, gsl, 16), op=ALU.mult), r=[bim, kim], w=[tb16])
            kb.op("dve", lambda e: e.tensor_tensor(out=bbre[:], in0=bbre[:], in1=tb16[:], op=ALU.subtract), r=[bbre, tb16], w=[bbre])
            kb.op("dve", lambda e: e.tensor_tensor(out=bbim[:], in0=bim[:], in1=bc3(kre, gsl, 16), op=ALU.mult), r=[bim, kre], w=[bbim])
            kb.op("dve", lambda e: e.tensor_tensor(out=tb16[:], in0=bre[:], in1=bc3(kim, gsl, 16), op=ALU.mult), r=[bre, kim, bbre], w=[tb16])
            kb.op("dve", lambda e: e.tensor_tensor(out=bbim[:], in0=bbim[:], in1=tb16[:], op=ALU.add), r=[bbim, tb16], w=[bbim])
            exb = EX[:].rearrange("p a b -> p (a b)").unsqueeze(1).broadcast_to([128, GC, 32])
            kb.op("dve", lambda e: e.tensor_tensor(out=marg[:], in0=bc3(lrdt, gsl, 32), in1=exb, op=ALU.mult), r=[lrdt, EX], w=[marg])
            kb.op("act", lambda e: e.activation(out=marg[:], in_=marg[:], func=AF.Exp), r=[marg], w=[marg])
            kb.op("dve", lambda e: e.tensor_tensor(out=turn[:], in0=bc3(f0, gsl, 32), in1=exb, op=ALU.mult), r=[f0, EX], w=[turn])
            frac_tiles(turn, tui, tuf)
            kb.op("dve", lambda e: e.tensor_scalar(turc[:], turn[:], 0.25, None, op0=ALU.add), r=[turn], w=[turc])
            frac_tiles(turc, tui, tuf)
            Ef_re = Ere[:].rearrange("p g a b -> p g (a b)")
            Ef_im = Eim[:].rearrange("p g a b -> p g (a b)")
            kb.op("act", lambda e: e.activation(out=Ef_im, in_=turn[:], func=AF.Sin, scale=TWO_PI), r=[turn], w=[Eim])
            kb.op("act", lambda e: e.activation(out=Ef_re, in_=turc[:], func=AF.Sin, scale=TWO_PI), r=[turc], w=[Ere])
            kb.op("dve", lambda e: e.tensor_tensor(out=Ef_im, in0=Ef_im, in1=marg[:], op=ALU.mult), r=[Eim, marg], w=[Eim])
            kb.op("dve", lambda e: e.tensor_tensor(out=Ef_re, in0=Ef_re, in1=marg[:], op=ALU.mult), r=[Ere, marg], w=[Ere])
            cmul_outer(Ere[:, :, 0, :], Eim[:, :, 0, :], bbre, bbim, Pre, Pim, False)
            cmul_outer(Ere[:, :, 1, :], Eim[:, :, 1, :], cre, cim, Qre, Qim, True)
            cmul_outer(Ere[:, :, 2, :], Eim[:, :, 2, :], bbre, bbim, Lre, Lim, False)
            cmul_outer(Ere[:, :, 3, :], Eim[:, :, 3, :], cre, cim, Xre, Xim, True)
            for d_, sc_ in ((0, nisb), (1, isb)):
                kb.op("act", lambda e, d_=d_, sc_=sc_: e.activation(out=QXreb[:, d_], in_=Xre[:].rearrange("p g a b -> p g (a b)"),
                                                                  func=AF.Copy, scale=sc_[:, 0:1]), r=[Xre, sc_], w=[QXreb])
                kb.op("act", lambda e, d_=d_, sc_=sc_: e.activation(out=QXimb[:, d_], in_=Xim[:].rearrange("p g a b -> p g (a b)"),
                                                                  func=AF.Copy, scale=sc_[:, 0:1]), r=[Xim, sc_], w=[QXimb])
            for nt in range(NNT):
                src = HS.ap()[nt * 1024:(nt + 1) * 1024, fc * 128:(fc + 1) * 128].rearrange("(n i) c -> n i c", i=8)
                kb.dma("sp", HN[:, nt, :, :], src, w=[HN])
            for nt in range(NNT):
                kb.op("act", lambda e, nt=nt: e.activation(out=HG[:, nt, :, :].rearrange("p g (i c) -> p g i c", c=16),
                                                           in_=HN[:, nt, :, :].rearrange("p i (g c) -> p g i c", c=16), func=AF.Copy),
                      r=[HN], w=[HG])
            Pr = Pre[:].rearrange("p g a b -> p g (a b)")
            Pi = Pim[:].rearrange("p g a b -> p g (a b)")
            Qr = Qre[:].rearrange("p g a b -> p g (a b)")
            Qi = Qim[:].rearrange("p g a b -> p g (a b)")
            Lr = Lre[:].rearrange("p g a b -> p g (a b)")
            Li = Lim[:].rearrange("p g a b -> p g (a b)")

            def prepA(g):
                gg = fc * GC + g
                U = Ug[g % 2]
                LTre, LTim = LTres[g % 2], LTims[g % 2]
                kb.op("dve", lambda e: e.tensor_scalar(tni[:], nrow[:], f8[:, gg:gg + 1], None, op0=ALU.mult), r=[nrow, f8], w=[tni])
                for nb in range(0, NNT, 4):
                    k4 = min(4, NNT - nb)
                    acc_group(psU, k4, lambda e, j, nb=nb: e.matmul(
                        psU[:, j * 128:(j + 1) * 128], lhsT=HG[:, nb + j, g, :], rhs=ident_b[:],
                        start=True, stop=True), r=[HG, ident_b])
                    kb.op("act", lambda e, nb=nb, k4=k4: e.activation(out=U[:, nb * 128:(nb + k4) * 128], in_=psU[:, 0:k4 * 128],
                                                                     func=AF.Copy), r=[psU], w=[U])
                for d_ in range(2):
                    rs = slice(64 * d_, 64 * d_ + 64)
                    cs_ = slice(128 * d_, 128 * d_ + 128)
                    acc_group(psM, 2, lambda e, j, rs=rs, cs_=cs_: e.matmul(
                        psM[:, cs_], lhsT=(Pr if j == 0 else Pi)[rs, g, :], rhs=(Qr if j == 0 else Qi)[rs, g, :],
                        start=(j == 0), stop=(j == 1)), r=[Pre, Pim, Qre, Qim])
                acc_group(psM2, 2, lambda e, j: e.transpose(out=psM2[:, j * 128:(j + 1) * 128],
                                                            in_=(Lr if j == 0 else Li)[:, g, :], identity=ident_f[:]),
                          r=[Lre, Lim, ident_f])
                kb.op("act", lambda e: e.activation(out=LTre[:], in_=psM2[:, 0:128], func=AF.Copy), r=[psM2], w=[LTre])
                kb.op("act", lambda e: e.activation(out=LTim[:], in_=psM2[:, 128:256], func=AF.Copy), r=[psM2], w=[LTim])

            def prepB(g):
                gg = fc * GC + g
                W0b = W0bs[g % 2]
                cn, sn, RK = cns[g % 2], sns[g % 2], RKs[g % 2]
                kb.op("dve", lambda e: e.tensor_copy(tnf[:], tni[:]), r=[tni], w=[tnf])
                kb.op("dve", lambda e: e.scalar_tensor_tensor(out=frt[:], in0=nrow[:], scalar=f8[:, gg:gg + 1], in1=tnf[:],
                                                             op0=ALU.mult, op1=ALU.subtract), r=[nrow, f8, tnf], w=[frt])
                kb.op("dve", lambda e: e.tensor_tensor(out=tW[:], in0=psM[:, 0:128], in1=MK[:, 0, :], op=ALU.mult), r=[psM, MK], w=[tW])
                kb.op("dve", lambda e: e.tensor_tensor(out=W0b[:], in0=psM[:, 128:256], in1=MK[:, 1, :], op=ALU.mult), r=[psM, MK], w=[W0b])
                kb.op("dve", lambda e: e.tensor_tensor(out=W0b[:], in0=W0b[:], in1=tW[:], op=ALU.add), r=[W0b, tW], w=[W0b])
                kb.op("act", lambda e: e.activation(out=sn[:], in_=frt[:], func=AF.Sin, scale=TWO_PI), r=[frt], w=[sn])
                kb.op("act", lambda e: e.activation(out=abt[:], in_=frt[:], func=AF.Abs), r=[frt], w=[abt])
                kb.op("act", lambda e: e.activation(out=cn[:], in_=abt[:], func=AF.Sin, scale=-TWO_PI, bias=halfpi[:, 0:1]),
                      r=[abt, halfpi], w=[cn])
                kb.op("act", lambda e: e.activation(out=RK[:], in_=KEEP[:], func=AF.Copy, scale=R8[:, gg:gg + 1]), r=[KEEP, R8], w=[RK])

            def main(g, mid_hook):
                gg = fc * GC + g
                U = Ug[g % 2]
                W0b, LTre, LTim = W0bs[g % 2], LTres[g % 2], LTims[g % 2]
                cn, sn, RK = cns[g % 2], sns[g % 2], RKs[g % 2]
                for h in range(NH):
                    cs_ = slice(h * CW, (h + 1) * CW)
                    kb.op("pe", lambda e, cs_=cs_: e.matmul(psS[0][:], lhsT=LTre[:], rhs=U[:, cs_], start=True, stop=True),
                          r=[LTre, U], w=[psS[0]])
                    kb.op("pe", lambda e, cs_=cs_: e.matmul(psS[1][:], lhsT=LTim[:], rhs=U[:, cs_], start=True, stop=True),
                          r=[LTim, U], w=[psS[1]])
                    kb.op("dve", lambda e, cs_=cs_: e.tensor_tensor(out=wr_[:, cs_], in0=psS[0][:], in1=cn[:, cs_], op=ALU.mult), r=[psS[0], cn], w=[wr_])
                    kb.op("dve", lambda e, cs_=cs_: e.tensor_tensor(out=ta[:, cs_], in0=psS[1][:], in1=sn[:, cs_], op=ALU.mult), r=[psS[1], sn], w=[ta])
                    kb.op("dve", lambda e, cs_=cs_: e.tensor_tensor(out=wi_[:, cs_], in0=psS[1][:], in1=cn[:, cs_], op=ALU.mult), r=[psS[1], cn], w=[wi_])
                    kb.op("dve", lambda e, cs_=cs_: e.tensor_tensor(out=tb2[:, cs_], in0=psS[0][:], in1=sn[:, cs_], op=ALU.mult), r=[psS[0], sn], w=[tb2])
                kb.op("pool", lambda e: e.tensor_tensor(out=wr_[:], in0=wr_[:], in1=ta[:], op=ALU.add), r=[wr_, ta], w=[wr_])
                kb.op("dve", lambda e: e.tensor_tensor(out=wi_[:], in0=wi_[:], in1=tb2[:], op=ALU.subtract), r=[wi_, tb2], w=[wi_])
                mid_hook()
                for (z_, w_) in ((zr, wr_), (zi, wi_)):
                    kb.op("dve", lambda e, z_=z_, w_=w_: e.tensor_tensor_scan(out=z_[0:64, :], data0=RK[0:64, :], data1=w_[0:64, :],
                                                                          initial=0.0, op0=ALU.mult, op1=ALU.add), r=[RK, w_], w=[z_])
                    kb.op("dve", lambda e, z_=z_, w_=w_: e.tensor_tensor_scan(out=z_[64:128, ::-1], data0=RK[64:128, ::-1], data1=w_[64:128, ::-1],
                                                                          initial=0.0, op0=ALU.mult, op1=ALU.add), r=[RK, w_], w=[z_])
                kb.op("dve", lambda e: e.tensor_tensor(out=ta[:], in0=zr[:], in1=cn[:], op=ALU.mult), r=[zr, cn], w=[ta])
                kb.op("dve", lambda e: e.tensor_tensor(out=tb2[:], in0=zi[:], in1=sn[:], op=ALU.mult), r=[zi, sn], w=[tb2])
                kb.op("dve", lambda e: e.tensor_tensor(out=XPr[:, 1:NB + 1], in0=ta[:], in1=tb2[:], op=ALU.subtract), r=[ta, tb2], w=[XPr])
                kb.op("dve", lambda e: e.tensor_tensor(out=ta[:], in0=zr[:], in1=sn[:], op=ALU.mult), r=[zr, sn, XPr], w=[ta])
                kb.op("dve", lambda e: e.tensor_tensor(out=tb2[:], in0=zi[:], in1=cn[:], op=ALU.mult), r=[zi, cn, XPr], w=[tb2])
                kb.op("dve", lambda e: e.tensor_tensor(out=XPi[:, 1:NB + 1], in0=ta[:], in1=tb2[:], op=ALU.add), r=[ta, tb2], w=[XPi])
                UB = UNIT // 8
                for XP in (XPr, XPi):
                    kb.op("dve", lambda e, XP=XP: e.tensor_tensor(out=XP[0:64, UB:3 * UB + 1:UB], in0=XP[0:64, UB:3 * UB + 1:UB],
                                                                 in1=mask_t[0:64, 1:4], op=ALU.mult), r=[XP, mask_t], w=[XP])
                    kb.op("dve", lambda e, XP=XP: e.tensor_tensor(out=XP[64:128, UB + 1:3 * UB + 2:UB], in0=XP[64:128, UB + 1:3 * UB + 2:UB],
                                                                 in1=mask_t[64:128, 1:4], op=ALU.mult), r=[XP, mask_t], w=[XP])
                for h in range(NH):
                    c0 = h * CW
                    cs_ = slice(c0, c0 + CW)
                    pY = psY[h]
                    ops = [(W0b[:], U[:, cs_]),
                           (QXreb[:, 0, g, :], XPr[:, c0:c0 + CW]), (QXreb[:, 1, g, :], XPr[:, c0 + 2:c0 + CW + 2]),
                           (QXimb[:, 0, g, :], XPi[:, c0:c0 + CW]), (QXimb[:, 1, g, :], XPi[:, c0 + 2:c0 + CW + 2])]
                    acc_group(pY, 5, lambda e, j, pY=pY, ops=ops: e.matmul(pY[:], lhsT=ops[j][0], rhs=ops[j][1],
                                                                          start=(j == 0), stop=(j == 4)),
                              r=[W0b, QXreb, QXimb, U, XPr, XPi])
                    kb.op("dve", lambda e, cs_=cs_, pY=pY: e.scalar_tensor_tensor(
                        out=ysk[:, cs_], in0=U[:, cs_], scalar=dcol[:, gg:gg + 1], in1=pY[:], op0=ALU.mult, op1=ALU.add),
                        r=[U, dcol, pY], w=[ysk])
                    kb.op("act", lambda e, cs_=cs_: e.activation(out=yg[:, cs_], in_=ysk[:, cs_], func=AF.Gelu_apprx_tanh), r=[ysk], w=[yg])
                for nb in range(0, NNT, 8):
                    k8 = min(8, NNT - nb)
                    acc_group(psB, k8, lambda e, j, nb=nb: e.transpose(out=psB[:, j * 128:(j + 1) * 128],
                                                                   in_=yg[:, (nb + j) * 128:(nb + j + 1) * 128], identity=ident_b[:]),
                              r=[yg, ident_b])
                    kb.op("act", lambda e, nb=nb, k8=k8: e.activation(
                        out=YN[:, nb:nb + k8, :, g * 16:(g + 1) * 16],
                        in_=psB[:, 0:k8 * 128].rearrange("p (a i c) -> p a i c", i=8, c=16), func=AF.Copy), r=[psB], w=[YN])

            prepA(0)
            prepB(0)
            for g in range(GC):
                if g + 1 < GC:
                    prepA(g + 1)
                    main(g, lambda g=g: prepB(g + 1))
                else:
                    main(g, lambda: None)
            for nt in range(NNT):
                dst = YS.ap()[nt * 1024:(nt + 1) * 1024, fc * 128:(fc + 1) * 128].rearrange("(n i) c -> n i c", i=8)
                kb.dma("sp", dst, YN[:, nt, :, :], r=[YN])
        kb.end()

    def stage_glu_out(li, x_src, x_dst):
        kb.begin()
        CGW = min(512, D)
        was = [kb.sb(f"wga{i}", [128, FC, CGW], BF16) for i in range(2)]
        wgs = [kb.sb(f"wgg{i}", [128, FC, CGW], BF16) for i in range(2)]
        hts = [kb.sb(f"htg{i}", [128, FC, 512], BF16) for i in range(2)]
        barow = kb.sb("barow", [128, D], F32)
        bgrow = kb.sb("bgrow", [128, D], F32)
        row_bcast(barow, ssm_b_glu.ap()[:, 0:D])
        row_bcast(bgrow, ssm_b_glu.ap()[:, D:2 * D])
        G_t = kb.sb("G1s", [128, D], F32)
        psa = [kb.ps(f"psga{i}", [128, CGW]) for i in range(2)]
        psg = [kb.ps(f"psgg{i}", [128, CGW]) for i in range(2)]
        sgs = [kb.sb(f"sgg{i}", [128, CGW], F32) for i in range(2)]
        ots = [kb.sb(f"otg{i}", [128, CGW], F32) for i in range(2)]
        xts = [kb.sb(f"xtg{i}", [128, CGW], F32) for i in range(2)]
        wv = ssm_w_glu.ap().rearrange("(kc p) n -> p kc n", p=128)
        it = 0
        def glu_w(cg_):
            kb.dma("gq", was[cg_ % 2][:], wv[:, :, cg_ * CGW:(cg_ + 1) * CGW], w=[was[cg_ % 2]])
            kb.dma("gq", wgs[cg_ % 2][:], wv[:, :, D + cg_ * CGW:D + (cg_ + 1) * CGW], w=[wgs[cg_ % 2]])

        glu_w(0)
        for cg in range(D // CGW):
            cs = slice(cg * CGW, (cg + 1) * CGW)
            wa, wg = was[cg % 2], wgs[cg % 2]
            if cg + 1 < D // CGW:
                glu_w(cg + 1)
            for tb in range(NTB):
                t0 = tb * 512
                if t0 % UNIT == 0:
                    row_bcast(G_t, MOD.ap()[li, t0 // UNIT:t0 // UNIT + 1, 2 * D:3 * D])
                ht = hts[tb % 2]
                kb.dma("sp", ht[:], HT.ap()[:, :, t0:t0 + 512].rearrange("fc p t -> p fc t"), w=[ht])
                for ts in range(4):
                    r0 = t0 + ts * 128
                    pa, pg, sg, ot, xt = psa[it % 2], psg[it % 2], sgs[it % 2], ots[it % 2], xts[it % 2]
                    it += 1
                    kb.dma("sp", xt[:], x_src.ap()[r0:r0 + 128, cs], w=[xt])
                    acc_group(pa, FC, lambda e, kc, pa=pa, wa=wa, ht=ht, ts=ts: e.matmul(
                        pa[:], lhsT=ht[:, kc, ts * 128:(ts + 1) * 128], rhs=wa[:, kc, :], start=(kc == 0), stop=(kc == FC - 1)), r=[ht, wa])
                    acc_group(pg, FC, lambda e, kc, pg=pg, wg=wg, ht=ht, ts=ts: e.matmul(
                        pg[:], lhsT=ht[:, kc, ts * 128:(ts + 1) * 128], rhs=wg[:, kc, :], start=(kc == 0), stop=(kc == FC - 1)), r=[ht, wg])
                    kb.op("dve", lambda e, pg=pg, sg=sg: e.tensor_tensor(out=sg[:], in0=pg[:], in1=bgrow[:, cs], op=ALU.add), r=[pg, bgrow], w=[sg])
                    kb.op("act", lambda e, sg=sg: e.activation(out=sg[:], in_=sg[:], func=AF.Sigmoid), r=[sg], w=[sg])
                    kb.op("dve", lambda e, pa=pa, ot=ot: e.tensor_tensor(out=ot[:], in0=pa[:], in1=barow[:, cs], op=ALU.add), r=[pa, barow], w=[ot])
                    kb.op("pool", lambda e, ot=ot, sg=sg: e.tensor_tensor(out=ot[:], in0=ot[:], in1=sg[:], op=ALU.mult), r=[ot, sg], w=[ot])
                    kb.op("dve", lambda e, ot=ot: e.tensor_tensor(out=ot[:], in0=ot[:], in1=G_t[:, cs], op=ALU.mult), r=[ot, G_t], w=[ot])
                    kb.op("dve", lambda e, ot=ot, xt=xt: e.tensor_tensor(out=ot[:], in0=ot[:], in1=xt[:], op=ALU.add), r=[ot, xt], w=[ot])
                    kb.dma("sp", x_dst.ap()[r0:r0 + 128, cs], ot[:], r=[ot])
        kb.end()

    stage_mod()
    stage_prenorm_T(0, x_in, norm_mix_g, 1, 0)
    stage_conv_in()
    stage_dwconv()
    stage_conv_out(0, x_in, y_out if debug_out == "conv" else X1)
    if debug_out != "conv":
        stage_moe(0, X1)
        stage_combine(0, X1, y_out if debug_out == "moe0" else X2, final=False)
    if debug_out not in ("conv", "moe0"):
        stage_prenorm_rows(1, X2, norm_mix_g, 1, 0, HS)
        stage_s5()
        stage_rows_to_T(YS)
        stage_glu_out(1, X2, y_out if debug_out == "s5" else X3)
        if debug_out != "s5":
            stage_moe(1, X3)
            stage_combine(1, X3, y_out, final=True)

    kb.barrier()
    const_stack.close()
    kb.es.close()
    return nc


def cols(v, FC):
    return np.ascontiguousarray(np.asarray(v, np.float32).reshape(FC, 128).T)


def make_core_inputs(cfg, x, c, unit_rows, masks, p, seq_len):
    FC, D = cfg.FC, cfg.D
    cu = np.asarray(c, np.float32)[unit_rows]
    cT = np.ascontiguousarray(cu.reshape(4, FC, 128).transpose(2, 1, 0))
    m = np.ascontiguousarray(np.broadcast_to(np.asarray(masks, np.float32)[None, :], (128, 4)))
    d = {
        "x": np.ascontiguousarray(x.reshape(-1, D), dtype=np.float32),
        "cT": cT, "masks": m,
        "ada_w": p["ada_w"], "ada_b": p["ada_b"],
        "norm_mix_g": p["norm_mix_g"], "norm_ffn_g": p["norm_ffn_g"],
        "final_norm_g": p["final_norm_g"].reshape(1, D),
        "conv_w_in": p["conv_w_in"][0],
        "conv_b_in_c": cols(p["conv_b_in"][0], 2 * FC),
        "conv_w_dw_c": np.ascontiguousarray(p["conv_w_dw"][0].T.reshape(FC, 128, CONV_W).transpose(1, 0, 2)),
        "conv_b_dw_c": cols(p["conv_b_dw"][0], FC),
        "conv_ln_g_c": cols(p["conv_ln_g"][0], FC),
        "conv_ln_b_c": cols(p["conv_ln_b"][0], FC),
        "conv_w_out": p["conv_w_out"][0],
        "conv_b_out": p["conv_b_out"][0].reshape(1, D),
        "moe_w_router_c": np.ascontiguousarray(p["moe_w_router"].reshape(cfg.depth, FC, 128, cfg.E).transpose(0, 2, 1, 3)),
    }
    G = cfg.G
    def dpg(a):
        return np.ascontiguousarray(np.asarray(a, np.float32).transpose(0, 2, 1).reshape(128, G))
    d["ssm_lre"] = dpg(p["ssm_lambda_re"][0])
    d["ssm_lim"] = dpg(p["ssm_lambda_im"][0])
    d["ssm_ls"] = dpg(np.broadcast_to(np.asarray(p["ssm_log_step"][0])[:, :, None], (2, G, 64)))
    d["ssm_bre"] = np.asarray(p["ssm_b_re"][0]).transpose(0, 2, 1, 3).reshape(128, G, 16)
    d["ssm_bim"] = np.asarray(p["ssm_b_im"][0]).transpose(0, 2, 1, 3).reshape(128, G, 16)
    d["ssm_cre"] = np.asarray(p["ssm_c_re"][0]).transpose(0, 3, 1, 2).reshape(128, G, 16)
    d["ssm_cim"] = np.asarray(p["ssm_c_im"][0]).transpose(0, 3, 1, 2).reshape(128, G, 16)
    dd = np.asarray(p["ssm_d"][0], np.float32).reshape(G, 16)
    d["ssm_dcol"] = np.ascontiguousarray(np.broadcast_to(dd.T[None, :, :], (8, 16, G)).reshape(128, G))
    d["ssm_w_glu"] = p["ssm_w_glu"][0]
    d["ssm_b_glu"] = p["ssm_b_glu"][0].reshape(1, 2 * D)
    NB = cfg.NT // 8
    seqb = seq_len // 8
    keep = np.ones((128, NB), np.float32)
    n = np.arange(NB)
    keep[:64, n % seqb == 0] = 0.0
    keep[64:, n % seqb == seqb - 1] = 0.0
    d["s5_keep"] = keep
    ip = np.arange(128) // 16
    mk = np.zeros((128, 2, 128), np.float32)
    mk[:, 0, :] = (ip[None, :] >= ip[:, None])
    mk[:, 1, :] = (ip[:, None] >= ip[None, :])
    d["s5_masks"] = mk
    EG = min(cfg.E, 8)
    for nm in ("gate", "up", "down"):
        w = p["moe_w_" + nm]
        for l in range(cfg.depth):
            for h in range(cfg.E // EG):
                d[f"moe_w_{nm}_{l}_{h}"] = w[l, h * EG:(h + 1) * EG]
    return {k: np.ascontiguousarray(v, dtype=np.float32) for k, v in d.items()}


def run(cfg, x_prompt, x_sample, c_prompt, c_sample, p, debug_out=None, n_cores=8):
    nc = build_program(cfg, debug_out=debug_out)
    bp, lp = x_prompt.shape[0], x_prompt.shape[1]
    bs, ls = x_sample.shape[0], x_sample.shape[1]

    def unit_info(b, l):
        upb = 4 // b
        rows = [u // upb for u in range(4)]
        masks = [0.0] + [1.0 if (u % upb) != 0 else 0.0 for u in range(1, 4)]
        return rows, masks

    rp, mp = unit_info(bp, lp)
    rs, ms = unit_info(bs, ls)
    inp_p = make_core_inputs(cfg, x_prompt, c_prompt, rp, mp, p, lp)
    inp_s = make_core_inputs(cfg, x_sample, c_sample, rs, ms, p, ls)
    for dct in (inp_p, inp_s):
        for k in list(dct):
            if (debug_out == "conv" and k.startswith("moe_")) or (debug_out in ("conv", "moe0") and (k.startswith("ssm_") or k.startswith("s5_"))):
                del dct[k]
    in_maps = [inp_p if (i % 2 == 0) else inp_s for i in range(n_cores)]
    res = run_bass_kernel_spmd(nc, in_maps, core_ids=list(range(n_cores)))
    yp = np.asarray(res.results[0]["y"], np.float32).reshape(x_prompt.shape)
    ys = np.asarray(res.results[1]["y"], np.float32).reshape(x_sample.shape)
    return yp, ys


def kernel(**inputs):
    cfg = Cfg()
    p = {k: np.asarray(v) for k, v in inputs.items()}
    return run(cfg, p["x_prompt"], p["x_sample"], p["c_prompt"], p["c_sample"], p, n_cores=2)
```

```python
import contextlib
import numpy as np
import concourse.bass as bass
import concourse.mybir as mybir
from concourse.bass_utils import run_bass_kernel_spmd

F32 = mybir.dt.float32
BF16 = mybir.dt.bfloat16
I32 = mybir.dt.int32
AF = mybir.ActivationFunctionType
ALU = mybir.AluOpType
AX = mybir.AxisListType

RMS_EPS = 1e-6
LN_EPS = 1e-5
CONV_W = 31
CONV_PAD = 15


class Cfg:
    def __init__(s, D=2048, NT=8192, E=16, depth=2):
        s.D = D
        s.FC = D // 128
        s.NT = NT
        s.UNIT = NT // 4
        s.E = E
        s.CAP = 2 * NT // E
        s.depth = depth
        s.G = D // 16


class Tile:
    def __init__(s, t):
        s.t = t
        s.w = None
        s.r = []

    def __getitem__(s, k):
        return s.t[k]


class Stream:
    def __init__(s, h, sem=None):
        s.h = h
        s.sem = sem
        s.cnt = 0
        s.seen = {}


class KB:
    NDS = 12
    verbose = False

    def __init__(s, nc):
        s.nc = nc
        s.es = contextlib.ExitStack()
        s.st = {}
        for nm, h in (("pe", nc.tensor), ("act", nc.scalar), ("dve", nc.vector), ("pool", nc.gpsimd), ("sp", nc.sync)):
            sem = s.es.enter_context(nc.semaphore("sem_" + nm))
            s.st[nm] = Stream(h, sem)
        s.dq = {}
        for q, stn in (("sp", "sp"), ("gq", "pool"), ("aq", "act")):
            sems = [s.es.enter_context(nc.semaphore(f"dq_{q}_{i}")) for i in range(s.NDS)]
            s.dq[q] = dict(stream=s.st[stn], sems=sems, cnt=[0] * s.NDS, i=0)
        s.stage = None
        s.uid = 0
        s.pending = []
        s.defer_stores = True

    def begin(s):
        if not hasattr(s, "stk"):
            s.stk = []
        s.stk.append(s.stage)
        s.stage = contextlib.ExitStack()

    def end(s):
        s.barrier()
        if KB.verbose:
            print("stage end:", {k: v.cnt for k, v in s.st.items()}, {q: max(Q["cnt"]) for q, Q in s.dq.items()}, flush=True)
        s.stage.close()
        s.stage = s.stk.pop()

    def sb(s, name, shape, dt):
        s.uid += 1
        return Tile(s.stage.enter_context(s.nc.sbuf_tensor(f"{name}_{s.uid}", list(shape), dt)))

    def ps(s, name, shape, dt=F32):
        s.uid += 1
        return Tile(s.stage.enter_context(s.nc.psum_tensor(f"{name}_{s.uid}", list(shape), dt)))

    def _wait(s, stream, sem, val):
        key = id(sem)
        if stream.seen.get(key, 0) >= val:
            return
        stream.h.wait_ge(sem, val)
        stream.seen[key] = val

    def _deps(s, stream, r, w):
        for b in r:
            if b.w is not None:
                s._wait(stream, *b.w)
        for b in w:
            if b.w is not None:
                s._wait(stream, *b.w)
            for tok in b.r:
                s._wait(stream, *tok)

    def _mark(s, tok, r, w):
        for b in r:
            b.r.append(tok)
            if len(b.r) > 24:
                d = {}
                for sem, v in b.r:
                    d[id(sem)] = (sem, max(v, d.get(id(sem), (sem, 0))[1]))
                b.r = list(d.values())
        for b in w:
            b.w = tok
            b.r = []

    def _flush(s, force=False, wset=None):
        if not s.pending:
            return
        keep = []
        emit_all_before = -1
        for i, p in enumerate(s.pending):
            hit = wset is not None and any(id(t) in wset for t in p["r"])
            if force or p["loads"] >= 1 or hit:
                emit_all_before = i
        pend, s.pending = s.pending, []
        for i, p in enumerate(pend):
            if i <= emit_all_before:
                s._dma_now(p["q"], p["out"], p["in_"], p["r"], (), None, p["kw"])
            else:
                keep.append(p)
        s.pending = keep + s.pending

    def op(s, eng, fn, r=(), w=()):
        if s.pending:
            s._flush(wset={id(t) for t in w})
        stream = s.st[eng]
        s._deps(stream, r, w)
        inst = fn(stream.h)
        stream.cnt += 1
        inst.then_inc(stream.sem, 1)
        tok = (stream.sem, stream.cnt)
        s._mark(tok, r, w)
        return tok

    def dma(s, q, out, in_, r=(), w=(), indirect=None, **kw):
        if q == "sp" and indirect is None and s.defer_stores:
            if not w:
                s.pending.append(dict(q=q, out=out, in_=in_, r=list(r), kw=kw, loads=0))
                return None
            if s.pending:
                s._flush(wset={id(t) for t in w})
            tok = s._dma_now(q, out, in_, r, w, indirect, kw)
            for p in s.pending:
                p["loads"] += 1
            return tok
        if s.pending:
            s._flush(wset={id(t) for t in w})
        return s._dma_now(q, out, in_, r, w, indirect, kw)

    def _dma_now(s, q, out, in_, r, w, indirect, kw):
        Q = s.dq[q]
        stream = Q["stream"]
        j = Q["i"] % s.NDS
        Q["i"] += 1
        sem = Q["sems"][j]
        if Q["cnt"][j] > 0:
            s._wait(stream, sem, Q["cnt"][j])
        s._deps(stream, r, w)
        if indirect is None:
            inst = stream.h.dma_start(out=out, in_=in_, **kw)
        else:
            inst = stream.h.indirect_dma_start(out=out, in_=in_, **indirect, **kw)
        inst.then_inc(sem, 16)
        Q["cnt"][j] += 16
        tok = (sem, Q["cnt"][j])
        s._mark(tok, r, w)
        return tok

    def barrier(s):
        s._flush(force=True)
        toks = []
        for stt in s.st.values():
            if stt.cnt > 0:
                toks.append((stt.sem, stt.cnt))
        for Q in s.dq.values():
            for sem, c in zip(Q["sems"], Q["cnt"]):
                if c > 0:
                    toks.append((sem, c))
        for stt in s.st.values():
            for sem, v in toks:
                if sem is stt.sem:
                    continue
                s._wait(stt, sem, v)


def build_program(cfg, debug_out=None):
    nc = bass.Bass("TRN2", target_bir_lowering=False)
    D, FC, NT, UNIT, E = cfg.D, cfg.FC, cfg.NT, cfg.UNIT, cfg.E
    NTT = NT // 128
    NTB = NT // 512
    D6 = 6 * D

    def inp(name, shape, dt=F32):
        return nc.dram_tensor(name, list(shape), dt, kind="ExternalInput")

    def scr(name, shape, dt):
        return nc.dram_tensor(name, list(shape), dt, kind="Internal")

    x_in = inp("x", [NT, D])
    cT = inp("cT", [128, FC, 4])
    masks = inp("masks", [128, 4])
    ada_w = inp("ada_w", [cfg.depth, D, D6])
    ada_b = inp("ada_b", [cfg.depth, D6])
    norm_mix_g = inp("norm_mix_g", [cfg.depth, D])
    norm_ffn_g = inp("norm_ffn_g", [cfg.depth, D])
    final_g = inp("final_norm_g", [1, D])
    conv_w_in = inp("conv_w_in", [D, 2 * D])
    conv_b_in = inp("conv_b_in_c", [128, 2 * FC])
    conv_w_dw = inp("conv_w_dw_c", [128, FC, CONV_W])
    conv_b_dw = inp("conv_b_dw_c", [128, FC])
    conv_ln_g = inp("conv_ln_g_c", [128, FC])
    conv_ln_b = inp("conv_ln_b_c", [128, FC])
    conv_w_out = inp("conv_w_out", [D, D])
    conv_b_out = inp("conv_b_out", [1, D])
    if debug_out != "conv":
        moe_w_router = inp("moe_w_router_c", [cfg.depth, 128, FC, E])
        EG = min(E, 8)
        moe_w = {nm: [[inp(f"moe_w_{nm}_{l}_{h}", [EG, D, D]) for h in range(E // EG)] for l in range(cfg.depth)]
                 for nm in ("gate", "up", "down")}
    G = cfg.G
    NB = NT // 8
    if debug_out not in ("conv", "moe0"):
        ssm_lre = inp("ssm_lre", [128, G])
        ssm_lim = inp("ssm_lim", [128, G])
        ssm_ls = inp("ssm_ls", [128, G])
        ssm_bre = inp("ssm_bre", [128, G, 16])
        ssm_bim = inp("ssm_bim", [128, G, 16])
        ssm_cre = inp("ssm_cre", [128, G, 16])
        ssm_cim = inp("ssm_cim", [128, G, 16])
        ssm_dcol = inp("ssm_dcol", [128, G])
        ssm_w_glu = inp("ssm_w_glu", [D, 2 * D])
        ssm_b_glu = inp("ssm_b_glu", [1, 2 * D])
        s5_keep = inp("s5_keep", [128, NB])
        s5_masks = inp("s5_masks", [128, 2, 128])
        HS = scr("HS", [NT, D], BF16)
        YS = scr("YS", [NT, D], BF16)
    y_out = nc.dram_tensor("y", [NT, D], F32, kind="ExternalOutput")
    CAP = cfg.CAP
    EXT = 2 + 2 * E
    DX = D + EXT
    OBW = min(512, D)
    NOB = D // OBW
    NST = CAP // 128
    H = scr("H", [NT, DX], BF16)
    XS = [scr(f"XS{e}", [CAP, DX], BF16) for e in range(E)]
    YB = [scr(f"YB{ob}", [NT, OBW], F32) for ob in range(NOB)]
    X2 = scr("X2", [NT, D], F32)
    X3 = scr("X3", [NT, D], F32)

    MOD = scr("MOD", [cfg.depth, 4, D6], F32)
    HT = scr("HT", [FC, 128, NT], BF16)
    UT = scr("UT", [FC, 128, NT], BF16)
    CU = scr("CU", [FC, 128, NT], F32)
    X1 = scr("X1", [NT, D], F32)

    kb = KB(nc)

    kb.begin()
    cst = kb.stage
    ident_f = kb.sb("identf", [128, 128], F32)
    ident_b = kb.sb("identb", [128, 128], BF16)
    ones_f = kb.sb("onesf", [128, 128], F32)
    kb.op("pool", lambda e: e.memset(ones_f[:], 1.0), w=[ones_f])
    iot = kb.sb("iot", [128, 128], I32)
    kb.op("pool", lambda e: e.iota(iot[:], pattern=[[1, 128]], base=0, channel_multiplier=-1), w=[iot])
    iotf = kb.sb("iotf", [128, 128], F32)
    kb.op("dve", lambda e: e.tensor_copy(iotf[:], iot[:]), r=[iot], w=[iotf])
    kb.op("dve", lambda e: e.tensor_scalar(ident_f[:], iotf[:], 0.0, None, op0=ALU.is_equal), r=[iotf], w=[ident_f])
    kb.op("dve", lambda e: e.tensor_copy(ident_b[:], ident_f[:]), r=[ident_f], w=[ident_b])
    mask_t = kb.sb("maskt", [128, 4], F32)
    kb.dma("sp", mask_t[:], masks.ap(), w=[mask_t])
    const_stack = kb.stage

    def row_bcast(tile, dram_row_ap):
        n = dram_row_ap.shape[-1]
        kb.dma("sp", tile[:, 0:n], dram_row_ap.partition_broadcast(128), w=[tile])

    def stage_mod():
        kb.begin()
        ct = kb.sb("ct", [128, FC, 4], F32)
        kb.dma("sp", ct[:], cT.ap(), w=[ct])
        cs_t = kb.sb("cs", [128, FC, 4], BF16)
        kb.op("act", lambda e: e.activation(out=cs_t[:], in_=ct[:], func=AF.Silu), r=[ct], w=[cs_t])
        NTL = D6 // 512
        wts = [kb.sb(f"adaw{i}", [128, FC, 512], BF16) for i in range(2)]
        pss = [kb.ps(f"modps{i}", [4, 512]) for i in range(2)]
        brs = [kb.sb(f"adab{i}", [4, 512], F32) for i in range(2)]
        mrs = [kb.sb(f"mrow{i}", [4, 512], F32) for i in range(2)]
        it = 0
        for li in range(cfg.depth):
            for nt in range(NTL):
                wt = wts[it % 2]
                ps = pss[it % 2]
                brow = brs[it % 2]
                mrow = mrs[it % 2]
                it += 1
                cs = slice(nt * 512, (nt + 1) * 512)
                src = ada_w.ap()[li].rearrange("(kc p) n -> p kc n", p=128)[:, :, cs]
                kb.dma("gq", wt[:], src, w=[wt])
                kb.dma("sp", brow[:], ada_b.ap()[li:li + 1, cs].partition_broadcast(4), w=[brow])
                for kc in range(FC):
                    kb.op("pe", lambda e, kc=kc: e.matmul(ps[:], lhsT=cs_t[:, kc, :], rhs=wt[:, kc, :],
                                                         start=(kc == 0), stop=(kc == FC - 1)),
                          r=[cs_t, wt], w=[ps] if kc == 0 else [], )
                    ps.w = (kb.st["pe"].sem, kb.st["pe"].cnt)
                kb.op("dve", lambda e: e.tensor_tensor(out=mrow[:], in0=ps[:], in1=brow[:], op=ALU.add),
                      r=[ps, brow], w=[mrow])
                kb.dma("sp", MOD.ap()[li, :, cs], mrow[:], r=[mrow])
        kb.end()

    def load_mod_rows(li, u, which_scale, which_shift, gain_ap, A_t, S_t, tmp):
        row_bcast(tmp, MOD.ap()[li, u:u + 1, which_scale * D:(which_scale + 1) * D])
        row_bcast(A_t, gain_ap)
        kb.op("dve", lambda e: e.scalar_tensor_tensor(out=A_t[:], in0=tmp[:], scalar=1.0, in1=A_t[:],
                                                     op0=ALU.add, op1=ALU.mult), r=[tmp, A_t], w=[A_t])
        row_bcast(S_t, MOD.ap()[li, u:u + 1, which_shift * D:(which_shift + 1) * D])

    def make_rms_ws():
        return dict(sq=kb.sb("rms_sq", [128, D], F32), tmp=[kb.sb(f"rms_tmp{i}", [128, D], F32) for i in range(2)],
                    ssum=[kb.sb(f"rms_ss{i}", [128, 1], F32) for i in range(2)],
                    rstd=[kb.sb(f"rms_rs{i}", [128, 3], F32) for i in range(2)], i=0)

    def rms_modulate(xt, A_t, S_t, h_out, ws):
        j = ws["i"] % 2
        ws["i"] += 1
        sq, junk, ssum, rstd = ws["sq"], ws["tmp"][j], ws["ssum"][j], ws["rstd"][j]
        kb.op("act", lambda e: e.activation(out=sq[:], in_=xt[:], func=AF.Square, accum_out=ssum[:, 0:1]),
              r=[xt], w=[sq, ssum])
        kb.op("dve", lambda e: e.tensor_scalar(rstd[:, 0:1], ssum[:, 0:1], 1.0 / D, RMS_EPS, op0=ALU.mult, op1=ALU.add),
              r=[ssum], w=[rstd])
        kb.op("act", lambda e: e.activation(out=rstd[:, 1:2], in_=rstd[:, 0:1], func=AF.Sqrt), r=[rstd], w=[rstd])
        kb.op("dve", lambda e: e.reciprocal(rstd[:, 2:3], rstd[:, 1:2]), r=[rstd], w=[rstd])
        kb.op("dve", lambda e: e.scalar_tensor_tensor(out=junk[:], in0=xt[:], scalar=rstd[:, 2:3], in1=A_t[:],
                                                     op0=ALU.mult, op1=ALU.mult), r=[xt, rstd, A_t], w=[junk])
        kb.op("dve", lambda e: e.tensor_tensor(out=h_out[:], in0=junk[:], in1=S_t[:], op=ALU.add),
              r=[junk, S_t], w=[h_out])

    def transpose_to_HT(h_bf, tt, psT, hT, dst):
        for fb in range(0, FC, 4):
            nb = min(4, FC - fb)
            ps = psT[(fb // 4) % 2]
            for j in range(nb):
                fc = fb + j
                kb.op("pe", lambda e, fc=fc, j=j: e.transpose(out=ps[:, j * 128:(j + 1) * 128],
                                                            in_=h_bf[:, fc * 128:(fc + 1) * 128], identity=ident_b[:]),
                      r=[h_bf, ident_b], w=[ps] if j == 0 else [])
                ps.w = (kb.st["pe"].sem, kb.st["pe"].cnt)
            kb.op("act", lambda e: e.activation(out=hT[:, fb:fb + nb, :],
                                                in_=ps[:, 0:nb * 128].rearrange("p (a b) -> p a b", b=128), func=AF.Copy),
                  r=[ps], w=[hT])
        kb.dma("sp", dst.ap()[:, :, tt * 128:(tt + 1) * 128].rearrange("fc p t -> p fc t"), hT[:], r=[hT])

    def stage_prenorm_T(li, x_src, gain, wsc, wsh):
        kb.begin()
        A_t = kb.sb("A", [128, D], F32)
        S_t = kb.sb("S", [128, D], F32)
        tmp = kb.sb("tmp", [128, D], F32)
        xts = [kb.sb(f"xt{i}", [128, D], F32) for i in range(2)]
        ws = make_rms_ws()
        hbs = [kb.sb(f"hb{i}", [128, D], BF16) for i in range(2)]
        hTs = [kb.sb(f"hT{i}", [128, FC, 128], BF16) for i in range(2)]
        psT = [kb.ps(f"psT{i}", [128, 512], BF16) for i in range(2)]
        for tt in range(NTT):
            u = (tt * 128) // UNIT
            if (tt * 128) % UNIT == 0:
                load_mod_rows(li, u, wsc, wsh, gain.ap()[li:li + 1, :], A_t, S_t, tmp)
            xt = xts[tt % 2]
            kb.dma("sp", xt[:], x_src.ap()[tt * 128:(tt + 1) * 128, :], w=[xt])
            hb = hbs[tt % 2]
            rms_modulate(xt, A_t, S_t, hb, ws)
            transpose_to_HT(hb, tt, psT, hTs[tt % 2], HT)
        kb.end()

    def stage_conv_in():
        kb.begin()
        FGS = min(4, FC)
        bin_t = kb.sb("bin", [128, 2 * FC], F32)
        kb.dma("sp", bin_t[:], conv_b_in.ap(), w=[bin_t])
        was = [kb.sb(f"wa{i}", [128, FC, FGS * 128], BF16) for i in range(2)]
        wgs = [kb.sb(f"wg{i}", [128, FC, FGS * 128], BF16) for i in range(2)]
        hts = [kb.sb(f"ht{i}", [128, FC, 512], BF16) for i in range(2)]
        psa = [kb.ps(f"psa{i}", [128, 512]) for i in range(2)]
        psg = [kb.ps(f"psg{i}", [128, 512]) for i in range(2)]
        sgs = [kb.sb(f"sg{i}", [128, 512], F32) for i in range(2)]
        uts = [kb.sb(f"ut{i}", [128, 512], BF16) for i in range(2)]
        wv = conv_w_in.ap().rearrange("(kc p) n -> p kc n", p=128)
        it = 0
        for fg in range(FC // FGS):
            wa, wg = was[fg % 2], wgs[fg % 2]
            kb.dma("gq", wa[:], wv[:, :, fg * FGS * 128:(fg + 1) * FGS * 128], w=[wa])
            kb.dma("gq", wg[:], wv[:, :, D + fg * FGS * 128:D + (fg + 1) * FGS * 128], w=[wg])
            for tb in range(NTB):
                ht = hts[tb % 2]
                kb.dma("sp", ht[:], HT.ap()[:, :, tb * 512:(tb + 1) * 512].rearrange("fc p t -> p fc t"), w=[ht])
                for j in range(FGS):
                    f = fg * FGS + j
                    pa, pg, sg, ut = psa[it % 2], psg[it % 2], sgs[it % 2], uts[it % 2]
                    it += 1
                    for kc in range(FC):
                        kb.op("pe", lambda e, kc=kc: e.matmul(pa[:], lhsT=wa[:, kc, j * 128:(j + 1) * 128], rhs=ht[:, kc, :],
                                                             start=(kc == 0), stop=(kc == FC - 1)),
                              r=[wa, ht], w=[pa] if kc == 0 else [])
                        pa.w = (kb.st["pe"].sem, kb.st["pe"].cnt)
                    for kc in range(FC):
                        kb.op("pe", lambda e, kc=kc: e.matmul(pg[:], lhsT=wg[:, kc, j * 128:(j + 1) * 128], rhs=ht[:, kc, :],
                                                             start=(kc == 0), stop=(kc == FC - 1)),
                              r=[wg, ht], w=[pg] if kc == 0 else [])
                        pg.w = (kb.st["pe"].sem, kb.st["pe"].cnt)
                    kb.op("act", lambda e: e.activation(out=sg[:], in_=pg[:], func=AF.Sigmoid,
                                                        bias=bin_t[:, FC + f:FC + f + 1]), r=[pg, bin_t], w=[sg])
                    kb.op("dve", lambda e: e.scalar_tensor_tensor(out=ut[:], in0=pa[:], scalar=bin_t[:, f:f + 1], in1=sg[:],
                                                                 op0=ALU.add, op1=ALU.mult), r=[pa, sg, bin_t], w=[ut])
                    kb.dma("sp", UT.ap()[f, :, tb * 512:(tb + 1) * 512], ut[:], r=[ut])
        kb.end()

    def stage_dwconv():
        kb.begin()
        wdw = kb.sb("wdw", [128, FC, CONV_W], F32)
        kb.dma("sp", wdw[:], conv_w_dw.ap(), w=[wdw])
        bdw = kb.sb("bdw", [128, FC], F32)
        kb.dma("sp", bdw[:], conv_b_dw.ap(), w=[bdw])
        dgs = [kb.sb(f"dg{i}", [128, CONV_W, 128], BF16) for i in range(2)]
        uws = [kb.sb(f"uw{i}", [128, 512 + 2 * CONV_PAD], BF16) for i in range(3)]
        pss = [kb.ps(f"cps{i}", [128, 512]) for i in range(2)]
        cus = [kb.sb(f"cu{i}", [128, 512], F32) for i in range(2)]
        it = 0
        for fc in range(FC):
            dg = dgs[fc % 2]
            for k in range(CONV_W):
                eng = "dve" if k % 2 == 0 else "pool"
                kb.op(eng, lambda e, k=k: e.tensor_scalar(dg[:, k, :], ident_f[:], wdw[:, fc, k:k + 1], None, op0=ALU.mult),
                      r=[ident_f, wdw], w=[dg])
            for tb in range(NTB):
                uw = uws[it % 3]
                ps = pss[it % 2]
                cu = cus[it % 2]
                it += 1
                t0 = tb * 512
                lo = max(t0 - CONV_PAD, 0)
                hi_ = min(t0 + 512 + CONV_PAD, NT)
                if t0 == 0:
                    kb.op("pool", lambda e: e.memset(uw[:, 0:CONV_PAD], 0.0), w=[uw])
                if t0 + 512 == NT:
                    kb.op("pool", lambda e: e.memset(uw[:, CONV_PAD + 512:], 0.0), w=[uw])
                kb.dma("sp", uw[:, lo - (t0 - CONV_PAD):hi_ - (t0 - CONV_PAD)], UT.ap()[fc, :, lo:hi_], w=[uw])
                if t0 > 0 and t0 % UNIT == 0:
                    b = t0 // UNIT
                    kb.op("dve", lambda e, b=b: e.tensor_scalar(uw[:, 0:CONV_PAD], uw[:, 0:CONV_PAD], mask_t[:, b:b + 1],
                                                             None, op0=ALU.mult), r=[uw, mask_t], w=[uw])
                if t0 + 512 < NT and (t0 + 512) % UNIT == 0:
                    b = (t0 + 512) // UNIT
                    kb.op("dve", lambda e, b=b: e.tensor_scalar(uw[:, CONV_PAD + 512:], uw[:, CONV_PAD + 512:],
                                                             mask_t[:, b:b + 1], None, op0=ALU.mult),
                          r=[uw, mask_t], w=[uw])
                for k in range(CONV_W):
                    kb.op("pe", lambda e, k=k: e.matmul(ps[:], lhsT=dg[:, k, :], rhs=uw[:, k:k + 512],
                                                       start=(k == 0), stop=(k == CONV_W - 1)),
                          r=[dg, uw], w=[ps] if k == 0 else [])
                    ps.w = (kb.st["pe"].sem, kb.st["pe"].cnt)
                kb.op("act", lambda e: e.activation(out=cu[:], in_=ps[:], func=AF.Identity, bias=bdw[:, fc:fc + 1]),
                      r=[ps, bdw], w=[cu])
                kb.dma("sp", CU.ap()[fc, :, t0:t0 + 512], cu[:], r=[cu])
        kb.end()

    def stage_conv_out(li, x_src, x_dst):
        kb.begin()
        lng = kb.sb("lng", [128, FC], F32)
        lnb = kb.sb("lnb", [128, FC], F32)
        kb.dma("sp", lng[:], conv_ln_g.ap(), w=[lng])
        kb.dma("sp", lnb[:], conv_ln_b.ap(), w=[lnb])
        wout = kb.sb("wout", [128, FC, D], BF16)
        wv = conv_w_out.ap().rearrange("(kc p) n -> p kc n", p=128)
        for kc in range(FC):
            kb.dma("gq", wout[:, kc, :], wv[:, kc, :], w=[wout])
        brow = kb.sb("brow", [128, D], F32)
        row_bcast(brow, conv_b_out.ap())
        G_t = kb.sb("G", [128, D], F32)
        cuts = [kb.sb(f"cut{i}", [128, FC, 512], F32) for i in range(2)]
        sq = kb.sb("sq", [128, 512], F32)
        ps1 = kb.ps("ps1", [128, 512])
        ps2 = kb.ps("ps2", [128, 512])
        mean = kb.sb("mean", [128, 512], F32)
        rstd = kb.sb("rstdln", [128, 512], F32)
        tmp = kb.sb("tmpln", [128, 512], F32)
        vTs = [kb.sb(f"vT{i}", [128, FC, 512], BF16) for i in range(2)]
        pso = [kb.ps(f"pso{i}", [128, 512]) for i in range(2)]
        xts = [kb.sb(f"xo{i}", [128, 512], F32) for i in range(2)]
        ots = [kb.sb(f"ot{i}", [128, 512], F32) for i in range(2)]
        it = 0
        NOB = D // 512 if D >= 512 else 1
        OBW = min(512, D)
        for tb in range(NTB):
            t0 = tb * 512
            if t0 % UNIT == 0:
                row_bcast(G_t, MOD.ap()[li, t0 // UNIT:t0 // UNIT + 1, 2 * D:3 * D])
            cut = cuts[tb % 2]
            vT = vTs[tb % 2]
            kb.dma("sp", cut[:], CU.ap()[:, :, t0:t0 + 512].rearrange("fc p t -> p fc t"), w=[cut])
            for fc in range(FC):
                kb.op("pe", lambda e, fc=fc: e.matmul(ps1[:], lhsT=ones_f[:], rhs=cut[:, fc, :], start=(fc == 0), stop=(fc == FC - 1)),
                      r=[ones_f, cut], w=[ps1] if fc == 0 else [])
                ps1.w = (kb.st["pe"].sem, kb.st["pe"].cnt)
            for fc in range(FC):
                kb.op("act", lambda e, fc=fc: e.activation(out=sq[:], in_=cut[:, fc, :], func=AF.Square), r=[cut], w=[sq])
                kb.op("pe", lambda e, fc=fc: e.matmul(ps2[:], lhsT=ones_f[:], rhs=sq[:], start=(fc == 0), stop=(fc == FC - 1)),
                      r=[ones_f, sq], w=[ps2] if fc == 0 else [])
                ps2.w = (kb.st["pe"].sem, kb.st["pe"].cnt)
            kb.op("dve", lambda e: e.tensor_scalar(mean[:], ps1[:], 1.0 / D, None, op0=ALU.mult), r=[ps1], w=[mean])
            kb.op("dve", lambda e: e.tensor_tensor(out=tmp[:], in0=mean[:], in1=mean[:], op=ALU.mult), r=[mean], w=[tmp])
            kb.op("dve", lambda e: e.scalar_tensor_tensor(out=tmp[:], in0=ps2[:], scalar=1.0 / D, in1=tmp[:],
                                                         op0=ALU.mult, op1=ALU.subtract), r=[ps2, tmp], w=[tmp])
            kb.op("dve", lambda e: e.tensor_scalar(tmp[:], tmp[:], LN_EPS, None, op0=ALU.add), r=[tmp], w=[tmp])
            kb.op("act", lambda e: e.activation(out=tmp[:], in_=tmp[:], func=AF.Sqrt), r=[tmp], w=[tmp])
            kb.op("dve", lambda e: e.reciprocal(rstd[:], tmp[:]), r=[tmp], w=[rstd])
            for fc in range(FC):
                kb.op("dve", lambda e, fc=fc: e.tensor_tensor(out=cut[:, fc, :], in0=cut[:, fc, :], in1=mean[:], op=ALU.subtract),
                      r=[cut, mean], w=[cut])
                kb.op("pool", lambda e, fc=fc: e.tensor_tensor(out=cut[:, fc, :], in0=cut[:, fc, :], in1=rstd[:], op=ALU.mult),
                      r=[cut, rstd], w=[cut])
                kb.op("act", lambda e, fc=fc: e.activation(out=vT[:, fc, :], in_=cut[:, fc, :], func=AF.Silu,
                                                           scale=lng[:, fc:fc + 1], bias=lnb[:, fc:fc + 1]),
                      r=[cut, lng, lnb], w=[vT])
            for ts in range(4):
                r0 = t0 + ts * 128
                for ob in range(NOB):
                    ps = pso[it % 2]
                    xt = xts[it % 2]
                    ot = ots[it % 2]
                    it += 1
                    cs = slice(ob * OBW, (ob + 1) * OBW)
                    kb.dma("sp", xt[:, 0:OBW], x_src.ap()[r0:r0 + 128, cs], w=[xt])
                    for kc in range(FC):
                        kb.op("pe", lambda e, kc=kc: e.matmul(ps[:, 0:OBW], lhsT=vT[:, kc, ts * 128:(ts + 1) * 128], rhs=wout[:, kc, cs],
                                                             start=(kc == 0), stop=(kc == FC - 1)),
                              r=[vT, wout], w=[ps] if kc == 0 else [])
                        ps.w = (kb.st["pe"].sem, kb.st["pe"].cnt)
                    kb.op("dve", lambda e: e.tensor_tensor(out=ot[:, 0:OBW], in0=ps[:, 0:OBW], in1=brow[:, cs], op=ALU.add),
                          r=[ps, brow], w=[ot])
                    kb.op("pool", lambda e: e.tensor_tensor(out=ot[:, 0:OBW], in0=ot[:, 0:OBW], in1=G_t[:, cs], op=ALU.mult),
                          r=[ot, G_t], w=[ot])
                    kb.op("dve", lambda e: e.tensor_tensor(out=ot[:, 0:OBW], in0=ot[:, 0:OBW], in1=xt[:, 0:OBW], op=ALU.add),
                          r=[ot, xt], w=[ot])
                    kb.dma("sp", x_dst.ap()[r0:r0 + 128, cs], ot[:, 0:OBW], r=[ot])
        kb.end()


    def acc_group(ps, n, mk, r):
        for i in range(n):
            kb.op("pe", lambda e, i=i: mk(e, i), r=r, w=[ps] if i == 0 else [])
            ps.w = (kb.st["pe"].sem, kb.st["pe"].cnt)

    def stage_moe(li, x_src):
        kb.begin()
        AFF = kb.sb("AFF", [128, NTT, E], F32)
        SLOT = kb.sb("SLOT", [128, E, NTT], I32)
        pidx = kb.sb("pidx", [128, 1], F32)
        pidi = kb.sb("pidi", [128, 1], I32)
        kb.op("pool", lambda e: e.iota(pidi[:], pattern=[[0, 1]], base=0, channel_multiplier=1), w=[pidi])
        kb.op("dve", lambda e: e.tensor_copy(pidx[:], pidi[:]), r=[pidi], w=[pidx])
        ltri = kb.sb("ltri", [128, 128], F32)
        kb.op("dve", lambda e: e.tensor_scalar(ltri[:], iotf[:], 0.0, None, op0=ALU.is_gt), r=[iotf], w=[ltri])
        kb.begin()
        zt = kb.sb("zt", [128, OBW], F32)
        kb.op("pool", lambda e: e.memset(zt[:], 0.0), w=[zt])
        for ob in range(NOB):
            for tt in range(NTT):
                kb.dma("sp", YB[ob].ap()[tt * 128:(tt + 1) * 128, :], zt[:], r=[zt])
        A_t = kb.sb("A", [128, D], F32)
        S_t = kb.sb("S", [128, D], F32)
        tmp = kb.sb("tmp", [128, D], F32)
        xts = [kb.sb(f"xt{i}", [128, D], F32) for i in range(2)]
        ws = make_rms_ws()
        hfs = [kb.sb(f"hf{i}", [128, D], F32) for i in range(2)]
        hxs = [kb.sb(f"hx{i}", [128, DX], BF16) for i in range(2)]
        hTs_ = [kb.sb(f"hTf{i}", [128, FC, 128], F32) for i in range(2)]
        sms = [kb.sb(f"sm{i}", [128, 4], F32) for i in range(2)]
        exs = [kb.sb(f"ex{i}", [128, E], F32) for i in range(2)]
        wr = kb.sb("wr", [128, FC, E], F32)
        kb.dma("sp", wr[:], moe_w_router.ap()[li], w=[wr])
        psT = [kb.ps(f"psTf{i}", [128, 512], F32) for i in range(2)]
        psr = kb.ps("psr", [128, E], F32)
        for tt in range(NTT):
            u = (tt * 128) // UNIT
            if (tt * 128) % UNIT == 0:
                load_mod_rows(li, u, 4, 3, norm_ffn_g.ap()[li:li + 1, :], A_t, S_t, tmp)
            xt = xts[tt % 2]
            hf = hfs[tt % 2]
            hx = hxs[tt % 2]
            kb.dma("sp", xt[:], x_src.ap()[tt * 128:(tt + 1) * 128, :], w=[xt])
            rms_modulate(xt, A_t, S_t, hf, ws)
            hT, sm, ex = hTs_[tt % 2], sms[tt % 2], exs[tt % 2]
            kb.op("act", lambda e: e.activation(out=hx[:, 0:D], in_=hf[:], func=AF.Copy), r=[hf], w=[hx])
            kb.op("pool", lambda e, tt=tt: e.memset(hx[:, D:D + 1], float(tt)), w=[hx])
            kb.op("pool", lambda e: e.tensor_copy(hx[:, D + 1:D + 2], pidx[:]), r=[pidx], w=[hx])
            for fb in range(0, FC, 4):
                nb = min(4, FC - fb)
                ps = psT[(fb // 4) % 2]
                acc_group(ps, nb, lambda e, j, fb=fb, ps=ps: e.transpose(out=ps[:, j * 128:(j + 1) * 128],
                                                                     in_=hf[:, (fb + j) * 128:(fb + j + 1) * 128],
                                                                     identity=ident_f[:]), r=[hf, ident_f])
                kb.op("dve", lambda e, fb=fb, nb=nb, ps=ps: e.tensor_copy(
                    hT[:, fb:fb + nb, :], ps[:, 0:nb * 128].rearrange("p (a b) -> p a b", b=128)), r=[ps], w=[hT])
            acc_group(psr, FC, lambda e, kc: e.matmul(psr[:], lhsT=hT[:, kc, :], rhs=wr[:, kc, :],
                                                     start=(kc == 0), stop=(kc == FC - 1)), r=[hT, wr])
            kb.op("dve", lambda e: e.tensor_reduce(out=sm[:, 0:1], in_=psr[:], axis=AX.X, op=ALU.max), r=[psr], w=[sm])
            kb.op("dve", lambda e: e.tensor_scalar(sm[:, 1:2], sm[:, 0:1], -1.0, None, op0=ALU.mult), r=[sm], w=[sm])
            kb.op("act", lambda e: e.activation(out=ex[:], in_=psr[:], func=AF.Exp, bias=sm[:, 1:2], accum_out=sm[:, 2:3]),
                  r=[psr, sm], w=[ex, sm])
            kb.op("dve", lambda e: e.reciprocal(sm[:, 3:4], sm[:, 2:3]), r=[sm], w=[sm])
            kb.op("dve", lambda e, tt=tt: e.tensor_scalar(AFF[:, tt, :], ex[:], sm[:, 3:4], None, op0=ALU.mult),
                  r=[ex, sm], w=[AFF])
            kb.op("dve", lambda e, tt=tt: e.tensor_copy(hx[:, D + 2:DX].bitcast(F32), AFF[:, tt, :]), r=[AFF], w=[hx])
            kb.dma("sp", H.ap()[tt * 128:(tt + 1) * 128, :], hx[:], r=[hx])
        kb.end()
        kb.begin()
        AFFv = AFF[:].rearrange("p t e -> p e t")
        lo = kb.sb("lo", [128, E], F32)
        hi = kb.sb("hi", [128, E], F32)
        mid = kb.sb("mid", [128, E], F32)
        ge = kb.sb("ge", [128, E], F32)
        nge = kb.sb("nge", [128, E], F32)
        ta = kb.sb("ta", [128, E], F32)
        tb_ = kb.sb("tb", [128, E], F32)
        cnt = kb.sb("cnt", [128, E], F32)
        cmp = kb.sb("cmp", [128, E, NTT], F32)
        pst = kb.ps("pst", [128, E], F32)
        kb.op("pool", lambda e: e.memset(lo[:], 0.0), w=[lo])
        kb.op("pool", lambda e: e.memset(hi[:], 1.0), w=[hi])

        def bc(t):
            return t[:, :].unsqueeze(2).broadcast_to([128, E, NTT])

        for it in range(34):
            kb.op("dve", lambda e: e.tensor_tensor(out=mid[:], in0=lo[:], in1=hi[:], op=ALU.add), r=[lo, hi], w=[mid])
            kb.op("dve", lambda e: e.tensor_scalar(mid[:], mid[:], 0.5, None, op0=ALU.mult), r=[mid], w=[mid])
            kb.op("dve", lambda e: e.tensor_tensor(out=cmp[:], in0=AFFv, in1=bc(mid), op=ALU.is_ge), r=[AFF, mid], w=[cmp])
            kb.op("dve", lambda e: e.tensor_reduce(out=cnt[:], in_=cmp[:], axis=AX.X, op=ALU.add), r=[cmp], w=[cnt])
            kb.op("pe", lambda e: e.matmul(pst[:], lhsT=ones_f[:], rhs=cnt[:], start=True, stop=True), r=[ones_f, cnt], w=[pst])
            kb.op("dve", lambda e: e.tensor_scalar(ge[:], pst[:], float(CAP), None, op0=ALU.is_ge), r=[pst], w=[ge])
            kb.op("dve", lambda e: e.tensor_scalar(nge[:], ge[:], -1.0, 1.0, op0=ALU.mult, op1=ALU.add), r=[ge], w=[nge])
            kb.op("dve", lambda e: e.tensor_tensor(out=ta[:], in0=ge[:], in1=mid[:], op=ALU.mult), r=[ge, mid], w=[ta])
            kb.op("dve", lambda e: e.tensor_tensor(out=tb_[:], in0=nge[:], in1=lo[:], op=ALU.mult), r=[nge, lo], w=[tb_])
            kb.op("dve", lambda e: e.tensor_tensor(out=lo[:], in0=ta[:], in1=tb_[:], op=ALU.add), r=[ta, tb_], w=[lo])
            kb.op("dve", lambda e: e.tensor_tensor(out=ta[:], in0=nge[:], in1=mid[:], op=ALU.mult), r=[nge, mid], w=[ta])
            kb.op("dve", lambda e: e.tensor_tensor(out=tb_[:], in0=ge[:], in1=hi[:], op=ALU.mult), r=[ge, hi], w=[tb_])
            kb.op("dve", lambda e: e.tensor_tensor(out=hi[:], in0=ta[:], in1=tb_[:], op=ALU.add), r=[ta, tb_], w=[hi])
        rst = kb.sb("rst", [128, E, NTT], F32)
        kb.op("pool", lambda e: e.memset(rst[:], 1.0), w=[rst])
        kb.op("pool", lambda e: e.memset(rst[:, :, 0:1], 0.0), w=[rst])
        pre = kb.sb("pre", [128, E, NTT], F32)
        kb.op("dve", lambda e: e.tensor_tensor(out=cmp[:], in0=AFFv, in1=bc(lo), op=ALU.is_ge), r=[AFF, lo], w=[cmp])
        kb.op("dve", lambda e: e.tensor_tensor_scan(out=pre[:].rearrange("p a b -> p (a b)"),
                                                   data0=rst[:].rearrange("p a b -> p (a b)"),
                                                   data1=cmp[:].rearrange("p a b -> p (a b)"),
                                                   initial=0.0, op0=ALU.mult, op1=ALU.add), r=[rst, cmp], w=[pre])
        kb.op("dve", lambda e: e.tensor_copy(cnt[:], pre[:, :, NTT - 1]), r=[pre], w=[cnt])
        kb.op("pe", lambda e: e.matmul(pst[:], lhsT=ltri[:], rhs=cnt[:], start=True, stop=True), r=[ltri, cnt], w=[pst])
        kb.op("dve", lambda e: e.tensor_scalar(ta[:], pst[:], -1.0, None, op0=ALU.add), r=[pst], w=[ta])
        BIG = 1000000.0
        kb.op("dve", lambda e: e.tensor_tensor(out=pre[:], in0=pre[:], in1=bc(ta), op=ALU.add), r=[pre, ta], w=[pre])
        kb.op("dve", lambda e: e.tensor_scalar(pre[:], pre[:], -BIG, None, op0=ALU.add), r=[pre], w=[pre])
        kb.op("dve", lambda e: e.tensor_tensor(out=pre[:], in0=pre[:], in1=cmp[:], op=ALU.mult), r=[pre, cmp], w=[pre])
        kb.op("dve", lambda e: e.tensor_scalar(pre[:], pre[:], BIG, None, op0=ALU.add), r=[pre], w=[pre])
        kb.op("dve", lambda e: e.tensor_copy(SLOT[:], pre[:]), r=[pre], w=[SLOT])
        hxs = [kb.sb(f"hxd{i}", [128, DX], BF16) for i in range(3)]
        bc_reg = nc.gpsimd.to_reg(CAP - 1)
        for tt in range(NTT):
            hx = hxs[tt % 3]
            kb.dma("sp", hx[:], H.ap()[tt * 128:(tt + 1) * 128, :], w=[hx])
            for ei in range(E):
                kb.dma("gq", XS[ei].ap()[:, :], hx[:, :], r=[hx, SLOT],
                       indirect=dict(out_offset=bass.IndirectOffsetOnAxis(ap=SLOT[:, ei, tt:tt + 1], axis=0), in_offset=None,
                                     bounds_check=bc_reg, oob_is_err=False))
        kb.end()
        kb.begin()
        FBW = min(256, D)
        xsT = kb.sb("xsT", [128, FC, CAP], BF16)
        hidT = kb.sb("hidT", [128, FC, CAP], BF16)
        IDX = kb.sb("IDX", [128, NST], I32)
        GT = kb.sb("GT", [128, NST], F32)
        idf = kb.sb("idf", [128, 4], F32)
        xss = [kb.sb(f"xs{i}", [128, DX], BF16) for i in range(2)]
        wgs = [kb.sb(f"wg{i}", [128, FC, FBW], BF16) for i in range(2)]
        wus = [kb.sb(f"wu{i}", [128, FC, FBW], BF16) for i in range(2)]
        wds = [kb.sb(f"wd{i}", [128, FC, OBW], BF16) for i in range(2)]
        sgs = [kb.sb(f"sgm{i}", [128, 512], F32) for i in range(2)]
        ots = [kb.sb(f"otm{i}", [128, OBW], F32) for i in range(3)]
        psT2 = [kb.ps(f"psTb{i}", [128, 512], BF16) for i in range(2)]
        psg = [kb.ps(f"psgm{i}", [128, 512]) for i in range(2)]
        psu = [kb.ps(f"psum{i}", [128, 512]) for i in range(2)]
        pso = [kb.ps(f"psom{i}", [128, OBW]) for i in range(2)]
        ytok = kb.sb("ytok", [1, 1], F32)
        nw = 0
        nd = 0
        ni = 0
        NFB = D // FBW
        wsched = []
        for ei_ in range(E):
            wsched += [("gu", ei_, fb_) for fb_ in range(NFB)] + [("d", ei_, ob_) for ob_ in range(NOB)]
        wstate = dict(issued=0, ngu=0, nd=0, bufs={})

        def w_issue(upto):
            while wstate["issued"] <= upto and wstate["issued"] < len(wsched):
                k = wstate["issued"]
                kind, e_, b_ = wsched[k]
                if kind == "gu":
                    gv_ = moe_w["gate"][li][e_ // EG].ap()[e_ % EG].rearrange("(kc p) f -> p kc f", p=128)
                    uv_ = moe_w["up"][li][e_ // EG].ap()[e_ % EG].rearrange("(kc p) f -> p kc f", p=128)
                    wg_, wu_ = wgs[wstate["ngu"] % 2], wus[wstate["ngu"] % 2]
                    wstate["ngu"] += 1
                    kb.dma("gq", wg_[:], gv_[:, :, b_ * FBW:(b_ + 1) * FBW], w=[wg_])
                    kb.dma("gq", wu_[:], uv_[:, :, b_ * FBW:(b_ + 1) * FBW], w=[wu_])
                    wstate["bufs"][k] = (wg_, wu_)
                else:
                    dv_ = moe_w["down"][li][e_ // EG].ap()[e_ % EG].rearrange("(kc p) f -> p kc f", p=128)
                    wd_ = wds[wstate["nd"] % 2]
                    wstate["nd"] += 1
                    kb.dma("gq", wd_[:], dv_[:, :, b_ * OBW:(b_ + 1) * OBW], w=[wd_])
                    wstate["bufs"][k] = (wd_,)
                wstate["issued"] += 1

        wk = 0
        w_issue(0)
        for ei in range(E):
            for st in range(NST):
                xs = xss[st % 2]
                kb.dma("sp", xs[:], XS[ei].ap()[st * 128:(st + 1) * 128, :], w=[xs])
                for fb in range(0, FC, 4):
                    nb = min(4, FC - fb)
                    ps = psT2[(fb // 4) % 2]
                    acc_group(ps, nb, lambda e, j, fb=fb, ps=ps, xs=xs: e.transpose(
                        out=ps[:, j * 128:(j + 1) * 128], in_=xs[:, (fb + j) * 128:(fb + j + 1) * 128], identity=ident_b[:]),
                        r=[xs, ident_b])
                    kb.op("act" if (fb // 4) % 2 == 0 else "dve",
                          (lambda e, fb=fb, nb=nb, ps=ps, st=st: e.activation(
                              out=xsT[:, fb:fb + nb, st * 128:(st + 1) * 128],
                              in_=ps[:, 0:nb * 128].rearrange("p (a b) -> p a b", b=128), func=AF.Copy))
                          if (fb // 4) % 2 == 0 else
                          (lambda e, fb=fb, nb=nb, ps=ps, st=st: e.tensor_copy(
                              xsT[:, fb:fb + nb, st * 128:(st + 1) * 128],
                              ps[:, 0:nb * 128].rearrange("p (a b) -> p a b", b=128))),
                          r=[ps], w=[xsT])
                kb.op("dve", lambda e, xs=xs: e.tensor_copy(idf[:, 0:2], xs[:, D:D + 2]), r=[xs], w=[idf])
                kb.op("dve", lambda e: e.scalar_tensor_tensor(out=idf[:, 2:3], in0=idf[:, 0:1], scalar=128.0, in1=idf[:, 1:2],
                                                             op0=ALU.mult, op1=ALU.add), r=[idf], w=[idf])
                kb.op("dve", lambda e, st=st: e.tensor_copy(IDX[:, st:st + 1], idf[:, 2:3]), r=[idf], w=[IDX])
                kb.op("dve", lambda e, st=st, xs=xs, ei=ei: e.tensor_copy(
                    GT[:, st:st + 1], xs[:, D + 2 + 2 * ei:D + 4 + 2 * ei].bitcast(F32)), r=[xs], w=[GT])
            gv = moe_w["gate"][li][ei // EG].ap()[ei % EG].rearrange("(kc p) f -> p kc f", p=128)
            uv = moe_w["up"][li][ei // EG].ap()[ei % EG].rearrange("(kc p) f -> p kc f", p=128)
            dv = moe_w["down"][li][ei // EG].ap()[ei % EG].rearrange("(kc p) f -> p kc f", p=128)
            for fb in range(D // FBW):
                w_issue(wk + 1)
                wg, wu = wstate["bufs"].pop(wk)
                wk += 1
                for j in range(FBW // 128):
                    f = fb * (FBW // 128) + j
                    for sb_ in range(CAP // 512):
                        pg, pu, sg = psg[ni % 2], psu[ni % 2], sgs[ni % 2]
                        ni += 1
                        cs = slice(sb_ * 512, (sb_ + 1) * 512)
                        acc_group(pg, FC, lambda e, kc, pg=pg, wg=wg, j=j, cs=cs: e.matmul(
                            pg[:], lhsT=wg[:, kc, j * 128:(j + 1) * 128], rhs=xsT[:, kc, cs],
                            start=(kc == 0), stop=(kc == FC - 1)), r=[wg, xsT])
                        acc_group(pu, FC, lambda e, kc, pu=pu, wu=wu, j=j, cs=cs: e.matmul(
                            pu[:], lhsT=wu[:, kc, j * 128:(j + 1) * 128], rhs=xsT[:, kc, cs],
                            start=(kc == 0), stop=(kc == FC - 1)), r=[wu, xsT])
                        kb.op("act", lambda e, pg=pg, sg=sg: e.activation(out=sg[:], in_=pg[:], func=AF.Silu), r=[pg], w=[sg])
                        kb.op("dve", lambda e, pu=pu, sg=sg, f=f, cs=cs: e.tensor_tensor(
                            out=hidT[:, f, cs], in0=pu[:], in1=sg[:], op=ALU.mult), r=[pu, sg], w=[hidT])
            for ob in range(NOB):
                w_issue(wk + 1)
                (wd,) = wstate["bufs"].pop(wk)
                wk += 1
                for st in range(NST):
                    po = pso[ni % 2]
                    ot = ots[ni % 3]
                    ni += 1
                    acc_group(po, FC, lambda e, kc, po=po, wd=wd, st=st: e.matmul(
                        po[:], lhsT=hidT[:, kc, st * 128:(st + 1) * 128], rhs=wd[:, kc, :],
                        start=(kc == 0), stop=(kc == FC - 1)), r=[hidT, wd])
                    kb.op("dve" if ni % 2 else "act",
                          (lambda e, po=po, ot=ot, st=st: e.tensor_scalar(ot[:], po[:], GT[:, st:st + 1], None, op0=ALU.mult))
                          if ni % 2 else
                          (lambda e, po=po, ot=ot, st=st: e.activation(out=ot[:], in_=po[:], func=AF.Identity, scale=GT[:, st:st + 1])),
                          r=[po, GT], w=[ot])
                    kb.dma("gq", YB[ob].ap()[:, :], ot[:, :], r=[ot, IDX], w=[ytok],
                           indirect=dict(out_offset=bass.IndirectOffsetOnAxis(ap=IDX[:, st:st + 1], axis=0), in_offset=None,
                                         compute_op=ALU.add))
        kb.end()
        kb.end()

    def stage_combine(li, x_src, x_dst, final):
        kb.begin()
        G_t = kb.sb("G2", [128, D], F32)
        F_t = kb.sb("Fg", [128, D], F32)
        if final:
            row_bcast(F_t, final_g.ap())
        xts = [kb.sb(f"xc{i}", [128, D], F32) for i in range(2)]
        yts = [kb.sb(f"yc{i}", [128, D], F32) for i in range(2)]
        junk = kb.sb("junkc", [128, D], F32)
        ssum = kb.sb("ssumc", [128, 1], F32)
        rstd = kb.sb("rstdc", [128, 3], F32)
        for tt in range(NTT):
            if (tt * 128) % UNIT == 0:
                u = (tt * 128) // UNIT
                row_bcast(G_t, MOD.ap()[li, u:u + 1, 5 * D:6 * D])
            xt, yt = xts[tt % 2], yts[tt % 2]
            kb.dma("sp", xt[:], x_src.ap()[tt * 128:(tt + 1) * 128, :], w=[xt])
            for ob in range(NOB):
                kb.dma("sp", yt[:, ob * OBW:(ob + 1) * OBW], YB[ob].ap()[tt * 128:(tt + 1) * 128, :], w=[yt])
            kb.op("pool", lambda e, yt=yt: e.tensor_tensor(out=yt[:], in0=yt[:], in1=G_t[:], op=ALU.mult), r=[yt, G_t], w=[yt])
            kb.op("dve", lambda e, xt=xt, yt=yt: e.tensor_tensor(out=xt[:], in0=xt[:], in1=yt[:], op=ALU.add), r=[xt, yt], w=[xt])
            if final:
                kb.op("act", lambda e, xt=xt: e.activation(out=junk[:], in_=xt[:], func=AF.Square, accum_out=ssum[:, 0:1]),
                      r=[xt], w=[junk, ssum])
                kb.op("dve", lambda e: e.tensor_scalar(rstd[:, 0:1], ssum[:, 0:1], 1.0 / D, RMS_EPS, op0=ALU.mult, op1=ALU.add),
                      r=[ssum], w=[rstd])
                kb.op("act", lambda e: e.activation(out=rstd[:, 1:2], in_=rstd[:, 0:1], func=AF.Sqrt), r=[rstd], w=[rstd])
                kb.op("dve", lambda e: e.reciprocal(rstd[:, 2:3], rstd[:, 1:2]), r=[rstd], w=[rstd])
                kb.op("dve", lambda e, xt=xt, yt=yt: e.scalar_tensor_tensor(out=yt[:], in0=xt[:], scalar=rstd[:, 2:3], in1=F_t[:],
                                                                       op0=ALU.mult, op1=ALU.mult), r=[xt, rstd, F_t], w=[yt])
                kb.dma("sp", x_dst.ap()[tt * 128:(tt + 1) * 128, :], yt[:], r=[yt])
            else:
                kb.dma("sp", x_dst.ap()[tt * 128:(tt + 1) * 128, :], xt[:], r=[xt])
        kb.end()


    def stage_prenorm_rows(li, x_src, gain, wsc, wsh, dst):
        kb.begin()
        A_t = kb.sb("A", [128, D], F32)
        S_t = kb.sb("S", [128, D], F32)
        tmp = kb.sb("tmp", [128, D], F32)
        xts = [kb.sb(f"xt{i}", [128, D], F32) for i in range(2)]
        ws = make_rms_ws()
        hbs = [kb.sb(f"hb{i}", [128, D], BF16) for i in range(2)]
        for tt in range(NTT):
            u = (tt * 128) // UNIT
            if (tt * 128) % UNIT == 0:
                load_mod_rows(li, u, wsc, wsh, gain.ap()[li:li + 1, :], A_t, S_t, tmp)
            xt = xts[tt % 2]
            kb.dma("sp", xt[:], x_src.ap()[tt * 128:(tt + 1) * 128, :], w=[xt])
            hb = hbs[tt % 2]
            rms_modulate(xt, A_t, S_t, hb, ws)
            kb.dma("sp", dst.ap()[tt * 128:(tt + 1) * 128, :], hb[:], r=[hb])
        kb.end()

    def stage_rows_to_T(src):
        kb.begin()
        hbs = [kb.sb(f"hbr{i}", [128, D], BF16) for i in range(2)]
        hTs = [kb.sb(f"hTr{i}", [128, FC, 128], BF16) for i in range(2)]
        psT = [kb.ps(f"psTr{i}", [128, 512], BF16) for i in range(2)]
        for tt in range(NTT):
            hb = hbs[tt % 2]
            kb.dma("sp", hb[:], src.ap()[tt * 128:(tt + 1) * 128, :], w=[hb])
            transpose_to_HT(hb, tt, psT, hTs[tt % 2], HT)
        kb.end()

    def stage_s5():
        kb.begin()
        CW = min(512, NB)
        NH = NB // CW
        NNT = NB // 128
        TWO_PI = 2.0 * np.pi
        isb = kb.sb("isb", [128, 1], F32)
        pidi = kb.sb("pidi5", [128, 1], I32)
        kb.op("pool", lambda e: e.iota(pidi[:], pattern=[[0, 1]], base=0, channel_multiplier=1), w=[pidi])
        kb.op("dve", lambda e: e.tensor_copy(isb[:], pidi[:]), r=[pidi], w=[isb])
        kb.op("dve", lambda e: e.tensor_scalar(isb[:], isb[:], 63.5, None, op0=ALU.is_gt), r=[isb], w=[isb])
        sign = kb.sb("sign", [128, 1], F32)
        kb.op("dve", lambda e: e.tensor_scalar(sign[:], isb[:], 2.0, -1.0, op0=ALU.mult, op1=ALU.add), r=[isb], w=[sign])
        nsign = kb.sb("nsign", [128, 1], F32)
        kb.op("dve", lambda e: e.tensor_scalar(nsign[:], sign[:], -1.0, None, op0=ALU.mult), r=[sign], w=[nsign])
        iri = kb.sb("iri", [128, 8], I32)
        kb.op("pool", lambda e: e.iota(iri[:], pattern=[[1, 8]], base=0, channel_multiplier=0), w=[iri])
        EX = kb.sb("EX", [128, 4, 8], F32)
        kb.op("dve", lambda e: e.tensor_copy(EX[:, 0, :], iri[:]), r=[iri], w=[EX])
        kb.op("dve", lambda e: e.tensor_scalar(EX[:, 0, :], EX[:, 0, :], sign[:, 0:1], None, op0=ALU.mult), r=[EX, sign], w=[EX])
        kb.op("dve", lambda e: e.tensor_scalar(EX[:, 1, :], EX[:, 0, :], -1.0, None, op0=ALU.mult), r=[EX], w=[EX])
        off3 = kb.sb("off3", [128, 2], F32)
        kb.op("dve", lambda e: e.tensor_scalar(off3[:, 0:1], isb[:], -7.0, 7.0, op0=ALU.mult, op1=ALU.add), r=[isb], w=[off3])
        kb.op("dve", lambda e: e.tensor_scalar(off3[:, 1:2], isb[:], 7.0, 1.0, op0=ALU.mult, op1=ALU.add), r=[isb], w=[off3])
        kb.op("dve", lambda e: e.tensor_scalar(EX[:, 2, :], EX[:, 0, :], off3[:, 0:1], None, op0=ALU.add), r=[EX, off3], w=[EX])
        kb.op("dve", lambda e: e.tensor_scalar(EX[:, 3, :], EX[:, 1, :], off3[:, 1:2], None, op0=ALU.add), r=[EX, off3], w=[EX])
        tni = kb.sb("tni", [128, NB], I32)
        kb.op("pool", lambda e: e.iota(tni[:], pattern=[[1, NB]], base=0, channel_multiplier=0), w=[tni])
        nrow = kb.sb("nrow", [128, NB], F32)
        kb.op("dve", lambda e: e.tensor_copy(nrow[:], tni[:]), r=[tni], w=[nrow])
        KEEP = kb.sb("KEEP", [128, NB], F32)
        kb.dma("sp", KEEP[:], s5_keep.ap(), w=[KEEP])
        MK = kb.sb("MK", [128, 2, 128], F32)
        kb.dma("sp", MK[:], s5_masks.ap(), w=[MK])
        lre = kb.sb("lre", [128, G], F32)
        lim = kb.sb("lim", [128, G], F32)
        lst = kb.sb("lst", [128, G], F32)
        kb.dma("sp", lre[:], ssm_lre.ap(), w=[lre])
        kb.dma("sp", lim[:], ssm_lim.ap(), w=[lim])
        kb.dma("sp", lst[:], ssm_ls.ap(), w=[lst])
        dcol = kb.sb("dcol", [128, G], F32)
        kb.dma("sp", dcol[:], ssm_dcol.ap(), w=[dcol])
        dt = kb.sb("dt", [128, G], F32)
        kb.op("act", lambda e: e.activation(out=dt[:], in_=lst[:], func=AF.Exp), r=[lst], w=[dt])
        lrdt = kb.sb("lrdt", [128, G], F32)
        kb.op("dve", lambda e: e.tensor_tensor(out=lrdt[:], in0=lre[:], in1=dt[:], op=ALU.mult), r=[lre, dt], w=[lrdt])
        f0 = kb.sb("f0", [128, G], F32)
        ti = kb.sb("ti", [128, G], I32)
        tf = kb.sb("tf", [128, G], F32)

        def frac_(t_f, t_i, t_tmp, eng="dve"):
            kb.op(eng, lambda e: e.tensor_copy(t_i, t_f), r=[], w=[])
            kb.op(eng, lambda e: e.tensor_copy(t_tmp, t_i), r=[], w=[])
            kb.op(eng, lambda e: e.tensor_tensor(out=t_f, in0=t_f, in1=t_tmp, op=ALU.subtract), r=[], w=[])

        kb.op("dve", lambda e: e.tensor_tensor(out=f0[:], in0=lim[:], in1=dt[:], op=ALU.mult), r=[lim, dt], w=[f0])
        kb.op("dve", lambda e: e.tensor_scalar(f0[:], f0[:], 1.0 / TWO_PI, None, op0=ALU.mult), r=[f0], w=[f0])
        def frac_tiles(F, I_, T, eng="dve"):
            kb.op(eng, lambda e: e.tensor_copy(I_[:], F[:]), r=[F], w=[I_])
            kb.op(eng, lambda e: e.tensor_copy(T[:], I_[:]), r=[I_], w=[T])
            kb.op(eng, lambda e: e.tensor_tensor(out=F[:], in0=F[:], in1=T[:], op=ALU.subtract), r=[F, T], w=[F])

        frac_tiles(f0, ti, tf)
        are = kb.sb("are", [128, G], F32)
        aim = kb.sb("aim", [128, G], F32)
        mag = kb.sb("mag", [128, G], F32)
        fc_ = kb.sb("fcq", [128, G], F32)
        kb.op("act", lambda e: e.activation(out=mag[:], in_=lrdt[:], func=AF.Exp), r=[lrdt], w=[mag])
        kb.op("act", lambda e: e.activation(out=aim[:], in_=f0[:], func=AF.Sin, scale=TWO_PI), r=[f0], w=[aim])
        kb.op("dve", lambda e: e.tensor_scalar(fc_[:], f0[:], 0.25, None, op0=ALU.add), r=[f0], w=[fc_])
        frac_tiles(fc_, ti, tf)
        kb.op("act", lambda e: e.activation(out=are[:], in_=fc_[:], func=AF.Sin, scale=TWO_PI), r=[fc_], w=[are])
        kb.op("dve", lambda e: e.tensor_tensor(out=are[:], in0=are[:], in1=mag[:], op=ALU.mult), r=[are, mag], w=[are])
        kb.op("dve", lambda e: e.tensor_tensor(out=aim[:], in0=aim[:], in1=mag[:], op=ALU.mult), r=[aim, mag], w=[aim])
        kre = kb.sb("kre", [128, G], F32)
        kim = kb.sb("kim", [128, G], F32)
        den = kb.sb("den", [128, G], F32)
        t1 = kb.sb("t1g", [128, G], F32)
        nr = kb.sb("nr", [128, G], F32)
        kb.op("dve", lambda e: e.tensor_scalar(nr[:], are[:], -1.0, None, op0=ALU.add), r=[are], w=[nr])
        kb.op("dve", lambda e: e.tensor_tensor(out=den[:], in0=lre[:], in1=lre[:], op=ALU.mult), r=[lre], w=[den])
        kb.op("dve", lambda e: e.tensor_tensor(out=t1[:], in0=lim[:], in1=lim[:], op=ALU.mult), r=[lim], w=[t1])
        kb.op("dve", lambda e: e.tensor_tensor(out=den[:], in0=den[:], in1=t1[:], op=ALU.add), r=[den, t1], w=[den])
        kb.op("dve", lambda e: e.reciprocal(den[:], den[:]), r=[den], w=[den])
        kb.op("dve", lambda e: e.tensor_tensor(out=kre[:], in0=nr[:], in1=lre[:], op=ALU.mult), r=[nr, lre], w=[kre])
        kb.op("dve", lambda e: e.tensor_tensor(out=t1[:], in0=aim[:], in1=lim[:], op=ALU.mult), r=[aim, lim], w=[t1])
        kb.op("dve", lambda e: e.tensor_tensor(out=kre[:], in0=kre[:], in1=t1[:], op=ALU.add), r=[kre, t1], w=[kre])
        kb.op("dve", lambda e: e.tensor_tensor(out=kre[:], in0=kre[:], in1=den[:], op=ALU.mult), r=[kre, den], w=[kre])
        kb.op("dve", lambda e: e.tensor_tensor(out=kim[:], in0=aim[:], in1=lre[:], op=ALU.mult), r=[aim, lre], w=[kim])
        kb.op("dve", lambda e: e.tensor_tensor(out=t1[:], in0=nr[:], in1=lim[:], op=ALU.mult), r=[nr, lim], w=[t1])
        kb.op("dve", lambda e: e.tensor_tensor(out=kim[:], in0=kim[:], in1=t1[:], op=ALU.subtract), r=[kim, t1], w=[kim])
        kb.op("dve", lambda e: e.tensor_tensor(out=kim[:], in0=kim[:], in1=den[:], op=ALU.mult), r=[kim, den], w=[kim])
        R8 = kb.sb("R8", [128, G], F32)
        kb.op("act", lambda e: e.activation(out=R8[:], in_=lrdt[:], func=AF.Exp, scale=8.0), r=[lrdt], w=[R8])
        f8 = kb.sb("f8", [128, G], F32)
        kb.op("dve", lambda e: e.tensor_scalar(f8[:], f0[:], 8.0, None, op0=ALU.mult), r=[f0], w=[f8])
        frac_tiles(f8, ti, tf)
        kb.op("dve", lambda e: e.tensor_scalar(f8[:], f8[:], nsign[:, 0:1], None, op0=ALU.mult), r=[f8, nsign], w=[f8])

        GC = 8
        bre = kb.sb("bre", [128, GC, 16], F32)
        bim = kb.sb("bim", [128, GC, 16], F32)
        cre = kb.sb("cre", [128, GC, 16], F32)
        cim = kb.sb("cim", [128, GC, 16], F32)
        bbre = kb.sb("bbre", [128, GC, 16], F32)
        bbim = kb.sb("bbim", [128, GC, 16], F32)
        tb16 = kb.sb("tb16", [128, GC, 16], F32)
        marg = kb.sb("marg", [128, GC, 32], F32)
        turn = kb.sb("turn", [128, GC, 32], F32)
        turc = kb.sb("turc", [128, GC, 32], F32)
        tui = kb.sb("tui", [128, GC, 32], I32)
        tuf = kb.sb("tuf", [128, GC, 32], F32)
        Ere = kb.sb("Ere", [128, GC, 4, 8], F32)
        Eim = kb.sb("Eim", [128, GC, 4, 8], F32)
        Pre = kb.sb("Pre", [128, GC, 8, 16], F32)
        Pim = kb.sb("Pim", [128, GC, 8, 16], F32)
        Qre = kb.sb("Qre", [128, GC, 8, 16], F32)
        Qim = kb.sb("Qim", [128, GC, 8, 16], F32)
        Lre = kb.sb("Lre", [128, GC, 8, 16], F32)
        Lim = kb.sb("Lim", [128, GC, 8, 16], F32)
        Xre = kb.sb("QXre", [128, GC, 8, 16], F32)
        Xim = kb.sb("QXim", [128, GC, 8, 16], F32)
        QXreb = kb.sb("QXreb", [128, 2, GC, 128], BF16)
        QXimb = kb.sb("QXimb", [128, 2, GC, 128], BF16)
        nisb = kb.sb("nisb", [128, 1], F32)
        kb.op("dve", lambda e: e.tensor_scalar(nisb[:], isb[:], -1.0, 1.0, op0=ALU.mult, op1=ALU.add), r=[isb], w=[nisb])
        ta4 = kb.sb("ta4", [128, GC, 8, 16], F32)
        tb4 = kb.sb("tb4", [128, GC, 8, 16], F32)
        HN = kb.sb("HN", [128, NNT, 8, 128], BF16)
        YN = kb.sb("YN", [128, NNT, 8, 128], BF16)
        HG = kb.sb("HG", [128, NNT, 8, 128], BF16)
        Ug = [kb.sb(f"Ug{i}", [128, NB], BF16) for i in range(2)]
        W0bs = [kb.sb(f"W0b{i}", [128, 128], BF16) for i in range(2)]
        tW = kb.sb("tW", [128, 128], F32)
        LTres = [kb.sb(f"LTre{i}", [128, 128], BF16) for i in range(2)]
        LTims = [kb.sb(f"LTim{i}", [128, 128], BF16) for i in range(2)]
        cns = [kb.sb(f"cn{i}", [128, NB], F32) for i in range(2)]
        sns = [kb.sb(f"sn{i}", [128, NB], F32) for i in range(2)]
        RKs = [kb.sb(f"RK{i}", [128, NB], F32) for i in range(2)]
        tnf = kb.sb("tnf", [128, NB], F32)
        frt = kb.sb("frt", [128, NB], F32)
        abt = tnf
        wr_ = kb.sb("wr5", [128, NB], F32)
        wi_ = kb.sb("wi5", [128, NB], F32)
        zr = kb.sb("zr", [128, NB], F32)
        zi = kb.sb("zi", [128, NB], F32)
        ta = kb.sb("ta5", [128, NB], F32)
        tb2 = kb.sb("tb5", [128, NB], F32)
        XPr = kb.sb("XPr", [128, NB + 2], BF16)
        XPi = kb.sb("XPi", [128, NB + 2], BF16)
        kb.op("pool", lambda e: e.memset(XPr[:], 0.0), w=[XPr])
        kb.op("pool", lambda e: e.memset(XPi[:], 0.0), w=[XPi])
        ysk = ta
        yg = kb.sb("yg", [128, NB], BF16)
        halfpi = kb.sb("halfpi", [128, 1], F32)
        kb.op("pool", lambda e: e.memset(halfpi[:], float(np.pi / 2)), w=[halfpi])
        psS = [kb.ps(f"psS{i}", [128, CW]) for i in range(2)]
        psY = [kb.ps(f"psY{i}", [128, CW]) for i in range(NH)] if NH <= 2 else None
        psU = kb.ps("psU", [128, 512])
        psM = kb.ps("psM", [128, 256])
        psM2 = kb.ps("psM2", [128, 256])
        psB = kb.ps("psB", [128, 1024], BF16)

        def bc3(t, gsl, n):
            return t[:, gsl].unsqueeze(2).broadcast_to([128, GC, n])

        def cmul_outer(Er, Ei, Br, Bi, outr, outi, neg_im):
            def eb(E_ap):
                return E_ap.unsqueeze(3).broadcast_to([128, GC, 8, 16])

            def bb(B):
                return B[:].unsqueeze(2).broadcast_to([128, GC, 8, 16])
            kb.op("dve", lambda e: e.tensor_tensor(out=ta4[:], in0=eb(Er), in1=bb(Br), op=ALU.mult), r=[Ere, Eim, Br], w=[ta4])
            kb.op("dve", lambda e: e.tensor_tensor(out=tb4[:], in0=eb(Ei), in1=bb(Bi), op=ALU.mult), r=[Ere, Eim, Bi], w=[tb4])
            kb.op("dve", lambda e: e.tensor_tensor(out=outr[:], in0=ta4[:], in1=tb4[:], op=ALU.subtract), r=[ta4, tb4], w=[outr])
            kb.op("dve", lambda e: e.tensor_tensor(out=ta4[:], in0=eb(Er), in1=bb(Bi), op=ALU.mult), r=[Ere, Eim, Bi, outr], w=[ta4])
            kb.op("dve", lambda e: e.tensor_tensor(out=tb4[:], in0=eb(Ei), in1=bb(Br), op=ALU.mult), r=[Ere, Eim, Br, outr], w=[tb4])
            if neg_im:
                kb.op("dve", lambda e: e.tensor_tensor(out=outi[:], in0=ta4[:], in1=tb4[:], op=ALU.add), r=[ta4, tb4], w=[outi])
                kb.op("dve", lambda e: e.tensor_scalar(outi[:], outi[:], -1.0, None, op0=ALU.mult), r=[outi], w=[outi])
            else:
                kb.op("dve", lambda e: e.tensor_tensor(out=outi[:], in0=ta4[:], in1=tb4[:], op=ALU.add), r=[ta4, tb4], w=[outi])

        for fc in range(FC):
            gsl = slice(fc * GC, (fc + 1) * GC)
            kb.dma("sp", bre[:], ssm_bre.ap()[:, gsl, :], w=[bre])
            kb.dma("sp", bim[:], ssm_bim.ap()[:, gsl, :], w=[bim])
            kb.dma("sp", cre[:], ssm_cre.ap()[:, gsl, :], w=[cre])
            kb.dma("sp", cim[:], ssm_cim.ap()[:, gsl, :], w=[cim])
            kb.op("dve", lambda e: e.tensor_tensor(out=bbre[:], in0=bre[:], in1=bc3(kre, gsl, 16), op=ALU.mult), r=[bre, kre], w=[bbre])
            kb.op("dve", lambda e: e.tensor_tensor(out=tb16[:], in0=bim[:], in1=bc3(kim, gsl, 16), op=ALU.mult), r=[bim, kim], w=[tb16])
            kb.op("dve", lambda e: e.tensor_tensor(out=bbre[:], in0=bbre[:], in1=tb16[:], op=ALU.subtract), r=[bbre, tb16], w=[bbre])
            kb.op("dve", lambda e: e.tensor_tensor(out=bbim[:], in0=bim[:], in1=bc3(kre, gsl, 16), op=ALU.mult), r=[bim, kre], w=[bbim])
            kb.op("dve", lambda e: e.tensor_tensor(out=tb16[:], in0=bre[:], in1=bc3(kim, gsl, 16), op=ALU.mult), r=[bre, kim, bbre], w=[tb16])
            kb.op("dve", lambda e: e.tensor_tensor(out=bbim[:], in0=bbim[:], in1=tb16[:], op=ALU.add), r=[bbim, tb16], w=[bbim])
            exb = EX[:].rearrange("p a b -> p (a b)").unsqueeze(1).broadcast_to([128, GC, 32])
            kb.op("dve", lambda e: e.tensor_tensor(out=marg[:], in0=bc3(lrdt, gsl, 32), in1=exb, op=ALU.mult), r=[lrdt, EX], w=[marg])
            kb.op("act", lambda e: e.activation(out=marg[:], in_=marg[:], func=AF.Exp), r=[marg], w=[marg])
            kb.op("dve", lambda e: e.tensor_tensor(out=turn[:], in0=bc3(f0, gsl, 32), in1=exb, op=ALU.mult), r=[f0, EX], w=[turn])
            frac_tiles(turn, tui, tuf)
            kb.op("dve", lambda e: e.tensor_scalar(turc[:], turn[:], 0.25, None, op0=ALU.add), r=[turn], w=[turc])
            frac_tiles(turc, tui, tuf)
            Ef_re = Ere[:].rearrange("p g a b -> p g (a b)")
            Ef_im = Eim[:].rearrange("p g a b -> p g (a b)")
            kb.op("act", lambda e: e.activation(out=Ef_im, in_=turn[:], func=AF.Sin, scale=TWO_PI), r=[turn], w=[Eim])
            kb.op("act", lambda e: e.activation(out=Ef_re, in_=turc[:], func=AF.Sin, scale=TWO_PI), r=[turc], w=[Ere])
            kb.op("dve", lambda e: e.tensor_tensor(out=Ef_im, in0=Ef_im, in1=marg[:], op=ALU.mult), r=[Eim, marg], w=[Eim])
            kb.op("dve", lambda e: e.tensor_tensor(out=Ef_re, in0=Ef_re, in1=marg[:], op=ALU.mult), r=[Ere, marg], w=[Ere])
            cmul_outer(Ere[:, :, 0, :], Eim[:, :, 0, :], bbre, bbim, Pre, Pim, False)
            cmul_outer(Ere[:, :, 1, :], Eim[:, :, 1, :], cre, cim, Qre, Qim, True)
            cmul_outer(Ere[:, :, 2, :], Eim[:, :, 2, :], bbre, bbim, Lre, Lim, False)
            cmul_outer(Ere[:, :, 3, :], Eim[:, :, 3, :], cre, cim, Xre, Xim, True)
            for d_, sc_ in ((0, nisb), (1, isb)):
                kb.op("act", lambda e, d_=d_, sc_=sc_: e.activation(out=QXreb[:, d_], in_=Xre[:].rearrange("p g a b -> p g (a b)"),
                                                                  func=AF.Copy, scale=sc_[:, 0:1]), r=[Xre, sc_], w=[QXreb])
                kb.op("act", lambda e, d_=d_, sc_=sc_: e.activation(out=QXimb[:, d_], in_=Xim[:].rearrange("p g a b -> p g (a b)"),
                                                                  func=AF.Copy, scale=sc_[:, 0:1]), r=[Xim, sc_], w=[QXimb])
            for nt in range(NNT):
                src = HS.ap()[nt * 1024:(nt + 1) * 1024, fc * 128:(fc + 1) * 128].rearrange("(n i) c -> n i c", i=8)
                kb.dma("sp", HN[:, nt, :, :], src, w=[HN])
            for nt in range(NNT):
                kb.op("act", lambda e, nt=nt: e.activation(out=HG[:, nt, :, :].rearrange("p g (i c) -> p g i c", c=16),
                                                           in_=HN[:, nt, :, :].rearrange("p i (g c) -> p g i c", c=16), func=AF.Copy),
                      r=[HN], w=[HG])
            Pr = Pre[:].rearrange("p g a b -> p g (a b)")
            Pi = Pim[:].rearrange("p g a b -> p g (a b)")
            Qr = Qre[:].rearrange("p g a b -> p g (a b)")
            Qi = Qim[:].rearrange("p g a b -> p g (a b)")
            Lr = Lre[:].rearrange("p g a b -> p g (a b)")
            Li = Lim[:].rearrange("p g a b -> p g (a b)")

            def prepA(g):
                gg = fc * GC + g
                U = Ug[g % 2]
                LTre, LTim = LTres[g % 2], LTims[g % 2]
                kb.op("dve", lambda e: e.tensor_scalar(tni[:], nrow[:], f8[:, gg:gg + 1], None, op0=ALU.mult), r=[nrow, f8], w=[tni])
                for nb in range(0, NNT, 4):
                    k4 = min(4, NNT - nb)
                    acc_group(psU, k4, lambda e, j, nb=nb: e.matmul(
                        psU[:, j * 128:(j + 1) * 128], lhsT=HG[:, nb + j, g, :], rhs=ident_b[:],
                        start=True, stop=True), r=[HG, ident_b])
                    kb.op("act", lambda e, nb=nb, k4=k4: e.activation(out=U[:, nb * 128:(nb + k4) * 128], in_=psU[:, 0:k4 * 128],
                                                                     func=AF.Copy), r=[psU], w=[U])
                for d_ in range(2):
                    rs = slice(64 * d_, 64 * d_ + 64)
                    cs_ = slice(128 * d_, 128 * d_ + 128)
                    acc_group(psM, 2, lambda e, j, rs=rs, cs_=cs_: e.matmul(
                        psM[:, cs_], lhsT=(Pr if j == 0 else Pi)[rs, g, :], rhs=(Qr if j == 0 else Qi)[rs, g, :],
                        start=(j == 0), stop=(j == 1)), r=[Pre, Pim, Qre, Qim])
                acc_group(psM2, 2, lambda e, j: e.transpose(out=psM2[:, j * 128:(j + 1) * 128],
                                                            in_=(Lr if j == 0 else Li)[:, g, :], identity=ident_f[:]),
                          r=[Lre, Lim, ident_f])
                kb.op("act", lambda e: e.activation(out=LTre[:], in_=psM2[:, 0:128], func=AF.Copy), r=[psM2], w=[LTre])
                kb.op("act", lambda e: e.activation(out=LTim[:], in_=psM2[:, 128:256], func=AF.Copy), r=[psM2], w=[LTim])

            def prepB(g):
                gg = fc * GC + g
                W0b = W0bs[g % 2]
                cn, sn, RK = cns[g % 2], sns[g % 2], RKs[g % 2]
                kb.op("dve", lambda e: e.tensor_copy(tnf[:], tni[:]), r=[tni], w=[tnf])
                kb.op("dve", lambda e: e.scalar_tensor_tensor(out=frt[:], in0=nrow[:], scalar=f8[:, gg:gg + 1], in1=tnf[:],
                                                             op0=ALU.mult, op1=ALU.subtract), r=[nrow, f8, tnf], w=[frt])
                kb.op("dve", lambda e: e.tensor_tensor(out=tW[:], in0=psM[:, 0:128], in1=MK[:, 0, :], op=ALU.mult), r=[psM, MK], w=[tW])
                kb.op("dve", lambda e: e.tensor_tensor(out=W0b[:], in0=psM[:, 128:256], in1=MK[:, 1, :], op=ALU.mult), r=[psM, MK], w=[W0b])
                kb.op("dve", lambda e: e.tensor_tensor(out=W0b[:], in0=W0b[:], in1=tW[:], op=ALU.add), r=[W0b, tW], w=[W0b])
                kb.op("act", lambda e: e.activation(out=sn[:], in_=frt[:], func=AF.Sin, scale=TWO_PI), r=[frt], w=[sn])
                kb.op("act", lambda e: e.activation(out=abt[:], in_=frt[:], func=AF.Abs), r=[frt], w=[abt])
                kb.op("act", lambda e: e.activation(out=cn[:], in_=abt[:], func=AF.Sin, scale=-TWO_PI, bias=halfpi[:, 0:1]),
                      r=[abt, halfpi], w=[cn])
                kb.op("act", lambda e: e.activation(out=RK[:], in_=KEEP[:], func=AF.Copy, scale=R8[:, gg:gg + 1]), r=[KEEP, R8], w=[RK])

            def main(g, mid_hook):
                gg = fc * GC + g
                U = Ug[g % 2]
                W0b, LTre, LTim = W0bs[g % 2], LTres[g % 2], LTims[g % 2]
                cn, sn, RK = cns[g % 2], sns[g % 2], RKs[g % 2]
                for h in range(NH):
                    cs_ = slice(h * CW, (h + 1) * CW)
                    kb.op("pe", lambda e, cs_=cs_: e.matmul(psS[0][:], lhsT=LTre[:], rhs=U[:, cs_], start=True, stop=True),
                          r=[LTre, U], w=[psS[0]])
                    kb.op("pe", lambda e, cs_=cs_: e.matmul(psS[1][:], lhsT=LTim[:], rhs=U[:, cs_], start=True, stop=True),
                          r=[LTim, U], w=[psS[1]])
                    kb.op("dve", lambda e, cs_=cs_: e.tensor_tensor(out=wr_[:, cs_], in0=psS[0][:], in1=cn[:, cs_], op=ALU.mult), r=[psS[0], cn], w=[wr_])
                    kb.op("dve", lambda e, cs_=cs_: e.tensor_tensor(out=ta[:, cs_], in0=psS[1][:], in1=sn[:, cs_], op=ALU.mult), r=[psS[1], sn], w=[ta])
                    kb.op("dve", lambda e, cs_=cs_: e.tensor_tensor(out=wi_[:, cs_], in0=psS[1][:], in1=cn[:, cs_], op=ALU.mult), r=[psS[1], cn], w=[wi_])
                    kb.op("dve", lambda e, cs_=cs_: e.tensor_tensor(out=tb2[:, cs_], in0=psS[0][:], in1=sn[:, cs_], op=ALU.mult), r=[psS[0], sn], w=[tb2])
                kb.op("pool", lambda e: e.tensor_tensor(out=wr_[:], in0=wr_[:], in1=ta[:], op=ALU.add), r=[wr_, ta], w=[wr_])
                kb.op("dve", lambda e: e.tensor_tensor(out=wi_[:], in0=wi_[:], in1=tb2[:], op=ALU.subtract), r=[wi_, tb2], w=[wi_])
                mid_hook()
                for (z_, w_) in ((zr, wr_), (zi, wi_)):
                    kb.op("dve", lambda e, z_=z_, w_=w_: e.tensor_tensor_scan(out=z_[0:64, :], data0=RK[0:64, :], data1=w_[0:64, :],
                                                                          initial=0.0, op0=ALU.mult, op1=ALU.add), r=[RK, w_], w=[z_])
                    kb.op("dve", lambda e, z_=z_, w_=w_: e.tensor_tensor_scan(out=z_[64:128, ::-1], data0=RK[64:128, ::-1], data1=w_[64:128, ::-1],
                                                                          initial=0.0, op0=ALU.mult, op1=ALU.add), r=[RK, w_], w=[z_])
                kb.op("dve", lambda e: e.tensor_tensor(out=ta[:], in0=zr[:], in1=cn[:], op=ALU.mult), r=[zr, cn], w=[ta])
                kb.op("dve", lambda e: e.tensor_tensor(out=tb2[:], in0=zi[:], in1=sn[:], op=ALU.mult), r=[zi, sn], w=[tb2])
                kb.op("dve", lambda e: e.tensor_tensor(out=XPr[:, 1:NB + 1], in0=ta[:], in1=tb2[:], op=ALU.subtract), r=[ta, tb2], w=[XPr])
                kb.op("dve", lambda e: e.tensor_tensor(out=ta[:], in0=zr[:], in1=sn[:], op=ALU.mult), r=[zr, sn, XPr], w=[ta])
                kb.op("dve", lambda e: e.tensor_tensor(out=tb2[:], in0=zi[:], in1=cn[:], op=ALU.mult), r=[zi, cn, XPr], w=[tb2])
                kb.op("dve", lambda e: e.tensor_tensor(out=XPi[:, 1:NB + 1], in0=ta[:], in1=tb2[:], op=ALU.add), r=[ta, tb2], w=[XPi])
                UB = UNIT // 8
                for XP in (XPr, XPi):
                    kb.op("dve", lambda e, XP=XP: e.tensor_tensor(out=XP[0:64, UB:3 * UB + 1:UB], in0=XP[0:64, UB:3 * UB + 1:UB],
                                                                 in1=mask_t[0:64, 1:4], op=ALU.mult), r=[XP, mask_t], w=[XP])
                    kb.op("dve", lambda e, XP=XP: e.tensor_tensor(out=XP[64:128, UB + 1:3 * UB + 2:UB], in0=XP[64:128, UB + 1:3 * UB + 2:UB],
                                                                 in1=mask_t[64:128, 1:4], op=ALU.mult), r=[XP, mask_t], w=[XP])
                for h in range(NH):
                    c0 = h * CW
                    cs_ = slice(c0, c0 + CW)
                    pY = psY[h]
                    ops = [(W0b[:], U[:, cs_]),
                           (QXreb[:, 0, g, :], XPr[:, c0:c0 + CW]), (QXreb[:, 1, g, :], XPr[:, c0 + 2:c0 + CW + 2]),
                           (QXimb[:, 0, g, :], XPi[:, c0:c0 + CW]), (QXimb[:, 1, g, :], XPi[:, c0 + 2:c0 + CW + 2])]
                    acc_group(pY, 5, lambda e, j, pY=pY, ops=ops: e.matmul(pY[:], lhsT=ops[j][0], rhs=ops[j][1],
                                                                          start=(j == 0), stop=(j == 4)),
                              r=[W0b, QXreb, QXimb, U, XPr, XPi])
                    kb.op("dve", lambda e, cs_=cs_, pY=pY: e.scalar_tensor_tensor(
                        out=ysk[:, cs_], in0=U[:, cs_], scalar=dcol[:, gg:gg + 1], in1=pY[:], op0=ALU.mult, op1=ALU.add),
                        r=[U, dcol, pY], w=[ysk])
                    kb.op("act", lambda e, cs_=cs_: e.activation(out=yg[:, cs_], in_=ysk[:, cs_], func=AF.Gelu_apprx_tanh), r=[ysk], w=[yg])
                for nb in range(0, NNT, 8):
                    k8 = min(8, NNT - nb)
                    acc_group(psB, k8, lambda e, j, nb=nb: e.transpose(out=psB[:, j * 128:(j + 1) * 128],
                                                                   in_=yg[:, (nb + j) * 128:(nb + j + 1) * 128], identity=ident_b[:]),
                              r=[yg, ident_b])
                    kb.op("act", lambda e, nb=nb, k8=k8: e.activation(
                        out=YN[:, nb:nb + k8, :, g * 16:(g + 1) * 16],
                        in_=psB[:, 0:k8 * 128].rearrange("p (a i c) -> p a i c", i=8, c=16), func=AF.Copy), r=[psB], w=[YN])

            prepA(0)
            prepB(0)
            for g in range(GC):
                if g + 1 < GC:
                    prepA(g + 1)
                    main(g, lambda g=g: prepB(g + 1))
                else:
                    main(g, lambda: None)
            for nt in range(NNT):
                dst = YS.ap()[nt * 1024:(nt + 1) * 1024, fc * 128:(fc + 1) * 128].rearrange("(n i) c -> n i c", i=8)
                kb.dma("sp", dst, YN[:, nt, :, :], r=[YN])
        kb.end()

    def stage_glu_out(li, x_src, x_dst):
        kb.begin()
        CGW = min(512, D)
        was = [kb.sb(f"wga{i}", [128, FC, CGW], BF16) for i in range(2)]
        wgs = [kb.sb(f"wgg{i}", [128, FC, CGW], BF16) for i in range(2)]
        hts = [kb.sb(f"htg{i}", [128, FC, 512], BF16) for i in range(2)]
        barow = kb.sb("barow", [128, D], F32)
        bgrow = kb.sb("bgrow", [128, D], F32)
        row_bcast(barow, ssm_b_glu.ap()[:, 0:D])
        row_bcast(bgrow, ssm_b_glu.ap()[:, D:2 * D])
        G_t = kb.sb("G1s", [128, D], F32)
        psa = [kb.ps(f"psga{i}", [128, CGW]) for i in range(2)]
        psg = [kb.ps(f"psgg{i}", [128, CGW]) for i in range(2)]
        sgs = [kb.sb(f"sgg{i}", [128, CGW], F32) for i in range(2)]
        ots = [kb.sb(f"otg{i}", [128, CGW], F32) for i in range(2)]
        xts = [kb.sb(f"xtg{i}", [128, CGW], F32) for i in range(2)]
        wv = ssm_w_glu.ap().rearrange("(kc p) n -> p kc n", p=128)
        it = 0
        def glu_w(cg_):
            kb.dma("gq", was[cg_ % 2][:], wv[:, :, cg_ * CGW:(cg_ + 1) * CGW], w=[was[cg_ % 2]])
            kb.dma("gq", wgs[cg_ % 2][:], wv[:, :, D + cg_ * CGW:D + (cg_ + 1) * CGW], w=[wgs[cg_ % 2]])

        glu_w(0)
        for cg in range(D // CGW):
            cs = slice(cg * CGW, (cg + 1) * CGW)
            wa, wg = was[cg % 2], wgs[cg % 2]
            if cg + 1 < D // CGW:
                glu_w(cg + 1)
            for tb in range(NTB):
                t0 = tb * 512
                if t0 % UNIT == 0:
                    row_bcast(G_t, MOD.ap()[li, t0 // UNIT:t0 // UNIT + 1, 2 * D:3 * D])
                ht = hts[tb % 2]
                kb.dma("sp", ht[:], HT.ap()[:, :, t0:t0 + 512].rearrange("fc p t -> p fc t"), w=[ht])
                for ts in range(4):
                    r0 = t0 + ts * 128
                    pa, pg, sg, ot, xt = psa[it % 2], psg[it % 2], sgs[it % 2], ots[it % 2], xts[it % 2]
                    it += 1
                    kb.dma("sp", xt[:], x_src.ap()[r0:r0 + 128, cs], w=[xt])
                    acc_group(pa, FC, lambda e, kc, pa=pa, wa=wa, ht=ht, ts=ts: e.matmul(
                        pa[:], lhsT=ht[:, kc, ts * 128:(ts + 1) * 128], rhs=wa[:, kc, :], start=(kc == 0), stop=(kc == FC - 1)), r=[ht, wa])
                    acc_group(pg, FC, lambda e, kc, pg=pg, wg=wg, ht=ht, ts=ts: e.matmul(
                        pg[:], lhsT=ht[:, kc, ts * 128:(ts + 1) * 128], rhs=wg[:, kc, :], start=(kc == 0), stop=(kc == FC - 1)), r=[ht, wg])
                    kb.op("dve", lambda e, pg=pg, sg=sg: e.tensor_tensor(out=sg[:], in0=pg[:], in1=bgrow[:, cs], op=ALU.add), r=[pg, bgrow], w=[sg])
                    kb.op("act", lambda e, sg=sg: e.activation(out=sg[:], in_=sg[:], func=AF.Sigmoid), r=[sg], w=[sg])
                    kb.op("dve", lambda e, pa=pa, ot=ot: e.tensor_tensor(out=ot[:], in0=pa[:], in1=barow[:, cs], op=ALU.add), r=[pa, barow], w=[ot])
                    kb.op("pool", lambda e, ot=ot, sg=sg: e.tensor_tensor(out=ot[:], in0=ot[:], in1=sg[:], op=ALU.mult), r=[ot, sg], w=[ot])
                    kb.op("dve", lambda e, ot=ot: e.tensor_tensor(out=ot[:], in0=ot[:], in1=G_t[:, cs], op=ALU.mult), r=[ot, G_t], w=[ot])
                    kb.op("dve", lambda e, ot=ot, xt=xt: e.tensor_tensor(out=ot[:], in0=ot[:], in1=xt[:], op=ALU.add), r=[ot, xt], w=[ot])
                    kb.dma("sp", x_dst.ap()[r0:r0 + 128, cs], ot[:], r=[ot])
        kb.end()

    stage_mod()
    stage_prenorm_T(0, x_in, norm_mix_g, 1, 0)
    stage_conv_in()
    stage_dwconv()
    stage_conv_out(0, x_in, y_out if debug_out == "conv" else X1)
    if debug_out != "conv":
        stage_moe(0, X1)
        stage_combine(0, X1, y_out if debug_out == "moe0" else X2, final=False)
    if debug_out not in ("conv", "moe0"):
        stage_prenorm_rows(1, X2, norm_mix_g, 1, 0, HS)
        stage_s5()
        stage_rows_to_T(YS)
        stage_glu_out(1, X2, y_out if debug_out == "s5" else X3)
        if debug_out != "s5":
            stage_moe(1, X3)
            stage_combine(1, X3, y_out, final=True)

    kb.barrier()
    const_stack.close()
    kb.es.close()
    return nc


def cols(v, FC):
    return np.ascontiguousarray(np.asarray(v, np.float32).reshape(FC, 128).T)


def make_core_inputs(cfg, x, c, unit_rows, masks, p, seq_len):
    FC, D = cfg.FC, cfg.D
    cu = np.asarray(c, np.float32)[unit_rows]
    cT = np.ascontiguousarray(cu.reshape(4, FC, 128).transpose(2, 1, 0))
    m = np.ascontiguousarray(np.broadcast_to(np.asarray(masks, np.float32)[None, :], (128, 4)))
    d = {
        "x": np.ascontiguousarray(x.reshape(-1, D), dtype=np.float32),
        "cT": cT, "masks": m,
        "ada_w": p["ada_w"], "ada_b": p["ada_b"],
        "norm_mix_g": p["norm_mix_g"], "norm_ffn_g": p["norm_ffn_g"],
        "final_norm_g": p["final_norm_g"].reshape(1, D),
        "conv_w_in": p["conv_w_in"][0],
        "conv_b_in_c": cols(p["conv_b_in"][0], 2 * FC),
        "conv_w_dw_c": np.ascontiguousarray(p["conv_w_dw"][0].T.reshape(FC, 128, CONV_W).transpose(1, 0, 2)),
        "conv_b_dw_c": cols(p["conv_b_dw"][0], FC),
        "conv_ln_g_c": cols(p["conv_ln_g"][0], FC),
        "conv_ln_b_c": cols(p["conv_ln_b"][0], FC),
        "conv_w_out": p["conv_w_out"][0],
        "conv_b_out": p["conv_b_out"][0].reshape(1, D),
        "moe_w_router_c": np.ascontiguousarray(p["moe_w_router"].reshape(cfg.depth, FC, 128, cfg.E).transpose(0, 2, 1, 3)),
    }
    G = cfg.G
    def dpg(a):
        return np.ascontiguousarray(np.asarray(a, np.float32).transpose(0, 2, 1).reshape(128, G))
    d["ssm_lre"] = dpg(p["ssm_lambda_re"][0])
    d["ssm_lim"] = dpg(p["ssm_lambda_im"][0])
    d["ssm_ls"] = dpg(np.broadcast_to(np.asarray(p["ssm_log_step"][0])[:, :, None], (2, G, 64)))
    d["ssm_bre"] = np.asarray(p["ssm_b_re"][0]).transpose(0, 2, 1, 3).reshape(128, G, 16)
    d["ssm_bim"] = np.asarray(p["ssm_b_im"][0]).transpose(0, 2, 1, 3).reshape(128, G, 16)
    d["ssm_cre"] = np.asarray(p["ssm_c_re"][0]).transpose(0, 3, 1, 2).reshape(128, G, 16)
    d["ssm_cim"] = np.asarray(p["ssm_c_im"][0]).transpose(0, 3, 1, 2).reshape(128, G, 16)
    dd = np.asarray(p["ssm_d"][0], np.float32).reshape(G, 16)
    d["ssm_dcol"] = np.ascontiguousarray(np.broadcast_to(dd.T[None, :, :], (8, 16, G)).reshape(128, G))
    d["ssm_w_glu"] = p["ssm_w_glu"][0]
    d["ssm_b_glu"] = p["ssm_b_glu"][0].reshape(1, 2 * D)
    NB = cfg.NT // 8
    seqb = seq_len // 8
    keep = np.ones((128, NB), np.float32)
    n = np.arange(NB)
    keep[:64, n % seqb == 0] = 0.0
    keep[64:, n % seqb == seqb - 1] = 0.0
    d["s5_keep"] = keep
    ip = np.arange(128) // 16
    mk = np.zeros((128, 2, 128), np.float32)
    mk[:, 0, :] = (ip[None, :] >= ip[:, None])
    mk[:, 1, :] = (ip[:, None] >= ip[None, :])
    d["s5_masks"] = mk
    EG = min(cfg.E, 8)
    for nm in ("gate", "up", "down"):
        w = p["moe_w_" + nm]
        for l in range(cfg.depth):
            for h in range(cfg.E // EG):
                d[f"moe_w_{nm}_{l}_{h}"] = w[l, h * EG:(h + 1) * EG]
    return {k: np.ascontiguousarray(v, dtype=np.float32) for k, v in d.items()}


def run(cfg, x_prompt, x_sample, c_prompt, c_sample, p, debug_out=None, n_cores=8):
    nc = build_program(cfg, debug_out=debug_out)
    bp, lp = x_prompt.shape[0], x_prompt.shape[1]
    bs, ls = x_sample.shape[0], x_sample.shape[1]

    def unit_info(b, l):
        upb = 4 // b
        rows = [u // upb for u in range(4)]
        masks = [0.0] + [1.0 if (u % upb) != 0 else 0.0 for u in range(1, 4)]
        return rows, masks

    rp, mp = unit_info(bp, lp)
    rs, ms = unit_info(bs, ls)
    inp_p = make_core_inputs(cfg, x_prompt, c_prompt, rp, mp, p, lp)
    inp_s = make_core_inputs(cfg, x_sample, c_sample, rs, ms, p, ls)
    for dct in (inp_p, inp_s):
        for k in list(dct):
            if (debug_out == "conv" and k.startswith("moe_")) or (debug_out in ("conv", "moe0") and (k.startswith("ssm_") or k.startswith("s5_"))):
                del dct[k]
    in_maps = [inp_p if (i % 2 == 0) else inp_s for i in range(n_cores)]
    res = run_bass_kernel_spmd(nc, in_maps, core_ids=list(range(n_cores)))
    yp = np.asarray(res.results[0]["y"], np.float32).reshape(x_prompt.shape)
    ys = np.asarray(res.results[1]["y"], np.float32).reshape(x_sample.shape)
    return yp, ys


def kernel(**inputs):
    cfg = Cfg()
    p = {k: np.asarray(v) for k, v in inputs.items()}
    return run(cfg, p["x_prompt"], p["x_sample"], p["c_prompt"], p["c_sample"], p, n_cores=2)
```

```python
import contextlib
import numpy as np
import concourse.bass as bass
import concourse.mybir as mybir
from concourse.bass_utils import run_bass_kernel_spmd

F32 = mybir.dt.float32
BF16 = mybir.dt.bfloat16
I32 = mybir.dt.int32
AF = mybir.ActivationFunctionType
ALU = mybir.AluOpType
AX = mybir.AxisListType

RMS_EPS = 1e-6
LN_EPS = 1e-5
CONV_W = 31
CONV_PAD = 15


class Cfg:
    def __init__(s, D=2048, NT=8192, E=16, depth=2):
        s.D = D
        s.FC = D // 128
        s.NT = NT
        s.UNIT = NT // 4
        s.E = E
        s.CAP = 2 * NT // E
        s.depth = depth
        s.G = D // 16


class Tile:
    def __init__(s, t):
        s.t = t
        s.w = None
        s.r = []

    def __getitem__(s, k):
        return s.t[k]


class Stream:
    def __init__(s, h, sem=None):
        s.h = h
        s.sem = sem
        s.cnt = 0
        s.seen = {}


class KB:
    NDS = 12
    verbose = False

    def __init__(s, nc):
        s.nc = nc
        s.es = contextlib.ExitStack()
        s.st = {}
        for nm, h in (("pe", nc.tensor), ("act", nc.scalar), ("dve", nc.vector), ("pool", nc.gpsimd), ("sp", nc.sync)):
            sem = s.es.enter_context(nc.semaphore("sem_" + nm))
            s.st[nm] = Stream(h, sem)
        s.dq = {}
        for q, stn in (("sp", "sp"), ("gq", "pool"), ("aq", "act")):
            sems = [s.es.enter_context(nc.semaphore(f"dq_{q}_{i}")) for i in range(s.NDS)]
            s.dq[q] = dict(stream=s.st[stn], sems=sems, cnt=[0] * s.NDS, i=0)
        s.stage = None
        s.uid = 0
        s.pending = []
        s.defer_stores = True

    def begin(s):
        if not hasattr(s, "stk"):
            s.stk = []
        s.stk.append(s.stage)
        s.stage = contextlib.ExitStack()

    def end(s):
        s.barrier()
        if KB.verbose:
            print("stage end:", {k: v.cnt for k, v in s.st.items()}, {q: max(Q["cnt"]) for q, Q in s.dq.items()}, flush=True)
        s.stage.close()
        s.stage = s.stk.pop()

    def sb(s, name, shape, dt):
        s.uid += 1
        return Tile(s.stage.enter_context(s.nc.sbuf_tensor(f"{name}_{s.uid}", list(shape), dt)))

    def ps(s, name, shape, dt=F32):
        s.uid += 1
        return Tile(s.stage.enter_context(s.nc.psum_tensor(f"{name}_{s.uid}", list(shape), dt)))

    def _wait(s, stream, sem, val):
        key = id(sem)
        if stream.seen.get(key, 0) >= val:
            return
        stream.h.wait_ge(sem, val)
        stream.seen[key] = val

    def _deps(s, stream, r, w):
        for b in r:
            if b.w is not None:
                s._wait(stream, *b.w)
        for b in w:
            if b.w is not None:
                s._wait(stream, *b.w)
            for tok in b.r:
                s._wait(stream, *tok)

    def _mark(s, tok, r, w):
        for b in r:
            b.r.append(tok)
            if len(b.r) > 24:
                d = {}
                for sem, v in b.r:
                    d[id(sem)] = (sem, max(v, d.get(id(sem), (sem, 0))[1]))
                b.r = list(d.values())
        for b in w:
            b.w = tok
            b.r = []

    def _flush(s, force=False, wset=None):
        if not s.pending:
            return
        keep = []
        emit_all_before = -1
        for i, p in enumerate(s.pending):
            hit = wset is not None and any(id(t) in wset for t in p["r"])
            if force or p["loads"] >= 1 or hit:
                emit_all_before = i
        pend, s.pending = s.pending, []
        for i, p in enumerate(pend):
            if i <= emit_all_before:
                s._dma_now(p["q"], p["out"], p["in_"], p["r"], (), None, p["kw"])
            else:
                keep.append(p)
        s.pending = keep + s.pending

    def op(s, eng, fn, r=(), w=()):
        if s.pending:
            s._flush(wset={id(t) for t in w})
        stream = s.st[eng]
        s._deps(stream, r, w)
        inst = fn(stream.h)
        stream.cnt += 1
        inst.then_inc(stream.sem, 1)
        tok = (stream.sem, stream.cnt)
        s._mark(tok, r, w)
        return tok

    def dma(s, q, out, in_, r=(), w=(), indirect=None, **kw):
        if q == "sp" and indirect is None and s.defer_stores:
            if not w:
                s.pending.append(dict(q=q, out=out, in_=in_, r=list(r), kw=kw, loads=0))
                return None
            if s.pending:
                s._flush(wset={id(t) for t in w})
            tok = s._dma_now(q, out, in_, r, w, indirect, kw)
            for p in s.pending:
                p["loads"] += 1
            return tok
        if s.pending:
            s._flush(wset={id(t) for t in w})
        return s._dma_now(q, out, in_, r, w, indirect, kw)

    def _dma_now(s, q, out, in_, r, w, indirect, kw):
        Q = s.dq[q]
        stream = Q["stream"]
        j = Q["i"] % s.NDS
        Q["i"] += 1
        sem = Q["sems"][j]
        if Q["cnt"][j] > 0:
            s._wait(stream, sem, Q["cnt"][j])
        s._deps(stream, r, w)
        if indirect is None:
            inst = stream.h.dma_start(out=out, in_=in_, **kw)
        else:
            inst = stream.h.indirect_dma_start(out=out, in_=in_, **indirect, **kw)
        inst.then_inc(sem, 16)
        Q["cnt"][j] += 16
        tok = (sem, Q["cnt"][j])
        s._mark(tok, r, w)
        return tok

    def barrier(s):
        s._flush(force=True)
        toks = []
        for stt in s.st.values():
            if stt.cnt > 0:
                toks.append((stt.sem, stt.cnt))
        for Q in s.dq.values():
            for sem, c in zip(Q["sems"], Q["cnt"]):
                if c > 0:
                    toks.append((sem, c))
        for stt in s.st.values():
            for sem, v in toks:
                if sem is stt.sem:
                    continue
                s._wait(stt, sem, v)


def build_program(cfg, debug_out=None):
    nc = bass.Bass("TRN2", target_bir_lowering=False)
    D, FC, NT, UNIT, E = cfg.D, cfg.FC, cfg.NT, cfg.UNIT, cfg.E
    NTT = NT // 128
    NTB = NT // 512
    D6 = 6 * D

    def inp(name, shape, dt=F32):
        return nc.dram_tensor(name, list(shape), dt, kind="ExternalInput")

    def scr(name, shape, dt):
        return nc.dram_tensor(name, list(shape), dt, kind="Internal")

    x_in = inp("x", [NT, D])
    cT = inp("cT", [128, FC, 4])
    masks = inp("masks", [128, 4])
    ada_w = inp("ada_w", [cfg.depth, D, D6])
    ada_b = inp("ada_b", [cfg.depth, D6])
    norm_mix_g = inp("norm_mix_g", [cfg.depth, D])
    norm_ffn_g = inp("norm_ffn_g", [cfg.depth, D])
    final_g = inp("final_norm_g", [1, D])
    conv_w_in = inp("conv_w_in", [D, 2 * D])
    conv_b_in = inp("conv_b_in_c", [128, 2 * FC])
    conv_w_dw = inp("conv_w_dw_c", [128, FC, CONV_W])
    conv_b_dw = inp("conv_b_dw_c", [128, FC])
    conv_ln_g = inp("conv_ln_g_c", [128, FC])
    conv_ln_b = inp("conv_ln_b_c", [128, FC])
    conv_w_out = inp("conv_w_out", [D, D])
    conv_b_out = inp("conv_b_out", [1, D])
    if debug_out != "conv":
        moe_w_router = inp("moe_w_router_c", [cfg.depth, 128, FC, E])
        EG = min(E, 8)
        moe_w = {nm: [[inp(f"moe_w_{nm}_{l}_{h}", [EG, D, D]) for h in range(E // EG)] for l in range(cfg.depth)]
                 for nm in ("gate", "up", "down")}
    G = cfg.G
    NB = NT // 8
    if debug_out not in ("conv", "moe0"):
        ssm_lre = inp("ssm_lre", [128, G])
        ssm_lim = inp("ssm_lim", [128, G])
        ssm_ls = inp("ssm_ls", [128, G])
        ssm_bre = inp("ssm_bre", [128, G, 16])
        ssm_bim = inp("ssm_bim", [128, G, 16])
        ssm_cre = inp("ssm_cre", [128, G, 16])
        ssm_cim = inp("ssm_cim", [128, G, 16])
        ssm_dcol = inp("ssm_dcol", [128, G])
        ssm_w_glu = inp("ssm_w_glu", [D, 2 * D])
        ssm_b_glu = inp("ssm_b_glu", [1, 2 * D])
        s5_keep = inp("s5_keep", [128, NB])
        s5_masks = inp("s5_masks", [128, 2, 128])
        HS = scr("HS", [NT, D], BF16)
        YS = scr("YS", [NT, D], BF16)
    y_out = nc.dram_tensor("y", [NT, D], F32, kind="ExternalOutput")
    CAP = cfg.CAP
    EXT = 2 + 2 * E
    DX = D + EXT
    OBW = min(512, D)
    NOB = D // OBW
    NST = CAP // 128
    H = scr("H", [NT, DX], BF16)
    XS = [scr(f"XS{e}", [CAP, DX], BF16) for e in range(E)]
    YBF = scr("YBF", [NT, D], F32)
    X2 = scr("X2", [NT, D], F32)
    X3 = scr("X3", [NT, D], F32)

    MOD = scr("MOD", [cfg.depth, 4, D6], F32)
    HT = scr("HT", [FC, 128, NT], BF16)
    UT = scr("UT", [FC, 128, NT], BF16)
    CU = scr("CU", [FC, 128, NT], F32)
    X1 = scr("X1", [NT, D], F32)

    kb = KB(nc)

    kb.begin()
    cst = kb.stage
    ident_f = kb.sb("identf", [128, 128], F32)
    ident_b = kb.sb("identb", [128, 128], BF16)
    ones_f = kb.sb("onesf", [128, 128], F32)
    kb.op("pool", lambda e: e.memset(ones_f[:], 1.0), w=[ones_f])
    iot = kb.sb("iot", [128, 128], I32)
    kb.op("pool", lambda e: e.iota(iot[:], pattern=[[1, 128]], base=0, channel_multiplier=-1), w=[iot])
    iotf = kb.sb("iotf", [128, 128], F32)
    kb.op("dve", lambda e: e.tensor_copy(iotf[:], iot[:]), r=[iot], w=[iotf])
    kb.op("dve", lambda e: e.tensor_scalar(ident_f[:], iotf[:], 0.0, None, op0=ALU.is_equal), r=[iotf], w=[ident_f])
    kb.op("dve", lambda e: e.tensor_copy(ident_b[:], ident_f[:]), r=[ident_f], w=[ident_b])
    mask_t = kb.sb("maskt", [128, 4], F32)
    kb.dma("sp", mask_t[:], masks.ap(), w=[mask_t])
    const_stack = kb.stage

    def row_bcast(tile, dram_row_ap):
        n = dram_row_ap.shape[-1]
        kb.dma("sp", tile[:, 0:n], dram_row_ap.partition_broadcast(128), w=[tile])

    def stage_mod():
        kb.begin()
        ct = kb.sb("ct", [128, FC, 4], F32)
        kb.dma("sp", ct[:], cT.ap(), w=[ct])
        cs_t = kb.sb("cs", [128, FC, 4], BF16)
        kb.op("act", lambda e: e.activation(out=cs_t[:], in_=ct[:], func=AF.Silu), r=[ct], w=[cs_t])
        NTL = D6 // 512
        wts = [kb.sb(f"adaw{i}", [128, FC, 512], BF16) for i in range(2)]
        pss = [kb.ps(f"modps{i}", [4, 512]) for i in range(2)]
        brs = [kb.sb(f"adab{i}", [4, 512], F32) for i in range(2)]
        mrs = [kb.sb(f"mrow{i}", [4, 512], F32) for i in range(2)]
        it = 0
        for li in range(cfg.depth):
            for nt in range(NTL):
                wt = wts[it % 2]
                ps = pss[it % 2]
                brow = brs[it % 2]
                mrow = mrs[it % 2]
                it += 1
                cs = slice(nt * 512, (nt + 1) * 512)
                src = ada_w.ap()[li].rearrange("(kc p) n -> p kc n", p=128)[:, :, cs]
                kb.dma("gq", wt[:], src, w=[wt])
                kb.dma("sp", brow[:], ada_b.ap()[li:li + 1, cs].partition_broadcast(4), w=[brow])
                for kc in range(FC):
                    kb.op("pe", lambda e, kc=kc: e.matmul(ps[:], lhsT=cs_t[:, kc, :], rhs=wt[:, kc, :],
                                                         start=(kc == 0), stop=(kc == FC - 1)),
                          r=[cs_t, wt], w=[ps] if kc == 0 else [], )
                    ps.w = (kb.st["pe"].sem, kb.st["pe"].cnt)
                kb.op("dve", lambda e: e.tensor_tensor(out=mrow[:], in0=ps[:], in1=brow[:], op=ALU.add),
                      r=[ps, brow], w=[mrow])
                kb.dma("sp", MOD.ap()[li, :, cs], mrow[:], r=[mrow])
        kb.end()

    def load_mod_rows(li, u, which_scale, which_shift, gain_ap, A_t, S_t, tmp):
        row_bcast(tmp, MOD.ap()[li, u:u + 1, which_scale * D:(which_scale + 1) * D])
        row_bcast(A_t, gain_ap)
        kb.op("dve", lambda e: e.scalar_tensor_tensor(out=A_t[:], in0=tmp[:], scalar=1.0, in1=A_t[:],
                                                     op0=ALU.add, op1=ALU.mult), r=[tmp, A_t], w=[A_t])
        row_bcast(S_t, MOD.ap()[li, u:u + 1, which_shift * D:(which_shift + 1) * D])

    def make_rms_ws():
        return dict(sq=kb.sb("rms_sq", [128, D], F32), tmp=[kb.sb(f"rms_tmp{i}", [128, D], F32) for i in range(2)],
                    ssum=[kb.sb(f"rms_ss{i}", [128, 1], F32) for i in range(2)],
                    rstd=[kb.sb(f"rms_rs{i}", [128, 3], F32) for i in range(2)], i=0)

    def rms_modulate(xt, A_t, S_t, h_out, ws):
        j = ws["i"] % 2
        ws["i"] += 1
        sq, junk, ssum, rstd = ws["sq"], ws["tmp"][j], ws["ssum"][j], ws["rstd"][j]
        kb.op("act", lambda e: e.activation(out=sq[:], in_=xt[:], func=AF.Square, accum_out=ssum[:, 0:1]),
              r=[xt], w=[sq, ssum])
        kb.op("dve", lambda e: e.tensor_scalar(rstd[:, 0:1], ssum[:, 0:1], 1.0 / D, RMS_EPS, op0=ALU.mult, op1=ALU.add),
              r=[ssum], w=[rstd])
        kb.op("act", lambda e: e.activation(out=rstd[:, 1:2], in_=rstd[:, 0:1], func=AF.Sqrt), r=[rstd], w=[rstd])
        kb.op("dve", lambda e: e.reciprocal(rstd[:, 2:3], rstd[:, 1:2]), r=[rstd], w=[rstd])
        kb.op("dve", lambda e: e.scalar_tensor_tensor(out=junk[:], in0=xt[:], scalar=rstd[:, 2:3], in1=A_t[:],
                                                     op0=ALU.mult, op1=ALU.mult), r=[xt, rstd, A_t], w=[junk])
        kb.op("dve", lambda e: e.tensor_tensor(out=h_out[:], in0=junk[:], in1=S_t[:], op=ALU.add),
              r=[junk, S_t], w=[h_out])

    def transpose_to_HT(h_bf, tt, psT, hT, dst):
        for fb in range(0, FC, 4):
            nb = min(4, FC - fb)
            ps = psT[(fb // 4) % 2]
            for j in range(nb):
                fc = fb + j
                kb.op("pe", lambda e, fc=fc, j=j: e.transpose(out=ps[:, j * 128:(j + 1) * 128],
                                                            in_=h_bf[:, fc * 128:(fc + 1) * 128], identity=ident_b[:]),
                      r=[h_bf, ident_b], w=[ps] if j == 0 else [])
                ps.w = (kb.st["pe"].sem, kb.st["pe"].cnt)
            kb.op("act", lambda e: e.activation(out=hT[:, fb:fb + nb, :],
                                                in_=ps[:, 0:nb * 128].rearrange("p (a b) -> p a b", b=128), func=AF.Copy),
                  r=[ps], w=[hT])
        kb.dma("sp", dst.ap()[:, :, tt * 128:(tt + 1) * 128].rearrange("fc p t -> p fc t"), hT[:], r=[hT])

    def stage_prenorm_T(li, x_src, gain, wsc, wsh):
        kb.begin()
        A_t = kb.sb("A", [128, D], F32)
        S_t = kb.sb("S", [128, D], F32)
        tmp = kb.sb("tmp", [128, D], F32)
        xts = [kb.sb(f"xt{i}", [128, D], F32) for i in range(2)]
        ws = make_rms_ws()
        hbs = [kb.sb(f"hb{i}", [128, D], BF16) for i in range(2)]
        hTs = [kb.sb(f"hT{i}", [128, FC, 128], BF16) for i in range(2)]
        psT = [kb.ps(f"psT{i}", [128, 512], BF16) for i in range(2)]
        for tt in range(NTT):
            u = (tt * 128) // UNIT
            if (tt * 128) % UNIT == 0:
                load_mod_rows(li, u, wsc, wsh, gain.ap()[li:li + 1, :], A_t, S_t, tmp)
            xt = xts[tt % 2]
            kb.dma("sp", xt[:], x_src.ap()[tt * 128:(tt + 1) * 128, :], w=[xt])
            hb = hbs[tt % 2]
            rms_modulate(xt, A_t, S_t, hb, ws)
            transpose_to_HT(hb, tt, psT, hTs[tt % 2], HT)
        kb.end()

    def stage_conv_in():
        kb.begin()
        FGS = min(4, FC)
        bin_t = kb.sb("bin", [128, 2 * FC], F32)
        kb.dma("sp", bin_t[:], conv_b_in.ap(), w=[bin_t])
        was = [kb.sb(f"wa{i}", [128, FC, FGS * 128], BF16) for i in range(2)]
        wgs = [kb.sb(f"wg{i}", [128, FC, FGS * 128], BF16) for i in range(2)]
        hts = [kb.sb(f"ht{i}", [128, FC, 512], BF16) for i in range(2)]
        psa = [kb.ps(f"psa{i}", [128, 512]) for i in range(2)]
        psg = [kb.ps(f"psg{i}", [128, 512]) for i in range(2)]
        sgs = [kb.sb(f"sg{i}", [128, 512], F32) for i in range(2)]
        uts = [kb.sb(f"ut{i}", [128, 512], BF16) for i in range(2)]
        wv = conv_w_in.ap().rearrange("(kc p) n -> p kc n", p=128)
        it = 0
        for fg in range(FC // FGS):
            wa, wg = was[fg % 2], wgs[fg % 2]
            kb.dma("gq", wa[:], wv[:, :, fg * FGS * 128:(fg + 1) * FGS * 128], w=[wa])
            kb.dma("gq", wg[:], wv[:, :, D + fg * FGS * 128:D + (fg + 1) * FGS * 128], w=[wg])
            for tb in range(NTB):
                ht = hts[tb % 2]
                kb.dma("sp", ht[:], HT.ap()[:, :, tb * 512:(tb + 1) * 512].rearrange("fc p t -> p fc t"), w=[ht])
                for j in range(FGS):
                    f = fg * FGS + j
                    pa, pg, sg, ut = psa[it % 2], psg[it % 2], sgs[it % 2], uts[it % 2]
                    it += 1
                    for kc in range(FC):
                        kb.op("pe", lambda e, kc=kc: e.matmul(pa[:], lhsT=wa[:, kc, j * 128:(j + 1) * 128], rhs=ht[:, kc, :],
                                                             start=(kc == 0), stop=(kc == FC - 1)),
                              r=[wa, ht], w=[pa] if kc == 0 else [])
                        pa.w = (kb.st["pe"].sem, kb.st["pe"].cnt)
                    for kc in range(FC):
                        kb.op("pe", lambda e, kc=kc: e.matmul(pg[:], lhsT=wg[:, kc, j * 128:(j + 1) * 128], rhs=ht[:, kc, :],
                                                             start=(kc == 0), stop=(kc == FC - 1)),
                              r=[wg, ht], w=[pg] if kc == 0 else [])
                        pg.w = (kb.st["pe"].sem, kb.st["pe"].cnt)
                    kb.op("act", lambda e: e.activation(out=sg[:], in_=pg[:], func=AF.Sigmoid,
                                                        bias=bin_t[:, FC + f:FC + f + 1]), r=[pg, bin_t], w=[sg])
                    kb.op("dve", lambda e: e.scalar_tensor_tensor(out=ut[:], in0=pa[:], scalar=bin_t[:, f:f + 1], in1=sg[:],
                                                                 op0=ALU.add, op1=ALU.mult), r=[pa, sg, bin_t], w=[ut])
                    kb.dma("sp", UT.ap()[f, :, tb * 512:(tb + 1) * 512], ut[:], r=[ut])
        kb.end()

    def stage_dwconv():
        kb.begin()
        wdw = kb.sb("wdw", [128, FC, CONV_W], F32)
        kb.dma("sp", wdw[:], conv_w_dw.ap(), w=[wdw])
        bdw = kb.sb("bdw", [128, FC], F32)
        kb.dma("sp", bdw[:], conv_b_dw.ap(), w=[bdw])
        dgs = [kb.sb(f"dg{i}", [128, CONV_W, 128], BF16) for i in range(2)]
        uws = [kb.sb(f"uw{i}", [128, 512 + 2 * CONV_PAD], BF16) for i in range(3)]
        pss = [kb.ps(f"cps{i}", [128, 512]) for i in range(2)]
        cus = [kb.sb(f"cu{i}", [128, 512], F32) for i in range(2)]
        it = 0
        for fc in range(FC):
            dg = dgs[fc % 2]
            for k in range(CONV_W):
                eng = "dve" if k % 2 == 0 else "pool"
                kb.op(eng, lambda e, k=k: e.tensor_scalar(dg[:, k, :], ident_f[:], wdw[:, fc, k:k + 1], None, op0=ALU.mult),
                      r=[ident_f, wdw], w=[dg])
            for tb in range(NTB):
                uw = uws[it % 3]
                ps = pss[it % 2]
                cu = cus[it % 2]
                it += 1
                t0 = tb * 512
                lo = max(t0 - CONV_PAD, 0)
                hi_ = min(t0 + 512 + CONV_PAD, NT)
                if t0 == 0:
                    kb.op("pool", lambda e: e.memset(uw[:, 0:CONV_PAD], 0.0), w=[uw])
                if t0 + 512 == NT:
                    kb.op("pool", lambda e: e.memset(uw[:, CONV_PAD + 512:], 0.0), w=[uw])
                kb.dma("sp", uw[:, lo - (t0 - CONV_PAD):hi_ - (t0 - CONV_PAD)], UT.ap()[fc, :, lo:hi_], w=[uw])
                if t0 > 0 and t0 % UNIT == 0:
                    b = t0 // UNIT
                    kb.op("dve", lambda e, b=b: e.tensor_scalar(uw[:, 0:CONV_PAD], uw[:, 0:CONV_PAD], mask_t[:, b:b + 1],
                                                             None, op0=ALU.mult), r=[uw, mask_t], w=[uw])
                if t0 + 512 < NT and (t0 + 512) % UNIT == 0:
                    b = (t0 + 512) // UNIT
                    kb.op("dve", lambda e, b=b: e.tensor_scalar(uw[:, CONV_PAD + 512:], uw[:, CONV_PAD + 512:],
                                                             mask_t[:, b:b + 1], None, op0=ALU.mult),
                          r=[uw, mask_t], w=[uw])
                for k in range(CONV_W):
                    kb.op("pe", lambda e, k=k: e.matmul(ps[:], lhsT=dg[:, k, :], rhs=uw[:, k:k + 512],
                                                       start=(k == 0), stop=(k == CONV_W - 1)),
                          r=[dg, uw], w=[ps] if k == 0 else [])
                    ps.w = (kb.st["pe"].sem, kb.st["pe"].cnt)
                kb.op("act", lambda e: e.activation(out=cu[:], in_=ps[:], func=AF.Identity, bias=bdw[:, fc:fc + 1]),
                      r=[ps, bdw], w=[cu])
                kb.dma("sp", CU.ap()[fc, :, t0:t0 + 512], cu[:], r=[cu])
        kb.end()

    def stage_conv_out(li, x_src, x_dst):
        kb.begin()
        lng = kb.sb("lng", [128, FC], F32)
        lnb = kb.sb("lnb", [128, FC], F32)
        kb.dma("sp", lng[:], conv_ln_g.ap(), w=[lng])
        kb.dma("sp", lnb[:], conv_ln_b.ap(), w=[lnb])
        wout = kb.sb("wout", [128, FC, D], BF16)
        wv = conv_w_out.ap().rearrange("(kc p) n -> p kc n", p=128)
        for kc in range(FC):
            kb.dma("gq", wout[:, kc, :], wv[:, kc, :], w=[wout])
        brow = kb.sb("brow", [128, D], F32)
        row_bcast(brow, conv_b_out.ap())
        G_t = kb.sb("G", [128, D], F32)
        cuts = [kb.sb(f"cut{i}", [128, FC, 512], F32) for i in range(2)]
        sq = kb.sb("sq", [128, 512], F32)
        ps1 = kb.ps("ps1", [128, 512])
        ps2 = kb.ps("ps2", [128, 512])
        mean = kb.sb("mean", [128, 512], F32)
        rstd = kb.sb("rstdln", [128, 512], F32)
        tmp = kb.sb("tmpln", [128, 512], F32)
        vTs = [kb.sb(f"vT{i}", [128, FC, 512], BF16) for i in range(2)]
        pso = [kb.ps(f"pso{i}", [128, 512]) for i in range(2)]
        xts = [kb.sb(f"xo{i}", [128, 512], F32) for i in range(2)]
        ots = [kb.sb(f"ot{i}", [128, 512], F32) for i in range(2)]
        it = 0
        NOB = D // 512 if D >= 512 else 1
        OBW = min(512, D)
        for tb in range(NTB):
            t0 = tb * 512
            if t0 % UNIT == 0:
                row_bcast(G_t, MOD.ap()[li, t0 // UNIT:t0 // UNIT + 1, 2 * D:3 * D])
            cut = cuts[tb % 2]
            vT = vTs[tb % 2]
            kb.dma("sp", cut[:], CU.ap()[:, :, t0:t0 + 512].rearrange("fc p t -> p fc t"), w=[cut])
            for fc in range(FC):
                kb.op("pe", lambda e, fc=fc: e.matmul(ps1[:], lhsT=ones_f[:], rhs=cut[:, fc, :], start=(fc == 0), stop=(fc == FC - 1)),
                      r=[ones_f, cut], w=[ps1] if fc == 0 else [])
                ps1.w = (kb.st["pe"].sem, kb.st["pe"].cnt)
            for fc in range(FC):
                kb.op("act", lambda e, fc=fc: e.activation(out=sq[:], in_=cut[:, fc, :], func=AF.Square), r=[cut], w=[sq])
                kb.op("pe", lambda e, fc=fc: e.matmul(ps2[:], lhsT=ones_f[:], rhs=sq[:], start=(fc == 0), stop=(fc == FC - 1)),
                      r=[ones_f, sq], w=[ps2] if fc == 0 else [])
                ps2.w = (kb.st["pe"].sem, kb.st["pe"].cnt)
            kb.op("dve", lambda e: e.tensor_scalar(mean[:], ps1[:], 1.0 / D, None, op0=ALU.mult), r=[ps1], w=[mean])
            kb.op("dve", lambda e: e.tensor_tensor(out=tmp[:], in0=mean[:], in1=mean[:], op=ALU.mult), r=[mean], w=[tmp])
            kb.op("dve", lambda e: e.scalar_tensor_tensor(out=tmp[:], in0=ps2[:], scalar=1.0 / D, in1=tmp[:],
                                                         op0=ALU.mult, op1=ALU.subtract), r=[ps2, tmp], w=[tmp])
            kb.op("dve", lambda e: e.tensor_scalar(tmp[:], tmp[:], LN_EPS, None, op0=ALU.add), r=[tmp], w=[tmp])
            kb.op("act", lambda e: e.activation(out=tmp[:], in_=tmp[:], func=AF.Sqrt), r=[tmp], w=[tmp])
            kb.op("dve", lambda e: e.reciprocal(rstd[:], tmp[:]), r=[tmp], w=[rstd])
            for fc in range(FC):
                kb.op("dve", lambda e, fc=fc: e.tensor_tensor(out=cut[:, fc, :], in0=cut[:, fc, :], in1=mean[:], op=ALU.subtract),
                      r=[cut, mean], w=[cut])
                kb.op("pool", lambda e, fc=fc: e.tensor_tensor(out=cut[:, fc, :], in0=cut[:, fc, :], in1=rstd[:], op=ALU.mult),
                      r=[cut, rstd], w=[cut])
                kb.op("act", lambda e, fc=fc: e.activation(out=vT[:, fc, :], in_=cut[:, fc, :], func=AF.Silu,
                                                           scale=lng[:, fc:fc + 1], bias=lnb[:, fc:fc + 1]),
                      r=[cut, lng, lnb], w=[vT])
            for ts in range(4):
                r0 = t0 + ts * 128
                for ob in range(NOB):
                    ps = pso[it % 2]
                    xt = xts[it % 2]
                    ot = ots[it % 2]
                    it += 1
                    cs = slice(ob * OBW, (ob + 1) * OBW)
                    kb.dma("sp", xt[:, 0:OBW], x_src.ap()[r0:r0 + 128, cs], w=[xt])
                    for kc in range(FC):
                        kb.op("pe", lambda e, kc=kc: e.matmul(ps[:, 0:OBW], lhsT=vT[:, kc, ts * 128:(ts + 1) * 128], rhs=wout[:, kc, cs],
                                                             start=(kc == 0), stop=(kc == FC - 1)),
                              r=[vT, wout], w=[ps] if kc == 0 else [])
                        ps.w = (kb.st["pe"].sem, kb.st["pe"].cnt)
                    kb.op("dve", lambda e: e.tensor_tensor(out=ot[:, 0:OBW], in0=ps[:, 0:OBW], in1=brow[:, cs], op=ALU.add),
                          r=[ps, brow], w=[ot])
                    kb.op("pool", lambda e: e.tensor_tensor(out=ot[:, 0:OBW], in0=ot[:, 0:OBW], in1=G_t[:, cs], op=ALU.mult),
                          r=[ot, G_t], w=[ot])
                    kb.op("dve", lambda e: e.tensor_tensor(out=ot[:, 0:OBW], in0=ot[:, 0:OBW], in1=xt[:, 0:OBW], op=ALU.add),
                          r=[ot, xt], w=[ot])
                    kb.dma("sp", x_dst.ap()[r0:r0 + 128, cs], ot[:, 0:OBW], r=[ot])
        kb.end()


    def acc_group(ps, n, mk, r):
        for i in range(n):
            kb.op("pe", lambda e, i=i: mk(e, i), r=r, w=[ps] if i == 0 else [])
            ps.w = (kb.st["pe"].sem, kb.st["pe"].cnt)

    def stage_moe(li, x_src):
        kb.begin()
        AFF = kb.sb("AFF", [128, NTT, E], F32)
        SLOT = kb.sb("SLOT", [128, E, NTT], I32)
        pidx = kb.sb("pidx", [128, 1], F32)
        pidi = kb.sb("pidi", [128, 1], I32)
        kb.op("pool", lambda e: e.iota(pidi[:], pattern=[[0, 1]], base=0, channel_multiplier=1), w=[pidi])
        kb.op("dve", lambda e: e.tensor_copy(pidx[:], pidi[:]), r=[pidi], w=[pidx])
        ltri = kb.sb("ltri", [128, 128], F32)
        kb.op("dve", lambda e: e.tensor_scalar(ltri[:], iotf[:], 0.0, None, op0=ALU.is_gt), r=[iotf], w=[ltri])
        kb.begin()
        zt = kb.sb("zt", [128, D], F32)
        kb.op("pool", lambda e: e.memset(zt[:], 0.0), w=[zt])
        for tt in range(NTT):
            kb.dma("sp", YBF.ap()[tt * 128:(tt + 1) * 128, :], zt[:], r=[zt])
        A_t = kb.sb("A", [128, D], F32)
        S_t = kb.sb("S", [128, D], F32)
        tmp = kb.sb("tmp", [128, D], F32)
        xts = [kb.sb(f"xt{i}", [128, D], F32) for i in range(2)]
        ws = make_rms_ws()
        hfs = [kb.sb(f"hf{i}", [128, D], F32) for i in range(2)]
        hxs = [kb.sb(f"hx{i}", [128, DX], BF16) for i in range(2)]
        hTs_ = [kb.sb(f"hTf{i}", [128, FC, 128], F32) for i in range(2)]
        sms = [kb.sb(f"sm{i}", [128, 4], F32) for i in range(2)]
        exs = [kb.sb(f"ex{i}", [128, E], F32) for i in range(2)]
        wr = kb.sb("wr", [128, FC, E], F32)
        kb.dma("sp", wr[:], moe_w_router.ap()[li], w=[wr])
        psT = [kb.ps(f"psTf{i}", [128, 512], F32) for i in range(2)]
        psr = kb.ps("psr", [128, E], F32)
        for tt in range(NTT):
            u = (tt * 128) // UNIT
            if (tt * 128) % UNIT == 0:
                load_mod_rows(li, u, 4, 3, norm_ffn_g.ap()[li:li + 1, :], A_t, S_t, tmp)
            xt = xts[tt % 2]
            hf = hfs[tt % 2]
            hx = hxs[tt % 2]
            kb.dma("sp", xt[:], x_src.ap()[tt * 128:(tt + 1) * 128, :], w=[xt])
            rms_modulate(xt, A_t, S_t, hf, ws)
            hT, sm, ex = hTs_[tt % 2], sms[tt % 2], exs[tt % 2]
            kb.op("act", lambda e: e.activation(out=hx[:, 0:D], in_=hf[:], func=AF.Copy), r=[hf], w=[hx])
            kb.op("pool", lambda e, tt=tt: e.memset(hx[:, D:D + 1], float(tt)), w=[hx])
            kb.op("pool", lambda e: e.tensor_copy(hx[:, D + 1:D + 2], pidx[:]), r=[pidx], w=[hx])
            for fb in range(0, FC, 4):
                nb = min(4, FC - fb)
                ps = psT[(fb // 4) % 2]
                acc_group(ps, nb, lambda e, j, fb=fb, ps=ps: e.transpose(out=ps[:, j * 128:(j + 1) * 128],
                                                                     in_=hf[:, (fb + j) * 128:(fb + j + 1) * 128],
                                                                     identity=ident_f[:]), r=[hf, ident_f])
                kb.op("dve", lambda e, fb=fb, nb=nb, ps=ps: e.tensor_copy(
                    hT[:, fb:fb + nb, :], ps[:, 0:nb * 128].rearrange("p (a b) -> p a b", b=128)), r=[ps], w=[hT])
            acc_group(psr, FC, lambda e, kc: e.matmul(psr[:], lhsT=hT[:, kc, :], rhs=wr[:, kc, :],
                                                     start=(kc == 0), stop=(kc == FC - 1)), r=[hT, wr])
            kb.op("dve", lambda e: e.tensor_reduce(out=sm[:, 0:1], in_=psr[:], axis=AX.X, op=ALU.max), r=[psr], w=[sm])
            kb.op("dve", lambda e: e.tensor_scalar(sm[:, 1:2], sm[:, 0:1], -1.0, None, op0=ALU.mult), r=[sm], w=[sm])
            kb.op("act", lambda e: e.activation(out=ex[:], in_=psr[:], func=AF.Exp, bias=sm[:, 1:2], accum_out=sm[:, 2:3]),
                  r=[psr, sm], w=[ex, sm])
            kb.op("dve", lambda e: e.reciprocal(sm[:, 3:4], sm[:, 2:3]), r=[sm], w=[sm])
            kb.op("dve", lambda e, tt=tt: e.tensor_scalar(AFF[:, tt, :], ex[:], sm[:, 3:4], None, op0=ALU.mult),
                  r=[ex, sm], w=[AFF])
            kb.op("dve", lambda e, tt=tt: e.tensor_copy(hx[:, D + 2:DX].bitcast(F32), AFF[:, tt, :]), r=[AFF], w=[hx])
            kb.dma("sp", H.ap()[tt * 128:(tt + 1) * 128, :], hx[:], r=[hx])
        kb.end()
        kb.begin()
        AFFv = AFF[:].rearrange("p t e -> p e t")
        lo = kb.sb("lo", [128, E], F32)
        hi = kb.sb("hi", [128, E], F32)
        mid = kb.sb("mid", [128, E], F32)
        ge = kb.sb("ge", [128, E], F32)
        nge = kb.sb("nge", [128, E], F32)
        ta = kb.sb("ta", [128, E], F32)
        tb_ = kb.sb("tb", [128, E], F32)
        cnt = kb.sb("cnt", [128, E], F32)
        cmp = kb.sb("cmp", [128, E, NTT], F32)
        pst = kb.ps("pst", [128, E], F32)
        kb.op("pool", lambda e: e.memset(lo[:], 0.0), w=[lo])
        kb.op("pool", lambda e: e.memset(hi[:], 1.0), w=[hi])

        def bc(t):
            return t[:, :].unsqueeze(2).broadcast_to([128, E, NTT])

        for it in range(34):
            kb.op("dve", lambda e: e.tensor_tensor(out=mid[:], in0=lo[:], in1=hi[:], op=ALU.add), r=[lo, hi], w=[mid])
            kb.op("dve", lambda e: e.tensor_scalar(mid[:], mid[:], 0.5, None, op0=ALU.mult), r=[mid], w=[mid])
            kb.op("dve", lambda e: e.tensor_tensor(out=cmp[:], in0=AFFv, in1=bc(mid), op=ALU.is_ge), r=[AFF, mid], w=[cmp])
            kb.op("dve", lambda e: e.tensor_reduce(out=cnt[:], in_=cmp[:], axis=AX.X, op=ALU.add), r=[cmp], w=[cnt])
            kb.op("pe", lambda e: e.matmul(pst[:], lhsT=ones_f[:], rhs=cnt[:], start=True, stop=True), r=[ones_f, cnt], w=[pst])
            kb.op("dve", lambda e: e.tensor_scalar(ge[:], pst[:], float(CAP), None, op0=ALU.is_ge), r=[pst], w=[ge])
            kb.op("dve", lambda e: e.tensor_scalar(nge[:], ge[:], -1.0, 1.0, op0=ALU.mult, op1=ALU.add), r=[ge], w=[nge])
            kb.op("dve", lambda e: e.tensor_tensor(out=ta[:], in0=ge[:], in1=mid[:], op=ALU.mult), r=[ge, mid], w=[ta])
            kb.op("dve", lambda e: e.tensor_tensor(out=tb_[:], in0=nge[:], in1=lo[:], op=ALU.mult), r=[nge, lo], w=[tb_])
            kb.op("dve", lambda e: e.tensor_tensor(out=lo[:], in0=ta[:], in1=tb_[:], op=ALU.add), r=[ta, tb_], w=[lo])
            kb.op("dve", lambda e: e.tensor_tensor(out=ta[:], in0=nge[:], in1=mid[:], op=ALU.mult), r=[nge, mid], w=[ta])
            kb.op("dve", lambda e: e.tensor_tensor(out=tb_[:], in0=ge[:], in1=hi[:], op=ALU.mult), r=[ge, hi], w=[tb_])
            kb.op("dve", lambda e: e.tensor_tensor(out=hi[:], in0=ta[:], in1=tb_[:], op=ALU.add), r=[ta, tb_], w=[hi])
        rst = kb.sb("rst", [128, E, NTT], F32)
        kb.op("pool", lambda e: e.memset(rst[:], 1.0), w=[rst])
        kb.op("pool", lambda e: e.memset(rst[:, :, 0:1], 0.0), w=[rst])
        pre = kb.sb("pre", [128, E, NTT], F32)
        kb.op("dve", lambda e: e.tensor_tensor(out=cmp[:], in0=AFFv, in1=bc(lo), op=ALU.is_ge), r=[AFF, lo], w=[cmp])
        kb.op("dve", lambda e: e.tensor_tensor_scan(out=pre[:].rearrange("p a b -> p (a b)"),
                                                   data0=rst[:].rearrange("p a b -> p (a b)"),
                                                   data1=cmp[:].rearrange("p a b -> p (a b)"),
                                                   initial=0.0, op0=ALU.mult, op1=ALU.add), r=[rst, cmp], w=[pre])
        kb.op("dve", lambda e: e.tensor_copy(cnt[:], pre[:, :, NTT - 1]), r=[pre], w=[cnt])
        kb.op("pe", lambda e: e.matmul(pst[:], lhsT=ltri[:], rhs=cnt[:], start=True, stop=True), r=[ltri, cnt], w=[pst])
        kb.op("dve", lambda e: e.tensor_scalar(ta[:], pst[:], -1.0, None, op0=ALU.add), r=[pst], w=[ta])
        BIG = 1000000.0
        kb.op("dve", lambda e: e.tensor_tensor(out=pre[:], in0=pre[:], in1=bc(ta), op=ALU.add), r=[pre, ta], w=[pre])
        kb.op("dve", lambda e: e.tensor_scalar(pre[:], pre[:], -BIG, None, op0=ALU.add), r=[pre], w=[pre])
        kb.op("dve", lambda e: e.tensor_tensor(out=pre[:], in0=pre[:], in1=cmp[:], op=ALU.mult), r=[pre, cmp], w=[pre])
        kb.op("dve", lambda e: e.tensor_scalar(pre[:], pre[:], BIG, None, op0=ALU.add), r=[pre], w=[pre])
        kb.op("dve", lambda e: e.tensor_copy(SLOT[:], pre[:]), r=[pre], w=[SLOT])
        hxs = [kb.sb(f"hxd{i}", [128, DX], BF16) for i in range(3)]
        bc_reg = nc.gpsimd.to_reg(CAP - 1)
        for tt in range(NTT):
            hx = hxs[tt % 3]
            kb.dma("sp", hx[:], H.ap()[tt * 128:(tt + 1) * 128, :], w=[hx])
            for ei in range(E):
                kb.dma("gq", XS[ei].ap()[:, :], hx[:, :], r=[hx, SLOT],
                       indirect=dict(out_offset=bass.IndirectOffsetOnAxis(ap=SLOT[:, ei, tt:tt + 1], axis=0), in_offset=None,
                                     bounds_check=bc_reg, oob_is_err=False))
        kb.end()
        kb.begin()
        FBW = min(256, D)
        xsT = kb.sb("xsT", [128, FC, CAP], BF16)
        hidT = kb.sb("hidT", [128, FC, CAP], BF16)
        IDX = kb.sb("IDX", [128, NST], I32)
        GT = kb.sb("GT", [128, NST], F32)
        idf = kb.sb("idf", [128, 4], F32)
        xss = [kb.sb(f"xs{i}", [128, DX], BF16) for i in range(2)]
        wgs = [kb.sb(f"wg{i}", [128, FC, FBW], BF16) for i in range(2)]
        wus = [kb.sb(f"wu{i}", [128, FC, FBW], BF16) for i in range(2)]
        wdp = [kb.sb(f"wdp{i}", [128, FC, OBW], BF16) for i in range(NOB)]
        outs = [kb.sb(f"outm{i}", [128, D], F32) for i in range(2)]
        sgs = [kb.sb(f"sgm{i}", [128, 512], F32) for i in range(2)]
        psT2 = [kb.ps(f"psTb{i}", [128, 512], BF16) for i in range(2)]
        psg = [kb.ps(f"psgm{i}", [128, 512]) for i in range(2)]
        psu = [kb.ps(f"psum{i}", [128, 512]) for i in range(2)]
        pso = [kb.ps(f"psom{i}", [128, OBW]) for i in range(2)]
        ytok = kb.sb("ytok", [1, 1], F32)
        nw = 0
        nd = 0
        ni = 0
        NFB = D // FBW
        wsched = []
        for ei_ in range(E):
            wsched += [("gu", ei_, fb_) for fb_ in range(NFB)]
        wstate = dict(issued=0, ngu=0, nd=0, bufs={})

        def w_issue(upto):
            while wstate["issued"] <= upto and wstate["issued"] < len(wsched):
                k = wstate["issued"]
                kind, e_, b_ = wsched[k]
                if kind == "gu":
                    gv_ = moe_w["gate"][li][e_ // EG].ap()[e_ % EG].rearrange("(kc p) f -> p kc f", p=128)
                    uv_ = moe_w["up"][li][e_ // EG].ap()[e_ % EG].rearrange("(kc p) f -> p kc f", p=128)
                    wg_, wu_ = wgs[wstate["ngu"] % 2], wus[wstate["ngu"] % 2]
                    wstate["ngu"] += 1
                    kb.dma("gq", wg_[:], gv_[:, :, b_ * FBW:(b_ + 1) * FBW], w=[wg_])
                    kb.dma("gq", wu_[:], uv_[:, :, b_ * FBW:(b_ + 1) * FBW], w=[wu_])
                    wstate["bufs"][k] = (wg_, wu_)
                wstate["issued"] += 1

        wk = 0
        w_issue(0)
        for ei in range(E):
            for st in range(NST):
                xs = xss[st % 2]
                kb.dma("sp", xs[:], XS[ei].ap()[st * 128:(st + 1) * 128, :], w=[xs])
                for fb in range(0, FC, 4):
                    nb = min(4, FC - fb)
                    ps = psT2[(fb // 4) % 2]
                    acc_group(ps, nb, lambda e, j, fb=fb, ps=ps, xs=xs: e.transpose(
                        out=ps[:, j * 128:(j + 1) * 128], in_=xs[:, (fb + j) * 128:(fb + j + 1) * 128], identity=ident_b[:]),
                        r=[xs, ident_b])
                    kb.op("act" if (fb // 4) % 2 == 0 else "dve",
                          (lambda e, fb=fb, nb=nb, ps=ps, st=st: e.activation(
                              out=xsT[:, fb:fb + nb, st * 128:(st + 1) * 128],
                              in_=ps[:, 0:nb * 128].rearrange("p (a b) -> p a b", b=128), func=AF.Copy))
                          if (fb // 4) % 2 == 0 else
                          (lambda e, fb=fb, nb=nb, ps=ps, st=st: e.tensor_copy(
                              xsT[:, fb:fb + nb, st * 128:(st + 1) * 128],
                              ps[:, 0:nb * 128].rearrange("p (a b) -> p a b", b=128))),
                          r=[ps], w=[xsT])
                kb.op("dve", lambda e, xs=xs: e.tensor_copy(idf[:, 0:2], xs[:, D:D + 2]), r=[xs], w=[idf])
                kb.op("dve", lambda e: e.scalar_tensor_tensor(out=idf[:, 2:3], in0=idf[:, 0:1], scalar=128.0, in1=idf[:, 1:2],
                                                             op0=ALU.mult, op1=ALU.add), r=[idf], w=[idf])
                kb.op("dve", lambda e, st=st: e.tensor_copy(IDX[:, st:st + 1], idf[:, 2:3]), r=[idf], w=[IDX])
                kb.op("dve", lambda e, st=st, xs=xs, ei=ei: e.tensor_copy(
                    GT[:, st:st + 1], xs[:, D + 2 + 2 * ei:D + 4 + 2 * ei].bitcast(F32)), r=[xs], w=[GT])
            gv = moe_w["gate"][li][ei // EG].ap()[ei % EG].rearrange("(kc p) f -> p kc f", p=128)
            uv = moe_w["up"][li][ei // EG].ap()[ei % EG].rearrange("(kc p) f -> p kc f", p=128)
            dv = moe_w["down"][li][ei // EG].ap()[ei % EG].rearrange("(kc p) f -> p kc f", p=128)
            for ob in range(NOB):
                kb.dma("gq", wdp[ob][:], dv[:, :, ob * OBW:(ob + 1) * OBW], w=[wdp[ob]])
            for fb in range(D // FBW):
                w_issue(wk + 1)
                wg, wu = wstate["bufs"].pop(wk)
                wk += 1
                for j in range(FBW // 128):
                    f = fb * (FBW // 128) + j
                    for sb_ in range(CAP // 512):
                        pg, pu, sg = psg[ni % 2], psu[ni % 2], sgs[ni % 2]
                        ni += 1
                        cs = slice(sb_ * 512, (sb_ + 1) * 512)
                        acc_group(pg, FC, lambda e, kc, pg=pg, wg=wg, j=j, cs=cs: e.matmul(
                            pg[:], lhsT=wg[:, kc, j * 128:(j + 1) * 128], rhs=xsT[:, kc, cs],
                            start=(kc == 0), stop=(kc == FC - 1)), r=[wg, xsT])
                        acc_group(pu, FC, lambda e, kc, pu=pu, wu=wu, j=j, cs=cs: e.matmul(
                            pu[:], lhsT=wu[:, kc, j * 128:(j + 1) * 128], rhs=xsT[:, kc, cs],
                            start=(kc == 0), stop=(kc == FC - 1)), r=[wu, xsT])
                        kb.op("act", lambda e, pg=pg, sg=sg: e.activation(out=sg[:], in_=pg[:], func=AF.Silu), r=[pg], w=[sg])
                        kb.op("dve", lambda e, pu=pu, sg=sg, f=f, cs=cs: e.tensor_tensor(
                            out=hidT[:, f, cs], in0=pu[:], in1=sg[:], op=ALU.mult), r=[pu, sg], w=[hidT])
            for st in range(NST):
                OUT = outs[st % 2]
                for ob in range(NOB):
                    po = pso[ni % 2]
                    ni += 1
                    wd = wdp[ob]
                    acc_group(po, FC, lambda e, kc, po=po, wd=wd, st=st: e.matmul(
                        po[:], lhsT=hidT[:, kc, st * 128:(st + 1) * 128], rhs=wd[:, kc, :],
                        start=(kc == 0), stop=(kc == FC - 1)), r=[hidT, wd])
                    osl = slice(ob * OBW, (ob + 1) * OBW)
                    kb.op("dve" if ni % 2 else "act",
                          (lambda e, po=po, OUT=OUT, st=st, osl=osl: e.tensor_scalar(OUT[:, osl], po[:], GT[:, st:st + 1], None, op0=ALU.mult))
                          if ni % 2 else
                          (lambda e, po=po, OUT=OUT, st=st, osl=osl: e.activation(out=OUT[:, osl], in_=po[:], func=AF.Identity, scale=GT[:, st:st + 1])),
                          r=[po, GT], w=[OUT])
                kb.dma("gq", YBF.ap()[:, :], OUT[:, :], r=[OUT, IDX], w=[ytok],
                       indirect=dict(out_offset=bass.IndirectOffsetOnAxis(ap=IDX[:, st:st + 1], axis=0), in_offset=None,
                                     compute_op=ALU.add))
        kb.end()
        kb.end()

    def stage_combine(li, x_src, x_dst, final):
        kb.begin()
        G_t = kb.sb("G2", [128, D], F32)
        F_t = kb.sb("Fg", [128, D], F32)
        if final:
            row_bcast(F_t, final_g.ap())
        xts = [kb.sb(f"xc{i}", [128, D], F32) for i in range(2)]
        yts = [kb.sb(f"yc{i}", [128, D], F32) for i in range(2)]
        junk = kb.sb("junkc", [128, D], F32)
        ssum = kb.sb("ssumc", [128, 1], F32)
        rstd = kb.sb("rstdc", [128, 3], F32)
        for tt in range(NTT):
            if (tt * 128) % UNIT == 0:
                u = (tt * 128) // UNIT
                row_bcast(G_t, MOD.ap()[li, u:u + 1, 5 * D:6 * D])
            xt, yt = xts[tt % 2], yts[tt % 2]
            kb.dma("sp", xt[:], x_src.ap()[tt * 128:(tt + 1) * 128, :], w=[xt])
            kb.dma("sp", yt[:], YBF.ap()[tt * 128:(tt + 1) * 128, :], w=[yt])
            kb.op("pool", lambda e, yt=yt: e.tensor_tensor(out=yt[:], in0=yt[:], in1=G_t[:], op=ALU.mult), r=[yt, G_t], w=[yt])
            kb.op("dve", lambda e, xt=xt, yt=yt: e.tensor_tensor(out=xt[:], in0=xt[:], in1=yt[:], op=ALU.add), r=[xt, yt], w=[xt])
            if final:
                kb.op("act", lambda e, xt=xt: e.activation(out=junk[:], in_=xt[:], func=AF.Square, accum_out=ssum[:, 0:1]),
                      r=[xt], w=[junk, ssum])
                kb.op("dve", lambda e: e.tensor_scalar(rstd[:, 0:1], ssum[:, 0:1], 1.0 / D, RMS_EPS, op0=ALU.mult, op1=ALU.add),
                      r=[ssum], w=[rstd])
                kb.op("act", lambda e: e.activation(out=rstd[:, 1:2], in_=rstd[:, 0:1], func=AF.Sqrt), r=[rstd], w=[rstd])
                kb.op("dve", lambda e: e.reciprocal(rstd[:, 2:3], rstd[:, 1:2]), r=[rstd], w=[rstd])
                kb.op("dve", lambda e, xt=xt, yt=yt: e.scalar_tensor_tensor(out=yt[:], in0=xt[:], scalar=rstd[:, 2:3], in1=F_t[:],
                                                                       op0=ALU.mult, op1=ALU.mult), r=[xt, rstd, F_t], w=[yt])
                kb.dma("sp", x_dst.ap()[tt * 128:(tt + 1) * 128, :], yt[:], r=[yt])
            else:
                kb.dma("sp", x_dst.ap()[tt * 128:(tt + 1) * 128, :], xt[:], r=[xt])
        kb.end()


    def stage_prenorm_rows(li, x_src, gain, wsc, wsh, dst):
        kb.begin()
        A_t = kb.sb("A", [128, D], F32)
        S_t = kb.sb("S", [128, D], F32)
        tmp = kb.sb("tmp", [128, D], F32)
        xts = [kb.sb(f"xt{i}", [128, D], F32) for i in range(2)]
        ws = make_rms_ws()
        hbs = [kb.sb(f"hb{i}", [128, D], BF16) for i in range(2)]
        for tt in range(NTT):
            u = (tt * 128) // UNIT
            if (tt * 128) % UNIT == 0:
                load_mod_rows(li, u, wsc, wsh, gain.ap()[li:li + 1, :], A_t, S_t, tmp)
            xt = xts[tt % 2]
            kb.dma("sp", xt[:], x_src.ap()[tt * 128:(tt + 1) * 128, :], w=[xt])
            hb = hbs[tt % 2]
            rms_modulate(xt, A_t, S_t, hb, ws)
            kb.dma("sp", dst.ap()[tt * 128:(tt + 1) * 128, :], hb[:], r=[hb])
        kb.end()

    def stage_rows_to_T(src):
        kb.begin()
        hbs = [kb.sb(f"hbr{i}", [128, D], BF16) for i in range(2)]
        hTs = [kb.sb(f"hTr{i}", [128, FC, 128], BF16) for i in range(2)]
        psT = [kb.ps(f"psTr{i}", [128, 512], BF16) for i in range(2)]
        for tt in range(NTT):
            hb = hbs[tt % 2]
            kb.dma("sp", hb[:], src.ap()[tt * 128:(tt + 1) * 128, :], w=[hb])
            transpose_to_HT(hb, tt, psT, hTs[tt % 2], HT)
        kb.end()

    def stage_s5():
        kb.begin()
        CW = min(512, NB)
        NH = NB // CW
        NNT = NB // 128
        TWO_PI = 2.0 * np.pi
        isb = kb.sb("isb", [128, 1], F32)
        pidi = kb.sb("pidi5", [128, 1], I32)
        kb.op("pool", lambda e: e.iota(pidi[:], pattern=[[0, 1]], base=0, channel_multiplier=1), w=[pidi])
        kb.op("dve", lambda e: e.tensor_copy(isb[:], pidi[:]), r=[pidi], w=[isb])
        kb.op("dve", lambda e: e.tensor_scalar(isb[:], isb[:], 63.5, None, op0=ALU.is_gt), r=[isb], w=[isb])
        sign = kb.sb("sign", [128, 1], F32)
        kb.op("dve", lambda e: e.tensor_scalar(sign[:], isb[:], 2.0, -1.0, op0=ALU.mult, op1=ALU.add), r=[isb], w=[sign])
        nsign = kb.sb("nsign", [128, 1], F32)
        kb.op("dve", lambda e: e.tensor_scalar(nsign[:], sign[:], -1.0, None, op0=ALU.mult), r=[sign], w=[nsign])
        iri = kb.sb("iri", [128, 8], I32)
        kb.op("pool", lambda e: e.iota(iri[:], pattern=[[1, 8]], base=0, channel_multiplier=0), w=[iri])
        EX = kb.sb("EX", [128, 4, 8], F32)
        kb.op("dve", lambda e: e.tensor_copy(EX[:, 0, :], iri[:]), r=[iri], w=[EX])
        kb.op("dve", lambda e: e.tensor_scalar(EX[:, 0, :], EX[:, 0, :], sign[:, 0:1], None, op0=ALU.mult), r=[EX, sign], w=[EX])
        kb.op("dve", lambda e: e.tensor_scalar(EX[:, 1, :], EX[:, 0, :], -1.0, None, op0=ALU.mult), r=[EX], w=[EX])
        off3 = kb.sb("off3", [128, 2], F32)
        kb.op("dve", lambda e: e.tensor_scalar(off3[:, 0:1], isb[:], -7.0, 7.0, op0=ALU.mult, op1=ALU.add), r=[isb], w=[off3])
        kb.op("dve", lambda e: e.tensor_scalar(off3[:, 1:2], isb[:], 7.0, 1.0, op0=ALU.mult, op1=ALU.add), r=[isb], w=[off3])
        kb.op("dve", lambda e: e.tensor_scalar(EX[:, 2, :], EX[:, 0, :], off3[:, 0:1], None, op0=ALU.add), r=[EX, off3], w=[EX])
        kb.op("dve", lambda e: e.tensor_scalar(EX[:, 3, :], EX[:, 1, :], off3[:, 1:2], None, op0=ALU.add), r=[EX, off3], w=[EX])
        tni = kb.sb("tni", [128, NB], I32)
        kb.op("pool", lambda e: e.iota(tni[:], pattern=[[1, NB]], base=0, channel_multiplier=0), w=[tni])
        nrow = kb.sb("nrow", [128, NB], F32)
        kb.op("dve", lambda e: e.tensor_copy(nrow[:], tni[:]), r=[tni], w=[nrow])
        KEEP = kb.sb("KEEP", [128, NB], F32)
        kb.dma("sp", KEEP[:], s5_keep.ap(), w=[KEEP])
        MK = kb.sb("MK", [128, 2, 128], F32)
        kb.dma("sp", MK[:], s5_masks.ap(), w=[MK])
        lre = kb.sb("lre", [128, G], F32)
        lim = kb.sb("lim", [128, G], F32)
        lst = kb.sb("lst", [128, G], F32)
        kb.dma("sp", lre[:], ssm_lre.ap(), w=[lre])
        kb.dma("sp", lim[:], ssm_lim.ap(), w=[lim])
        kb.dma("sp", lst[:], ssm_ls.ap(), w=[lst])
        dcol = kb.sb("dcol", [128, G], F32)
        kb.dma("sp", dcol[:], ssm_dcol.ap(), w=[dcol])
        dt = kb.sb("dt", [128, G], F32)
        kb.op("act", lambda e: e.activation(out=dt[:], in_=lst[:], func=AF.Exp), r=[lst], w=[dt])
        lrdt = kb.sb("lrdt", [128, G], F32)
        kb.op("dve", lambda e: e.tensor_tensor(out=lrdt[:], in0=lre[:], in1=dt[:], op=ALU.mult), r=[lre, dt], w=[lrdt])
        f0 = kb.sb("f0", [128, G], F32)
        ti = kb.sb("ti", [128, G], I32)
        tf = kb.sb("tf", [128, G], F32)

        def frac_(t_f, t_i, t_tmp, eng="dve"):
            kb.op(eng, lambda e: e.tensor_copy(t_i, t_f), r=[], w=[])
            kb.op(eng, lambda e: e.tensor_copy(t_tmp, t_i), r=[], w=[])
            kb.op(eng, lambda e: e.tensor_tensor(out=t_f, in0=t_f, in1=t_tmp, op=ALU.subtract), r=[], w=[])

        kb.op("dve", lambda e: e.tensor_tensor(out=f0[:], in0=lim[:], in1=dt[:], op=ALU.mult), r=[lim, dt], w=[f0])
        kb.op("dve", lambda e: e.tensor_scalar(f0[:], f0[:], 1.0 / TWO_PI, None, op0=ALU.mult), r=[f0], w=[f0])
        def frac_tiles(F, I_, T, eng="dve"):
            kb.op(eng, lambda e: e.tensor_copy(I_[:], F[:]), r=[F], w=[I_])
            kb.op(eng, lambda e: e.tensor_copy(T[:], I_[:]), r=[I_], w=[T])
            kb.op(eng, lambda e: e.tensor_tensor(out=F[:], in0=F[:], in1=T[:], op=ALU.subtract), r=[F, T], w=[F])

        frac_tiles(f0, ti, tf)
        are = kb.sb("are", [128, G], F32)
        aim = kb.sb("aim", [128, G], F32)
        mag = kb.sb("mag", [128, G], F32)
        fc_ = kb.sb("fcq", [128, G], F32)
        kb.op("act", lambda e: e.activation(out=mag[:], in_=lrdt[:], func=AF.Exp), r=[lrdt], w=[mag])
        kb.op("act", lambda e: e.activation(out=aim[:], in_=f0[:], func=AF.Sin, scale=TWO_PI), r=[f0], w=[aim])
        kb.op("dve", lambda e: e.tensor_scalar(fc_[:], f0[:], 0.25, None, op0=ALU.add), r=[f0], w=[fc_])
        frac_tiles(fc_, ti, tf)
        kb.op("act", lambda e: e.activation(out=are[:], in_=fc_[:], func=AF.Sin, scale=TWO_PI), r=[fc_], w=[are])
        kb.op("dve", lambda e: e.tensor_tensor(out=are[:], in0=are[:], in1=mag[:], op=ALU.mult), r=[are, mag], w=[are])
        kb.op("dve", lambda e: e.tensor_tensor(out=aim[:], in0=aim[:], in1=mag[:], op=ALU.mult), r=[aim, mag], w=[aim])
        kre = kb.sb("kre", [128, G], F32)
        kim = kb.sb("kim", [128, G], F32)
        den = kb.sb("den", [128, G], F32)
        t1 = kb.sb("t1g", [128, G], F32)
        nr = kb.sb("nr", [128, G], F32)
        kb.op("dve", lambda e: e.tensor_scalar(nr[:], are[:], -1.0, None, op0=ALU.add), r=[are], w=[nr])
        kb.op("dve", lambda e: e.tensor_tensor(out=den[:], in0=lre[:], in1=lre[:], op=ALU.mult), r=[lre], w=[den])
        kb.op("dve", lambda e: e.tensor_tensor(out=t1[:], in0=lim[:], in1=lim[:], op=ALU.mult), r=[lim], w=[t1])
        kb.op("dve", lambda e: e.tensor_tensor(out=den[:], in0=den[:], in1=t1[:], op=ALU.add), r=[den, t1], w=[den])
        kb.op("dve", lambda e: e.reciprocal(den[:], den[:]), r=[den], w=[den])
        kb.op("dve", lambda e: e.tensor_tensor(out=kre[:], in0=nr[:], in1=lre[:], op=ALU.mult), r=[nr, lre], w=[kre])
        kb.op("dve", lambda e: e.tensor_tensor(out=t1[:], in0=aim[:], in1=lim[:], op=ALU.mult), r=[aim, lim], w=[t1])
        kb.op("dve", lambda e: e.tensor_tensor(out=kre[:], in0=kre[:], in1=t1[:], op=ALU.add), r=[kre, t1], w=[kre])
        kb.op("dve", lambda e: e.tensor_tensor(out=kre[:], in0=kre[:], in1=den[:], op=ALU.mult), r=[kre, den], w=[kre])
        kb.op("dve", lambda e: e.tensor_tensor(out=kim[:], in0=aim[:], in1=lre[:], op=ALU.mult), r=[aim, lre], w=[kim])
        kb.op("dve", lambda e: e.tensor_tensor(out=t1[:], in0=nr[:], in1=lim[:], op=ALU.mult), r=[nr, lim], w=[t1])
        kb.op("dve", lambda e: e.tensor_tensor(out=kim[:], in0=kim[:], in1=t1[:], op=ALU.subtract), r=[kim, t1], w=[kim])
        kb.op("dve", lambda e: e.tensor_tensor(out=kim[:], in0=kim[:], in1=den[:], op=ALU.mult), r=[kim, den], w=[kim])
        R8 = kb.sb("R8", [128, G], F32)
        kb.op("act", lambda e: e.activation(out=R8[:], in_=lrdt[:], func=AF.Exp, scale=8.0), r=[lrdt], w=[R8])
        f8 = kb.sb("f8", [128, G], F32)
        kb.op("dve", lambda e: e.tensor_scalar(f8[:], f0[:], 8.0, None, op0=ALU.mult), r=[f0], w=[f8])
        frac_tiles(f8, ti, tf)
        kb.op("dve", lambda e: e.tensor_scalar(f8[:], f8[:], nsign[:, 0:1], None, op0=ALU.mult), r=[f8, nsign], w=[f8])

        GC = 8
        bre = kb.sb("bre", [128, GC, 16], F32)
        bim = kb.sb("bim", [128, GC, 16], F32)
        cre = kb.sb("cre", [128, GC, 16], F32)
        cim = kb.sb("cim", [128, GC, 16], F32)
        bbre = kb.sb("bbre", [128, GC, 16], F32)
        bbim = kb.sb("bbim", [128, GC, 16], F32)
        tb16 = kb.sb("tb16", [128, GC, 16], F32)
        marg = kb.sb("marg", [128, GC, 32], F32)
        turn = kb.sb("turn", [128, GC, 32], F32)
        turc = kb.sb("turc", [128, GC, 32], F32)
        tui = kb.sb("tui", [128, GC, 32], I32)
        tuf = kb.sb("tuf", [128, GC, 32], F32)
        Ere = kb.sb("Ere", [128, GC, 4, 8], F32)
        Eim = kb.sb("Eim", [128, GC, 4, 8], F32)
        Pre = kb.sb("Pre", [128, GC, 8, 16], F32)
        Pim = kb.sb("Pim", [128, GC, 8, 16], F32)
        Qre = kb.sb("Qre", [128, GC, 8, 16], F32)
        Qim = kb.sb("Qim", [128, GC, 8, 16], F32)
        Lre = kb.sb("Lre", [128, GC, 8, 16], F32)
        Lim = kb.sb("Lim", [128, GC, 8, 16], F32)
        Xre = kb.sb("QXre", [128, GC, 8, 16], F32)
        Xim = kb.sb("QXim", [128, GC, 8, 16], F32)
        QXreb = kb.sb("QXreb", [128, 2, GC, 128], BF16)
        QXimb = kb.sb("QXimb", [128, 2, GC, 128], BF16)
        nisb = kb.sb("nisb", [128, 1], F32)
        kb.op("dve", lambda e: e.tensor_scalar(nisb[:], isb[:], -1.0, 1.0, op0=ALU.mult, op1=ALU.add), r=[isb], w=[nisb])
        ta4 = kb.sb("ta4", [128, GC, 8, 16], F32)
        tb4 = kb.sb("tb4", [128, GC, 8, 16], F32)
        HN = kb.sb("HN", [128, NNT, 8, 128], BF16)
        YN = kb.sb("YN", [128, NNT, 8, 128], BF16)
        HG = kb.sb("HG", [128, NNT, 8, 128], BF16)
        Ug = [kb.sb(f"Ug{i}", [128, NB], BF16) for i in range(2)]
        W0bs = [kb.sb(f"W0b{i}", [128, 128], BF16) for i in range(2)]
        tW = kb.sb("tW", [128, 128], F32)
        LTres = [kb.sb(f"LTre{i}", [128, 128], BF16) for i in range(2)]
        LTims = [kb.sb(f"LTim{i}", [128, 128], BF16) for i in range(2)]
        cns = [kb.sb(f"cn{i}", [128, NB], F32) for i in range(2)]
        sns = [kb.sb(f"sn{i}", [128, NB], F32) for i in range(2)]
        RKs = [kb.sb(f"RK{i}", [128, NB], F32) for i in range(2)]
        tnf = kb.sb("tnf", [128, NB], F32)
        frt = kb.sb("frt", [128, NB], F32)
        abt = tnf
        wr_ = kb.sb("wr5", [128, NB], F32)
        wi_ = kb.sb("wi5", [128, NB], F32)
        zr = kb.sb("zr", [128, NB], F32)
        zi = kb.sb("zi", [128, NB], F32)
        ta = kb.sb("ta5", [128, NB], F32)
        tb2 = kb.sb("tb5", [128, NB], F32)
        XPr = kb.sb("XPr", [128, NB + 2], BF16)
        XPi = kb.sb("XPi", [128, NB + 2], BF16)
        kb.op("pool", lambda e: e.memset(XPr[:], 0.0), w=[XPr])
        kb.op("pool", lambda e: e.memset(XPi[:], 0.0), w=[XPi])
        ysk = ta
        yg = kb.sb("yg", [128, NB], BF16)
        halfpi = kb.sb("halfpi", [128, 1], F32)
        kb.op("pool", lambda e: e.memset(halfpi[:], float(np.pi / 2)), w=[halfpi])
        psS = [kb.ps(f"psS{i}", [128, CW]) for i in range(2)]
        psY = [kb.ps(f"psY{i}", [128, CW]) for i in range(NH)] if NH <= 2 else None
        psU = kb.ps("psU", [128, 512])
        psM = kb.ps("psM", [128, 256])
        psM2 = kb.ps("psM2", [128, 256])
        psB = kb.ps("psB", [128, 1024], BF16)

        def bc3(t, gsl, n):
            return t[:, gsl].unsqueeze(2).broadcast_to([128, GC, n])

        def cmul_outer(Er, Ei, Br, Bi, outr, outi, neg_im):
            def eb(E_ap):
                return E_ap.unsqueeze(3).broadcast_to([128, GC, 8, 16])

            def bb(B):
                return B[:].unsqueeze(2).broadcast_to([128, GC, 8, 16])
            kb.op("dve", lambda e: e.tensor_tensor(out=ta4[:], in0=eb(Er), in1=bb(Br), op=ALU.mult), r=[Ere, Eim, Br], w=[ta4])
            kb.op("dve", lambda e: e.tensor_tensor(out=tb4[:], in0=eb(Ei), in1=bb(Bi), op=ALU.mult), r=[Ere, Eim, Bi], w=[tb4])
            kb.op("dve", lambda e: e.tensor_tensor(out=outr[:], in0=ta4[:], in1=tb4[:], op=ALU.subtract), r=[ta4, tb4], w=[outr])
            kb.op("dve", lambda e: e.tensor_tensor(out=ta4[:], in0=eb(Er), in1=bb(Bi), op=ALU.mult), r=[Ere, Eim, Bi, outr], w=[ta4])
            kb.op("dve", lambda e: e.tensor_tensor(out=tb4[:], in0=eb(Ei), in1=bb(Br), op=ALU.mult), r=[Ere, Eim, Br, outr], w=[tb4])
            if neg_im:
                kb.op("dve", lambda e: e.tensor_tensor(out=outi[:], in0=ta4[:], in1=tb4[:], op=ALU.add), r=[ta4, tb4], w=[outi])
                kb.op("dve", lambda e: e.tensor_scalar(outi[:], outi[:], -1.0, None, op0=ALU.mult), r=[outi], w=[outi])
            else:
                kb.op("dve", lambda e: e.tensor_tensor(out=outi[:], in0=ta4[:], in1=tb4[:], op=ALU.add), r=[ta4, tb4], w=[outi])

        for fc in range(FC):
            gsl = slice(fc * GC, (fc + 1) * GC)
            kb.dma("sp", bre[:], ssm_bre.ap()[:, gsl, :], w=[bre])
            kb.dma("sp", bim[:], ssm_bim.ap()[:, gsl, :], w=[bim])
            kb.dma("sp", cre[:], ssm_cre.ap()[:, gsl, :], w=[cre])
            kb.dma("sp", cim[:], ssm_cim.ap()[:, gsl, :], w=[cim])
            kb.op("dve", lambda e: e.tensor_tensor(out=bbre[:], in0=bre[:], in1=bc3(kre, gsl, 16), op=ALU.mult), r=[bre, kre], w=[bbre])
            kb.op("dve", lambda e: e.tensor_tensor(out=tb16[:], in0=bim[:], in1=bc3(kim, gsl, 16), op=ALU.mult), r=[bim, kim], w=[tb16])
            kb.op("dve", lambda e: e.tensor_tensor(out=bbre[:], in0=bbre[:], in1=tb16[:], op=ALU.subtract), r=[bbre, tb16], w=[bbre])
            kb.op("dve", lambda e: e.tensor_tensor(out=bbim[:], in0=bim[:], in1=bc3(kre, gsl, 16), op=ALU.mult), r=[bim, kre], w=[bbim])
            kb.op("dve", lambda e: e.tensor_tensor(out=tb16[:], in0=bre[:], in1=bc3(kim, gsl, 16), op=ALU.mult), r=[bre, kim, bbre], w=[tb16])
            kb.op("dve", lambda e: e.tensor_tensor(out=bbim[:], in0=bbim[:], in1=tb16[:], op=ALU.add), r=[bbim, tb16], w=[bbim])
            exb = EX[:].rearrange("p a b -> p (a b)").unsqueeze(1).broadcast_to([128, GC, 32])
            kb.op("dve", lambda e: e.tensor_tensor(out=marg[:], in0=bc3(lrdt, gsl, 32), in1=exb, op=ALU.mult), r=[lrdt, EX], w=[marg])
            kb.op("act", lambda e: e.activation(out=marg[:], in_=marg[:], func=AF.Exp), r=[marg], w=[marg])
            kb.op("dve", lambda e: e.tensor_tensor(out=turn[:], in0=bc3(f0, gsl, 32), in1=exb, op=ALU.mult), r=[f0, EX], w=[turn])
            frac_tiles(turn, tui, tuf)
            kb.op("dve", lambda e: e.tensor_scalar(turc[:], turn[:], 0.25, None, op0=ALU.add), r=[turn], w=[turc])
            frac_tiles(turc, tui, tuf)
            Ef_re = Ere[:].rearrange("p g a b -> p g (a b)")
            Ef_im = Eim[:].rearrange("p g a b -> p g (a b)")
            kb.op("act", lambda e: e.activation(out=Ef_im, in_=turn[:], func=AF.Sin, scale=TWO_PI), r=[turn], w=[Eim])
            kb.op("act", lambda e: e.activation(out=Ef_re, in_=turc[:], func=AF.Sin, scale=TWO_PI), r=[turc], w=[Ere])
            kb.op("dve", lambda e: e.tensor_tensor(out=Ef_im, in0=Ef_im, in1=marg[:], op=ALU.mult), r=[Eim, marg], w=[Eim])
            kb.op("dve", lambda e: e.tensor_tensor(out=Ef_re, in0=Ef_re, in1=marg[:], op=ALU.mult), r=[Ere, marg], w=[Ere])
            cmul_outer(Ere[:, :, 0, :], Eim[:, :, 0, :], bbre, bbim, Pre, Pim, False)
            cmul_outer(Ere[:, :, 1, :], Eim[:, :, 1, :], cre, cim, Qre, Qim, True)
            cmul_outer(Ere[:, :, 2, :], Eim[:, :, 2, :], bbre, bbim, Lre, Lim, False)
            cmul_outer(Ere[:, :, 3, :], Eim[:, :, 3, :], cre, cim, Xre, Xim, True)
            for d_, sc_ in ((0, nisb), (1, isb)):
                kb.op("act", lambda e, d_=d_, sc_=sc_: e.activation(out=QXreb[:, d_], in_=Xre[:].rearrange("p g a b -> p g (a b)"),
                                                                  func=AF.Copy, scale=sc_[:, 0:1]), r=[Xre, sc_], w=[QXreb])
                kb.op("act", lambda e, d_=d_, sc_=sc_: e.activation(out=QXimb[:, d_], in_=Xim[:].rearrange("p g a b -> p g (a b)"),
                                                                  func=AF.Copy, scale=sc_[:, 0:1]), r=[Xim, sc_], w=[QXimb])
            for nt in range(NNT):
                src = HS.ap()[nt * 1024:(nt + 1) * 1024, fc * 128:(fc + 1) * 128].rearrange("(n i) c -> n i c", i=8)
                kb.dma("sp", HN[:, nt, :, :], src, w=[HN])
            for nt in range(NNT):
                kb.op("act", lambda e, nt=nt: e.activation(out=HG[:, nt, :, :].rearrange("p g (i c) -> p g i c", c=16),
                                                           in_=HN[:, nt, :, :].rearrange("p i (g c) -> p g i c", c=16), func=AF.Copy),
                      r=[HN], w=[HG])
            Pr = Pre[:].rearrange("p g a b -> p g (a b)")
            Pi = Pim[:].rearrange("p g a b -> p g (a b)")
            Qr = Qre[:].rearrange("p g a b -> p g (a b)")
            Qi = Qim[:].rearrange("p g a b -> p g (a b)")
            Lr = Lre[:].rearrange("p g a b -> p g (a b)")
            Li = Lim[:].rearrange("p g a b -> p g (a b)")

            def prepA(g):
                gg = fc * GC + g
                U = Ug[g % 2]
                LTre, LTim = LTres[g % 2], LTims[g % 2]
                kb.op("dve", lambda e: e.tensor_scalar(tni[:], nrow[:], f8[:, gg:gg + 1], None, op0=ALU.mult), r=[nrow, f8], w=[tni])
                for nb in range(0, NNT, 4):
                    k4 = min(4, NNT - nb)
                    acc_group(psU, k4, lambda e, j, nb=nb: e.matmul(
                        psU[:, j * 128:(j + 1) * 128], lhsT=HG[:, nb + j, g, :], rhs=ident_b[:],
                        start=True, stop=True), r=[HG, ident_b])
                    kb.op("act", lambda e, nb=nb, k4=k4: e.activation(out=U[:, nb * 128:(nb + k4) * 128], in_=psU[:, 0:k4 * 128],
                                                                     func=AF.Copy), r=[psU], w=[U])
                for d_ in range(2):
                    rs = slice(64 * d_, 64 * d_ + 64)
                    cs_ = slice(128 * d_, 128 * d_ + 128)
                    acc_group(psM, 2, lambda e, j, rs=rs, cs_=cs_: e.matmul(
                        psM[:, cs_], lhsT=(Pr if j == 0 else Pi)[rs, g, :], rhs=(Qr if j == 0 else Qi)[rs, g, :],
                        start=(j == 0), stop=(j == 1)), r=[Pre, Pim, Qre, Qim])
                acc_group(psM2, 2, lambda e, j: e.transpose(out=psM2[:, j * 128:(j + 1) * 128],
                                                            in_=(Lr if j == 0 else Li)[:, g, :], identity=ident_f[:]),
                          r=[Lre, Lim, ident_f])
                kb.op("act", lambda e: e.activation(out=LTre[:], in_=psM2[:, 0:128], func=AF.Copy), r=[psM2], w=[LTre])
                kb.op("act", lambda e: e.activation(out=LTim[:], in_=psM2[:, 128:256], func=AF.Copy), r=[psM2], w=[LTim])

            def prepB(g):
                gg = fc * GC + g
                W0b = W0bs[g % 2]
                cn, sn, RK = cns[g % 2], sns[g % 2], RKs[g % 2]
                kb.op("dve", lambda e: e.tensor_copy(tnf[:], tni[:]), r=[tni], w=[tnf])
                kb.op("dve", lambda e: e.scalar_tensor_tensor(out=frt[:], in0=nrow[:], scalar=f8[:, gg:gg + 1], in1=tnf[:],
                                                             op0=ALU.mult, op1=ALU.subtract), r=[nrow, f8, tnf], w=[frt])
                kb.op("dve", lambda e: e.tensor_tensor(out=tW[:], in0=psM[:, 0:128], in1=MK[:, 0, :], op=ALU.mult), r=[psM, MK], w=[tW])
                kb.op("dve", lambda e: e.tensor_tensor(out=W0b[:], in0=psM[:, 128:256], in1=MK[:, 1, :], op=ALU.mult), r=[psM, MK], w=[W0b])
                kb.op("dve", lambda e: e.tensor_tensor(out=W0b[:], in0=W0b[:], in1=tW[:], op=ALU.add), r=[W0b, tW], w=[W0b])
                kb.op("act", lambda e: e.activation(out=sn[:], in_=frt[:], func=AF.Sin, scale=TWO_PI), r=[frt], w=[sn])
                kb.op("act", lambda e: e.activation(out=abt[:], in_=frt[:], func=AF.Abs), r=[frt], w=[abt])
                kb.op("act", lambda e: e.activation(out=cn[:], in_=abt[:], func=AF.Sin, scale=-TWO_PI, bias=halfpi[:, 0:1]),
                      r=[abt, halfpi], w=[cn])
                kb.op("act", lambda e: e.activation(out=RK[:], in_=KEEP[:], func=AF.Copy, scale=R8[:, gg:gg + 1]), r=[KEEP, R8], w=[RK])

            def main(g, mid_hook):
                gg = fc * GC + g
                U = Ug[g % 2]
                W0b, LTre, LTim = W0bs[g % 2], LTres[g % 2], LTims[g % 2]
                cn, sn, RK = cns[g % 2], sns[g % 2], RKs[g % 2]
                for h in range(NH):
                    cs_ = slice(h * CW, (h + 1) * CW)
                    kb.op("pe", lambda e, cs_=cs_: e.matmul(psS[0][:], lhsT=LTre[:], rhs=U[:, cs_], start=True, stop=True),
                          r=[LTre, U], w=[psS[0]])
                    kb.op("pe", lambda e, cs_=cs_: e.matmul(psS[1][:], lhsT=LTim[:], rhs=U[:, cs_], start=True, stop=True),
                          r=[LTim, U], w=[psS[1]])
                    kb.op("dve", lambda e, cs_=cs_: e.tensor_tensor(out=wr_[:, cs_], in0=psS[0][:], in1=cn[:, cs_], op=ALU.mult), r=[psS[0], cn], w=[wr_])
                    kb.op("dve", lambda e, cs_=cs_: e.tensor_tensor(out=ta[:, cs_], in0=psS[1][:], in1=sn[:, cs_], op=ALU.mult), r=[psS[1], sn], w=[ta])
                    kb.op("dve", lambda e, cs_=cs_: e.tensor_tensor(out=wi_[:, cs_], in0=psS[1][:], in1=cn[:, cs_], op=ALU.mult), r=[psS[1], cn], w=[wi_])
                    kb.op("dve", lambda e, cs_=cs_: e.tensor_tensor(out=tb2[:, cs_], in0=psS[0][:], in1=sn[:, cs_], op=ALU.mult), r=[psS[0], sn], w=[tb2])
                kb.op("pool", lambda e: e.tensor_tensor(out=wr_[:], in0=wr_[:], in1=ta[:], op=ALU.add), r=[wr_, ta], w=[wr_])
                kb.op("dve", lambda e: e.tensor_tensor(out=wi_[:], in0=wi_[:], in1=tb2[:], op=ALU.subtract), r=[wi_, tb2], w=[wi_])
                mid_hook()
                for (z_, w_) in ((zr, wr_), (zi, wi_)):
                    kb.op("dve", lambda e, z_=z_, w_=w_: e.tensor_tensor_scan(out=z_[0:64, :], data0=RK[0:64, :], data1=w_[0:64, :],
                                                                          initial=0.0, op0=ALU.mult, op1=ALU.add), r=[RK, w_], w=[z_])
                    kb.op("dve", lambda e, z_=z_, w_=w_: e.tensor_tensor_scan(out=z_[64:128, ::-1], data0=RK[64:128, ::-1], data1=w_[64:128, ::-1],
                                                                          initial=0.0, op0=ALU.mult, op1=ALU.add), r=[RK, w_], w=[z_])
                kb.op("dve", lambda e: e.tensor_tensor(out=ta[:], in0=zr[:], in1=cn[:], op=ALU.mult), r=[zr, cn], w=[ta])
                kb.op("dve", lambda e: e.tensor_tensor(out=tb2[:], in0=zi[:], in1=sn[:], op=ALU.mult), r=[zi, sn], w=[tb2])
                kb.op("dve", lambda e: e.tensor_tensor(out=XPr[:, 1:NB + 1], in0=ta[:], in1=tb2[:], op=ALU.subtract), r=[ta, tb2], w=[XPr])
                kb.op("dve", lambda e: e.tensor_tensor(out=ta[:], in0=zr[:], in1=sn[:], op=ALU.mult), r=[zr, sn, XPr], w=[ta])
                kb.op("dve", lambda e: e.tensor_tensor(out=tb2[:], in0=zi[:], in1=cn[:], op=ALU.mult), r=[zi, cn, XPr], w=[tb2])
                kb.op("dve", lambda e: e.tensor_tensor(out=XPi[:, 1:NB + 1], in0=ta[:], in1=tb2[:], op=ALU.add), r=[ta, tb2], w=[XPi])
                UB = UNIT // 8
                for XP in (XPr, XPi):
                    kb.op("dve", lambda e, XP=XP: e.tensor_tensor(out=XP[0:64, UB:3 * UB + 1:UB], in0=XP[0:64, UB:3 * UB + 1:UB],
                                                                 in1=mask_t[0:64, 1:4], op=ALU.mult), r=[XP, mask_t], w=[XP])
                    kb.op("dve", lambda e, XP=XP: e.tensor_tensor(out=XP[64:128, UB + 1:3 * UB + 2:UB], in0=XP[64:128, UB + 1:3 * UB + 2:UB],
                                                                 in1=mask_t[64:128, 1:4], op=ALU.mult), r=[XP, mask_t], w=[XP])
                for h in range(NH):
                    c0 = h * CW
                    cs_ = slice(c0, c0 + CW)
                    pY = psY[h]
                    ops = [(W0b[:], U[:, cs_]),
                           (QXreb[:, 0, g, :], XPr[:, c0:c0 + CW]), (QXreb[:, 1, g, :], XPr[:, c0 + 2:c0 + CW + 2]),
                           (QXimb[:, 0, g, :], XPi[:, c0:c0 + CW]), (QXimb[:, 1, g, :], XPi[:, c0 + 2:c0 + CW + 2])]
                    acc_group(pY, 5, lambda e, j, pY=pY, ops=ops: e.matmul(pY[:], lhsT=ops[j][0], rhs=ops[j][1],
                                                                          start=(j == 0), stop=(j == 4)),
                              r=[W0b, QXreb, QXimb, U, XPr, XPi])
                    kb.op("dve", lambda e, cs_=cs_, pY=pY: e.scalar_tensor_tensor(
                        out=ysk[:, cs_], in0=U[:, cs_], scalar=dcol[:, gg:gg + 1], in1=pY[:], op0=ALU.mult, op1=ALU.add),
                        r=[U, dcol, pY], w=[ysk])
                    kb.op("act", lambda e, cs_=cs_: e.activation(out=yg[:, cs_], in_=ysk[:, cs_], func=AF.Gelu_apprx_tanh), r=[ysk], w=[yg])
                for nb in range(0, NNT, 8):
                    k8 = min(8, NNT - nb)
                    acc_group(psB, k8, lambda e, j, nb=nb: e.transpose(out=psB[:, j * 128:(j + 1) * 128],
                                                                   in_=yg[:, (nb + j) * 128:(nb + j + 1) * 128], identity=ident_b[:]),
                              r=[yg, ident_b])
                    kb.op("act", lambda e, nb=nb, k8=k8: e.activation(
                        out=YN[:, nb:nb + k8, :, g * 16:(g + 1) * 16],
                        in_=psB[:, 0:k8 * 128].rearrange("p (a i c) -> p a i c", i=8, c=16), func=AF.Copy), r=[psB], w=[YN])

            prepA(0)
            prepB(0)
            for g in range(GC):
                if g + 1 < GC:
                    prepA(g + 1)
                    main(g, lambda g=g: prepB(g + 1))
                else:
                    main(g, lambda: None)
            for nt in range(NNT):
                dst = YS.ap()[nt * 1024:(nt + 1) * 1024, fc * 128:(fc + 1) * 128].rearrange("(n i) c -> n i c", i=8)
                kb.dma("sp", dst, YN[:, nt, :, :], r=[YN])
        kb.end()

    def stage_glu_out(li, x_src, x_dst):
        kb.begin()
        CGW = min(512, D)
        was = [kb.sb(f"wga{i}", [128, FC, CGW], BF16) for i in range(2)]
        wgs = [kb.sb(f"wgg{i}", [128, FC, CGW], BF16) for i in range(2)]
        hts = [kb.sb(f"htg{i}", [128, FC, 512], BF16) for i in range(2)]
        barow = kb.sb("barow", [128, D], F32)
        bgrow = kb.sb("bgrow", [128, D], F32)
        row_bcast(barow, ssm_b_glu.ap()[:, 0:D])
        row_bcast(bgrow, ssm_b_glu.ap()[:, D:2 * D])
        G_t = kb.sb("G1s", [128, D], F32)
        psa = [kb.ps(f"psga{i}", [128, CGW]) for i in range(2)]
        psg = [kb.ps(f"psgg{i}", [128, CGW]) for i in range(2)]
        sgs = [kb.sb(f"sgg{i}", [128, CGW], F32) for i in range(2)]
        ots = [kb.sb(f"otg{i}", [128, CGW], F32) for i in range(2)]
        xts = [kb.sb(f"xtg{i}", [128, CGW], F32) for i in range(2)]
        wv = ssm_w_glu.ap().rearrange("(kc p) n -> p kc n", p=128)
        it = 0
        def glu_w(cg_):
            kb.dma("gq", was[cg_ % 2][:], wv[:, :, cg_ * CGW:(cg_ + 1) * CGW], w=[was[cg_ % 2]])
            kb.dma("gq", wgs[cg_ % 2][:], wv[:, :, D + cg_ * CGW:D + (cg_ + 1) * CGW], w=[wgs[cg_ % 2]])

        glu_w(0)
        for cg in range(D // CGW):
            cs = slice(cg * CGW, (cg + 1) * CGW)
            wa, wg = was[cg % 2], wgs[cg % 2]
            if cg + 1 < D // CGW:
                glu_w(cg + 1)
            for tb in range(NTB):
                t0 = tb * 512
                if t0 % UNIT == 0:
                    row_bcast(G_t, MOD.ap()[li, t0 // UNIT:t0 // UNIT + 1, 2 * D:3 * D])
                ht = hts[tb % 2]
                kb.dma("sp", ht[:], HT.ap()[:, :, t0:t0 + 512].rearrange("fc p t -> p fc t"), w=[ht])
                for ts in range(4):
                    r0 = t0 + ts * 128
                    pa, pg, sg, ot, xt = psa[it % 2], psg[it % 2], sgs[it % 2], ots[it % 2], xts[it % 2]
                    it += 1
                    kb.dma("sp", xt[:], x_src.ap()[r0:r0 + 128, cs], w=[xt])
                    acc_group(pa, FC, lambda e, kc, pa=pa, wa=wa, ht=ht, ts=ts: e.matmul(
                        pa[:], lhsT=ht[:, kc, ts * 128:(ts + 1) * 128], rhs=wa[:, kc, :], start=(kc == 0), stop=(kc == FC - 1)), r=[ht, wa])
                    acc_group(pg, FC, lambda e, kc, pg=pg, wg=wg, ht=ht, ts=ts: e.matmul(
                        pg[:], lhsT=ht[:, kc, ts * 128:(ts + 1) * 128], rhs=wg[:, kc, :], start=(kc == 0), stop=(kc == FC - 1)), r=[ht, wg])
                    kb.op("dve", lambda e, pg=pg, sg=sg: e.tensor_tensor(out=sg[:], in0=pg[:], in1=bgrow[:, cs], op=ALU.add), r=[pg, bgrow], w=[sg])
                    kb.op("act", lambda e, sg=sg: e.activation(out=sg[:], in_=sg[:], func=AF.Sigmoid), r=[sg], w=[sg])
                    kb.op("dve", lambda e, pa=pa, ot=ot: e.tensor_tensor(out=ot[:], in0=pa[:], in1=barow[:, cs], op=ALU.add), r=[pa, barow], w=[ot])
                    kb.op("pool", lambda e, ot=ot, sg=sg: e.tensor_tensor(out=ot[:], in0=ot[:], in1=sg[:], op=ALU.mult), r=[ot, sg], w=[ot])
                    kb.op("dve", lambda e, ot=ot: e.tensor_tensor(out=ot[:], in0=ot[:], in1=G_t[:, cs], op=ALU.mult), r=[ot, G_t], w=[ot])
                    kb.op("dve", lambda e, ot=ot, xt=xt: e.tensor_tensor(out=ot[:], in0=ot[:], in1=xt[:], op=ALU.add), r=[ot, xt], w=[ot])
                    kb.dma("sp", x_dst.ap()[r0:r0 + 128, cs], ot[:], r=[ot])
        kb.end()

    stage_mod()
    stage_prenorm_T(0, x_in, norm_mix_g, 1, 0)
    stage_conv_in()
    stage_dwconv()
    stage_conv_out(0, x_in, y_out if debug_out == "conv" else X1)
    if debug_out != "conv":
        stage_moe(0, X1)
        stage_combine(0, X1, y_out if debug_out == "moe0" else X2, final=False)
    if debug_out not in ("conv", "moe0"):
        stage_prenorm_rows(1, X2, norm_mix_g, 1, 0, HS)
        stage_s5()
        stage_rows_to_T(YS)
        stage_glu_out(1, X2, y_out if debug_out == "s5" else X3)
        if debug_out != "s5":
            stage_moe(1, X3)
            stage_combine(1, X3, y_out, final=True)

    kb.barrier()
    const_stack.close()
    kb.es.close()
    return nc


def cols(v, FC):
    return np.ascontiguousarray(np.asarray(v, np.float32).reshape(FC, 128).T)


def make_core_inputs(cfg, x, c, unit_rows, masks, p, seq_len):
    FC, D = cfg.FC, cfg.D
    cu = np.asarray(c, np.float32)[unit_rows]
    cT = np.ascontiguousarray(cu.reshape(4, FC, 128).transpose(2, 1, 0))
    m = np.ascontiguousarray(np.broadcast_to(np.asarray(masks, np.float32)[None, :], (128, 4)))
    d = {
        "x": np.ascontiguousarray(x.reshape(-1, D), dtype=np.float32),
        "cT": cT, "masks": m,
        "ada_w": p["ada_w"], "ada_b": p["ada_b"],
        "norm_mix_g": p["norm_mix_g"], "norm_ffn_g": p["norm_ffn_g"],
        "final_norm_g": p["final_norm_g"].reshape(1, D),
        "conv_w_in": p["conv_w_in"][0],
        "conv_b_in_c": cols(p["conv_b_in"][0], 2 * FC),
        "conv_w_dw_c": np.ascontiguousarray(p["conv_w_dw"][0].T.reshape(FC, 128, CONV_W).transpose(1, 0, 2)),
        "conv_b_dw_c": cols(p["conv_b_dw"][0], FC),
        "conv_ln_g_c": cols(p["conv_ln_g"][0], FC),
        "conv_ln_b_c": cols(p["conv_ln_b"][0], FC),
        "conv_w_out": p["conv_w_out"][0],
        "conv_b_out": p["conv_b_out"][0].reshape(1, D),
        "moe_w_router_c": np.ascontiguousarray(p["moe_w_router"].reshape(cfg.depth, FC, 128, cfg.E).transpose(0, 2, 1, 3)),
    }
    G = cfg.G
    def dpg(a):
        return np.ascontiguousarray(np.asarray(a, np.float32).transpose(0, 2, 1).reshape(128, G))
    d["ssm_lre"] = dpg(p["ssm_lambda_re"][0])
    d["ssm_lim"] = dpg(p["ssm_lambda_im"][0])
    d["ssm_ls"] = dpg(np.broadcast_to(np.asarray(p["ssm_log_step"][0])[:, :, None], (2, G, 64)))
    d["ssm_bre"] = np.asarray(p["ssm_b_re"][0]).transpose(0, 2, 1, 3).reshape(128, G, 16)
    d["ssm_bim"] = np.asarray(p["ssm_b_im"][0]).transpose(0, 2, 1, 3).reshape(128, G, 16)
    d["ssm_cre"] = np.asarray(p["ssm_c_re"][0]).transpose(0, 3, 1, 2).reshape(128, G, 16)
    d["ssm_cim"] = np.asarray(p["ssm_c_im"][0]).transpose(0, 3, 1, 2).reshape(128, G, 16)
    dd = np.asarray(p["ssm_d"][0], np.float32).reshape(G, 16)
    d["ssm_dcol"] = np.ascontiguousarray(np.broadcast_to(dd.T[None, :, :], (8, 16, G)).reshape(128, G))
    d["ssm_w_glu"] = p["ssm_w_glu"][0]
    d["ssm_b_glu"] = p["ssm_b_glu"][0].reshape(1, 2 * D)
    NB = cfg.NT // 8
    seqb = seq_len // 8
    keep = np.ones((128, NB), np.float32)
    n = np.arange(NB)
    keep[:64, n % seqb == 0] = 0.0
    keep[64:, n % seqb == seqb - 1] = 0.0
    d["s5_keep"] = keep
    ip = np.arange(128) // 16
    mk = np.zeros((128, 2, 128), np.float32)
    mk[:, 0, :] = (ip[None, :] >= ip[:, None])
    mk[:, 1, :] = (ip[:, None] >= ip[None, :])
    d["s5_masks"] = mk
    EG = min(cfg.E, 8)
    for nm in ("gate", "up", "down"):
        w = p["moe_w_" + nm]
        for l in range(cfg.depth):
            for h in range(cfg.E // EG):
                d[f"moe_w_{nm}_{l}_{h}"] = w[l, h * EG:(h + 1) * EG]
    return {k: np.ascontiguousarray(v, dtype=np.float32) for k, v in d.items()}


def run(cfg, x_prompt, x_sample, c_prompt, c_sample, p, debug_out=None, n_cores=8):
    nc = build_program(cfg, debug_out=debug_out)
    bp, lp = x_prompt.shape[0], x_prompt.shape[1]
    bs, ls = x_sample.shape[0], x_sample.shape[1]

    def unit_info(b, l):
        upb = 4 // b
        rows = [u // upb for u in range(4)]
        masks = [0.0] + [1.0 if (u % upb) != 0 else 0.0 for u in range(1, 4)]
        return rows, masks

    rp, mp = unit_info(bp, lp)
    rs, ms = unit_info(bs, ls)
    inp_p = make_core_inputs(cfg, x_prompt, c_prompt, rp, mp, p, lp)
    inp_s = make_core_inputs(cfg, x_sample, c_sample, rs, ms, p, ls)
    for dct in (inp_p, inp_s):
        for k in list(dct):
            if (debug_out == "conv" and k.startswith("moe_")) or (debug_out in ("conv", "moe0") and (k.startswith("ssm_") or k.startswith("s5_"))):
                del dct[k]
    in_maps = [inp_p if (i % 2 == 0) else inp_s for i in range(n_cores)]
    res = run_bass_kernel_spmd(nc, in_maps, core_ids=list(range(n_cores)))
    yp = np.asarray(res.results[0]["y"], np.float32).reshape(x_prompt.shape)
    ys = np.asarray(res.results[1]["y"], np.float32).reshape(x_sample.shape)
    return yp, ys


def kernel(**inputs):
    cfg = Cfg()
    p = {k: np.asarray(v) for k, v in inputs.items()}
    return run(cfg, p["x_prompt"], p["x_sample"], p["c_prompt"], p["c_sample"], p, n_cores=2)
```

```python
import contextlib
import numpy as np
import concourse.bass as bass
import concourse.mybir as mybir
from concourse.bass_utils import run_bass_kernel_spmd

F32 = mybir.dt.float32
BF16 = mybir.dt.bfloat16
I32 = mybir.dt.int32
AF = mybir.ActivationFunctionType
ALU = mybir.AluOpType
AX = mybir.AxisListType

RMS_EPS = 1e-6
LN_EPS = 1e-5
CONV_W = 31
CONV_PAD = 15


class Cfg:
    def __init__(s, D=2048, NT=8192, E=16, depth=2):
        s.D = D
        s.FC = D // 128
        s.NT = NT
        s.UNIT = NT // 4
        s.E = E
        s.CAP = 2 * NT // E
        s.depth = depth
        s.G = D // 16


class Tile:
    def __init__(s, t):
        s.t = t
        s.w = None
        s.r = []

    def __getitem__(s, k):
        return s.t[k]


class Stream:
    def __init__(s, h, sem=None):
        s.h = h
        s.sem = sem
        s.cnt = 0
        s.seen = {}


class KB:
    NDS = 12
    verbose = False

    def __init__(s, nc):
        s.nc = nc
        s.es = contextlib.ExitStack()
        s.st = {}
        for nm, h in (("pe", nc.tensor), ("act", nc.scalar), ("dve", nc.vector), ("pool", nc.gpsimd), ("sp", nc.sync)):
            sem = s.es.enter_context(nc.semaphore("sem_" + nm))
            s.st[nm] = Stream(h, sem)
        s.dq = {}
        for q, stn in (("sp", "sp"), ("gq", "pool"), ("aq", "act")):
            sems = [s.es.enter_context(nc.semaphore(f"dq_{q}_{i}")) for i in range(s.NDS)]
            s.dq[q] = dict(stream=s.st[stn], sems=sems, cnt=[0] * s.NDS, i=0)
        s.stage = None
        s.uid = 0
        s.pending = []
        s.defer_stores = True

    def begin(s):
        if not hasattr(s, "stk"):
            s.stk = []
        s.stk.append(s.stage)
        s.stage = contextlib.ExitStack()

    def end(s):
        s.barrier()
        if KB.verbose:
            print("stage end:", {k: v.cnt for k, v in s.st.items()}, {q: max(Q["cnt"]) for q, Q in s.dq.items()}, flush=True)
        s.stage.close()
        s.stage = s.stk.pop()

    def sb(s, name, shape, dt):
        s.uid += 1
        return Tile(s.stage.enter_context(s.nc.sbuf_tensor(f"{name}_{s.uid}", list(shape), dt)))

    def ps(s, name, shape, dt=F32):
        s.uid += 1
        return Tile(s.stage.enter_context(s.nc.psum_tensor(f"{name}_{s.uid}", list(shape), dt)))

    def _wait(s, stream, sem, val):
        key = id(sem)
        if stream.seen.get(key, 0) >= val:
            return
        stream.h.wait_ge(sem, val)
        stream.seen[key] = val

    def _deps(s, stream, r, w):
        for b in r:
            if b.w is not None:
                s._wait(stream, *b.w)
        for b in w:
            if b.w is not None:
                s._wait(stream, *b.w)
            for tok in b.r:
                s._wait(stream, *tok)

    def _mark(s, tok, r, w):
        for b in r:
            b.r.append(tok)
            if len(b.r) > 24:
                d = {}
                for sem, v in b.r:
                    d[id(sem)] = (sem, max(v, d.get(id(sem), (sem, 0))[1]))
                b.r = list(d.values())
        for b in w:
            b.w = tok
            b.r = []

    def _flush(s, force=False, wset=None):
        if not s.pending:
            return
        keep = []
        emit_all_before = -1
        for i, p in enumerate(s.pending):
            hit = wset is not None and any(id(t) in wset for t in p["r"])
            if force or p["loads"] >= 1 or hit:
                emit_all_before = i
        pend, s.pending = s.pending, []
        for i, p in enumerate(pend):
            if i <= emit_all_before:
                s._dma_now(p["q"], p["out"], p["in_"], p["r"], (), None, p["kw"])
            else:
                keep.append(p)
        s.pending = keep + s.pending

    def op(s, eng, fn, r=(), w=()):
        if s.pending:
            s._flush(wset={id(t) for t in w})
        stream = s.st[eng]
        s._deps(stream, r, w)
        inst = fn(stream.h)
        stream.cnt += 1
        inst.then_inc(stream.sem, 1)
        tok = (stream.sem, stream.cnt)
        s._mark(tok, r, w)
        return tok

    def dma(s, q, out, in_, r=(), w=(), indirect=None, **kw):
        if q == "sp" and indirect is None and s.defer_stores:
            if not w:
                s.pending.append(dict(q=q, out=out, in_=in_, r=list(r), kw=kw, loads=0))
                return None
            if s.pending:
                s._flush(wset={id(t) for t in w})
            tok = s._dma_now(q, out, in_, r, w, indirect, kw)
            for p in s.pending:
                p["loads"] += 1
            return tok
        if s.pending:
            s._flush(wset={id(t) for t in w})
        return s._dma_now(q, out, in_, r, w, indirect, kw)

    def _dma_now(s, q, out, in_, r, w, indirect, kw):
        Q = s.dq[q]
        stream = Q["stream"]
        j = Q["i"] % s.NDS
        Q["i"] += 1
        sem = Q["sems"][j]
        if Q["cnt"][j] > 0:
            s._wait(stream, sem, Q["cnt"][j])
        s._deps(stream, r, w)
        if indirect is None:
            inst = stream.h.dma_start(out=out, in_=in_, **kw)
        else:
            inst = stream.h.indirect_dma_start(out=out, in_=in_, **indirect, **kw)
        inst.then_inc(sem, 16)
        Q["cnt"][j] += 16
        tok = (sem, Q["cnt"][j])
        s._mark(tok, r, w)
        return tok

    def barrier(s):
        s._flush(force=True)
        toks = []
        for stt in s.st.values():
            if stt.cnt > 0:
                toks.append((stt.sem, stt.cnt))
        for Q in s.dq.values():
            for sem, c in zip(Q["sems"], Q["cnt"]):
                if c > 0:
                    toks.append((sem, c))
        for stt in s.st.values():
            for sem, v in toks:
                if sem is stt.sem:
                    continue
                s._wait(stt, sem, v)


def build_program(cfg, debug_out=None):
    nc = bass.Bass("TRN2", target_bir_lowering=False)
    D, FC, NT, UNIT, E = cfg.D, cfg.FC, cfg.NT, cfg.UNIT, cfg.E
    NTT = NT // 128
    NTB = NT // 512
    D6 = 6 * D

    def inp(name, shape, dt=F32):
        return nc.dram_tensor(name, list(shape), dt, kind="ExternalInput")

    def scr(name, shape, dt):
        return nc.dram_tensor(name, list(shape), dt, kind="Internal")

    x_in = inp("x", [NT, D])
    cT = inp("cT", [128, FC, 4])
    masks = inp("masks", [128, 4])
    ada_w = inp("ada_w", [cfg.depth, D, D6])
    ada_b = inp("ada_b", [cfg.depth, D6])
    norm_mix_g = inp("norm_mix_g", [cfg.depth, D])
    norm_ffn_g = inp("norm_ffn_g", [cfg.depth, D])
    final_g = inp("final_norm_g", [1, D])
    conv_w_in = inp("conv_w_in", [D, 2 * D])
    conv_b_in = inp("conv_b_in_c", [128, 2 * FC])
    conv_w_dw = inp("conv_w_dw_c", [128, FC, CONV_W])
    conv_b_dw = inp("conv_b_dw_c", [128, FC])
    conv_ln_g = inp("conv_ln_g_c", [128, FC])
    conv_ln_b = inp("conv_ln_b_c", [128, FC])
    conv_w_out = inp("conv_w_out", [D, D])
    conv_b_out = inp("conv_b_out", [1, D])
    if debug_out != "conv":
        moe_w_router = inp("moe_w_router_c", [cfg.depth, 128, FC, E])
        EG = min(E, 8)
        moe_w = {nm: [[inp(f"moe_w_{nm}_{l}_{h}", [EG, D, D]) for h in range(E // EG)] for l in range(cfg.depth)]
                 for nm in ("gate", "up", "down")}
    G = cfg.G
    NB = NT // 8
    if debug_out not in ("conv", "moe0"):
        ssm_lre = inp("ssm_lre", [128, G])
        ssm_lim = inp("ssm_lim", [128, G])
        ssm_ls = inp("ssm_ls", [128, G])
        ssm_bre = inp("ssm_bre", [128, G, 16])
        ssm_bim = inp("ssm_bim", [128, G, 16])
        ssm_cre = inp("ssm_cre", [128, G, 16])
        ssm_cim = inp("ssm_cim", [128, G, 16])
        ssm_dcol = inp("ssm_dcol", [128, G])
        ssm_w_glu = inp("ssm_w_glu", [D, 2 * D])
        ssm_b_glu = inp("ssm_b_glu", [1, 2 * D])
        s5_keep = inp("s5_keep", [128, NB])
        s5_masks = inp("s5_masks", [128, 2, 128])
        HS = scr("HS", [NT, D], BF16)
        YS = scr("YS", [NT, D], BF16)
    y_out = nc.dram_tensor("y", [NT, D], F32, kind="ExternalOutput")
    CAP = cfg.CAP
    EXT = 2 + 2 * E
    DX = D + EXT
    OBW = min(512, D)
    NOB = D // OBW
    NST = CAP // 128
    H = scr("H", [NT, DX], BF16)
    XS = [scr(f"XS{e}", [CAP, DX], BF16) for e in range(E)]
    YBF = scr("YBF", [NT, D], F32)
    X2 = scr("X2", [NT, D], F32)
    X3 = scr("X3", [NT, D], F32)

    MOD = scr("MOD", [cfg.depth, 4, D6], F32)
    HT = scr("HT", [FC, 128, NT], BF16)
    UT = scr("UT", [FC, 128, NT], BF16)
    CU = scr("CU", [FC, 128, NT], F32)
    X1 = scr("X1", [NT, D], F32)

    kb = KB(nc)

    kb.begin()
    cst = kb.stage
    ident_f = kb.sb("identf", [128, 128], F32)
    ident_b = kb.sb("identb", [128, 128], BF16)
    ones_f = kb.sb("onesf", [128, 128], F32)
    kb.op("pool", lambda e: e.memset(ones_f[:], 1.0), w=[ones_f])
    iot = kb.sb("iot", [128, 128], I32)
    kb.op("pool", lambda e: e.iota(iot[:], pattern=[[1, 128]], base=0, channel_multiplier=-1), w=[iot])
    iotf = kb.sb("iotf", [128, 128], F32)
    kb.op("dve", lambda e: e.tensor_copy(iotf[:], iot[:]), r=[iot], w=[iotf])
    kb.op("dve", lambda e: e.tensor_scalar(ident_f[:], iotf[:], 0.0, None, op0=ALU.is_equal), r=[iotf], w=[ident_f])
    kb.op("dve", lambda e: e.tensor_copy(ident_b[:], ident_f[:]), r=[ident_f], w=[ident_b])
    mask_t = kb.sb("maskt", [128, 4], F32)
    kb.dma("sp", mask_t[:], masks.ap(), w=[mask_t])
    const_stack = kb.stage

    def row_bcast(tile, dram_row_ap):
        n = dram_row_ap.shape[-1]
        kb.dma("sp", tile[:, 0:n], dram_row_ap.partition_broadcast(128), w=[tile])

    def stage_mod():
        kb.begin()
        ct = kb.sb("ct", [128, FC, 4], F32)
        kb.dma("sp", ct[:], cT.ap(), w=[ct])
        cs_t = kb.sb("cs", [128, FC, 4], BF16)
        kb.op("act", lambda e: e.activation(out=cs_t[:], in_=ct[:], func=AF.Silu), r=[ct], w=[cs_t])
        NTL = D6 // 512
        wts = [kb.sb(f"adaw{i}", [128, FC, 512], BF16) for i in range(2)]
        pss = [kb.ps(f"modps{i}", [4, 512]) for i in range(2)]
        brs = [kb.sb(f"adab{i}", [4, 512], F32) for i in range(2)]
        mrs = [kb.sb(f"mrow{i}", [4, 512], F32) for i in range(2)]
        it = 0
        for li in range(cfg.depth):
            for nt in range(NTL):
                wt = wts[it % 2]
                ps = pss[it % 2]
                brow = brs[it % 2]
                mrow = mrs[it % 2]
                it += 1
                cs = slice(nt * 512, (nt + 1) * 512)
                src = ada_w.ap()[li].rearrange("(kc p) n -> p kc n", p=128)[:, :, cs]
                kb.dma("gq", wt[:], src, w=[wt])
                kb.dma("sp", brow[:], ada_b.ap()[li:li + 1, cs].partition_broadcast(4), w=[brow])
                for kc in range(FC):
                    kb.op("pe", lambda e, kc=kc: e.matmul(ps[:], lhsT=cs_t[:, kc, :], rhs=wt[:, kc, :],
                                                         start=(kc == 0), stop=(kc == FC - 1)),
                          r=[cs_t, wt], w=[ps] if kc == 0 else [], )
                    ps.w = (kb.st["pe"].sem, kb.st["pe"].cnt)
                kb.op("dve", lambda e: e.tensor_tensor(out=mrow[:], in0=ps[:], in1=brow[:], op=ALU.add),
                      r=[ps, brow], w=[mrow])
                kb.dma("sp", MOD.ap()[li, :, cs], mrow[:], r=[mrow])
        kb.end()

    def load_mod_rows(li, u, which_scale, which_shift, gain_ap, A_t, S_t, tmp):
        row_bcast(tmp, MOD.ap()[li, u:u + 1, which_scale * D:(which_scale + 1) * D])
        row_bcast(A_t, gain_ap)
        kb.op("dve", lambda e: e.scalar_tensor_tensor(out=A_t[:], in0=tmp[:], scalar=1.0, in1=A_t[:],
                                                     op0=ALU.add, op1=ALU.mult), r=[tmp, A_t], w=[A_t])
        row_bcast(S_t, MOD.ap()[li, u:u + 1, which_shift * D:(which_shift + 1) * D])

    def make_rms_ws():
        return dict(sq=kb.sb("rms_sq", [128, D], F32), tmp=[kb.sb(f"rms_tmp{i}", [128, D], F32) for i in range(2)],
                    ssum=[kb.sb(f"rms_ss{i}", [128, 1], F32) for i in range(2)],
                    rstd=[kb.sb(f"rms_rs{i}", [128, 3], F32) for i in range(2)], i=0)

    def rms_modulate(xt, A_t, S_t, h_out, ws):
        j = ws["i"] % 2
        ws["i"] += 1
        sq, junk, ssum, rstd = ws["sq"], ws["tmp"][j], ws["ssum"][j], ws["rstd"][j]
        kb.op("act", lambda e: e.activation(out=sq[:], in_=xt[:], func=AF.Square, accum_out=ssum[:, 0:1]),
              r=[xt], w=[sq, ssum])
        kb.op("dve", lambda e: e.tensor_scalar(rstd[:, 0:1], ssum[:, 0:1], 1.0 / D, RMS_EPS, op0=ALU.mult, op1=ALU.add),
              r=[ssum], w=[rstd])
        kb.op("act", lambda e: e.activation(out=rstd[:, 1:2], in_=rstd[:, 0:1], func=AF.Sqrt), r=[rstd], w=[rstd])
        kb.op("dve", lambda e: e.reciprocal(rstd[:, 2:3], rstd[:, 1:2]), r=[rstd], w=[rstd])
        kb.op("dve", lambda e: e.scalar_tensor_tensor(out=junk[:], in0=xt[:], scalar=rstd[:, 2:3], in1=A_t[:],
                                                     op0=ALU.mult, op1=ALU.mult), r=[xt, rstd, A_t], w=[junk])
        kb.op("dve", lambda e: e.tensor_tensor(out=h_out[:], in0=junk[:], in1=S_t[:], op=ALU.add),
              r=[junk, S_t], w=[h_out])

    def transpose_to_HT(h_bf, tt, psT, hT, dst):
        for fb in range(0, FC, 4):
            nb = min(4, FC - fb)
            ps = psT[(fb // 4) % 2]
            for j in range(nb):
                fc = fb + j
                kb.op("pe", lambda e, fc=fc, j=j: e.transpose(out=ps[:, j * 128:(j + 1) * 128],
                                                            in_=h_bf[:, fc * 128:(fc + 1) * 128], identity=ident_b[:]),
                      r=[h_bf, ident_b], w=[ps] if j == 0 else [])
                ps.w = (kb.st["pe"].sem, kb.st["pe"].cnt)
            kb.op("act", lambda e: e.activation(out=hT[:, fb:fb + nb, :],
                                                in_=ps[:, 0:nb * 128].rearrange("p (a b) -> p a b", b=128), func=AF.Copy),
                  r=[ps], w=[hT])
        kb.dma("sp", dst.ap()[:, :, tt * 128:(tt + 1) * 128].rearrange("fc p t -> p fc t"), hT[:], r=[hT])

    def stage_prenorm_T(li, x_src, gain, wsc, wsh):
        kb.begin()
        A_t = kb.sb("A", [128, D], F32)
        S_t = kb.sb("S", [128, D], F32)
        tmp = kb.sb("tmp", [128, D], F32)
        xts = [kb.sb(f"xt{i}", [128, D], F32) for i in range(2)]
        ws = make_rms_ws()
        hbs = [kb.sb(f"hb{i}", [128, D], BF16) for i in range(2)]
        hTs = [kb.sb(f"hT{i}", [128, FC, 128], BF16) for i in range(2)]
        psT = [kb.ps(f"psT{i}", [128, 512], BF16) for i in range(2)]
        for tt in range(NTT):
            u = (tt * 128) // UNIT
            if (tt * 128) % UNIT == 0:
                load_mod_rows(li, u, wsc, wsh, gain.ap()[li:li + 1, :], A_t, S_t, tmp)
            xt = xts[tt % 2]
            kb.dma("sp", xt[:], x_src.ap()[tt * 128:(tt + 1) * 128, :], w=[xt])
            hb = hbs[tt % 2]
            rms_modulate(xt, A_t, S_t, hb, ws)
            transpose_to_HT(hb, tt, psT, hTs[tt % 2], HT)
        kb.end()

    def stage_conv_in():
        kb.begin()
        FGS = min(4, FC)
        bin_t = kb.sb("bin", [128, 2 * FC], F32)
        kb.dma("sp", bin_t[:], conv_b_in.ap(), w=[bin_t])
        was = [kb.sb(f"wa{i}", [128, FC, FGS * 128], BF16) for i in range(2)]
        wgs = [kb.sb(f"wg{i}", [128, FC, FGS * 128], BF16) for i in range(2)]
        hts = [kb.sb(f"ht{i}", [128, FC, 512], BF16) for i in range(2)]
        psa = [kb.ps(f"psa{i}", [128, 512]) for i in range(2)]
        psg = [kb.ps(f"psg{i}", [128, 512]) for i in range(2)]
        sgs = [kb.sb(f"sg{i}", [128, 512], F32) for i in range(2)]
        uts = [kb.sb(f"ut{i}", [128, 512], BF16) for i in range(2)]
        wv = conv_w_in.ap().rearrange("(kc p) n -> p kc n", p=128)
        it = 0
        for fg in range(FC // FGS):
            wa, wg = was[fg % 2], wgs[fg % 2]
            kb.dma("gq", wa[:], wv[:, :, fg * FGS * 128:(fg + 1) * FGS * 128], w=[wa])
            kb.dma("gq", wg[:], wv[:, :, D + fg * FGS * 128:D + (fg + 1) * FGS * 128], w=[wg])
            for tb in range(NTB):
                ht = hts[tb % 2]
                kb.dma("sp", ht[:], HT.ap()[:, :, tb * 512:(tb + 1) * 512].rearrange("fc p t -> p fc t"), w=[ht])
                for j in range(FGS):
                    f = fg * FGS + j
                    pa, pg, sg, ut = psa[it % 2], psg[it % 2], sgs[it % 2], uts[it % 2]
                    it += 1
                    for kc in range(FC):
                        kb.op("pe", lambda e, kc=kc: e.matmul(pa[:], lhsT=wa[:, kc, j * 128:(j + 1) * 128], rhs=ht[:, kc, :],
                                                             start=(kc == 0), stop=(kc == FC - 1)),
                              r=[wa, ht], w=[pa] if kc == 0 else [])
                        pa.w = (kb.st["pe"].sem, kb.st["pe"].cnt)
                    for kc in range(FC):
                        kb.op("pe", lambda e, kc=kc: e.matmul(pg[:], lhsT=wg[:, kc, j * 128:(j + 1) * 128], rhs=ht[:, kc, :],
                                                             start=(kc == 0), stop=(kc == FC - 1)),
                              r=[wg, ht], w=[pg] if kc == 0 else [])
                        pg.w = (kb.st["pe"].sem, kb.st["pe"].cnt)
                    kb.op("act", lambda e: e.activation(out=sg[:], in_=pg[:], func=AF.Sigmoid,
                                                        bias=bin_t[:, FC + f:FC + f + 1]), r=[pg, bin_t], w=[sg])
                    kb.op("dve", lambda e: e.scalar_tensor_tensor(out=ut[:], in0=pa[:], scalar=bin_t[:, f:f + 1], in1=sg[:],
                                                                 op0=ALU.add, op1=ALU.mult), r=[pa, sg, bin_t], w=[ut])
                    kb.dma("sp", UT.ap()[f, :, tb * 512:(tb + 1) * 512], ut[:], r=[ut])
        kb.end()

    def stage_dwconv():
        kb.begin()
        wdw = kb.sb("wdw", [128, FC, CONV_W], F32)
        kb.dma("sp", wdw[:], conv_w_dw.ap(), w=[wdw])
        bdw = kb.sb("bdw", [128, FC], F32)
        kb.dma("sp", bdw[:], conv_b_dw.ap(), w=[bdw])
        dgs = [kb.sb(f"dg{i}", [128, CONV_W, 128], BF16) for i in range(2)]
        uws = [kb.sb(f"uw{i}", [128, 512 + 2 * CONV_PAD], BF16) for i in range(3)]
        pss = [kb.ps(f"cps{i}", [128, 512]) for i in range(2)]
        cus = [kb.sb(f"cu{i}", [128, 512], F32) for i in range(2)]
        it = 0
        for fc in range(FC):
            dg = dgs[fc % 2]
            for k in range(CONV_W):
                eng = "dve" if k % 2 == 0 else "pool"
                kb.op(eng, lambda e, k=k: e.tensor_scalar(dg[:, k, :], ident_f[:], wdw[:, fc, k:k + 1], None, op0=ALU.mult),
                      r=[ident_f, wdw], w=[dg])
            for tb in range(NTB):
                uw = uws[it % 3]
                ps = pss[it % 2]
                cu = cus[it % 2]
                it += 1
                t0 = tb * 512
                lo = max(t0 - CONV_PAD, 0)
                hi_ = min(t0 + 512 + CONV_PAD, NT)
                if t0 == 0:
                    kb.op("pool", lambda e: e.memset(uw[:, 0:CONV_PAD], 0.0), w=[uw])
                if t0 + 512 == NT:
                    kb.op("pool", lambda e: e.memset(uw[:, CONV_PAD + 512:], 0.0), w=[uw])
                kb.dma("sp", uw[:, lo - (t0 - CONV_PAD):hi_ - (t0 - CONV_PAD)], UT.ap()[fc, :, lo:hi_], w=[uw])
                if t0 > 0 and t0 % UNIT == 0:
                    b = t0 // UNIT
                    kb.op("dve", lambda e, b=b: e.tensor_scalar(uw[:, 0:CONV_PAD], uw[:, 0:CONV_PAD], mask_t[:, b:b + 1],
                                                             None, op0=ALU.mult), r=[uw, mask_t], w=[uw])
                if t0 + 512 < NT and (t0 + 512) % UNIT == 0:
                    b = (t0 + 512) // UNIT
                    kb.op("dve", lambda e, b=b: e.tensor_scalar(uw[:, CONV_PAD + 512:], uw[:, CONV_PAD + 512:],
                                                             mask_t[:, b:b + 1], None, op0=ALU.mult),
                          r=[uw, mask_t], w=[uw])
                for k in range(CONV_W):
                    kb.op("pe", lambda e, k=k: e.matmul(ps[:], lhsT=dg[:, k, :], rhs=uw[:, k:k + 512],
                                                       start=(k == 0), stop=(k == CONV_W - 1)),
                          r=[dg, uw], w=[ps] if k == 0 else [])
                    ps.w = (kb.st["pe"].sem, kb.st["pe"].cnt)
                kb.op("act", lambda e: e.activation(out=cu[:], in_=ps[:], func=AF.Identity, bias=bdw[:, fc:fc + 1]),
                      r=[ps, bdw], w=[cu])
                kb.dma("sp", CU.ap()[fc, :, t0:t0 + 512], cu[:], r=[cu])
        kb.end()

    def stage_conv_out(li, x_src, x_dst):
        kb.begin()
        lng = kb.sb("lng", [128, FC], F32)
        lnb = kb.sb("lnb", [128, FC], F32)
        kb.dma("sp", lng[:], conv_ln_g.ap(), w=[lng])
        kb.dma("sp", lnb[:], conv_ln_b.ap(), w=[lnb])
        wout = kb.sb("wout", [128, FC, D], BF16)
        wv = conv_w_out.ap().rearrange("(kc p) n -> p kc n", p=128)
        for kc in range(FC):
            kb.dma("gq", wout[:, kc, :], wv[:, kc, :], w=[wout])
        brow = kb.sb("brow", [128, D], F32)
        row_bcast(brow, conv_b_out.ap())
        G_t = kb.sb("G", [128, D], F32)
        cuts = [kb.sb(f"cut{i}", [128, FC, 512], F32) for i in range(2)]
        sq = kb.sb("sq", [128, 512], F32)
        ps1 = kb.ps("ps1", [128, 512])
        ps2 = kb.ps("ps2", [128, 512])
        mean = kb.sb("mean", [128, 512], F32)
        rstd = kb.sb("rstdln", [128, 512], F32)
        tmp = kb.sb("tmpln", [128, 512], F32)
        vTs = [kb.sb(f"vT{i}", [128, FC, 512], BF16) for i in range(2)]
        pso = [kb.ps(f"pso{i}", [128, 512]) for i in range(2)]
        xts = [kb.sb(f"xo{i}", [128, 512], F32) for i in range(2)]
        ots = [kb.sb(f"ot{i}", [128, 512], F32) for i in range(2)]
        it = 0
        NOB = D // 512 if D >= 512 else 1
        OBW = min(512, D)
        for tb in range(NTB):
            t0 = tb * 512
            if t0 % UNIT == 0:
                row_bcast(G_t, MOD.ap()[li, t0 // UNIT:t0 // UNIT + 1, 2 * D:3 * D])
            cut = cuts[tb % 2]
            vT = vTs[tb % 2]
            kb.dma("sp", cut[:], CU.ap()[:, :, t0:t0 + 512].rearrange("fc p t -> p fc t"), w=[cut])
            for fc in range(FC):
                kb.op("pe", lambda e, fc=fc: e.matmul(ps1[:], lhsT=ones_f[:], rhs=cut[:, fc, :], start=(fc == 0), stop=(fc == FC - 1)),
                      r=[ones_f, cut], w=[ps1] if fc == 0 else [])
                ps1.w = (kb.st["pe"].sem, kb.st["pe"].cnt)
            for fc in range(FC):
                kb.op("act", lambda e, fc=fc: e.activation(out=sq[:], in_=cut[:, fc, :], func=AF.Square), r=[cut], w=[sq])
                kb.op("pe", lambda e, fc=fc: e.matmul(ps2[:], lhsT=ones_f[:], rhs=sq[:], start=(fc == 0), stop=(fc == FC - 1)),
                      r=[ones_f, sq], w=[ps2] if fc == 0 else [])
                ps2.w = (kb.st["pe"].sem, kb.st["pe"].cnt)
            kb.op("dve", lambda e: e.tensor_scalar(mean[:], ps1[:], 1.0 / D, None, op0=ALU.mult), r=[ps1], w=[mean])
            kb.op("dve", lambda e: e.tensor_tensor(out=tmp[:], in0=mean[:], in1=mean[:], op=ALU.mult), r=[mean], w=[tmp])
            kb.op("dve", lambda e: e.scalar_tensor_tensor(out=tmp[:], in0=ps2[:], scalar=1.0 / D, in1=tmp[:],
                                                         op0=ALU.mult, op1=ALU.subtract), r=[ps2, tmp], w=[tmp])
            kb.op("dve", lambda e: e.tensor_scalar(tmp[:], tmp[:], LN_EPS, None, op0=ALU.add), r=[tmp], w=[tmp])
            kb.op("act", lambda e: e.activation(out=tmp[:], in_=tmp[:], func=AF.Sqrt), r=[tmp], w=[tmp])
            kb.op("dve", lambda e: e.reciprocal(rstd[:], tmp[:]), r=[tmp], w=[rstd])
            for fc in range(FC):
                kb.op("dve", lambda e, fc=fc: e.tensor_tensor(out=cut[:, fc, :], in0=cut[:, fc, :], in1=mean[:], op=ALU.subtract),
                      r=[cut, mean], w=[cut])
                kb.op("pool", lambda e, fc=fc: e.tensor_tensor(out=cut[:, fc, :], in0=cut[:, fc, :], in1=rstd[:], op=ALU.mult),
                      r=[cut, rstd], w=[cut])
                kb.op("act", lambda e, fc=fc: e.activation(out=vT[:, fc, :], in_=cut[:, fc, :], func=AF.Silu,
                                                           scale=lng[:, fc:fc + 1], bias=lnb[:, fc:fc + 1]),
                      r=[cut, lng, lnb], w=[vT])
            for ts in range(4):
                r0 = t0 + ts * 128
                for ob in range(NOB):
                    ps = pso[it % 2]
                    xt = xts[it % 2]
                    ot = ots[it % 2]
                    it += 1
                    cs = slice(ob * OBW, (ob + 1) * OBW)
                    kb.dma("sp", xt[:, 0:OBW], x_src.ap()[r0:r0 + 128, cs], w=[xt])
                    for kc in range(FC):
                        kb.op("pe", lambda e, kc=kc: e.matmul(ps[:, 0:OBW], lhsT=vT[:, kc, ts * 128:(ts + 1) * 128], rhs=wout[:, kc, cs],
                                                             start=(kc == 0), stop=(kc == FC - 1)),
                              r=[vT, wout], w=[ps] if kc == 0 else [])
                        ps.w = (kb.st["pe"].sem, kb.st["pe"].cnt)
                    kb.op("dve", lambda e: e.tensor_tensor(out=ot[:, 0:OBW], in0=ps[:, 0:OBW], in1=brow[:, cs], op=ALU.add),
                          r=[ps, brow], w=[ot])
                    kb.op("pool", lambda e: e.tensor_tensor(out=ot[:, 0:OBW], in0=ot[:, 0:OBW], in1=G_t[:, cs], op=ALU.mult),
                          r=[ot, G_t], w=[ot])
                    kb.op("dve", lambda e: e.tensor_tensor(out=ot[:, 0:OBW], in0=ot[:, 0:OBW], in1=xt[:, 0:OBW], op=ALU.add),
                          r=[ot, xt], w=[ot])
                    kb.dma("sp", x_dst.ap()[r0:r0 + 128, cs], ot[:, 0:OBW], r=[ot])
        kb.end()


    def acc_group(ps, n, mk, r):
        for i in range(n):
            kb.op("pe", lambda e, i=i: mk(e, i), r=r, w=[ps] if i == 0 else [])
            ps.w = (kb.st["pe"].sem, kb.st["pe"].cnt)

    def stage_moe(li, x_src):
        kb.begin()
        AFF = kb.sb("AFF", [128, NTT, E], F32)
        SLOT = kb.sb("SLOT", [128, E, NTT], I32)
        pidx = kb.sb("pidx", [128, 1], F32)
        pidi = kb.sb("pidi", [128, 1], I32)
        kb.op("pool", lambda e: e.iota(pidi[:], pattern=[[0, 1]], base=0, channel_multiplier=1), w=[pidi])
        kb.op("dve", lambda e: e.tensor_copy(pidx[:], pidi[:]), r=[pidi], w=[pidx])
        ltri = kb.sb("ltri", [128, 128], F32)
        kb.op("dve", lambda e: e.tensor_scalar(ltri[:], iotf[:], 0.0, None, op0=ALU.is_gt), r=[iotf], w=[ltri])
        kb.begin()
        zt = kb.sb("zt", [128, D], F32)
        kb.op("pool", lambda e: e.memset(zt[:], 0.0), w=[zt])
        for tt in range(NTT):
            kb.dma("sp", YBF.ap()[tt * 128:(tt + 1) * 128, :], zt[:], r=[zt])
        A_t = kb.sb("A", [128, D], F32)
        S_t = kb.sb("S", [128, D], F32)
        tmp = kb.sb("tmp", [128, D], F32)
        xts = [kb.sb(f"xt{i}", [128, D], F32) for i in range(2)]
        ws = make_rms_ws()
        hfs = [kb.sb(f"hf{i}", [128, D], F32) for i in range(2)]
        hxs = [kb.sb(f"hx{i}", [128, DX], BF16) for i in range(2)]
        hTs_ = [kb.sb(f"hTf{i}", [128, FC, 128], F32) for i in range(2)]
        sms = [kb.sb(f"sm{i}", [128, 4], F32) for i in range(2)]
        exs = [kb.sb(f"ex{i}", [128, E], F32) for i in range(2)]
        wr = kb.sb("wr", [128, FC, E], F32)
        kb.dma("sp", wr[:], moe_w_router.ap()[li], w=[wr])
        psT = [kb.ps(f"psTf{i}", [128, 512], F32) for i in range(2)]
        psr = kb.ps("psr", [128, E], F32)
        def c1_front(tt):
            u = (tt * 128) // UNIT
            if (tt * 128) % UNIT == 0:
                load_mod_rows(li, u, 4, 3, norm_ffn_g.ap()[li:li + 1, :], A_t, S_t, tmp)
            xt = xts[tt % 2]
            hf = hfs[tt % 2]
            hx = hxs[tt % 2]
            hT = hTs_[tt % 2]
            kb.dma("sp", xt[:], x_src.ap()[tt * 128:(tt + 1) * 128, :], w=[xt])
            rms_modulate(xt, A_t, S_t, hf, ws)
            kb.op("act", lambda e: e.activation(out=hx[:, 0:D], in_=hf[:], func=AF.Copy), r=[hf], w=[hx])
            kb.op("pool", lambda e: e.memset(hx[:, D:D + 1], float(tt)), w=[hx])
            kb.op("pool", lambda e: e.tensor_copy(hx[:, D + 1:D + 2], pidx[:]), r=[pidx], w=[hx])
            for fb in range(0, FC, 4):
                nb = min(4, FC - fb)
                ps = psT[(fb // 4) % 2]
                acc_group(ps, nb, lambda e, j, fb=fb, ps=ps: e.transpose(out=ps[:, j * 128:(j + 1) * 128],
                                                                     in_=hf[:, (fb + j) * 128:(fb + j + 1) * 128],
                                                                     identity=ident_f[:]), r=[hf, ident_f])
                kb.op("dve", lambda e, fb=fb, nb=nb, ps=ps: e.tensor_copy(
                    hT[:, fb:fb + nb, :], ps[:, 0:nb * 128].rearrange("p (a b) -> p a b", b=128)), r=[ps], w=[hT])

        def c1_back(tt):
            hx = hxs[tt % 2]
            hT, sm, ex = hTs_[tt % 2], sms[tt % 2], exs[tt % 2]
            acc_group(psr, FC, lambda e, kc: e.matmul(psr[:], lhsT=hT[:, kc, :], rhs=wr[:, kc, :],
                                                     start=(kc == 0), stop=(kc == FC - 1)), r=[hT, wr])
            kb.op("dve", lambda e: e.tensor_reduce(out=sm[:, 0:1], in_=psr[:], axis=AX.X, op=ALU.max), r=[psr], w=[sm])
            kb.op("dve", lambda e: e.tensor_scalar(sm[:, 1:2], sm[:, 0:1], -1.0, None, op0=ALU.mult), r=[sm], w=[sm])
            kb.op("act", lambda e: e.activation(out=ex[:], in_=psr[:], func=AF.Exp, bias=sm[:, 1:2], accum_out=sm[:, 2:3]),
                  r=[psr, sm], w=[ex, sm])
            kb.op("dve", lambda e: e.reciprocal(sm[:, 3:4], sm[:, 2:3]), r=[sm], w=[sm])
            kb.op("dve", lambda e: e.tensor_scalar(AFF[:, tt, :], ex[:], sm[:, 3:4], None, op0=ALU.mult),
                  r=[ex, sm], w=[AFF])
            kb.op("dve", lambda e: e.tensor_copy(hx[:, D + 2:DX].bitcast(F32), AFF[:, tt, :]), r=[AFF], w=[hx])
            kb.dma("sp", H.ap()[tt * 128:(tt + 1) * 128, :], hx[:], r=[hx])

        c1_front(0)
        for tt in range(NTT):
            if tt + 1 < NTT:
                c1_front(tt + 1)
            c1_back(tt)
        kb.end()
        kb.begin()
        AFFv = AFF[:].rearrange("p t e -> p e t")
        lo = kb.sb("lo", [128, E], F32)
        hi = kb.sb("hi", [128, E], F32)
        mid = kb.sb("mid", [128, E], F32)
        ge = kb.sb("ge", [128, E], F32)
        nge = kb.sb("nge", [128, E], F32)
        ta = kb.sb("ta", [128, E], F32)
        tb_ = kb.sb("tb", [128, E], F32)
        cnt = kb.sb("cnt", [128, E], F32)
        cmp = kb.sb("cmp", [128, E, NTT], F32)
        pst = kb.ps("pst", [128, E], F32)
        kb.op("pool", lambda e: e.memset(lo[:], 0.0), w=[lo])
        kb.op("pool", lambda e: e.memset(hi[:], 1.0), w=[hi])

        def bc(t):
            return t[:, :].unsqueeze(2).broadcast_to([128, E, NTT])

        for it in range(34):
            kb.op("dve", lambda e: e.tensor_tensor(out=mid[:], in0=lo[:], in1=hi[:], op=ALU.add), r=[lo, hi], w=[mid])
            kb.op("dve", lambda e: e.tensor_scalar(mid[:], mid[:], 0.5, None, op0=ALU.mult), r=[mid], w=[mid])
            kb.op("dve", lambda e: e.tensor_tensor(out=cmp[:], in0=AFFv, in1=bc(mid), op=ALU.is_ge), r=[AFF, mid], w=[cmp])
            kb.op("dve", lambda e: e.tensor_reduce(out=cnt[:], in_=cmp[:], axis=AX.X, op=ALU.add), r=[cmp], w=[cnt])
            kb.op("pe", lambda e: e.matmul(pst[:], lhsT=ones_f[:], rhs=cnt[:], start=True, stop=True), r=[ones_f, cnt], w=[pst])
            kb.op("dve", lambda e: e.tensor_scalar(ge[:], pst[:], float(CAP), None, op0=ALU.is_ge), r=[pst], w=[ge])
            kb.op("dve", lambda e: e.tensor_scalar(nge[:], ge[:], -1.0, 1.0, op0=ALU.mult, op1=ALU.add), r=[ge], w=[nge])
            kb.op("dve", lambda e: e.tensor_tensor(out=ta[:], in0=ge[:], in1=mid[:], op=ALU.mult), r=[ge, mid], w=[ta])
            kb.op("dve", lambda e: e.tensor_tensor(out=tb_[:], in0=nge[:], in1=lo[:], op=ALU.mult), r=[nge, lo], w=[tb_])
            kb.op("dve", lambda e: e.tensor_tensor(out=lo[:], in0=ta[:], in1=tb_[:], op=ALU.add), r=[ta, tb_], w=[lo])
            kb.op("dve", lambda e: e.tensor_tensor(out=ta[:], in0=nge[:], in1=mid[:], op=ALU.mult), r=[nge, mid], w=[ta])
            kb.op("dve", lambda e: e.tensor_tensor(out=tb_[:], in0=ge[:], in1=hi[:], op=ALU.mult), r=[ge, hi], w=[tb_])
            kb.op("dve", lambda e: e.tensor_tensor(out=hi[:], in0=ta[:], in1=tb_[:], op=ALU.add), r=[ta, tb_], w=[hi])
        rst = kb.sb("rst", [128, E, NTT], F32)
        kb.op("pool", lambda e: e.memset(rst[:], 1.0), w=[rst])
        kb.op("pool", lambda e: e.memset(rst[:, :, 0:1], 0.0), w=[rst])
        pre = kb.sb("pre", [128, E, NTT], F32)
        kb.op("dve", lambda e: e.tensor_tensor(out=cmp[:], in0=AFFv, in1=bc(lo), op=ALU.is_ge), r=[AFF, lo], w=[cmp])
        kb.op("dve", lambda e: e.tensor_tensor_scan(out=pre[:].rearrange("p a b -> p (a b)"),
                                                   data0=rst[:].rearrange("p a b -> p (a b)"),
                                                   data1=cmp[:].rearrange("p a b -> p (a b)"),
                                                   initial=0.0, op0=ALU.mult, op1=ALU.add), r=[rst, cmp], w=[pre])
        kb.op("dve", lambda e: e.tensor_copy(cnt[:], pre[:, :, NTT - 1]), r=[pre], w=[cnt])
        kb.op("pe", lambda e: e.matmul(pst[:], lhsT=ltri[:], rhs=cnt[:], start=True, stop=True), r=[ltri, cnt], w=[pst])
        kb.op("dve", lambda e: e.tensor_scalar(ta[:], pst[:], -1.0, None, op0=ALU.add), r=[pst], w=[ta])
        BIG = 1000000.0
        kb.op("dve", lambda e: e.tensor_tensor(out=pre[:], in0=pre[:], in1=bc(ta), op=ALU.add), r=[pre, ta], w=[pre])
        kb.op("dve", lambda e: e.tensor_scalar(pre[:], pre[:], -BIG, None, op0=ALU.add), r=[pre], w=[pre])
        kb.op("dve", lambda e: e.tensor_tensor(out=pre[:], in0=pre[:], in1=cmp[:], op=ALU.mult), r=[pre, cmp], w=[pre])
        kb.op("dve", lambda e: e.tensor_scalar(pre[:], pre[:], BIG, None, op0=ALU.add), r=[pre], w=[pre])
        kb.op("dve", lambda e: e.tensor_copy(SLOT[:], pre[:]), r=[pre], w=[SLOT])
        hxs = [kb.sb(f"hxd{i}", [128, DX], BF16) for i in range(3)]
        bc_reg = nc.gpsimd.to_reg(CAP - 1)
        for tt in range(NTT):
            hx = hxs[tt % 3]
            kb.dma("sp", hx[:], H.ap()[tt * 128:(tt + 1) * 128, :], w=[hx])
            for ei in range(E):
                kb.dma("gq", XS[ei].ap()[:, :], hx[:, :], r=[hx, SLOT],
                       indirect=dict(out_offset=bass.IndirectOffsetOnAxis(ap=SLOT[:, ei, tt:tt + 1], axis=0), in_offset=None,
                                     bounds_check=bc_reg, oob_is_err=False))
        kb.end()
        kb.begin()
        FBW = min(256, D)
        xsT = kb.sb("xsT", [128, FC, CAP], BF16)
        hidT = kb.sb("hidT", [128, FC, CAP], BF16)
        IDX = kb.sb("IDX", [128, NST], I32)
        GT = kb.sb("GT", [128, NST], F32)
        idf = kb.sb("idf", [128, 4], F32)
        xss = [kb.sb(f"xs{i}", [128, DX], BF16) for i in range(2)]
        wgs = [kb.sb(f"wg{i}", [128, FC, FBW], BF16) for i in range(2)]
        wus = [kb.sb(f"wu{i}", [128, FC, FBW], BF16) for i in range(2)]
        wdp = [kb.sb(f"wdp{i}", [128, FC, OBW], BF16) for i in range(NOB)]
        outs = [kb.sb(f"outm{i}", [128, D], F32) for i in range(2)]
        sgs = [kb.sb(f"sgm{i}", [128, 512], F32) for i in range(2)]
        psT2 = [kb.ps(f"psTb{i}", [128, 512], BF16) for i in range(2)]
        psg = [kb.ps(f"psgm{i}", [128, 512]) for i in range(2)]
        psu = [kb.ps(f"psum{i}", [128, 512]) for i in range(2)]
        pso = [kb.ps(f"psom{i}", [128, OBW]) for i in range(2)]
        ytok = kb.sb("ytok", [1, 1], F32)
        nw = 0
        nd = 0
        ni = 0
        NFB = D // FBW
        wsched = []
        for ei_ in range(E):
            wsched += [("gu", ei_, fb_) for fb_ in range(NFB)]
        wstate = dict(issued=0, ngu=0, nd=0, bufs={})

        def w_issue(upto):
            while wstate["issued"] <= upto and wstate["issued"] < len(wsched):
                k = wstate["issued"]
                kind, e_, b_ = wsched[k]
                if kind == "gu":
                    gv_ = moe_w["gate"][li][e_ // EG].ap()[e_ % EG].rearrange("(kc p) f -> p kc f", p=128)
                    uv_ = moe_w["up"][li][e_ // EG].ap()[e_ % EG].rearrange("(kc p) f -> p kc f", p=128)
                    wg_, wu_ = wgs[wstate["ngu"] % 2], wus[wstate["ngu"] % 2]
                    wstate["ngu"] += 1
                    kb.dma("gq", wg_[:], gv_[:, :, b_ * FBW:(b_ + 1) * FBW], w=[wg_])
                    kb.dma("gq", wu_[:], uv_[:, :, b_ * FBW:(b_ + 1) * FBW], w=[wu_])
                    wstate["bufs"][k] = (wg_, wu_)
                wstate["issued"] += 1

        wk = 0
        w_issue(0)
        for ei in range(E):
            for st in range(NST):
                xs = xss[st % 2]
                kb.dma("sp", xs[:], XS[ei].ap()[st * 128:(st + 1) * 128, :], w=[xs])
                for fb in range(0, FC, 4):
                    nb = min(4, FC - fb)
                    ps = psT2[(fb // 4) % 2]
                    acc_group(ps, nb, lambda e, j, fb=fb, ps=ps, xs=xs: e.transpose(
                        out=ps[:, j * 128:(j + 1) * 128], in_=xs[:, (fb + j) * 128:(fb + j + 1) * 128], identity=ident_b[:]),
                        r=[xs, ident_b])
                    kb.op("act" if (fb // 4) % 2 == 0 else "dve",
                          (lambda e, fb=fb, nb=nb, ps=ps, st=st: e.activation(
                              out=xsT[:, fb:fb + nb, st * 128:(st + 1) * 128],
                              in_=ps[:, 0:nb * 128].rearrange("p (a b) -> p a b", b=128), func=AF.Copy))
                          if (fb // 4) % 2 == 0 else
                          (lambda e, fb=fb, nb=nb, ps=ps, st=st: e.tensor_copy(
                              xsT[:, fb:fb + nb, st * 128:(st + 1) * 128],
                              ps[:, 0:nb * 128].rearrange("p (a b) -> p a b", b=128))),
                          r=[ps], w=[xsT])
                kb.op("dve", lambda e, xs=xs: e.tensor_copy(idf[:, 0:2], xs[:, D:D + 2]), r=[xs], w=[idf])
                kb.op("dve", lambda e: e.scalar_tensor_tensor(out=idf[:, 2:3], in0=idf[:, 0:1], scalar=128.0, in1=idf[:, 1:2],
                                                             op0=ALU.mult, op1=ALU.add), r=[idf], w=[idf])
                kb.op("dve", lambda e, st=st: e.tensor_copy(IDX[:, st:st + 1], idf[:, 2:3]), r=[idf], w=[IDX])
                kb.op("dve", lambda e, st=st, xs=xs, ei=ei: e.tensor_copy(
                    GT[:, st:st + 1], xs[:, D + 2 + 2 * ei:D + 4 + 2 * ei].bitcast(F32)), r=[xs], w=[GT])
            gv = moe_w["gate"][li][ei // EG].ap()[ei % EG].rearrange("(kc p) f -> p kc f", p=128)
            uv = moe_w["up"][li][ei // EG].ap()[ei % EG].rearrange("(kc p) f -> p kc f", p=128)
            dv = moe_w["down"][li][ei // EG].ap()[ei % EG].rearrange("(kc p) f -> p kc f", p=128)
            for ob in range(NOB):
                kb.dma("gq", wdp[ob][:], dv[:, :, ob * OBW:(ob + 1) * OBW], w=[wdp[ob]])
            for fb in range(D // FBW):
                w_issue(wk + 1)
                wg, wu = wstate["bufs"].pop(wk)
                wk += 1
                for j in range(FBW // 128):
                    f = fb * (FBW // 128) + j
                    for sb_ in range(CAP // 512):
                        pg, pu, sg = psg[ni % 2], psu[ni % 2], sgs[ni % 2]
                        ni += 1
                        cs = slice(sb_ * 512, (sb_ + 1) * 512)
                        acc_group(pg, FC, lambda e, kc, pg=pg, wg=wg, j=j, cs=cs: e.matmul(
                            pg[:], lhsT=wg[:, kc, j * 128:(j + 1) * 128], rhs=xsT[:, kc, cs],
                            start=(kc == 0), stop=(kc == FC - 1)), r=[wg, xsT])
                        acc_group(pu, FC, lambda e, kc, pu=pu, wu=wu, j=j, cs=cs: e.matmul(
                            pu[:], lhsT=wu[:, kc, j * 128:(j + 1) * 128], rhs=xsT[:, kc, cs],
                            start=(kc == 0), stop=(kc == FC - 1)), r=[wu, xsT])
                        kb.op("act", lambda e, pg=pg, sg=sg: e.activation(out=sg[:], in_=pg[:], func=AF.Silu), r=[pg], w=[sg])
                        kb.op("dve", lambda e, pu=pu, sg=sg, f=f, cs=cs: e.tensor_tensor(
                            out=hidT[:, f, cs], in0=pu[:], in1=sg[:], op=ALU.mult), r=[pu, sg], w=[hidT])
            for st in range(NST):
                OUT = outs[st % 2]
                for ob in range(NOB):
                    po = pso[ni % 2]
                    ni += 1
                    wd = wdp[ob]
                    acc_group(po, FC, lambda e, kc, po=po, wd=wd, st=st: e.matmul(
                        po[:], lhsT=hidT[:, kc, st * 128:(st + 1) * 128], rhs=wd[:, kc, :],
                        start=(kc == 0), stop=(kc == FC - 1)), r=[hidT, wd])
                    osl = slice(ob * OBW, (ob + 1) * OBW)
                    kb.op("dve" if ni % 2 else "act",
                          (lambda e, po=po, OUT=OUT, st=st, osl=osl: e.tensor_scalar(OUT[:, osl], po[:], GT[:, st:st + 1], None, op0=ALU.mult))
                          if ni % 2 else
                          (lambda e, po=po, OUT=OUT, st=st, osl=osl: e.activation(out=OUT[:, osl], in_=po[:], func=AF.Identity, scale=GT[:, st:st + 1])),
                          r=[po, GT], w=[OUT])
                kb.dma("gq", YBF.ap()[:, :], OUT[:, :], r=[OUT, IDX], w=[ytok],
                       indirect=dict(out_offset=bass.IndirectOffsetOnAxis(ap=IDX[:, st:st + 1], axis=0), in_offset=None,
                                     compute_op=ALU.add))
        kb.end()
        kb.end()

    def stage_combine(li, x_src, x_dst, final):
        kb.begin()
        G_t = kb.sb("G2", [128, D], F32)
        F_t = kb.sb("Fg", [128, D], F32)
        if final:
            row_bcast(F_t, final_g.ap())
        xts = [kb.sb(f"xc{i}", [128, D], F32) for i in range(2)]
        yts = [kb.sb(f"yc{i}", [128, D], F32) for i in range(2)]
        junk = kb.sb("junkc", [128, D], F32)
        ssum = kb.sb("ssumc", [128, 1], F32)
        rstd = kb.sb("rstdc", [128, 3], F32)
        for tt in range(NTT):
            if (tt * 128) % UNIT == 0:
                u = (tt * 128) // UNIT
                row_bcast(G_t, MOD.ap()[li, u:u + 1, 5 * D:6 * D])
            xt, yt = xts[tt % 2], yts[tt % 2]
            kb.dma("sp", xt[:], x_src.ap()[tt * 128:(tt + 1) * 128, :], w=[xt])
            kb.dma("sp", yt[:], YBF.ap()[tt * 128:(tt + 1) * 128, :], w=[yt])
            kb.op("pool", lambda e, yt=yt: e.tensor_tensor(out=yt[:], in0=yt[:], in1=G_t[:], op=ALU.mult), r=[yt, G_t], w=[yt])
            kb.op("dve", lambda e, xt=xt, yt=yt: e.tensor_tensor(out=xt[:], in0=xt[:], in1=yt[:], op=ALU.add), r=[xt, yt], w=[xt])
            if final:
                kb.op("act", lambda e, xt=xt: e.activation(out=junk[:], in_=xt[:], func=AF.Square, accum_out=ssum[:, 0:1]),
                      r=[xt], w=[junk, ssum])
                kb.op("dve", lambda e: e.tensor_scalar(rstd[:, 0:1], ssum[:, 0:1], 1.0 / D, RMS_EPS, op0=ALU.mult, op1=ALU.add),
                      r=[ssum], w=[rstd])
                kb.op("act", lambda e: e.activation(out=rstd[:, 1:2], in_=rstd[:, 0:1], func=AF.Sqrt), r=[rstd], w=[rstd])
                kb.op("dve", lambda e: e.reciprocal(rstd[:, 2:3], rstd[:, 1:2]), r=[rstd], w=[rstd])
                kb.op("dve", lambda e, xt=xt, yt=yt: e.scalar_tensor_tensor(out=yt[:], in0=xt[:], scalar=rstd[:, 2:3], in1=F_t[:],
                                                                       op0=ALU.mult, op1=ALU.mult), r=[xt, rstd, F_t], w=[yt])
                kb.dma("sp", x_dst.ap()[tt * 128:(tt + 1) * 128, :], yt[:], r=[yt])
            else:
                kb.dma("sp", x_dst.ap()[tt * 128:(tt + 1) * 128, :], xt[:], r=[xt])
        kb.end()


    def stage_prenorm_rows(li, x_src, gain, wsc, wsh, dst):
        kb.begin()
        A_t = kb.sb("A", [128, D], F32)
        S_t = kb.sb("S", [128, D], F32)
        tmp = kb.sb("tmp", [128, D], F32)
        xts = [kb.sb(f"xt{i}", [128, D], F32) for i in range(2)]
        ws = make_rms_ws()
        hbs = [kb.sb(f"hb{i}", [128, D], BF16) for i in range(2)]
        for tt in range(NTT):
            u = (tt * 128) // UNIT
            if (tt * 128) % UNIT == 0:
                load_mod_rows(li, u, wsc, wsh, gain.ap()[li:li + 1, :], A_t, S_t, tmp)
            xt = xts[tt % 2]
            kb.dma("sp", xt[:], x_src.ap()[tt * 128:(tt + 1) * 128, :], w=[xt])
            hb = hbs[tt % 2]
            rms_modulate(xt, A_t, S_t, hb, ws)
            kb.dma("sp", dst.ap()[tt * 128:(tt + 1) * 128, :], hb[:], r=[hb])
        kb.end()

    def stage_rows_to_T(src):
        kb.begin()
        hbs = [kb.sb(f"hbr{i}", [128, D], BF16) for i in range(2)]
        hTs = [kb.sb(f"hTr{i}", [128, FC, 128], BF16) for i in range(2)]
        psT = [kb.ps(f"psTr{i}", [128, 512], BF16) for i in range(2)]
        for tt in range(NTT):
            hb = hbs[tt % 2]
            kb.dma("sp", hb[:], src.ap()[tt * 128:(tt + 1) * 128, :], w=[hb])
            transpose_to_HT(hb, tt, psT, hTs[tt % 2], HT)
        kb.end()

    def stage_s5():
        kb.begin()
        CW = min(512, NB)
        NH = NB // CW
        NNT = NB // 128
        TWO_PI = 2.0 * np.pi
        isb = kb.sb("isb", [128, 1], F32)
        pidi = kb.sb("pidi5", [128, 1], I32)
        kb.op("pool", lambda e: e.iota(pidi[:], pattern=[[0, 1]], base=0, channel_multiplier=1), w=[pidi])
        kb.op("dve", lambda e: e.tensor_copy(isb[:], pidi[:]), r=[pidi], w=[isb])
        kb.op("dve", lambda e: e.tensor_scalar(isb[:], isb[:], 63.5, None, op0=ALU.is_gt), r=[isb], w=[isb])
        sign = kb.sb("sign", [128, 1], F32)
        kb.op("dve", lambda e: e.tensor_scalar(sign[:], isb[:], 2.0, -1.0, op0=ALU.mult, op1=ALU.add), r=[isb], w=[sign])
        nsign = kb.sb("nsign", [128, 1], F32)
        kb.op("dve", lambda e: e.tensor_scalar(nsign[:], sign[:], -1.0, None, op0=ALU.mult), r=[sign], w=[nsign])
        iri = kb.sb("iri", [128, 8], I32)
        kb.op("pool", lambda e: e.iota(iri[:], pattern=[[1, 8]], base=0, channel_multiplier=0), w=[iri])
        EX = kb.sb("EX", [128, 4, 8], F32)
        kb.op("dve", lambda e: e.tensor_copy(EX[:, 0, :], iri[:]), r=[iri], w=[EX])
        kb.op("dve", lambda e: e.tensor_scalar(EX[:, 0, :], EX[:, 0, :], sign[:, 0:1], None, op0=ALU.mult), r=[EX, sign], w=[EX])
        kb.op("dve", lambda e: e.tensor_scalar(EX[:, 1, :], EX[:, 0, :], -1.0, None, op0=ALU.mult), r=[EX], w=[EX])
        off3 = kb.sb("off3", [128, 2], F32)
        kb.op("dve", lambda e: e.tensor_scalar(off3[:, 0:1], isb[:], -7.0, 7.0, op0=ALU.mult, op1=ALU.add), r=[isb], w=[off3])
        kb.op("dve", lambda e: e.tensor_scalar(off3[:, 1:2], isb[:], 7.0, 1.0, op0=ALU.mult, op1=ALU.add), r=[isb], w=[off3])
        kb.op("dve", lambda e: e.tensor_scalar(EX[:, 2, :], EX[:, 0, :], off3[:, 0:1], None, op0=ALU.add), r=[EX, off3], w=[EX])
        kb.op("dve", lambda e: e.tensor_scalar(EX[:, 3, :], EX[:, 1, :], off3[:, 1:2], None, op0=ALU.add), r=[EX, off3], w=[EX])
        tni = kb.sb("tni", [128, NB], I32)
        kb.op("pool", lambda e: e.iota(tni[:], pattern=[[1, NB]], base=0, channel_multiplier=0), w=[tni])
        nrow = kb.sb("nrow", [128, NB], F32)
        kb.op("dve", lambda e: e.tensor_copy(nrow[:], tni[:]), r=[tni], w=[nrow])
        KEEP = kb.sb("KEEP", [128, NB], F32)
        kb.dma("sp", KEEP[:], s5_keep.ap(), w=[KEEP])
        MK = kb.sb("MK", [128, 2, 128], F32)
        kb.dma("sp", MK[:], s5_masks.ap(), w=[MK])
        lre = kb.sb("lre", [128, G], F32)
        lim = kb.sb("lim", [128, G], F32)
        lst = kb.sb("lst", [128, G], F32)
        kb.dma("sp", lre[:], ssm_lre.ap(), w=[lre])
        kb.dma("sp", lim[:], ssm_lim.ap(), w=[lim])
        kb.dma("sp", lst[:], ssm_ls.ap(), w=[lst])
        dcol = kb.sb("dcol", [128, G], F32)
        kb.dma("sp", dcol[:], ssm_dcol.ap(), w=[dcol])
        dt = kb.sb("dt", [128, G], F32)
        kb.op("act", lambda e: e.activation(out=dt[:], in_=lst[:], func=AF.Exp), r=[lst], w=[dt])
        lrdt = kb.sb("lrdt", [128, G], F32)
        kb.op("dve", lambda e: e.tensor_tensor(out=lrdt[:], in0=lre[:], in1=dt[:], op=ALU.mult), r=[lre, dt], w=[lrdt])
        f0 = kb.sb("f0", [128, G], F32)
        ti = kb.sb("ti", [128, G], I32)
        tf = kb.sb("tf", [128, G], F32)

        def frac_(t_f, t_i, t_tmp, eng="dve"):
            kb.op(eng, lambda e: e.tensor_copy(t_i, t_f), r=[], w=[])
            kb.op(eng, lambda e: e.tensor_copy(t_tmp, t_i), r=[], w=[])
            kb.op(eng, lambda e: e.tensor_tensor(out=t_f, in0=t_f, in1=t_tmp, op=ALU.subtract), r=[], w=[])

        kb.op("dve", lambda e: e.tensor_tensor(out=f0[:], in0=lim[:], in1=dt[:], op=ALU.mult), r=[lim, dt], w=[f0])
        kb.op("dve", lambda e: e.tensor_scalar(f0[:], f0[:], 1.0 / TWO_PI, None, op0=ALU.mult), r=[f0], w=[f0])
        def frac_tiles(F, I_, T, eng="dve"):
            kb.op(eng, lambda e: e.tensor_copy(I_[:], F[:]), r=[F], w=[I_])
            kb.op(eng, lambda e: e.tensor_copy(T[:], I_[:]), r=[I_], w=[T])
            kb.op(eng, lambda e: e.tensor_tensor(out=F[:], in0=F[:], in1=T[:], op=ALU.subtract), r=[F, T], w=[F])

        frac_tiles(f0, ti, tf)
        are = kb.sb("are", [128, G], F32)
        aim = kb.sb("aim", [128, G], F32)
        mag = kb.sb("mag", [128, G], F32)
        fc_ = kb.sb("fcq", [128, G], F32)
        kb.op("act", lambda e: e.activation(out=mag[:], in_=lrdt[:], func=AF.Exp), r=[lrdt], w=[mag])
        kb.op("act", lambda e: e.activation(out=aim[:], in_=f0[:], func=AF.Sin, scale=TWO_PI), r=[f0], w=[aim])
        kb.op("dve", lambda e: e.tensor_scalar(fc_[:], f0[:], 0.25, None, op0=ALU.add), r=[f0], w=[fc_])
        frac_tiles(fc_, ti, tf)
        kb.op("act", lambda e: e.activation(out=are[:], in_=fc_[:], func=AF.Sin, scale=TWO_PI), r=[fc_], w=[are])
        kb.op("dve", lambda e: e.tensor_tensor(out=are[:], in0=are[:], in1=mag[:], op=ALU.mult), r=[are, mag], w=[are])
        kb.op("dve", lambda e: e.tensor_tensor(out=aim[:], in0=aim[:], in1=mag[:], op=ALU.mult), r=[aim, mag], w=[aim])
        kre = kb.sb("kre", [128, G], F32)
        kim = kb.sb("kim", [128, G], F32)
        den = kb.sb("den", [128, G], F32)
        t1 = kb.sb("t1g", [128, G], F32)
        nr = kb.sb("nr", [128, G], F32)
        kb.op("dve", lambda e: e.tensor_scalar(nr[:], are[:], -1.0, None, op0=ALU.add), r=[are], w=[nr])
        kb.op("dve", lambda e: e.tensor_tensor(out=den[:], in0=lre[:], in1=lre[:], op=ALU.mult), r=[lre], w=[den])
        kb.op("dve", lambda e: e.tensor_tensor(out=t1[:], in0=lim[:], in1=lim[:], op=ALU.mult), r=[lim], w=[t1])
        kb.op("dve", lambda e: e.tensor_tensor(out=den[:], in0=den[:], in1=t1[:], op=ALU.add), r=[den, t1], w=[den])
        kb.op("dve", lambda e: e.reciprocal(den[:], den[:]), r=[den], w=[den])
        kb.op("dve", lambda e: e.tensor_tensor(out=kre[:], in0=nr[:], in1=lre[:], op=ALU.mult), r=[nr, lre], w=[kre])
        kb.op("dve", lambda e: e.tensor_tensor(out=t1[:], in0=aim[:], in1=lim[:], op=ALU.mult), r=[aim, lim], w=[t1])
        kb.op("dve", lambda e: e.tensor_tensor(out=kre[:], in0=kre[:], in1=t1[:], op=ALU.add), r=[kre, t1], w=[kre])
        kb.op("dve", lambda e: e.tensor_tensor(out=kre[:], in0=kre[:], in1=den[:], op=ALU.mult), r=[kre, den], w=[kre])
        kb.op("dve", lambda e: e.tensor_tensor(out=kim[:], in0=aim[:], in1=lre[:], op=ALU.mult), r=[aim, lre], w=[kim])
        kb.op("dve", lambda e: e.tensor_tensor(out=t1[:], in0=nr[:], in1=lim[:], op=ALU.mult), r=[nr, lim], w=[t1])
        kb.op("dve", lambda e: e.tensor_tensor(out=kim[:], in0=kim[:], in1=t1[:], op=ALU.subtract), r=[kim, t1], w=[kim])
        kb.op("dve", lambda e: e.tensor_tensor(out=kim[:], in0=kim[:], in1=den[:], op=ALU.mult), r=[kim, den], w=[kim])
        R8 = kb.sb("R8", [128, G], F32)
        kb.op("act", lambda e: e.activation(out=R8[:], in_=lrdt[:], func=AF.Exp, scale=8.0), r=[lrdt], w=[R8])
        f8 = kb.sb("f8", [128, G], F32)
        kb.op("dve", lambda e: e.tensor_scalar(f8[:], f0[:], 8.0, None, op0=ALU.mult), r=[f0], w=[f8])
        frac_tiles(f8, ti, tf)
        kb.op("dve", lambda e: e.tensor_scalar(f8[:], f8[:], nsign[:, 0:1], None, op0=ALU.mult), r=[f8, nsign], w=[f8])

        GC = 8
        bre = kb.sb("bre", [128, GC, 16], F32)
        bim = kb.sb("bim", [128, GC, 16], F32)
        cre = kb.sb("cre", [128, GC, 16], F32)
        cim = kb.sb("cim", [128, GC, 16], F32)
        bbre = kb.sb("bbre", [128, GC, 16], F32)
        bbim = kb.sb("bbim", [128, GC, 16], F32)
        tb16 = kb.sb("tb16", [128, GC, 16], F32)
        marg = kb.sb("marg", [128, GC, 32], F32)
        turn = kb.sb("turn", [128, GC, 32], F32)
        turc = kb.sb("turc", [128, GC, 32], F32)
        tui = kb.sb("tui", [128, GC, 32], I32)
        tuf = kb.sb("tuf", [128, GC, 32], F32)
        Ere = kb.sb("Ere", [128, GC, 4, 8], F32)
        Eim = kb.sb("Eim", [128, GC, 4, 8], F32)
        Pre = kb.sb("Pre", [128, GC, 8, 16], F32)
        Pim = kb.sb("Pim", [128, GC, 8, 16], F32)
        Qre = kb.sb("Qre", [128, GC, 8, 16], F32)
        Qim = kb.sb("Qim", [128, GC, 8, 16], F32)
        Lre = kb.sb("Lre", [128, GC, 8, 16], F32)
        Lim = kb.sb("Lim", [128, GC, 8, 16], F32)
        Xre = kb.sb("QXre", [128, GC, 8, 16], F32)
        Xim = kb.sb("QXim", [128, GC, 8, 16], F32)
        QXreb = kb.sb("QXreb", [128, 2, GC, 128], BF16)
        QXimb = kb.sb("QXimb", [128, 2, GC, 128], BF16)
        nisb = kb.sb("nisb", [128, 1], F32)
        kb.op("dve", lambda e: e.tensor_scalar(nisb[:], isb[:], -1.0, 1.0, op0=ALU.mult, op1=ALU.add), r=[isb], w=[nisb])
        ta4 = kb.sb("ta4", [128, GC, 8, 16], F32)
        tb4 = kb.sb("tb4", [128, GC, 8, 16], F32)
        HN = kb.sb("HN", [128, NNT, 8, 128], BF16)
        YN = kb.sb("YN", [128, NNT, 8, 128], BF16)
        HG = kb.sb("HG", [128, NNT, 8, 128], BF16)
        Ug = [kb.sb(f"Ug{i}", [128, NB], BF16) for i in range(2)]
        W0bs = [kb.sb(f"W0b{i}", [128, 128], BF16) for i in range(2)]
        tW = kb.sb("tW", [128, 128], F32)
        LTres = [kb.sb(f"LTre{i}", [128, 128], BF16) for i in range(2)]
        LTims = [kb.sb(f"LTim{i}", [128, 128], BF16) for i in range(2)]
        cns = [kb.sb(f"cn{i}", [128, NB], F32) for i in range(2)]
        sns = [kb.sb(f"sn{i}", [128, NB], F32) for i in range(2)]
        RKs = [kb.sb(f"RK{i}", [128, NB], F32) for i in range(2)]
        tnf = kb.sb("tnf", [128, NB], F32)
        frt = kb.sb("frt", [128, NB], F32)
        abt = tnf
        wr_ = kb.sb("wr5", [128, NB], F32)
        wi_ = kb.sb("wi5", [128, NB], F32)
        zr = kb.sb("zr", [128, NB], F32)
        zi = kb.sb("zi", [128, NB], F32)
        ta = kb.sb("ta5", [128, NB], F32)
        tb2 = kb.sb("tb5", [128, NB], F32)
        XPr = kb.sb("XPr", [128, NB + 2], BF16)
        XPi = kb.sb("XPi", [128, NB + 2], BF16)
        kb.op("pool", lambda e: e.memset(XPr[:], 0.0), w=[XPr])
        kb.op("pool", lambda e: e.memset(XPi[:], 0.0), w=[XPi])
        ysk = ta
        yg = kb.sb("yg", [128, NB], BF16)
        halfpi = kb.sb("halfpi", [128, 1], F32)
        kb.op("pool", lambda e: e.memset(halfpi[:], float(np.pi / 2)), w=[halfpi])
        psS = [kb.ps(f"psS{i}", [128, CW]) for i in range(2)]
        psY = [kb.ps(f"psY{i}", [128, CW]) for i in range(NH)] if NH <= 2 else None
        psU = kb.ps("psU", [128, 512])
        psM = kb.ps("psM", [128, 256])
        psM2 = kb.ps("psM2", [128, 256])
        psB = kb.ps("psB", [128, 1024], BF16)

        def bc3(t, gsl, n):
            return t[:, gsl].unsqueeze(2).broadcast_to([128, GC, n])

        def cmul_outer(Er, Ei, Br, Bi, outr, outi, neg_im):
            def eb(E_ap):
                return E_ap.unsqueeze(3).broadcast_to([128, GC, 8, 16])

            def bb(B):
                return B[:].unsqueeze(2).broadcast_to([128, GC, 8, 16])
            kb.op("dve", lambda e: e.tensor_tensor(out=ta4[:], in0=eb(Er), in1=bb(Br), op=ALU.mult), r=[Ere, Eim, Br], w=[ta4])
            kb.op("dve", lambda e: e.tensor_tensor(out=tb4[:], in0=eb(Ei), in1=bb(Bi), op=ALU.mult), r=[Ere, Eim, Bi], w=[tb4])
            kb.op("dve", lambda e: e.tensor_tensor(out=outr[:], in0=ta4[:], in1=tb4[:], op=ALU.subtract), r=[ta4, tb4], w=[outr])
            kb.op("dve", lambda e: e.tensor_tensor(out=ta4[:], in0=eb(Er), in1=bb(Bi), op=ALU.mult), r=[Ere, Eim, Bi, outr], w=[ta4])
            kb.op("dve", lambda e: e.tensor_tensor(out=tb4[:], in0=eb(Ei), in1=bb(Br), op=ALU.mult), r=[Ere, Eim, Br, outr], w=[tb4])
            if neg_im:
                kb.op("dve", lambda e: e.tensor_tensor(out=outi[:], in0=ta4[:], in1=tb4[:], op=ALU.add), r=[ta4, tb4], w=[outi])
                kb.op("dve", lambda e: e.tensor_scalar(outi[:], outi[:], -1.0, None, op0=ALU.mult), r=[outi], w=[outi])
            else:
                kb.op("dve", lambda e: e.tensor_tensor(out=outi[:], in0=ta4[:], in1=tb4[:], op=ALU.add), r=[ta4, tb4], w=[outi])

        for fc in range(FC):
            gsl = slice(fc * GC, (fc + 1) * GC)
            kb.dma("sp", bre[:], ssm_bre.ap()[:, gsl, :], w=[bre])
            kb.dma("sp", bim[:], ssm_bim.ap()[:, gsl, :], w=[bim])
            kb.dma("sp", cre[:], ssm_cre.ap()[:, gsl, :], w=[cre])
            kb.dma("sp", cim[:], ssm_cim.ap()[:, gsl, :], w=[cim])
            kb.op("dve", lambda e: e.tensor_tensor(out=bbre[:], in0=bre[:], in1=bc3(kre, gsl, 16), op=ALU.mult), r=[bre, kre], w=[bbre])
            kb.op("dve", lambda e: e.tensor_tensor(out=tb16[:], in0=bim[:], in1=bc3(kim, gsl, 16), op=ALU.mult), r=[bim, kim], w=[tb16])
            kb.op("dve", lambda e: e.tensor_tensor(out=bbre[:], in0=bbre[:], in1=tb16[:], op=ALU.subtract), r=[bbre, tb16], w=[bbre])
            kb.op("dve", lambda e: e.tensor_tensor(out=bbim[:], in0=bim[:], in1=bc3(kre, gsl, 16), op=ALU.mult), r=[bim, kre], w=[bbim])
            kb.op("dve", lambda e: e.tensor_tensor(out=tb16[:], in0=bre[:], in1=bc3(kim, gsl, 16), op=ALU.mult), r=[bre, kim, bbre], w=[tb16])
            kb.op("dve", lambda e: e.tensor_tensor(out=bbim[:], in0=bbim[:], in1=tb16[:], op=ALU.add), r=[bbim, tb16], w=[bbim])
            exb = EX[:].rearrange("p a b -> p (a b)").unsqueeze(1).broadcast_to([128, GC, 32])
            kb.op("dve", lambda e: e.tensor_tensor(out=marg[:], in0=bc3(lrdt, gsl, 32), in1=exb, op=ALU.mult), r=[lrdt, EX], w=[marg])
            kb.op("act", lambda e: e.activation(out=marg[:], in_=marg[:], func=AF.Exp), r=[marg], w=[marg])
            kb.op("dve", lambda e: e.tensor_tensor(out=turn[:], in0=bc3(f0, gsl, 32), in1=exb, op=ALU.mult), r=[f0, EX], w=[turn])
            frac_tiles(turn, tui, tuf)
            kb.op("dve", lambda e: e.tensor_scalar(turc[:], turn[:], 0.25, None, op0=ALU.add), r=[turn], w=[turc])
            frac_tiles(turc, tui, tuf)
            Ef_re = Ere[:].rearrange("p g a b -> p g (a b)")
            Ef_im = Eim[:].rearrange("p g a b -> p g (a b)")
            kb.op("act", lambda e: e.activation(out=Ef_im, in_=turn[:], func=AF.Sin, scale=TWO_PI), r=[turn], w=[Eim])
            kb.op("act", lambda e: e.activation(out=Ef_re, in_=turc[:], func=AF.Sin, scale=TWO_PI), r=[turc], w=[Ere])
            kb.op("dve", lambda e: e.tensor_tensor(out=Ef_im, in0=Ef_im, in1=marg[:], op=ALU.mult), r=[Eim, marg], w=[Eim])
            kb.op("dve", lambda e: e.tensor_tensor(out=Ef_re, in0=Ef_re, in1=marg[:], op=ALU.mult), r=[Ere, marg], w=[Ere])
            cmul_outer(Ere[:, :, 0, :], Eim[:, :, 0, :], bbre, bbim, Pre, Pim, False)
            cmul_outer(Ere[:, :, 1, :], Eim[:, :, 1, :], cre, cim, Qre, Qim, True)
            cmul_outer(Ere[:, :, 2, :], Eim[:, :, 2, :], bbre, bbim, Lre, Lim, False)
            cmul_outer(Ere[:, :, 3, :], Eim[:, :, 3, :], cre, cim, Xre, Xim, True)
            for d_, sc_ in ((0, nisb), (1, isb)):
                kb.op("act", lambda e, d_=d_, sc_=sc_: e.activation(out=QXreb[:, d_], in_=Xre[:].rearrange("p g a b -> p g (a b)"),
                                                                  func=AF.Copy, scale=sc_[:, 0:1]), r=[Xre, sc_], w=[QXreb])
                kb.op("act", lambda e, d_=d_, sc_=sc_: e.activation(out=QXimb[:, d_], in_=Xim[:].rearrange("p g a b -> p g (a b)"),
                                                                  func=AF.Copy, scale=sc_[:, 0:1]), r=[Xim, sc_], w=[QXimb])
            for nt in range(NNT):
                src = HS.ap()[nt * 1024:(nt + 1) * 1024, fc * 128:(fc + 1) * 128].rearrange("(n i) c -> n i c", i=8)
                kb.dma("sp", HN[:, nt, :, :], src, w=[HN])
            for nt in range(NNT):
                kb.op("act", lambda e, nt=nt: e.activation(out=HG[:, nt, :, :].rearrange("p g (i c) -> p g i c", c=16),
                                                           in_=HN[:, nt, :, :].rearrange("p i (g c) -> p g i c", c=16), func=AF.Copy),
                      r=[HN], w=[HG])
            Pr = Pre[:].rearrange("p g a b -> p g (a b)")
            Pi = Pim[:].rearrange("p g a b -> p g (a b)")
            Qr = Qre[:].rearrange("p g a b -> p g (a b)")
            Qi = Qim[:].rearrange("p g a b -> p g (a b)")
            Lr = Lre[:].rearrange("p g a b -> p g (a b)")
            Li = Lim[:].rearrange("p g a b -> p g (a b)")

            def prepA(g):
                gg = fc * GC + g
                U = Ug[g % 2]
                LTre, LTim = LTres[g % 2], LTims[g % 2]
                kb.op("dve", lambda e: e.tensor_scalar(tni[:], nrow[:], f8[:, gg:gg + 1], None, op0=ALU.mult), r=[nrow, f8], w=[tni])
                for nb in range(0, NNT, 4):
                    k4 = min(4, NNT - nb)
                    acc_group(psU, k4, lambda e, j, nb=nb: e.matmul(
                        psU[:, j * 128:(j + 1) * 128], lhsT=HG[:, nb + j, g, :], rhs=ident_b[:],
                        start=True, stop=True), r=[HG, ident_b])
                    kb.op("act", lambda e, nb=nb, k4=k4: e.activation(out=U[:, nb * 128:(nb + k4) * 128], in_=psU[:, 0:k4 * 128],
                                                                     func=AF.Copy), r=[psU], w=[U])
                for d_ in range(2):
                    rs = slice(64 * d_, 64 * d_ + 64)
                    cs_ = slice(128 * d_, 128 * d_ + 128)
                    acc_group(psM, 2, lambda e, j, rs=rs, cs_=cs_: e.matmul(
                        psM[:, cs_], lhsT=(Pr if j == 0 else Pi)[rs, g, :], rhs=(Qr if j == 0 else Qi)[rs, g, :],
                        start=(j == 0), stop=(j == 1)), r=[Pre, Pim, Qre, Qim])
                acc_group(psM2, 2, lambda e, j: e.transpose(out=psM2[:, j * 128:(j + 1) * 128],
                                                            in_=(Lr if j == 0 else Li)[:, g, :], identity=ident_f[:]),
                          r=[Lre, Lim, ident_f])
                kb.op("act", lambda e: e.activation(out=LTre[:], in_=psM2[:, 0:128], func=AF.Copy), r=[psM2], w=[LTre])
                kb.op("act", lambda e: e.activation(out=LTim[:], in_=psM2[:, 128:256], func=AF.Copy), r=[psM2], w=[LTim])

            def prepB(g):
                gg = fc * GC + g
                W0b = W0bs[g % 2]
                cn, sn, RK = cns[g % 2], sns[g % 2], RKs[g % 2]
                kb.op("dve", lambda e: e.tensor_copy(tnf[:], tni[:]), r=[tni], w=[tnf])
                kb.op("dve", lambda e: e.scalar_tensor_tensor(out=frt[:], in0=nrow[:], scalar=f8[:, gg:gg + 1], in1=tnf[:],
                                                             op0=ALU.mult, op1=ALU.subtract), r=[nrow, f8, tnf], w=[frt])
                kb.op("dve", lambda e: e.tensor_tensor(out=tW[:], in0=psM[:, 0:128], in1=MK[:, 0, :], op=ALU.mult), r=[psM, MK], w=[tW])
                kb.op("dve", lambda e: e.tensor_tensor(out=W0b[:], in0=psM[:, 128:256], in1=MK[:, 1, :], op=ALU.mult), r=[psM, MK], w=[W0b])
                kb.op("dve", lambda e: e.tensor_tensor(out=W0b[:], in0=W0b[:], in1=tW[:], op=ALU.add), r=[W0b, tW], w=[W0b])
                kb.op("act", lambda e: e.activation(out=sn[:], in_=frt[:], func=AF.Sin, scale=TWO_PI), r=[frt], w=[sn])
                kb.op("act", lambda e: e.activation(out=abt[:], in_=frt[:], func=AF.Abs), r=[frt], w=[abt])
                kb.op("act", lambda e: e.activation(out=cn[:], in_=abt[:], func=AF.Sin, scale=-TWO_PI, bias=halfpi[:, 0:1]),
                      r=[abt, halfpi], w=[cn])
                kb.op("act", lambda e: e.activation(out=RK[:], in_=KEEP[:], func=AF.Copy, scale=R8[:, gg:gg + 1]), r=[KEEP, R8], w=[RK])

            def main(g, mid_hook):
                gg = fc * GC + g
                U = Ug[g % 2]
                W0b, LTre, LTim = W0bs[g % 2], LTres[g % 2], LTims[g % 2]
                cn, sn, RK = cns[g % 2], sns[g % 2], RKs[g % 2]
                for h in range(NH):
                    cs_ = slice(h * CW, (h + 1) * CW)
                    kb.op("pe", lambda e, cs_=cs_: e.matmul(psS[0][:], lhsT=LTre[:], rhs=U[:, cs_], start=True, stop=True),
                          r=[LTre, U], w=[psS[0]])
                    kb.op("pe", lambda e, cs_=cs_: e.matmul(psS[1][:], lhsT=LTim[:], rhs=U[:, cs_], start=True, stop=True),
                          r=[LTim, U], w=[psS[1]])
                    kb.op("dve", lambda e, cs_=cs_: e.tensor_tensor(out=wr_[:, cs_], in0=psS[0][:], in1=cn[:, cs_], op=ALU.mult), r=[psS[0], cn], w=[wr_])
                    kb.op("dve", lambda e, cs_=cs_: e.tensor_tensor(out=ta[:, cs_], in0=psS[1][:], in1=sn[:, cs_], op=ALU.mult), r=[psS[1], sn], w=[ta])
                    kb.op("dve", lambda e, cs_=cs_: e.tensor_tensor(out=wi_[:, cs_], in0=psS[1][:], in1=cn[:, cs_], op=ALU.mult), r=[psS[1], cn], w=[wi_])
                    kb.op("dve", lambda e, cs_=cs_: e.tensor_tensor(out=tb2[:, cs_], in0=psS[0][:], in1=sn[:, cs_], op=ALU.mult), r=[psS[0], sn], w=[tb2])
                kb.op("pool", lambda e: e.tensor_tensor(out=wr_[:], in0=wr_[:], in1=ta[:], op=ALU.add), r=[wr_, ta], w=[wr_])
                kb.op("dve", lambda e: e.tensor_tensor(out=wi_[:], in0=wi_[:], in1=tb2[:], op=ALU.subtract), r=[wi_, tb2], w=[wi_])
                mid_hook()
                for (z_, w_) in ((zr, wr_), (zi, wi_)):
                    kb.op("dve", lambda e, z_=z_, w_=w_: e.tensor_tensor_scan(out=z_[0:64, :], data0=RK[0:64, :], data1=w_[0:64, :],
                                                                          initial=0.0, op0=ALU.mult, op1=ALU.add), r=[RK, w_], w=[z_])
                    kb.op("dve", lambda e, z_=z_, w_=w_: e.tensor_tensor_scan(out=z_[64:128, ::-1], data0=RK[64:128, ::-1], data1=w_[64:128, ::-1],
                                                                          initial=0.0, op0=ALU.mult, op1=ALU.add), r=[RK, w_], w=[z_])
                kb.op("dve", lambda e: e.tensor_tensor(out=ta[:], in0=zr[:], in1=cn[:], op=ALU.mult), r=[zr, cn], w=[ta])
                kb.op("dve", lambda e: e.tensor_tensor(out=tb2[:], in0=zi[:], in1=sn[:], op=ALU.mult), r=[zi, sn], w=[tb2])
                kb.op("dve", lambda e: e.tensor_tensor(out=XPr[:, 1:NB + 1], in0=ta[:], in1=tb2[:], op=ALU.subtract), r=[ta, tb2], w=[XPr])
                kb.op("dve", lambda e: e.tensor_tensor(out=ta[:], in0=zr[:], in1=sn[:], op=ALU.mult), r=[zr, sn, XPr], w=[ta])
                kb.op("dve", lambda e: e.tensor_tensor(out=tb2[:], in0=zi[:], in1=cn[:], op=ALU.mult), r=[zi, cn, XPr], w=[tb2])
                kb.op("dve", lambda e: e.tensor_tensor(out=XPi[:, 1:NB + 1], in0=ta[:], in1=tb2[:], op=ALU.add), r=[ta, tb2], w=[XPi])
                UB = UNIT // 8
                for XP in (XPr, XPi):
                    kb.op("dve", lambda e, XP=XP: e.tensor_tensor(out=XP[0:64, UB:3 * UB + 1:UB], in0=XP[0:64, UB:3 * UB + 1:UB],
                                                                 in1=mask_t[0:64, 1:4], op=ALU.mult), r=[XP, mask_t], w=[XP])
                    kb.op("dve", lambda e, XP=XP: e.tensor_tensor(out=XP[64:128, UB + 1:3 * UB + 2:UB], in0=XP[64:128, UB + 1:3 * UB + 2:UB],
                                                                 in1=mask_t[64:128, 1:4], op=ALU.mult), r=[XP, mask_t], w=[XP])
                for h in range(NH):
                    c0 = h * CW
                    cs_ = slice(c0, c0 + CW)
                    pY = psY[h]
                    ops = [(W0b[:], U[:, cs_]),
                           (QXreb[:, 0, g, :], XPr[:, c0:c0 + CW]), (QXreb[:, 1, g, :], XPr[:, c0 + 2:c0 + CW + 2]),
                           (QXimb[:, 0, g, :], XPi[:, c0:c0 + CW]), (QXimb[:, 1, g, :], XPi[:, c0 + 2:c0 + CW + 2])]
                    acc_group(pY, 5, lambda e, j, pY=pY, ops=ops: e.matmul(pY[:], lhsT=ops[j][0], rhs=ops[j][1],
                                                                          start=(j == 0), stop=(j == 4)),
                              r=[W0b, QXreb, QXimb, U, XPr, XPi])
                    kb.op("dve", lambda e, cs_=cs_, pY=pY: e.scalar_tensor_tensor(
                        out=ysk[:, cs_], in0=U[:, cs_], scalar=dcol[:, gg:gg + 1], in1=pY[:], op0=ALU.mult, op1=ALU.add),
                        r=[U, dcol, pY], w=[ysk])
                    kb.op("act", lambda e, cs_=cs_: e.activation(out=yg[:, cs_], in_=ysk[:, cs_], func=AF.Gelu_apprx_tanh), r=[ysk], w=[yg])
                for nb in range(0, NNT, 8):
                    k8 = min(8, NNT - nb)
                    acc_group(psB, k8, lambda e, j, nb=nb: e.transpose(out=psB[:, j * 128:(j + 1) * 128],
                                                                   in_=yg[:, (nb + j) * 128:(nb + j + 1) * 128], identity=ident_b[:]),
                              r=[yg, ident_b])
                    kb.op("act", lambda e, nb=nb, k8=k8: e.activation(
                        out=YN[:, nb:nb + k8, :, g * 16:(g + 1) * 16],
                        in_=psB[:, 0:k8 * 128].rearrange("p (a i c) -> p a i c", i=8, c=16), func=AF.Copy), r=[psB], w=[YN])

            prepA(0)
            prepB(0)
            for g in range(GC):
                if g + 1 < GC:
                    prepA(g + 1)
                    main(g, lambda g=g: prepB(g + 1))
                else:
                    main(g, lambda: None)
            for nt in range(NNT):
                dst = YS.ap()[nt * 1024:(nt + 1) * 1024, fc * 128:(fc + 1) * 128].rearrange("(n i) c -> n i c", i=8)
                kb.dma("sp", dst, YN[:, nt, :, :], r=[YN])
        kb.end()

    def stage_glu_out(li, x_src, x_dst):
        kb.begin()
        CGW = min(512, D)
        was = [kb.sb(f"wga{i}", [128, FC, CGW], BF16) for i in range(2)]
        wgs = [kb.sb(f"wgg{i}", [128, FC, CGW], BF16) for i in range(2)]
        hts = [kb.sb(f"htg{i}", [128, FC, 512], BF16) for i in range(2)]
        barow = kb.sb("barow", [128, D], F32)
        bgrow = kb.sb("bgrow", [128, D], F32)
        row_bcast(barow, ssm_b_glu.ap()[:, 0:D])
        row_bcast(bgrow, ssm_b_glu.ap()[:, D:2 * D])
        G_t = kb.sb("G1s", [128, D], F32)
        psa = [kb.ps(f"psga{i}", [128, CGW]) for i in range(2)]
        psg = [kb.ps(f"psgg{i}", [128, CGW]) for i in range(2)]
        sgs = [kb.sb(f"sgg{i}", [128, CGW], F32) for i in range(2)]
        ots = [kb.sb(f"otg{i}", [128, CGW], F32) for i in range(2)]
        xts = [kb.sb(f"xtg{i}", [128, CGW], F32) for i in range(2)]
        wv = ssm_w_glu.ap().rearrange("(kc p) n -> p kc n", p=128)
        it = 0
        def glu_w(cg_):
            kb.dma("gq", was[cg_ % 2][:], wv[:, :, cg_ * CGW:(cg_ + 1) * CGW], w=[was[cg_ % 2]])
            kb.dma("gq", wgs[cg_ % 2][:], wv[:, :, D + cg_ * CGW:D + (cg_ + 1) * CGW], w=[wgs[cg_ % 2]])

        glu_w(0)
        for cg in range(D // CGW):
            cs = slice(cg * CGW, (cg + 1) * CGW)
            wa, wg = was[cg % 2], wgs[cg % 2]
            if cg + 1 < D // CGW:
                glu_w(cg + 1)
            for tb in range(NTB):
                t0 = tb * 512
                if t0 % UNIT == 0:
                    row_bcast(G_t, MOD.ap()[li, t0 // UNIT:t0 // UNIT + 1, 2 * D:3 * D])
                ht = hts[tb % 2]
                kb.dma("sp", ht[:], HT.ap()[:, :, t0:t0 + 512].rearrange("fc p t -> p fc t"), w=[ht])
                for ts in range(4):
                    r0 = t0 + ts * 128
                    pa, pg, sg, ot, xt = psa[it % 2], psg[it % 2], sgs[it % 2], ots[it % 2], xts[it % 2]
                    it += 1
                    kb.dma("sp", xt[:], x_src.ap()[r0:r0 + 128, cs], w=[xt])
                    acc_group(pa, FC, lambda e, kc, pa=pa, wa=wa, ht=ht, ts=ts: e.matmul(
                        pa[:], lhsT=ht[:, kc, ts * 128:(ts + 1) * 128], rhs=wa[:, kc, :], start=(kc == 0), stop=(kc == FC - 1)), r=[ht, wa])
                    acc_group(pg, FC, lambda e, kc, pg=pg, wg=wg, ht=ht, ts=ts: e.matmul(
                        pg[:], lhsT=ht[:, kc, ts * 128:(ts + 1) * 128], rhs=wg[:, kc, :], start=(kc == 0), stop=(kc == FC - 1)), r=[ht, wg])
                    kb.op("dve", lambda e, pg=pg, sg=sg: e.tensor_tensor(out=sg[:], in0=pg[:], in1=bgrow[:, cs], op=ALU.add), r=[pg, bgrow], w=[sg])
                    kb.op("act", lambda e, sg=sg: e.activation(out=sg[:], in_=sg[:], func=AF.Sigmoid), r=[sg], w=[sg])
                    kb.op("dve", lambda e, pa=pa, ot=ot: e.tensor_tensor(out=ot[:], in0=pa[:], in1=barow[:, cs], op=ALU.add), r=[pa, barow], w=[ot])
                    kb.op("pool", lambda e, ot=ot, sg=sg: e.tensor_tensor(out=ot[:], in0=ot[:], in1=sg[:], op=ALU.mult), r=[ot, sg], w=[ot])
                    kb.op("dve", lambda e, ot=ot: e.tensor_tensor(out=ot[:], in0=ot[:], in1=G_t[:, cs], op=ALU.mult), r=[ot, G_t], w=[ot])
                    kb.op("dve", lambda e, ot=ot, xt=xt: e.tensor_tensor(out=ot[:], in0=ot[:], in1=xt[:], op=ALU.add), r=[ot, xt], w=[ot])
                    kb.dma("sp", x_dst.ap()[r0:r0 + 128, cs], ot[:], r=[ot])
        kb.end()

    stage_mod()
    stage_prenorm_T(0, x_in, norm_mix_g, 1, 0)
    stage_conv_in()
    stage_dwconv()
    stage_conv_out(0, x_in, y_out if debug_out == "conv" else X1)
    if debug_out != "conv":
        stage_moe(0, X1)
        stage_combine(0, X1, y_out if debug_out == "moe0" else X2, final=False)
    if debug_out not in ("conv", "moe0"):
        stage_prenorm_rows(1, X2, norm_mix_g, 1, 0, HS)
        stage_s5()
        stage_rows_to_T(YS)
        stage_glu_out(1, X2, y_out if debug_out == "s5" else X3)
        if debug_out != "s5":
            stage_moe(1, X3)
            stage_combine(1, X3, y_out, final=True)

    kb.barrier()
    const_stack.close()
    kb.es.close()
    return nc


def cols(v, FC):
    return np.ascontiguousarray(np.asarray(v, np.float32).reshape(FC, 128).T)


def make_core_inputs(cfg, x, c, unit_rows, masks, p, seq_len):
    FC, D = cfg.FC, cfg.D
    cu = np.asarray(c, np.float32)[unit_rows]
    cT = np.ascontiguousarray(cu.reshape(4, FC, 128).transpose(2, 1, 0))
    m = np.ascontiguousarray(np.broadcast_to(np.asarray(masks, np.float32)[None, :], (128, 4)))
    d = {
        "x": np.ascontiguousarray(x.reshape(-1, D), dtype=np.float32),
        "cT": cT, "masks": m,
        "ada_w": p["ada_w"], "ada_b": p["ada_b"],
        "norm_mix_g": p["norm_mix_g"], "norm_ffn_g": p["norm_ffn_g"],
        "final_norm_g": p["final_norm_g"].reshape(1, D),
        "conv_w_in": p["conv_w_in"][0],
        "conv_b_in_c": cols(p["conv_b_in"][0], 2 * FC),
        "conv_w_dw_c": np.ascontiguousarray(p["conv_w_dw"][0].T.reshape(FC, 128, CONV_W).transpose(1, 0, 2)),
        "conv_b_dw_c": cols(p["conv_b_dw"][0], FC),
        "conv_ln_g_c": cols(p["conv_ln_g"][0], FC),
        "conv_ln_b_c": cols(p["conv_ln_b"][0], FC),
        "conv_w_out": p["conv_w_out"][0],
        "conv_b_out": p["conv_b_out"][0].reshape(1, D),
        "moe_w_router_c": np.ascontiguousarray(p["moe_w_router"].reshape(cfg.depth, FC, 128, cfg.E).transpose(0, 2, 1, 3)),
    }
    G = cfg.G
    def dpg(a):
        return np.ascontiguousarray(np.asarray(a, np.float32).transpose(0, 2, 1).reshape(128, G))
    d["ssm_lre"] = dpg(p["ssm_lambda_re"][0])
    d["ssm_lim"] = dpg(p["ssm_lambda_im"][0])
    d["ssm_ls"] = dpg(np.broadcast_to(np.asarray(p["ssm_log_step"][0])[:, :, None], (2, G, 64)))
    d["ssm_bre"] = np.asarray(p["ssm_b_re"][0]).transpose(0, 2, 1, 3).reshape(128, G, 16)
    d["ssm_bim"] = np.asarray(p["ssm_b_im"][0]).transpose(0, 2, 1, 3).reshape(128, G, 16)
    d["ssm_cre"] = np.asarray(p["ssm_c_re"][0]).transpose(0, 3, 1, 2).reshape(128, G, 16)
    d["ssm_cim"] = np.asarray(p["ssm_c_im"][0]).transpose(0, 3, 1, 2).reshape(128, G, 16)
    dd = np.asarray(p["ssm_d"][0], np.float32).reshape(G, 16)
    d["ssm_dcol"] = np.ascontiguousarray(np.broadcast_to(dd.T[None, :, :], (8, 16, G)).reshape(128, G))
    d["ssm_w_glu"] = p["ssm_w_glu"][0]
    d["ssm_b_glu"] = p["ssm_b_glu"][0].reshape(1, 2 * D)
    NB = cfg.NT // 8
    seqb = seq_len // 8
    keep = np.ones((128, NB), np.float32)
    n = np.arange(NB)
    keep[:64, n % seqb == 0] = 0.0
    keep[64:, n % seqb == seqb - 1] = 0.0
    d["s5_keep"] = keep
    ip = np.arange(128) // 16
    mk = np.zeros((128, 2, 128), np.float32)
    mk[:, 0, :] = (ip[None, :] >= ip[:, None])
    mk[:, 1, :] = (ip[:, None] >= ip[None, :])
    d["s5_masks"] = mk
    EG = min(cfg.E, 8)
    for nm in ("gate", "up", "down"):
        w = p["moe_w_" + nm]
        for l in range(cfg.depth):
            for h in range(cfg.E // EG):
                d[f"moe_w_{nm}_{l}_{h}"] = w[l, h * EG:(h + 1) * EG]
    return {k: np.ascontiguousarray(v, dtype=np.float32) for k, v in d.items()}


def run(cfg, x_prompt, x_sample, c_prompt, c_sample, p, debug_out=None, n_cores=8):
    nc = build_program(cfg, debug_out=debug_out)
    bp, lp = x_prompt.shape[0], x_prompt.shape[1]
    bs, ls = x_sample.shape[0], x_sample.shape[1]

    def unit_info(b, l):
        upb = 4 // b
        rows = [u // upb for u in range(4)]
        masks = [0.0] + [1.0 if (u % upb) != 0 else 0.0 for u in range(1, 4)]
        return rows, masks

    rp, mp = unit_info(bp, lp)
    rs, ms = unit_info(bs, ls)
    inp_p = make_core_inputs(cfg, x_prompt, c_prompt, rp, mp, p, lp)
    inp_s = make_core_inputs(cfg, x_sample, c_sample, rs, ms, p, ls)
    for dct in (inp_p, inp_s):
        for k in list(dct):
            if (debug_out == "conv" and k.startswith("moe_")) or (debug_out in ("conv", "moe0") and (k.startswith("ssm_") or k.startswith("s5_"))):
                del dct[k]
    in_maps = [inp_p if (i % 2 == 0) else inp_s for i in range(n_cores)]
    res = run_bass_kernel_spmd(nc, in_maps, core_ids=list(range(n_cores)))
    yp = np.asarray(res.results[0]["y"], np.float32).reshape(x_prompt.shape)
    ys = np.asarray(res.results[1]["y"], np.float32).reshape(x_sample.shape)
    return yp, ys


def kernel(**inputs):
    cfg = Cfg()
    p = {k: np.asarray(v) for k, v in inputs.items()}
    return run(cfg, p["x_prompt"], p["x_sample"], p["c_prompt"], p["c_sample"], p, n_cores=2)
```
